# Optimizing a Trainium2 kernel written in Bass

```python
import math
import jax
import jax.numpy as jnp
from jax import lax
import numpy as np

D_MODEL = 1024
BATCH = 8
SEQ = 2048
DEPTH = 2

CTX_LEN = 256
GRID_W = 64

HG_HEADS = 4
HG_DK = 128
HG_DV = 128
HG_K = HG_HEADS * HG_DK
HG_V = HG_HEADS * HG_DV
HG_CHUNK = 64

RW_HEADS = 8
RW_HD = 64
RW_C = RW_HEADS * RW_HD
RW_W_RANK = 64
RW_A_RANK = 64
RW_G_RANK = 128
RW_LN_EPS = 64e-5

DA_HEADS = 4
DA_HD = 64
DA_QK = DA_HEADS * 2 * DA_HD
DA_V = DA_HEADS * 2 * DA_HD
DA_Q_BLOCK = 128
DA_SUBLN_EPS = 1e-5
DA_SCALE = DA_HD ** -0.5
ROPE_AXIS_DIM = DA_HD // 2
ROPE_BASE = 10000.0

FFN_DIM = 2816
N_EXPERTS = 8
TOP_K = 2
N_DENSE = (DEPTH + 1) // 2
N_MOE = DEPTH // 2
NORM_EPS = 1e-6

HG_SPLITS = (('q', HG_K), ('f_fwd', HG_K), ('f_bwd', HG_K), ('i', HG_V), ('g', HG_V))
RW_SPLITS = (('r', RW_C), ('k', RW_C), ('v', RW_C), ('wd_fwd', RW_W_RANK), ('wd_bwd', RW_W_RANK), ('ad', RW_A_RANK), ('gd', RW_G_RANK))
DA_SPLITS = (('q', DA_QK), ('k', DA_QK), ('v', DA_V))
GATE_SPLITS = (('hg', D_MODEL), ('rw', D_MODEL), ('da', D_MODEL))
HG_COLS = sum(w for _, w in HG_SPLITS)
RW_COLS = sum(w for _, w in RW_SPLITS)
DA_COLS = sum(w for _, w in DA_SPLITS)
GATE_COLS = sum(w for _, w in GATE_SPLITS)
IN_COLS = HG_COLS + RW_COLS + DA_COLS + GATE_COLS
GROUP_BOUNDS = (HG_COLS, HG_COLS + RW_COLS, HG_COLS + RW_COLS + DA_COLS)

kernel_name = 'hybrid_flow_hgrn2_rwkv7_diffattn_moe'


def rms_norm(x, g, eps=NORM_EPS):
    xf = x.astype(jnp.float32)
    y = xf * lax.rsqrt(jnp.mean(xf * xf, axis=-1, keepdims=True) + eps)
    return (y * g).astype(x.dtype)


def modulate(h, shift, scale):
    return h * (1.0 + scale) + shift


def split_named(p, splits):
    bounds = np.cumsum([w for _, w in splits])[:-1].tolist()
    return dict(zip([n for n, _ in splits], jnp.split(p, bounds, axis=-1)))


def to_heads(a, n_heads):
    bsz, t, _ = a.shape
    return a.reshape(bsz, t, n_heads, -1).transpose(0, 2, 1, 3)


def hgrn2_lower_bounds(lb_logits):
    p = jax.nn.softmax(lb_logits.astype(jnp.float32), axis=0)
    cs = jnp.cumsum(p, axis=0)
    return cs - cs[:1]


def hgrn2_log_forget(z, lb):
    z = z.astype(jnp.float32)
    return jnp.logaddexp(jnp.log(lb), jnp.log1p(-lb) + jax.nn.log_sigmoid(z))


def gla_chunk_scan(q, k, v, logf, s0, need_out):
    bsz, nh, t, _ = q.shape
    n = t // HG_CHUNK

    def chunks(a):
        return a.reshape(bsz, nh, n, HG_CHUNK, a.shape[-1]).transpose(2, 0, 1, 3, 4)

    incl = jnp.tril(jnp.ones((HG_CHUNK, HG_CHUNK), bool))[:, :, None]

    def step(state, inp):
        qc, kc, vc, lc = inp
        b = jnp.cumsum(lc, axis=2)
        b_end = b[:, :, -1:, :]
        new_state = (jnp.swapaxes(jnp.exp(b_end), 2, 3) * state
                     + jnp.einsum('bhsk,bhsv->bhkv', kc * jnp.exp(b_end - b), vc))
        if not need_out:
            return new_state, None
        o_inter = jnp.einsum('bhtk,bhkv->bhtv', qc * jnp.exp(b), state)
        decay = jnp.exp(jnp.where(incl, b[:, :, :, None, :] - b[:, :, None, :, :], -jnp.inf))
        attn = jnp.einsum('bhtk,bhsk,bhtsk->bhts', qc, kc, decay)
        return new_state, o_inter + jnp.einsum('bhts,bhsv->bhtv', attn, vc)

    state, o = lax.scan(step, s0, (chunks(q), chunks(k), chunks(v), chunks(logf)))
    if need_out:
        o = o.transpose(1, 2, 0, 3, 4).reshape(bsz, nh, t, -1)
    return state, o


def hgrn2_branch(p_lat, p_ctx, lb, norm_g, proj, need_ctx):
    streams = []
    for p in (p_lat, p_ctx):
        pd = split_named(p, HG_SPLITS)
        q = to_heads(jax.nn.silu(pd['q'].astype(jnp.float32)), HG_HEADS)
        v = to_heads(pd['i'].astype(jnp.float32), HG_HEADS)
        logfs = [to_heads(hgrn2_log_forget(pd[nm], lb[d]), HG_HEADS) for d, nm in enumerate(('f_fwd', 'f_bwd'))]
        streams.append((pd, q, v, logfs))
    (pd_l, q_l, v_l, lf_l), (pd_c, q_c, v_c, lf_c) = streams
    bsz = q_l.shape[0]
    s0 = jnp.zeros((bsz, HG_HEADS, HG_DK, HG_DV), jnp.float32)
    o_lat, o_ctx = 0.0, 0.0
    for d in range(2):
        flip = (lambda a: jnp.flip(a, axis=2)) if d == 1 else (lambda a: a)
        s_ctx, oc = gla_chunk_scan(flip(q_c), flip(-jnp.expm1(lf_c[d])), flip(v_c), flip(lf_c[d]), s0, need_ctx)
        _, ol = gla_chunk_scan(flip(q_l), flip(-jnp.expm1(lf_l[d])), flip(v_l), flip(lf_l[d]), s_ctx, True)
        o_lat = o_lat + flip(ol)
        if need_ctx:
            o_ctx = o_ctx + flip(oc)

    def finish(o, pd):
        b_, _, t, _ = o.shape
        o = rms_norm(o, norm_g).transpose(0, 2, 1, 3).reshape(b_, t, HG_V)
        return (o * jax.nn.silu(pd['g'].astype(jnp.float32))) @ proj

    return finish(o_lat, pd_l), (finish(o_ctx, pd_c) if need_ctx else None)


def token_shift_mix(p, mu):
    pad = jnp.pad(p, ((0, 0), (1, 1), (0, 0)))
    nb = 0.5 * (pad[:, :-2] + pad[:, 2:])
    return p + mu * (nb - p)


def rwkv7_prepare(p, mu, w0, w2, a0, a2, g2, k_k, k_a):
    pd = split_named(token_shift_mix(p, mu).astype(jnp.float32), RW_SPLITS)
    bsz, t, _ = p.shape
    rh = lambda z: z.reshape(bsz, t, RW_HEADS, RW_HD)
    a = jax.nn.sigmoid(a0 + pd['ad'] @ a2)
    kk = rh(pd['k'] * k_k)
    kk = kk / jnp.maximum(jnp.sqrt(jnp.sum(kk * kk, axis=-1, keepdims=True)), 1e-12)
    k = pd['k'] * (1.0 + (a - 1.0) * k_a)
    decays = []
    for d, nm in enumerate(('wd_fwd', 'wd_bwd')):
        wlog = -jax.nn.softplus(-(w0[d] + jnp.tanh(pd[nm]) @ w2[d])) - 0.5
        decays.append(rh(jnp.exp(-jnp.exp(wlog))))
    return dict(r=rh(pd['r']), k=rh(k), v=rh(pd['v']), kk=kk, a=rh(a),
                g=jax.nn.sigmoid(pd['gd']) @ g2, decays=decays)


def rwkv7_scan(r, w, k, v, a, b, s0, need_out, reverse):
    tm = lambda z: jnp.moveaxis(z, 1, 0)

    def step(state, inp):
        rt, wt, kt, vt, at, bt = inp
        sa = jnp.einsum('bhvk,bhk->bhv', state, at)
        state = state * wt[:, :, None, :] + sa[..., None] * bt[:, :, None, :] + vt[..., None] * kt[:, :, None, :]
        if not need_out:
            return state, None
        return state, jnp.einsum('bhvk,bhk->bhv', state, rt)

    state, y = lax.scan(step, s0, (tm(r), tm(w), tm(k), tm(v), tm(a), tm(b)), reverse=reverse)
    return state, (jnp.moveaxis(y, 0, 1) if need_out else None)


def rwkv7_finish(y, st, r_k, ln_g, ln_b, proj):
    bsz, t = y.shape[:2]
    mean = jnp.mean(y, axis=-1, keepdims=True)
    var = jnp.mean(jnp.square(y - mean), axis=-1, keepdims=True)
    yn = ((y - mean) * lax.rsqrt(var + RW_LN_EPS)).reshape(bsz, t, RW_C) * ln_g + ln_b
    bonus = jnp.sum(st['r'] * st['k'] * r_k, axis=-1, keepdims=True) * st['v']
    return ((yn + bonus.reshape(bsz, t, RW_C)) * st['g']) @ proj


def rwkv7_branch(p_lat, p_ctx, mu, w0, w2, a0, a2, g2, k_k, k_a, r_k, ln_g, ln_b, proj, need_ctx):
    sl = rwkv7_prepare(p_lat, mu, w0, w2, a0, a2, g2, k_k, k_a)
    sc = rwkv7_prepare(p_ctx, mu, w0, w2, a0, a2, g2, k_k, k_a)
    bsz = p_lat.shape[0]
    s0 = jnp.zeros((bsz, RW_HEADS, RW_HD, RW_HD), jnp.float32)
    y_lat, y_ctx = 0.0, 0.0
    for d in range(2):
        rev = d == 1
        s_ctx, yc = rwkv7_scan(sc['r'], sc['decays'][d], sc['k'], sc['v'], -sc['kk'], sc['kk'] * sc['a'], s0, need_ctx, rev)
        _, yl = rwkv7_scan(sl['r'], sl['decays'][d], sl['k'], sl['v'], -sl['kk'], sl['kk'] * sl['a'], s_ctx, True, rev)
        y_lat = y_lat + yl
        if need_ctx:
            y_ctx = y_ctx + yc
    out_l = rwkv7_finish(y_lat, sl, r_k, ln_g, ln_b, proj)
    out_c = rwkv7_finish(y_ctx, sc, r_k, ln_g, ln_b, proj) if need_ctx else None
    return out_l, out_c


def axial_rope_tables(rows):
    row = jnp.repeat(jnp.arange(rows), GRID_W).astype(jnp.float32)
    col = jnp.tile(jnp.arange(GRID_W), rows).astype(jnp.float32)
    inv_freq = 1.0 / (ROPE_BASE ** (jnp.arange(0, ROPE_AXIS_DIM, 2, dtype=jnp.float32) / ROPE_AXIS_DIM))
    ang_r = row[:, None] * inv_freq
    ang_c = col[:, None] * inv_freq
    return (jnp.cos(ang_r), jnp.sin(ang_r), jnp.cos(ang_c), jnp.sin(ang_c))


def rotate_pairs(x, cos, sin):
    x1, x2 = jnp.split(x, 2, axis=-1)
    return jnp.concatenate([x1 * cos - x2 * sin, x2 * cos + x1 * sin], axis=-1)


def apply_axial_rope(x, tables):
    cr, sr, cc, sc = [tb[:, None, None, :] for tb in tables]
    xr, xc = jnp.split(x.astype(jnp.float32), 2, axis=-1)
    return jnp.concatenate([rotate_pairs(xr, cr, sr), rotate_pairs(xc, cc, sc)], axis=-1).astype(x.dtype)


def diff_softmax_attend(q, k, v, lam):
    s = jnp.einsum('bqhmd,bkhmd->bhmqk', q, k).astype(jnp.float32) * DA_SCALE
    p = jax.nn.softmax(s, axis=-1)
    w = p[:, :, 0] - lam * p[:, :, 1]
    return jnp.einsum('bhqk,bkhe->bqhe', w.astype(v.dtype), v)


def diff_attn_branch(p_lat, p_ctx, lam_p, subln_g, proj, layer, rope, need_ctx):
    lat = split_named(p_lat, DA_SPLITS)
    cx = split_named(p_ctx, DA_SPLITS)
    bsz, seq, _ = p_lat.shape
    qk = lambda a: a.reshape(a.shape[0], a.shape[1], DA_HEADS, 2, DA_HD)
    vv = lambda a: a.reshape(a.shape[0], a.shape[1], DA_HEADS, 2 * DA_HD)
    q_l = apply_axial_rope(qk(lat['q']), rope)
    k_l = apply_axial_rope(qk(lat['k']), rope)
    k_c, v_c = qk(cx['k']), vv(cx['v'])
    lam_init = 0.8 - 0.6 * math.exp(-0.3 * layer)
    lp = lam_p.astype(jnp.float32)
    lam = jnp.exp(jnp.sum(lp[0] * lp[1])) - jnp.exp(jnp.sum(lp[2] * lp[3])) + lam_init
    k_all = jnp.concatenate([k_c, k_l], axis=1)
    v_all = jnp.concatenate([v_c, vv(lat['v'])], axis=1)
    nb = seq // DA_Q_BLOCK
    qb = q_l.reshape(bsz, nb, DA_Q_BLOCK, DA_HEADS, 2, DA_HD).swapaxes(0, 1)
    ob = lax.map(lambda qq: diff_softmax_attend(qq, k_all, v_all, lam), qb)
    o_l = ob.swapaxes(0, 1).reshape(bsz, seq, DA_HEADS, 2 * DA_HD)

    def finish(o):
        o = rms_norm(o, subln_g, DA_SUBLN_EPS) * (1.0 - lam_init)
        return o.reshape(o.shape[0], o.shape[1], DA_V) @ proj

    y_c = finish(diff_softmax_attend(qk(cx['q']), k_c, v_c, lam)) if need_ctx else None
    return finish(o_l), y_c


def gated_merge(p_gate, y_hg, y_rw, y_da, w_out):
    g = split_named(p_gate, GATE_SPLITS)
    m = jax.nn.sigmoid(g['hg']) * y_hg + jax.nn.sigmoid(g['rw']) * y_rw + jax.nn.sigmoid(g['da']) * y_da
    return m @ w_out


def swiglu(h, w1, w3, w2):
    return (jax.nn.silu(h @ w1) * (h @ w3)) @ w2


def moe_swiglu(h, router, w1, w3, w2):
    shp = h.shape
    t = h.reshape(-1, shp[-1])
    logits = (t @ router).astype(jnp.float32)
    top_v, top_i = lax.top_k(logits, TOP_K)
    wts = jax.nn.softmax(top_v, axis=-1)
    combine = jnp.sum(jax.nn.one_hot(top_i, N_EXPERTS, dtype=jnp.float32) * wts[..., None], axis=1)
    out = jnp.zeros(t.shape, jnp.float32)
    for e in range(N_EXPERTS):
        out = out + combine[:, e:e + 1] * swiglu(t, w1[e], w3[e], w2[e])
    return out.astype(h.dtype).reshape(shp)


def channel_mix(h, layer, ffn_w1, ffn_w3, ffn_w2, moe_router, moe_w1, moe_w3, moe_w2):
    j = layer // 2
    if layer % 2 == 0:
        return swiglu(h, ffn_w1[j], ffn_w3[j], ffn_w2[j])
    return moe_swiglu(h, moe_router[j], moe_w1[j], moe_w3[j], moe_w2[j])


def setup_inputs(seed: int = 0) -> dict:
    key = jax.random.key(seed)
    keys = list(jax.random.split(key, 48))

    def nrm(shape, scale):
        return jax.random.normal(keys.pop(), shape, jnp.float32) * scale

    def unif(shape, lo, hi):
        return jax.random.uniform(keys.pop(), shape, jnp.float32, lo, hi)

    def gain(shape):
        return 1.0 + nrm(shape, 0.02)

    D = D_MODEL
    return {
        'x': nrm((BATCH, SEQ, D), 1.0),
        'c': nrm((BATCH, D), 1.0),
        'ctx': nrm((BATCH, CTX_LEN, D), 1.0),
        'c_ctx': nrm((D,), 1.0),
        'ada_w': nrm((DEPTH, D, 6 * D), 0.5 * D ** -0.5),
        'ada_b': nrm((DEPTH, 6 * D), 0.01),
        'norm_mix_g': gain((DEPTH, D)),
        'norm_ffn_g': gain((DEPTH, D)),
        'final_norm_g': gain((D,)),
        'w_in': nrm((DEPTH, D, IN_COLS), D ** -0.5),
        'hg_lb_logits': nrm((DEPTH, 2, HG_K), 1.0),
        'hg_norm_g': gain((DEPTH, HG_DV)),
        'hg_proj': nrm((DEPTH, HG_V, D), HG_V ** -0.5),
        'rw_mu': unif((DEPTH, RW_COLS), 0.0, 1.0),
        'rw_w0': unif((DEPTH, 2, RW_C), -6.0, -1.0),
        'rw_w2': nrm((DEPTH, 2, RW_W_RANK, RW_C), 0.1 * RW_W_RANK ** -0.5),
        'rw_a0': nrm((DEPTH, RW_C), 0.5),
        'rw_a2': nrm((DEPTH, RW_A_RANK, RW_C), 0.5 * RW_A_RANK ** -0.5),
        'rw_g2': nrm((DEPTH, RW_G_RANK, RW_C), RW_G_RANK ** -0.5),
        'rw_k_k': 0.85 + nrm((DEPTH, RW_C), 0.05),
        'rw_k_a': 1.0 + nrm((DEPTH, RW_C), 0.05),
        'rw_r_k': nrm((DEPTH, RW_HEADS, RW_HD), 0.1),
        'rw_ln_g': gain((DEPTH, RW_C)),
        'rw_ln_b': nrm((DEPTH, RW_C), 0.01),
        'rw_proj': nrm((DEPTH, RW_C, D), RW_C ** -0.5),
        'da_lambda': nrm((DEPTH, 4, DA_HD), 0.1),
        'da_subln_g': gain((DEPTH, 2 * DA_HD)),
        'da_proj': nrm((DEPTH, DA_V, D), DA_V ** -0.5),
        'w_out': nrm((DEPTH, D, D), D ** -0.5),
        'ffn_w1': nrm((N_DENSE, D, FFN_DIM), D ** -0.5),
        'ffn_w3': nrm((N_DENSE, D, FFN_DIM), D ** -0.5),
        'ffn_w2': nrm((N_DENSE, FFN_DIM, D), FFN_DIM ** -0.5),
        'moe_router': nrm((N_MOE, D, N_EXPERTS), D ** -0.5),
        'moe_w1': nrm((N_MOE, N_EXPERTS, D, FFN_DIM), D ** -0.5),
        'moe_w3': nrm((N_MOE, N_EXPERTS, D, FFN_DIM), D ** -0.5),
        'moe_w2': nrm((N_MOE, N_EXPERTS, FFN_DIM, D), FFN_DIM ** -0.5),
    }


def reference(x, c, ctx, c_ctx, ada_w, ada_b, norm_mix_g, norm_ffn_g, final_norm_g, w_in,
              hg_lb_logits, hg_norm_g, hg_proj,
              rw_mu, rw_w0, rw_w2, rw_a0, rw_a2, rw_g2, rw_k_k, rw_k_a, rw_r_k, rw_ln_g, rw_ln_b, rw_proj,
              da_lambda, da_subln_g, da_proj, w_out,
              ffn_w1, ffn_w3, ffn_w2, moe_router, moe_w1, moe_w3, moe_w2):
    seq = x.shape[1]
    rows = seq // GRID_W
    rope = axial_rope_tables(rows)
    lower_bounds = hgrn2_lower_bounds(hg_lb_logits)
    sc_lat = jax.nn.silu(c)
    sc_ctx = jax.nn.silu(c_ctx)
    xc = ctx
    for l in range(DEPTH):
        need_ctx = l < DEPTH - 1
        ml = [m[:, None, :] for m in jnp.split(sc_lat @ ada_w[l] + ada_b[l], 6, axis=-1)]
        mc = jnp.split(sc_ctx @ ada_w[l] + ada_b[l], 6, axis=-1)
        pl = modulate(rms_norm(x, norm_mix_g[l]), ml[0], ml[1]) @ w_in[l]
        pc = modulate(rms_norm(xc, norm_mix_g[l]), mc[0], mc[1]) @ w_in[l]
        hg_l, rw_l, da_l, gt_l = jnp.split(pl, GROUP_BOUNDS, axis=-1)
        hg_c, rw_c, da_c, gt_c = jnp.split(pc, GROUP_BOUNDS, axis=-1)
        ya_l, ya_c = hgrn2_branch(hg_l, hg_c, lower_bounds[l], hg_norm_g[l], hg_proj[l], need_ctx)
        yb_l, yb_c = rwkv7_branch(rw_l, rw_c, rw_mu[l], rw_w0[l], rw_w2[l], rw_a0[l], rw_a2[l], rw_g2[l],
                                  rw_k_k[l], rw_k_a[l], rw_r_k[l], rw_ln_g[l], rw_ln_b[l], rw_proj[l], need_ctx)
        yc_l, yc_c = diff_attn_branch(da_l, da_c, da_lambda[l], da_subln_g[l], da_proj[l], l, rope, need_ctx)
        x = x + (ml[2] * gated_merge(gt_l, ya_l, yb_l, yc_l, w_out[l])).astype(x.dtype)
        if need_ctx:
            xc = xc + (mc[2] * gated_merge(gt_c, ya_c, yb_c, yc_c, w_out[l])).astype(xc.dtype)
        hl = modulate(rms_norm(x, norm_ffn_g[l]), ml[3], ml[4])
        x = x + (ml[5] * channel_mix(hl, l, ffn_w1, ffn_w3, ffn_w2, moe_router, moe_w1, moe_w3, moe_w2)).astype(x.dtype)
        if need_ctx:
            hc = modulate(rms_norm(xc, norm_ffn_g[l]), mc[3], mc[4])
            xc = xc + (mc[5] * channel_mix(hc, l, ffn_w1, ffn_w3, ffn_w2, moe_router, moe_w1, moe_w3, moe_w2)).astype(xc.dtype)
    return rms_norm(x, final_norm_g)
```

```python
import math
import numpy as np
from contextlib import ExitStack
import concourse.bass as bass
import concourse.mybir as mybir
from concourse.bass_utils import run_bass_kernel_spmd

F32 = mybir.dt.float32
BF16 = mybir.dt.bfloat16
AF = mybir.ActivationFunctionType
ALU = mybir.AluOpType
AX = mybir.AxisListType

SAME_ENGINE_SYNC = True
N_DMA_SEMS = 40
SEM_EPOCH = 16000

T = 2304
TC = 256
TL = 2048
D = 1024
INC = 9024
FF = 2816
NE = 8
BLKS = [(0, 256, 1), (256, 512, 0), (768, 512, 0), (1280, 512, 0), (1792, 512, 0)]
HG0 = 0
RW0 = 2560
DA0 = 4416
GT0 = 5952
RW_R, RW_K, RW_V, RW_WF, RW_WB, RW_AD, RW_GD = RW0, RW0 + 512, RW0 + 1024, RW0 + 1536, RW0 + 1600, RW0 + 1664, RW0 + 1728


class Buf:
    def __init__(self, name, h):
        self.name = name
        self.h = h
        self.w = None
        self.r = {}

    def __getitem__(self, idx):
        return self.h[idx]


class Prog:
    ENG = ("pe", "act", "dve", "pool", "sp")

    def __init__(self, nc, es):
        self.nc = nc
        self.es = es
        self.e = dict(pe=nc.tensor, act=nc.scalar, dve=nc.vector, pool=nc.gpsimd, sp=nc.sync)
        self.sem = {}
        self.ekey = {}
        for k in self.ENG:
            self.ekey[k] = (k, 0)
            self.sem[(k, 0)] = es.enter_context(nc.semaphore("s_" + k))
        self.cnt = {k: 0 for k in self.ENG}
        self.dsem = [es.enter_context(nc.semaphore("d%d" % i)) for i in range(N_DMA_SEMS)]
        for i in range(N_DMA_SEMS):
            self.sem[("d", i)] = self.dsem[i]
        self.dval = [0] * N_DMA_SEMS
        self.dnext = 0
        self.seen = {k: {} for k in self.ENG}
        self.ninstr = {k: 0 for k in self.ENG}
        self.nwait = 0
        self.uid = 0

    def sb(self, name, shape, dtype=F32, es=None):
        self.uid += 1
        h = (es or self.es).enter_context(self.nc.sbuf_tensor("%s_%d" % (name, self.uid), list(shape), dtype))
        return Buf(name, h)

    def ps(self, name, shape, dtype=F32, es=None):
        self.uid += 1
        h = (es or self.es).enter_context(self.nc.psum_tensor("%s_%d" % (name, self.uid), list(shape), dtype))
        return Buf(name, h)

    def dram(self, name, shape, dtype=F32, kind="Internal"):
        h = self.nc.dram_tensor(name, list(shape), dtype, kind=kind)
        return Buf(name, h.ap())

    def _wait(self, ek, k, v):
        if self.seen[ek].get(k, 0) >= v:
            return
        self.e[ek].wait_ge(self.sem[k], v)
        self.seen[ek][k] = v
        self.nwait += 1

    def _deps(self, ek, r, w):
        deps = {}

        def add(ev):
            if ev is None:
                return
            k, v = ev
            if deps.get(k, 0) < v:
                deps[k] = v

        for b in r:
            add(b.w)
        for b in w:
            add(b.w)
            for k, v in b.r.items():
                add((k, v))
        for k, v in deps.items():
            if k[0] == ek and (not SAME_ENGINE_SYNC or ek in ("pe", "sp")):
                continue
            self._wait(ek, k, v)

    def _commit(self, ev, r, w):
        k, v = ev
        for b in w:
            b.w = ev
            b.r = {}
        for b in r:
            if b.r.get(k, 0) < v:
                b.r[k] = v

    def op(self, ek, fn, w=(), r=()):
        if self.cnt[ek] >= SEM_EPOCH:
            ep = self.ekey[ek][1] + 1
            self.ekey[ek] = (ek, ep)
            self.sem[(ek, ep)] = self.es.enter_context(self.nc.semaphore("s_%s_%d" % (ek, ep)))
            self.cnt[ek] = 0
        self._deps(ek, r, w)
        ins = fn(self.e[ek])
        self.cnt[ek] += 1
        key = self.ekey[ek]
        ins.then_inc(self.sem[key], 1)
        self.ninstr[ek] += 1
        self._commit((key, self.cnt[ek]), r, w)
        return ins

    def V(self, fn, w=(), r=()):
        return self.op("dve", fn, w, r)

    def A(self, fn, w=(), r=()):
        return self.op("act", fn, w, r)

    def G(self, fn, w=(), r=()):
        return self.op("pool", fn, w, r)

    def T(self, fn, w=(), r=()):
        return self.op("pe", fn, w, r)

    def dma(self, out, in_, w=(), r=(), q="sp", **kw):
        i = self.dnext
        self.dnext = (self.dnext + 1) % N_DMA_SEMS
        key = ("d", i)
        if self.dval[i] > 0:
            self._wait(q, key, self.dval[i])
        self._deps(q, r, w)
        ins = self.e[q].dma_start(out=out, in_=in_, **kw)
        self.dval[i] += 16
        ins.then_inc(self.dsem[i], 16)
        self.ninstr[q] += 1
        self._commit((key, self.dval[i]), r, w)
        return ins

    def barrier(self):
        for ek in self.ENG:
            for k in self.ENG:
                if k == ek:
                    continue
                key = self.ekey[k]
                if key[1] > 0:
                    self._wait(ek, (k, key[1] - 1), SEM_EPOCH)
                if self.cnt[k] > 0:
                    self._wait(ek, key, self.cnt[k])
            for i in range(N_DMA_SEMS):
                if self.dval[i] > 0:
                    self._wait(ek, ("d", i), self.dval[i])


def build(n_layers=2, dbg=(), stop_after=None, skip=()):
    nc = bass.Bass("TRN2", target_bir_lowering=False)
    with ExitStack() as es:
        p = Prog(nc, es)
        I = {}

        def inp(name, shape, dt=F32):
            I[name] = p.dram(name, shape, dt, kind="ExternalInput")
            return I[name]

        inp("xT", [D, T]); inp("c2", [128, 8, 2])
        inp("ada_w", [2, D, 6 * D]); inp("ada_bT", [2, 128, 48])
        inp("nmgT", [2, 128, 8]); inp("nfgT", [2, 128, 8]); inp("fngT", [128, 8])
        inp("w_in", [2, D, INC]); inp("muT", [2, 128, 71])
        inp("ident", [128, 128]); inp("masks", [64, 4, 64])
        inp("hg_lbT", [2, 2, 128, 4]); inp("hg_ngT", [2, 128, 1]); inp("hg_proj", [2, 512, D])
        inp("w_out", [2, D, D])
        inp("ffn_w1", [1, D, FF]); inp("ffn_w3", [1, D, FF]); inp("ffn_w2", [1, FF, D])
        inp("moe_router", [1, D, NE]); inp("moe_w1", [1, NE, D, FF]); inp("moe_w3", [1, NE, D, FF]); inp("moe_w2", [1, NE, FF, D])
        inp("sel8", [8, 8, 128])
        inp("da_lambda", [2, 256]); inp("da_sg", [2, 128]); inp("da_proj", [2, 512, D])
        inp("ropeCS", [64, 2, TL]); inp("ropeR", [64, 64])
        inp("rw_vecT", [2, 64, 8, 8]); inp("rw_w0T", [2, 2, 64, 8]); inp("rw_w2", [2, 2, 64, 512]); inp("rw_a2", [2, 64, 512])
        inp("rw_g2", [2, 128, 512]); inp("rw_proj", [2, 512, D])
        outT = p.dram("outT", [D, TL], F32, kind="ExternalOutput")

        X = p.dram("Xs", [D, T])
        PT = p.dram("PT", [INC, T])
        VHG = p.dram("VHG", [T, 512])
        VDA = p.dram("VDA", [T, 512])
        OHG = p.dram("OHG", [2, 512, T])
        YG = [p.dram("YG%d" % i, [D, T]) for i in range(3)]
        UT = p.dram("UT", [FF, T], BF16)
        FACC = p.dram("FACC", [D, T])
        dbg_out = {}

        ident = p.sb("ident", [128, 128]); p.dma(ident[:], I["ident"][:], w=[ident], r=[I["ident"]])
        masks = p.sb("masks", [64, 4, 64]); p.dma(masks[:], I["masks"][:], w=[masks], r=[I["masks"]])
        ones_bf = p.sb("ones_bf", [128, 128], BF16); p.V(lambda e: e.memset(ones_bf[:], 1.0), w=[ones_bf])
        ones_f = p.sb("ones_f", [128, 128]); p.V(lambda e: e.memset(ones_f[:], 1.0), w=[ones_f])
        sc = p.sb("sc", [128, 8, 2]); p.dma(sc[:], I["c2"][:], w=[sc], r=[I["c2"]])
        p.A(lambda e: e.activation(out=sc[:], in_=sc[:], func=AF.Silu), w=[sc], r=[sc])
        mod = [p.sb("mod%d" % l, [128, 48, 2]) for l in range(2)]
        gsm = [p.sb("gsm%d" % l, [128, 8, 2]) for l in range(2)]
        gsf = [p.sb("gsf%d" % l, [128, 8, 2]) for l in range(2)]
        fng = p.sb("fng", [128, 8]); p.dma(fng[:], I["fngT"][:], w=[fng], r=[I["fngT"]])

        p.dma(X[:], I["xT"][:], w=[X], r=[I["xT"]])

        def stage_ada(l):
            with ExitStack() as st:
                wt = [p.sb("adaw", [128, 8, 512], es=st) for _ in range(2)]
                adab = p.sb("adab", [128, 48], es=st)
                ng = p.sb("ng", [128, 8], es=st); nf = p.sb("nf", [128, 8], es=st)
                pm = p.ps("pmod", [128, 48, 2], es=st)
                p.dma(adab[:], I["ada_bT"][l], w=[adab], r=[I["ada_bT"]])
                p.dma(ng[:], I["nmgT"][l], w=[ng], r=[I["nmgT"]])
                p.dma(nf[:], I["nfgT"][l], w=[nf], r=[I["nfgT"]])
                for cb in range(12):
                    w_ = wt[cb % 2]
                    p.dma(w_[:], I["ada_w"][l, :, cb * 512:(cb + 1) * 512].rearrange("(kc p) c -> p kc c", p=128),
                          w=[w_], r=[I["ada_w"]], q=("sp" if cb % 2 == 0 else "pool"))
                    for sub in range(4):
                        cc = cb * 4 + sub
                        for kc in range(8):
                            p.T(lambda e, w_=w_, cc=cc, kc=kc, sub=sub: e.matmul(pm[:, cc, :], lhsT=w_[:, kc, sub * 128:(sub + 1) * 128],
                                                                                rhs=sc[:, kc, :], start=(kc == 0), stop=(kc == 7)),
                                w=[pm], r=[w_, sc])
                m = mod[l]
                p.V(lambda e: e.tensor_tensor(out=m[:], in0=pm[:], in1=adab[:].unsqueeze(2).broadcast_to([128, 48, 2]), op=ALU.add),
                    w=[m], r=[pm, adab])
                for (gs, g, j) in ((gsm[l], ng, 1), (gsf[l], nf, 4)):
                    p.V(lambda e, gs=gs, g=g, j=j: e.scalar_tensor_tensor(out=gs[:], in0=m[:, j * 8:(j + 1) * 8, :], scalar=1.0,
                                                                         in1=g[:].unsqueeze(2).broadcast_to([128, 8, 2]),
                                                                         op0=ALU.add, op1=ALU.mult), w=[gs], r=[m, g])
                p.barrier()

        def norm_block(st_tiles, b0, n, gs_ap, sh_ap, hT, h32=None):
            xb, sq, pss, rstd, tmp = st_tiles
            p.dma(xb[:, :, :n], X[:, b0:b0 + n].rearrange("(kc p) t -> p kc t", p=128), w=[xb], r=[X])
            for kc in range(8):
                p.A(lambda e, kc=kc: e.activation(out=sq[:, kc, :n], in_=xb[:, kc, :n], func=AF.Square), w=[sq], r=[xb])
            for kc in range(8):
                p.T(lambda e, kc=kc: e.matmul(pss[:, :n], lhsT=ones_bf[:], rhs=sq[:, kc, :n], start=(kc == 0), stop=(kc == 7)),
                    w=[pss], r=[ones_bf, sq])
            p.A(lambda e: e.activation(out=rstd[:, :n], in_=pss[:, :n], func=AF.Sqrt, bias=1e-6, scale=1.0 / D), w=[rstd], r=[pss])
            p.V(lambda e: e.reciprocal(out=rstd[:, :n], in_=rstd[:, :n]), w=[rstd], r=[rstd])
            for kc in range(8):
                p.V(lambda e, kc=kc: e.scalar_tensor_tensor(out=tmp[:, kc, :n], in0=xb[:, kc, :n], scalar=gs_ap(kc), in1=rstd[:, :n],
                                                           op0=ALU.mult, op1=ALU.mult), w=[tmp], r=[xb, rstd])
                if sh_ap is not None:
                    if h32 is not None:
                        p.A(lambda e, kc=kc: e.activation(out=h32[:, kc, :n], in_=tmp[:, kc, :n], func=AF.Identity, bias=sh_ap(kc), scale=1.0),
                            w=[h32], r=[tmp])
                        p.G(lambda e, kc=kc: e.tensor_copy(out=hT[:, kc, b0:b0 + n], in_=h32[:, kc, :n]), w=[hT], r=[h32])
                    else:
                        p.A(lambda e, kc=kc: e.activation(out=hT[:, kc, b0:b0 + n], in_=tmp[:, kc, :n], func=AF.Identity, bias=sh_ap(kc), scale=1.0),
                            w=[hT], r=[tmp])

        def alloc_norm_tiles(st):
            return (p.sb("xb", [128, 8, 512], es=st), p.sb("sq", [128, 8, 512], BF16, es=st), p.ps("pss", [128, 512], es=st),
                    p.sb("rstd", [128, 512], es=st), p.sb("ntmp", [128, 8, 512], es=st))

        def stage_proj(l):
            with ExitStack() as st:
                hT = p.sb("hT", [128, 8, T], BF16, es=st)
                with ExitStack() as st2:
                    nt = alloc_norm_tiles(st2)
                    m = mod[l]
                    for (b0, n, seg) in BLKS:
                        norm_block(nt, b0, n, lambda kc, seg=seg: gsm[l][:, kc, seg:seg + 1], lambda kc, seg=seg: m[:, kc, seg:seg + 1], hT)
                    p.barrier()
                if "h" in dbg and l == dbg["h"]:
                    hd = p.dram("dbg_h", [D, T], BF16, kind="ExternalOutput")
                    p.dma(hd[:].rearrange("(kc p) t -> p kc t", p=128), hT[:], w=[hd], r=[hT])
                wf = [p.sb("wf", [128, 8, 512], es=st) for _ in range(2)]
                wb = [p.sb("wb", [128, 8, 512], BF16, es=st) for _ in range(2)]
                row = [p.sb("row", [128, T + 4], es=st) for _ in range(2)]
                tsm = p.sb("tsm", [128, T], es=st)
                mu = p.sb("mu", [128, 71], es=st); om = p.sb("om", [128, 71], es=st)
                pc = [p.ps("pc", [128, 512], es=st) for _ in range(4)]
                p.dma(mu[:], I["muT"][l], w=[mu], r=[I["muT"]])
                p.V(lambda e: e.tensor_scalar(out=om[:], in0=mu[:], scalar1=-1.0, scalar2=1.0, op0=ALU.mult, op1=ALU.add), w=[om], r=[mu])
                p.V(lambda e: e.tensor_scalar(out=mu[:], in0=mu[:], scalar1=0.5, scalar2=None, op0=ALU.mult), w=[mu], r=[mu])
                for r_ in row:
                    p.G(lambda e, r_=r_: e.memset(r_[:], 0.0), w=[r_])
                def rcol(t0):
                    return t0 + 1 if t0 < TC else t0 + 3
                ngrp = (INC + 511) // 512
                ei = 0
                for g in range(ngrp):
                    c0 = g * 512
                    ncg = min(512, INC - c0)
                    wf_, wb_ = wf[g % 2], wb[g % 2]
                    p.dma(wf_[:, :, :ncg], I["w_in"][l, :, c0:c0 + ncg].rearrange("(kc p) c -> p kc c", p=128), w=[wf_], r=[I["w_in"]],
                          q=("sp" if g % 2 == 0 else "pool"))
                    for kc in range(8):
                        if kc % 2 == 0:
                            p.V(lambda e, kc=kc: e.tensor_copy(out=wb_[:, kc, :ncg], in_=wf_[:, kc, :ncg]), w=[wb_], r=[wf_])
                        else:
                            p.G(lambda e, kc=kc: e.tensor_copy(out=wb_[:, kc, :ncg], in_=wf_[:, kc, :ncg]), w=[wb_], r=[wf_])
                    for sub in range((ncg + 127) // 128):
                        cb = g * 4 + sub
                        ncol = min(128, ncg - sub * 128)
                        rw_ = row[cb % 2]
                        for bi, (b0, n, seg) in enumerate(BLKS):
                            ps_ = pc[ei % 4]
                            for kc in range(8):
                                p.T(lambda e, kc=kc, ps_=ps_, n=n, b0=b0, sub=sub, ncol=ncol: e.matmul(
                                    ps_[:ncol, :n], lhsT=wb_[:, kc, sub * 128:sub * 128 + ncol], rhs=hT[:, kc, b0:b0 + n],
                                    start=(kc == 0), stop=(kc == 7)), w=[ps_], r=[wb_, hT])
                            rc = rcol(b0)
                            if ei % 2 == 0:
                                p.A(lambda e, ps_=ps_, rc=rc, n=n, ncol=ncol: e.copy(out=rw_[:ncol, rc:rc + n], in_=ps_[:ncol, :n]), w=[rw_], r=[ps_])
                            else:
                                p.V(lambda e, ps_=ps_, rc=rc, n=n, ncol=ncol: e.tensor_copy(out=rw_[:ncol, rc:rc + n], in_=ps_[:ncol, :n]), w=[rw_], r=[ps_])
                            ei += 1
                        col0 = cb * 128
                        if RW0 // 128 <= cb <= (DA0 - 1) // 128:
                            for (t0, n) in ((0, TC), (TC, TL)):
                                rc = rcol(t0)
                                p.V(lambda e, rc=rc, n=n, t0=t0: e.tensor_tensor(out=tsm[:ncol, t0:t0 + n], in0=rw_[:ncol, rc - 1:rc - 1 + n],
                                                                                 in1=rw_[:ncol, rc + 1:rc + 1 + n], op=ALU.add), w=[tsm], r=[rw_])
                                p.G(lambda e, n=n, t0=t0, cb=cb: e.tensor_scalar(out=tsm[:ncol, t0:t0 + n], in0=tsm[:ncol, t0:t0 + n],
                                                                                 scalar1=mu[:ncol, cb:cb + 1], scalar2=None, op0=ALU.mult), w=[tsm], r=[tsm, mu])
                                p.V(lambda e, rc=rc, n=n, t0=t0, cb=cb: e.scalar_tensor_tensor(out=tsm[:ncol, t0:t0 + n], in0=rw_[:ncol, rc:rc + n],
                                                                                               scalar=om[:ncol, cb:cb + 1], in1=tsm[:ncol, t0:t0 + n],
                                                                                               op0=ALU.mult, op1=ALU.add), w=[tsm], r=[rw_, om, tsm])
                            p.dma(PT[col0:col0 + ncol, :], tsm[:ncol, :], w=[PT], r=[tsm])
                        else:
                            p.dma(PT[col0:col0 + ncol, 0:TC], rw_[:ncol, 1:1 + TC], w=[PT], r=[rw_])
                            p.dma(PT[col0:col0 + ncol, TC:T], rw_[:ncol, TC + 3:T + 3], w=[PT], r=[rw_], q="pool")
                for (cstart, dst) in ((HG0 + 1536, VHG), (DA0 + 1024, VDA)):
                    wf_, wb_ = wf[0], wb[0]
                    p.dma(wf_[:], I["w_in"][l, :, cstart:cstart + 512].rearrange("(kc p) c -> p kc c", p=128), w=[wf_], r=[I["w_in"]])
                    for kc in range(8):
                        p.V(lambda e, kc=kc: e.tensor_copy(out=wb_[:, kc, :], in_=wf_[:, kc, :]), w=[wb_], r=[wf_])
                    for tt in range(18):
                        ps_ = pc[tt % 4]
                        for kc in range(8):
                            p.T(lambda e, kc=kc, ps_=ps_, tt=tt: e.matmul(ps_[:, :], lhsT=hT[:, kc, tt * 128:(tt + 1) * 128], rhs=wb_[:, kc, :],
                                                                          start=(kc == 0), stop=(kc == 7)), w=[ps_], r=[wb_, hT])
                        o_ = row[tt % 2]
                        if tt % 2 == 0:
                            p.A(lambda e, ps_=ps_, o_=o_: e.copy(out=o_[:, 0:512], in_=ps_[:, :]), w=[o_], r=[ps_])
                        else:
                            p.V(lambda e, ps_=ps_, o_=o_: e.tensor_copy(out=o_[:, 0:512], in_=ps_[:, :]), w=[o_], r=[ps_])
                        p.dma(dst[tt * 128:(tt + 1) * 128, :], o_[:, 0:512], w=[dst], r=[o_])
                p.barrier()


        def load_w_bf(st, src_ap, nk, cols, name, pk=128):
            wf_ = p.sb(name + "_f", [pk, nk, cols], es=st)
            wb_ = p.sb(name + "_b", [pk, nk, cols], BF16, es=st)
            p.dma(wf_[:], src_ap, w=[wf_], r=[])
            for k in range(nk):
                p.V(lambda e, k=k: e.tensor_copy(out=wb_[:, k, :], in_=wf_[:, k, :]), w=[wb_], r=[wf_])
            return wb_

        def branch_out(bt, zT, nk, pk, wproj, gi, b0, n):
            gt, sgt, po, yo = bt
            for oc in range(8):
                p.dma(gt[:, :n], PT[GT0 + gi * 1024 + oc * 128:GT0 + gi * 1024 + (oc + 1) * 128, b0:b0 + n], w=[gt], r=[PT],
                      q=("sp" if oc % 2 == 0 else "pool"))
                p.A(lambda e: e.activation(out=sgt[:, :n], in_=gt[:, :n], func=AF.Sigmoid), w=[sgt], r=[gt])
                po_ = po[oc % 2]
                for k in range(nk):
                    p.T(lambda e, k=k, oc=oc, po_=po_: e.matmul(po_[:, :n], lhsT=wproj[:pk, k, oc * 128:(oc + 1) * 128], rhs=zT[:pk, k, :n],
                                                               start=(k == 0), stop=(k == nk - 1)), w=[po_], r=[wproj, zT])
                yo_ = yo[oc % 2]
                p.V(lambda e, po_=po_, yo_=yo_: e.tensor_tensor(out=yo_[:, :n], in0=po_[:, :n], in1=sgt[:, :n], op=ALU.mult), w=[yo_], r=[po_, sgt])
                p.dma(YG[gi][oc * 128:(oc + 1) * 128, b0:b0 + n], yo_[:, :n], w=[YG[gi]], r=[yo_], q=("pool" if oc % 2 == 0 else "sp"))

        def alloc_branch_tiles(st):
            return (p.sb("gt", [128, 512], es=st), p.sb("sgt", [128, 512], es=st),
                    [p.ps("po", [128, 512], es=st) for _ in range(2)], [p.sb("yo", [128, 512], es=st) for _ in range(2)])

        def stage_hg(l):
            with ExitStack() as st:
                lb = p.sb("lb", [128, 2, 4], es=st); oml = p.sb("oml", [128, 2, 4], es=st)
                if l == 0:
                    p.V(lambda e: e.memset(lb[:], 0.0), w=[lb])
                else:
                    lg0 = p.sb("lg0", [128, 2, 4], es=st)
                    p.dma(lg0[:], I["hg_lbT"][0].rearrange("d p h -> p d h"), w=[lg0], r=[I["hg_lbT"]])
                    p.dma(lb[:], I["hg_lbT"][1].rearrange("d p h -> p d h"), w=[lb], r=[I["hg_lbT"]])
                    p.V(lambda e: e.tensor_tensor(out=lb[:], in0=lb[:], in1=lg0[:], op=ALU.subtract), w=[lb], r=[lb, lg0])
                    p.A(lambda e: e.activation(out=lb[:], in_=lb[:], func=AF.Sigmoid), w=[lb], r=[lb])
                p.V(lambda e: e.tensor_scalar(out=oml[:], in0=lb[:], scalar1=-1.0, scalar2=1.0, op0=ALU.mult, op1=ALU.add), w=[oml], r=[lb])
                S = [p.sb("S", [128, 4, 128], es=st) for _ in range(2)]
                for d in range(2):
                    p.G(lambda e, d=d: e.memset(S[d][:], 0.0), w=[S[d]])
                names = ("qtl", "ktl", "qh", "kh")
                prep = [[{nm: p.sb(nm, [128, 4, 256], es=st) for nm in names} for _ in range(2)] for _ in range(2)]
                ebC = [[p.sb("ebC", [128, 4, 8], es=st) for _ in range(2)] for _ in range(2)]
                Vg = [p.sb("Vg", [32, 8, 512], es=st) for _ in range(2)]
                og = [p.sb("og", [128, 4, 256], es=st) for _ in range(2)]
                tmp = {nm: [p.sb(nm, [128, 256], es=st) for _ in range(2)] for nm in ("zt", "qt", "lf", "F", "E", "kg", "X", "ex")}
                ones256 = p.sb("ones256", [128, 256], es=st); p.V(lambda e: e.memset(ones256[:], 1.0), w=[ones256])
                khT = [p.sb("khT", [32, 512], es=st) for _ in range(2)]
                AT = [p.sb("AT", [32, 4, 32], es=st) for _ in range(2)]
                pT = [p.ps("pT", [32, 512], es=st) for _ in range(2)]
                pA = [p.ps("pA", [32, 4, 32], es=st) for _ in range(2)]
                pO = [p.ps("pO", [128, 4, 32], es=st) for _ in range(2)]
                pS = [p.ps("pS", [128, 4, 128], es=st) for _ in range(2)]
                bwd_groups = [0, 8, 7, 6, 5, 4, 3, 2, 1]
                ti = 0
                for step in range(9):
                    for d in range(2):
                        g = step if d == 0 else bwd_groups[step]
                        t0 = g * 256
                        pr = prep[d][step % 2]
                        eb = ebC[d][step % 2]
                        p.dma(Vg[d][:], VHG[t0:t0 + 256, :].rearrange("(c s) v -> s c v", s=32), w=[Vg[d]], r=[VHG], q="pool")
                        for h in range(4):
                            tt = {nm: tmp[nm][ti % 2] for nm in tmp}
                            ti += 1
                            zt, qt, lf, Ft, Et, kg, Xt, ex = (tt[nm] for nm in ("zt", "qt", "lf", "F", "E", "kg", "X", "ex"))
                            zr = 512 * (1 + d) + h * 128
                            p.dma(zt[:], PT[zr:zr + 128, t0:t0 + 256], w=[zt], r=[PT])
                            p.dma(qt[:], PT[h * 128:(h + 1) * 128, t0:t0 + 256], w=[qt], r=[PT])
                            p.A(lambda e: e.activation(out=zt[:], in_=zt[:], func=AF.Sigmoid), w=[zt], r=[zt])
                            p.V(lambda e, d=d, h=h: e.tensor_scalar(out=zt[:], in0=zt[:], scalar1=oml[:, d, h:h + 1], scalar2=lb[:, d, h:h + 1],
                                                                   op0=ALU.mult, op1=ALU.add), w=[zt], r=[zt, oml, lb])
                            p.A(lambda e: e.activation(out=lf[:], in_=zt[:], func=AF.Ln), w=[lf], r=[zt])
                            p.G(lambda e: e.tensor_scalar(out=kg[:], in0=zt[:], scalar1=-1.0, scalar2=1.0, op0=ALU.mult, op1=ALU.add), w=[kg], r=[zt])
                            p.A(lambda e: e.activation(out=qt[:], in_=qt[:], func=AF.Silu), w=[qt], r=[qt])
                            p.V(lambda e: e.tensor_tensor_scan(out=Ft[:], data0=ones256[:], data1=lf[:], initial=0.0, op0=ALU.mult, op1=ALU.add),
                                w=[Ft], r=[ones256, lf])
                            p.G(lambda e: e.tensor_tensor(out=Et[:], in0=Ft[:], in1=lf[:], op=ALU.subtract), w=[Et], r=[Ft, lf])
                            F3 = Ft[:].rearrange("p (c t) -> p c t", t=32); E3 = Et[:].rearrange("p (c t) -> p c t", t=32)
                            X3 = Xt[:].rearrange("p (c t) -> p c t", t=32)
                            bc = lambda ap: ap.broadcast_to([128, 8, 32])
                            if d == 0:
                                plan = [(F3, F3[:, :, 15:16], [("qtl", qt, 1.0), ("ktl", kg, -1.0)]),
                                        (F3, E3[:, :, 0:1], [("qh", qt, 1.0)]),
                                        (F3, F3[:, :, 31:32], [("kh", kg, -1.0)])]
                            else:
                                plan = [(E3, E3[:, :, 16:17], [("qtl", qt, -1.0), ("ktl", kg, 1.0)]),
                                        (E3, F3[:, :, 31:32], [("qh", qt, -1.0)]),
                                        (E3, E3[:, :, 0:1], [("kh", kg, 1.0)])]
                            for (src3, ref, outs) in plan:
                                p.V(lambda e, src3=src3, ref=ref: e.tensor_tensor(out=X3, in0=src3, in1=bc(ref), op=ALU.subtract), w=[Xt], r=[Ft, Et])
                                for (nm, mul, sgn) in outs:
                                    p.A(lambda e, sgn=sgn: e.activation(out=ex[:], in_=Xt[:], func=AF.Exp, scale=sgn), w=[ex], r=[Xt])
                                    p.V(lambda e, nm=nm, mul=mul, h=h: e.tensor_tensor(out=pr[nm][:, h, :], in0=mul[:], in1=ex[:], op=ALU.mult),
                                        w=[pr[nm]], r=[mul, ex])
                            p.V(lambda e, h=h: e.tensor_tensor(out=eb[:, h, :], in0=F3[:, :, 31], in1=E3[:, :, 0], op=ALU.subtract), w=[eb], r=[Ft, Et])
                            p.A(lambda e, h=h: e.activation(out=eb[:, h, :], in_=eb[:, h, :], func=AF.Exp), w=[eb], r=[eb])
                        mi = 1 if d == 0 else 3
                        order = range(8) if d == 0 else range(7, -1, -1)
                        for c in order:
                            cs = c * 32
                            for h in range(4):
                                p.T(lambda e, h=h, cs=cs: e.transpose(pT[d][:32, h * 128:(h + 1) * 128], pr["kh"][:, h, cs:cs + 32], ident[:]),
                                    w=[pT[d]], r=[pr["kh"], ident])
                            p.A(lambda e: e.copy(out=khT[d][:], in_=pT[d][:]), w=[khT[d]], r=[pT[d]])
                            for h in range(4):
                                p.T(lambda e, h=h, cs=cs: e.matmul(pA[d][:, h, :], lhsT=pr["ktl"][:, h, cs:cs + 32], rhs=pr["qtl"][:, h, cs:cs + 32],
                                                                  start=True, stop=True), w=[pA[d]], r=[pr["ktl"], pr["qtl"]])
                            p.V(lambda e: e.tensor_tensor(out=AT[d][:], in0=pA[d][:], in1=masks[0:32, mi, 0:32].unsqueeze(1).broadcast_to([32, 4, 32]),
                                                          op=ALU.mult), w=[AT[d]], r=[pA[d], masks])
                            for h in range(4):
                                p.T(lambda e, h=h, c=c: e.matmul(pO[d][:, h, :], lhsT=Vg[d][:, c, h * 128:(h + 1) * 128], rhs=AT[d][:, h, :],
                                                                start=True, stop=False), w=[pO[d]], r=[Vg[d], AT[d]])
                                p.T(lambda e, h=h, cs=cs: e.matmul(pO[d][:, h, :], lhsT=S[d][:, h, :], rhs=pr["qh"][:, h, cs:cs + 32],
                                                                  start=False, stop=True), w=[pO[d]], r=[S[d], pr["qh"]])
                            p.A(lambda e, cs=cs: e.copy(out=og[d][:, :, cs:cs + 32], in_=pO[d][:]), w=[og[d]], r=[pO[d]])
                            for h in range(4):
                                p.T(lambda e, h=h, c=c: e.matmul(pS[d][:, h, :], lhsT=khT[d][:, h * 128:(h + 1) * 128], rhs=Vg[d][:, c, h * 128:(h + 1) * 128],
                                                                start=True, stop=True), w=[pS[d]], r=[khT[d], Vg[d]])
                            for h in range(4):
                                p.V(lambda e, h=h, c=c: e.scalar_tensor_tensor(out=S[d][:, h, :], in0=S[d][:, h, :], scalar=eb[:, h, c:c + 1], in1=pS[d][:, h, :],
                                                                              op0=ALU.mult, op1=ALU.add), w=[S[d]], r=[S[d], eb, pS[d]])
                        p.dma(OHG[d, :, t0:t0 + 256].rearrange("(h v) t -> v h t", v=128), og[d][:], w=[OHG], r=[og[d]])
                p.barrier()
            with ExitStack() as st:
                wproj = load_w_bf(st, I["hg_proj"][l].rearrange("(k p) c -> p k c", p=128), 4, D, "hgp")
                ngv = p.sb("ngv", [128, 1], es=st); p.dma(ngv[:], I["hg_ngT"][l], w=[ngv], r=[I["hg_ngT"]])
                bt = alloc_branch_tiles(st)
                oa = p.sb("oa", [128, 4, 512], es=st); ob = p.sb("ob", [128, 4, 512], es=st); gg = p.sb("gg", [128, 4, 512], es=st)
                sq = p.sb("sqh", [128, 4, 512], BF16, es=st); zT = p.sb("zT", [128, 4, 512], BF16, es=st)
                pn = p.ps("pn", [128, 512], es=st); rs = p.sb("rs", [128, 512], es=st)
                for (b0, n, seg) in BLKS:
                    p.dma(oa[:, :, :n], OHG[0, :, b0:b0 + n].rearrange("(h v) t -> v h t", v=128), w=[oa], r=[OHG])
                    p.dma(ob[:, :, :n], OHG[1, :, b0:b0 + n].rearrange("(h v) t -> v h t", v=128), w=[ob], r=[OHG], q="pool")
                    p.dma(gg[:, :, :n], PT[2048:2560, b0:b0 + n].rearrange("(h v) t -> v h t", v=128), w=[gg], r=[PT])
                    p.V(lambda e: e.tensor_tensor(out=oa[:, :, :n], in0=oa[:, :, :n], in1=ob[:, :, :n], op=ALU.add), w=[oa], r=[oa, ob])
                    p.A(lambda e: e.activation(out=gg[:, :, :n], in_=gg[:, :, :n], func=AF.Silu), w=[gg], r=[gg])
                    p.G(lambda e: e.tensor_tensor(out=sq[:, :, :n], in0=oa[:, :, :n], in1=oa[:, :, :n], op=ALU.mult), w=[sq], r=[oa])
                    for h in range(4):
                        p.T(lambda e, h=h: e.matmul(pn[:, :n], lhsT=ones_bf[:], rhs=sq[:, h, :n], start=True, stop=True), w=[pn], r=[ones_bf, sq])
                        p.A(lambda e: e.activation(out=rs[:, :n], in_=pn[:, :n], func=AF.Sqrt, bias=1e-6, scale=1.0 / 128), w=[rs], r=[pn])
                        p.V(lambda e: e.reciprocal(out=rs[:, :n], in_=rs[:, :n]), w=[rs], r=[rs])
                        p.V(lambda e, h=h: e.scalar_tensor_tensor(out=oa[:, h, :n], in0=oa[:, h, :n], scalar=ngv[:, 0:1], in1=rs[:, :n],
                                                                 op0=ALU.mult, op1=ALU.mult), w=[oa], r=[oa, ngv, rs])
                        p.V(lambda e, h=h: e.tensor_tensor(out=zT[:, h, :n], in0=oa[:, h, :n], in1=gg[:, h, :n], op=ALU.mult), w=[zT], r=[oa, gg])
                    branch_out(bt, zT, 4, 128, wproj, 0, b0, n)
                p.barrier()


        def stage_da(l):
            lam_init = 0.8 - 0.6 * math.exp(-0.3 * l)
            scale = 64 ** -0.5
            with ExitStack() as st:
                lp = p.sb("lp", [128, 4, 64], es=st); s12 = p.sb("s12", [128, 2], es=st); nlam = p.sb("nlam", [128, 1], es=st)
                pr_ = p.sb("lpp", [128, 2, 64], es=st)
                p.dma(lp[:].rearrange("p a b -> p (a b)"), I["da_lambda"][l].partition_broadcast(128), w=[lp], r=[I["da_lambda"]])
                p.V(lambda e: e.tensor_tensor(out=pr_[:, 0, :], in0=lp[:, 0, :], in1=lp[:, 1, :], op=ALU.mult), w=[pr_], r=[lp])
                p.V(lambda e: e.tensor_tensor(out=pr_[:, 1, :], in0=lp[:, 2, :], in1=lp[:, 3, :], op=ALU.mult), w=[pr_], r=[lp])
                p.V(lambda e: e.reduce_sum(out=s12[:], in_=pr_[:], axis=AX.X), w=[s12], r=[pr_])
                p.A(lambda e: e.activation(out=s12[:], in_=s12[:], func=AF.Exp), w=[s12], r=[s12])
                p.V(lambda e: e.tensor_tensor(out=nlam[:], in0=s12[:, 1:2], in1=s12[:, 0:1], op=ALU.subtract), w=[nlam], r=[s12])
                p.V(lambda e: e.tensor_scalar(out=nlam[:], in0=nlam[:], scalar1=-lam_init, scalar2=None, op0=ALU.add), w=[nlam], r=[nlam])
                sg = p.sb("sg", [128, 128], es=st)
                p.dma(sg[:], I["da_sg"][l].partition_broadcast(128), w=[sg], r=[I["da_sg"]])
                p.V(lambda e: e.tensor_scalar(out=sg[:], in0=sg[:], scalar1=1.0 - lam_init, scalar2=None, op0=ALU.mult), w=[sg], r=[sg])
                KT = p.sb("KT", [64, 8, T], BF16, es=st); QT = p.sb("QT", [64, 8, T], BF16, es=st)
                Vb = p.sb("Vb", [128, 18, 512], BF16, es=st)
                with ExitStack() as st2:
                    cs = p.sb("cs", [64, 2, TL], es=st2); p.dma(cs[:], I["ropeCS"][:], w=[cs], r=[I["ropeCS"]])
                    rR = p.sb("rR", [64, 64], es=st2); p.dma(rR[:], I["ropeR"][:], w=[rR], r=[I["ropeR"]])
                    xr = [p.sb("xr", [64, T], es=st2) for _ in range(2)]
                    t1 = [p.sb("t1", [64, 512], es=st2) for _ in range(2)]; t2 = [p.sb("t2", [64, 512], es=st2) for _ in range(2)]
                    pr2 = [p.ps("prp", [64, 512], es=st2) for _ in range(2)]
                    vf = [p.sb("vf", [128, 512], es=st2) for _ in range(2)]
                    i2 = 0
                    for qi, (dst, roff) in enumerate(((QT, DA0), (KT, DA0 + 512))):
                        for hm in range(8):
                            x_ = xr[hm % 2]
                            p.dma(x_[:], PT[roff + hm * 64:roff + (hm + 1) * 64, :], w=[x_], r=[PT], q=("sp" if hm % 2 == 0 else "pool"))
                            p.A(lambda e, hm=hm, dst=dst, x_=x_: e.copy(out=dst[:, hm, 0:TC], in_=x_[:, 0:TC]), w=[dst], r=[x_])
                            for bi in range(4):
                                a0 = TC + bi * 512
                                pp = pr2[i2 % 2]; t1_ = t1[i2 % 2]; t2_ = t2[i2 % 2]; i2 += 1
                                p.T(lambda e, pp=pp, x_=x_, a0=a0: e.matmul(pp[:], lhsT=rR[:], rhs=x_[:, a0:a0 + 512], start=True, stop=True), w=[pp], r=[rR, x_])
                                p.V(lambda e, t1_=t1_, x_=x_, a0=a0, bi=bi: e.tensor_tensor(out=t1_[:], in0=x_[:, a0:a0 + 512], in1=cs[:, 0, bi * 512:(bi + 1) * 512], op=ALU.mult),
                                    w=[t1_], r=[x_, cs])
                                p.V(lambda e, t2_=t2_, pp=pp, bi=bi: e.tensor_tensor(out=t2_[:], in0=pp[:], in1=cs[:, 1, bi * 512:(bi + 1) * 512], op=ALU.mult),
                                    w=[t2_], r=[pp, cs])
                                p.G(lambda e, dst=dst, hm=hm, a0=a0, t1_=t1_, t2_=t2_: e.tensor_tensor(out=dst[:, hm, a0:a0 + 512], in0=t1_[:], in1=t2_[:], op=ALU.add),
                                    w=[dst], r=[t1_, t2_])
                    for kt in range(18):
                        v_ = vf[kt % 2]
                        p.dma(v_[:], VDA[kt * 128:(kt + 1) * 128, :], w=[v_], r=[VDA], q=("sp" if kt % 2 == 0 else "pool"))
                        p.V(lambda e, kt=kt, v_=v_: e.tensor_copy(out=Vb[:, kt, :], in_=v_[:]), w=[Vb], r=[v_])
                    p.barrier()
                wproj = load_w_bf(st, I["da_proj"][l].rearrange("(k p) c -> p k c", p=128), 4, D, "dap")
                bt = alloc_branch_tiles(st)
                pS = [p.ps("pSc", [128, 512], es=st) for _ in range(2)]
                Eb = [p.sb("Eb", [128, 18, 512], BF16, es=st) for _ in range(2)]
                acc = [p.ps("acc", [128, 4, 128], es=st) for _ in range(2)]
                den = p.ps("den", [128, 2, 4], es=st)
                pZ = p.ps("pZ", [128, 512], es=st)
                rden = p.sb("rden", [128, 2, 4], es=st); rl = p.sb("rl", [128, 4], es=st)
                o1 = p.sb("o1", [128, 4, 128], es=st); o2 = p.sb("o2", [128, 4, 128], es=st); ssq = p.sb("ssq", [128, 4], es=st)
                zT = p.sb("zTd", [128, 4, 512], BF16, es=st)
                qblocks = [(0, 256, [0, 1])] + [(TC + i * 512, 512, list(range(18))) for i in range(4)]
                ei = [0]

                def phase1(q0, nq, kts, hm):
                    Eb_ = Eb[hm % 2]
                    for kt in kts:
                        pS_ = pS[ei[0] % 2]; ei[0] += 1
                        p.T(lambda e, pS_=pS_, kt=kt: e.matmul(pS_[:, :nq], lhsT=KT[:, hm, kt * 128:(kt + 1) * 128], rhs=QT[:, hm, q0:q0 + nq],
                                                              start=True, stop=True), w=[pS_], r=[KT, QT])
                        p.A(lambda e, pS_=pS_, kt=kt: e.activation(out=Eb_[:, kt, :nq], in_=pS_[:, :nq], func=AF.Exp, scale=scale), w=[Eb_], r=[pS_])

                def phase2(q0, nq, kts, hm):
                    Eb_ = Eb[hm % 2]; h = hm // 2; m = hm % 2
                    for j in range(nq // 128):
                        for kt in kts:
                            p.T(lambda e, j=j, kt=kt: e.matmul(acc[m][:, j, :], lhsT=Eb_[:, kt, j * 128:(j + 1) * 128], rhs=Vb[:, kt, h * 128:(h + 1) * 128],
                                                              start=(kt == kts[0]), stop=(kt == kts[-1])), w=[acc[m]], r=[Eb_, Vb])
                        for kt in kts:
                            p.T(lambda e, j=j, kt=kt: e.matmul(den[:, m, j:j + 1], lhsT=Eb_[:, kt, j * 128:(j + 1) * 128], rhs=ones_bf[:, 0:1],
                                                              start=(kt == kts[0]), stop=(kt == kts[-1])), w=[den], r=[Eb_, ones_bf])

                for (q0, nq, kts) in qblocks:
                    nj = nq // 128
                    phase1(q0, nq, kts, 0)
                    for hm in range(8):
                        if hm + 1 < 8:
                            phase1(q0, nq, kts, hm + 1)
                        phase2(q0, nq, kts, hm)
                        if hm % 2 == 0:
                            continue
                        h = hm // 2
                        p.V(lambda e: e.reciprocal(out=rden[:, :, :nj], in_=den[:, :, :nj]), w=[rden], r=[den])
                        p.V(lambda e: e.tensor_scalar(out=rl[:, :nj], in0=rden[:, 1, :nj], scalar1=nlam[:, 0:1], scalar2=None, op0=ALU.mult), w=[rl], r=[rden, nlam])
                        p.V(lambda e: e.tensor_tensor(out=o1[:, :nj, :], in0=acc[0][:, :nj, :], in1=rden[:, 0, :nj].unsqueeze(2).broadcast_to([128, nj, 128]), op=ALU.mult),
                            w=[o1], r=[acc[0], rden])
                        p.V(lambda e: e.tensor_tensor(out=o2[:, :nj, :], in0=acc[1][:, :nj, :], in1=rl[:, :nj].unsqueeze(2).broadcast_to([128, nj, 128]), op=ALU.mult),
                            w=[o2], r=[acc[1], rl])
                        p.G(lambda e: e.tensor_tensor(out=o1[:, :nj, :], in0=o1[:, :nj, :], in1=o2[:, :nj, :], op=ALU.add), w=[o1], r=[o1, o2])
                        p.G(lambda e: e.tensor_tensor(out=o2[:, :nj, :], in0=o1[:, :nj, :], in1=o1[:, :nj, :], op=ALU.mult), w=[o2], r=[o1])
                        p.V(lambda e: e.reduce_sum(out=ssq[:, :nj], in_=o2[:, :nj, :], axis=AX.X), w=[ssq], r=[o2])
                        p.A(lambda e: e.activation(out=ssq[:, :nj], in_=ssq[:, :nj], func=AF.Sqrt, bias=1e-5, scale=1.0 / 128), w=[ssq], r=[ssq])
                        p.V(lambda e: e.reciprocal(out=ssq[:, :nj], in_=ssq[:, :nj]), w=[ssq], r=[ssq])
                        p.V(lambda e: e.tensor_tensor(out=o1[:, :nj, :], in0=o1[:, :nj, :], in1=ssq[:, :nj].unsqueeze(2).broadcast_to([128, nj, 128]), op=ALU.mult),
                            w=[o1], r=[o1, ssq])
                        p.G(lambda e: e.tensor_tensor(out=o1[:, :nj, :], in0=o1[:, :nj, :], in1=sg[:].unsqueeze(1).broadcast_to([128, nj, 128]), op=ALU.mult),
                            w=[o1], r=[o1, sg])
                        for j in range(nj):
                            p.T(lambda e, j=j: e.transpose(pZ[:, j * 128:(j + 1) * 128], o1[:, j, :], ident[:]), w=[pZ], r=[o1, ident])
                        p.A(lambda e, h=h: e.copy(out=zT[:, h, :nq], in_=pZ[:, :nq]), w=[zT], r=[pZ])
                    branch_out(bt, zT, 4, 128, wproj, 2, q0, nq)
                p.barrier()


        RWN = ("R", "K", "V", "AN", "B", "LW0", "LW1", "BON", "G")
        RWS = {nm: p.dram("RWS_" + nm, [64, 8, T]) for nm in RWN}
        YRW = [p.dram("YRW%d" % d, [64, 8, T]) for d in range(2)]

        def stage_rw_prep(l):
            with ExitStack() as st:
                vec = p.sb("rwvec", [64, 8, 8], es=st); p.dma(vec[:], I["rw_vecT"][l], w=[vec], r=[I["rw_vecT"]])
                w0 = p.sb("rww0", [64, 2, 8], es=st); p.dma(w0[:], I["rw_w0T"][l].rearrange("d k h -> k d h"), w=[w0], r=[I["rw_w0T"]])
                w2 = p.sb("rww2", [64, 2, 512], es=st); p.dma(w2[:], I["rw_w2"][l].rearrange("d j c -> j d c"), w=[w2], r=[I["rw_w2"]])
                a2 = p.sb("rwa2", [64, 512], es=st); p.dma(a2[:], I["rw_a2"][l], w=[a2], r=[I["rw_a2"]])
                g2 = p.sb("rwg2", [128, 512], es=st); p.dma(g2[:], I["rw_g2"][l], w=[g2], r=[I["rw_g2"]])
                n = 256
                mk = lambda nm: p.sb(nm, [64, 8, n], es=st)
                r_, kr, v_, a_, kk, sq, nrm, k_, b_, an, rk, bon, g_ = (mk(x) for x in ("r_", "kr", "v_", "a_", "kk", "sq", "nrm", "k_", "b_", "an", "rk", "bon", "g_"))
                lw = [mk("lw0"), mk("lw1")]
                adt = p.sb("adt", [64, n], es=st); wd = [p.sb("wdf", [64, n], es=st), p.sb("wdb", [64, n], es=st)]
                gdt = p.sb("gdt", [128, n], es=st); th = p.sb("th", [64, n], es=st)
                pool = [p.ps("prw", [64, 2, n], es=st) for _ in range(4)]
                pi = [0]

                def nps():
                    pi[0] += 1
                    return pool[pi[0] % 4]
                bc = lambda i: vec[:, i, :].unsqueeze(2).broadcast_to([64, 8, n])
                for blk in range(9):
                    b0 = blk * n
                    hk = lambda c0: PT[c0:c0 + 512, b0:b0 + n].rearrange("(h k) t -> k h t", k=64)
                    p.dma(r_[:], hk(RW_R), w=[r_], r=[PT]); p.dma(kr[:], hk(RW_K), w=[kr], r=[PT], q="pool"); p.dma(v_[:], hk(RW_V), w=[v_], r=[PT])
                    p.dma(adt[:], PT[RW_AD:RW_AD + 64, b0:b0 + n], w=[adt], r=[PT], q="pool")
                    p.dma(wd[0][:], PT[RW_WF:RW_WF + 64, b0:b0 + n], w=[wd[0]], r=[PT]); p.dma(wd[1][:], PT[RW_WB:RW_WB + 64, b0:b0 + n], w=[wd[1]], r=[PT], q="pool")
                    p.dma(gdt[:], PT[RW_GD:RW_GD + 128, b0:b0 + n], w=[gdt], r=[PT])
                    for hp in range(4):
                        ps_ = nps()
                        for hh in range(2):
                            h = hp * 2 + hh
                            p.T(lambda e, h=h, hh=hh, ps_=ps_: e.matmul(ps_[:, hh, :], lhsT=a2[:, h * 64:(h + 1) * 64], rhs=adt[:], start=True, stop=True), w=[ps_], r=[a2, adt])
                        for hh in range(2):
                            h = hp * 2 + hh
                            p.A(lambda e, h=h, hh=hh, ps_=ps_: e.activation(out=a_[:, h, :], in_=ps_[:, hh, :], func=AF.Sigmoid, bias=vec[:, 2, h:h + 1], scale=1.0),
                                w=[a_], r=[ps_, vec])
                    p.V(lambda e: e.tensor_tensor(out=kk[:], in0=kr[:], in1=bc(0), op=ALU.mult), w=[kk], r=[kr, vec])
                    p.G(lambda e: e.tensor_tensor(out=sq[:], in0=kk[:], in1=kk[:], op=ALU.mult), w=[sq], r=[kk])
                    for hp in range(4):
                        ps_ = nps()
                        p.T(lambda e, hp=hp, ps_=ps_: e.matmul(ps_[:].rearrange("p a b -> p (a b)"), lhsT=ones_f[:64, :64],
                                                              rhs=sq[:, 2 * hp:2 * hp + 2, :].rearrange("p a b -> p (a b)"), start=True, stop=True), w=[ps_], r=[ones_f, sq])
                        p.A(lambda e, hp=hp, ps_=ps_: e.activation(out=nrm[:, 2 * hp:2 * hp + 2, :], in_=ps_[:], func=AF.Sqrt), w=[nrm], r=[ps_])
                    p.V(lambda e: e.tensor_scalar(out=nrm[:], in0=nrm[:], scalar1=1e-12, scalar2=None, op0=ALU.max), w=[nrm], r=[nrm])
                    p.V(lambda e: e.reciprocal(out=nrm[:], in_=nrm[:]), w=[nrm], r=[nrm])
                    p.V(lambda e: e.tensor_tensor(out=kk[:], in0=kk[:], in1=nrm[:], op=ALU.mult), w=[kk], r=[kk, nrm])
                    p.V(lambda e: e.scalar_tensor_tensor(out=k_[:], in0=a_[:], scalar=-1.0, in1=bc(1), op0=ALU.add, op1=ALU.mult), w=[k_], r=[a_, vec])
                    p.V(lambda e: e.scalar_tensor_tensor(out=k_[:], in0=k_[:], scalar=1.0, in1=kr[:], op0=ALU.add, op1=ALU.mult), w=[k_], r=[k_, kr])
                    p.G(lambda e: e.tensor_tensor(out=b_[:], in0=kk[:], in1=a_[:], op=ALU.mult), w=[b_], r=[kk, a_])
                    p.G(lambda e: e.tensor_scalar(out=an[:], in0=kk[:], scalar1=-1.0, scalar2=None, op0=ALU.mult), w=[an], r=[kk])
                    p.V(lambda e: e.tensor_tensor(out=rk[:], in0=r_[:], in1=k_[:], op=ALU.mult), w=[rk], r=[r_, k_])
                    p.V(lambda e: e.tensor_tensor(out=rk[:], in0=rk[:], in1=bc(3), op=ALU.mult), w=[rk], r=[rk, vec])
                    for hp in range(4):
                        ps_ = nps()
                        p.T(lambda e, hp=hp, ps_=ps_: e.matmul(ps_[:].rearrange("p a b -> p (a b)"), lhsT=ones_f[:64, :64],
                                                              rhs=rk[:, 2 * hp:2 * hp + 2, :].rearrange("p a b -> p (a b)"), start=True, stop=True), w=[ps_], r=[ones_f, rk])
                        p.V(lambda e, hp=hp, ps_=ps_: e.tensor_tensor(out=bon[:, 2 * hp:2 * hp + 2, :], in0=ps_[:], in1=v_[:, 2 * hp:2 * hp + 2, :], op=ALU.mult),
                            w=[bon], r=[ps_, v_])
                    p.A(lambda e: e.activation(out=gdt[:], in_=gdt[:], func=AF.Sigmoid), w=[gdt], r=[gdt])
                    for hp in range(4):
                        ps_ = nps()
                        for hh in range(2):
                            h = hp * 2 + hh
                            p.T(lambda e, h=h, hh=hh, ps_=ps_: e.matmul(ps_[:, hh, :], lhsT=g2[:, h * 64:(h + 1) * 64], rhs=gdt[:], start=True, stop=True), w=[ps_], r=[g2, gdt])
                        p.A(lambda e, hp=hp, ps_=ps_: e.copy(out=g_[:, 2 * hp:2 * hp + 2, :], in_=ps_[:]), w=[g_], r=[ps_])
                    for d in range(2):
                        p.A(lambda e, d=d: e.activation(out=th[:], in_=wd[d][:], func=AF.Tanh), w=[th], r=[wd[d]])
                        for hp in range(4):
                            ps_ = nps()
                            for hh in range(2):
                                h = hp * 2 + hh
                                p.T(lambda e, h=h, hh=hh, ps_=ps_, d=d: e.matmul(ps_[:, hh, :], lhsT=w2[:, d, h * 64:(h + 1) * 64], rhs=th[:], start=True, stop=True), w=[ps_], r=[w2, th])
                            for hh in range(2):
                                h = hp * 2 + hh
                                p.A(lambda e, h=h, hh=hh, ps_=ps_, d=d: e.activation(out=lw[d][:, h, :], in_=ps_[:, hh, :], func=AF.Sigmoid, bias=w0[:, d, h:h + 1], scale=1.0),
                                    w=[lw[d]], r=[ps_, w0])
                        p.V(lambda e, d=d: e.tensor_scalar(out=lw[d][:], in0=lw[d][:], scalar1=-math.exp(-0.5), scalar2=None, op0=ALU.mult), w=[lw[d]], r=[lw[d]])
                    for qi, (nm, tl) in enumerate((("R", r_), ("K", k_), ("V", v_), ("AN", an), ("B", b_), ("LW0", lw[0]), ("LW1", lw[1]), ("BON", bon), ("G", g_))):
                        p.dma(RWS[nm][:, :, b0:b0 + n], tl[:], w=[RWS[nm]], r=[tl], q=("sp" if qi % 2 == 0 else "pool"))
                p.barrier()

        def stage_rw_scan(l):
            with ExitStack() as st:
                mk = lambda nm, shp=(64, 8, 64): [p.sb(nm, list(shp), es=st) for _ in range(2)]
                ST = mk("ST")
                for d in range(2):
                    p.G(lambda e, d=d: e.memset(ST[d][:], 0.0), w=[ST[d]])
                rmask = p.sb("rmask", [64, 8, 64], es=st)
                p.V(lambda e: e.memset(rmask[:], 1.0), w=[rmask]); p.V(lambda e: e.memset(rmask[:, :, 0:1], 0.0), w=[rmask])
                ld = {nm: mk("l" + nm) for nm in ("R", "K", "V", "AN", "B", "LW")}
                Ft, Et, Gt, Ht = mk("F"), mk("E"), mk("G"), mk("H")
                ex = [mk("ex0"), mk("ex1")]
                AR = mk("AR", (64, 8, 2, 64)); bt, kt_, bh, kh = mk("bt"), mk("kt"), mk("bh"), mk("kh")
                Vtm, Btm, Ktm = mk("Vtm"), mk("Btm"), mk("Ktm")
                W1 = mk("W1", (64, 8, 128)); W2 = mk("W2", (64, 8, 128)); Lm = mk("Lm"); Xm = mk("Xm")
                Pb = [mk("Pb0"), mk("Pb1")]; PTb = [mk("PTb0"), mk("PTb1")]
                RH, Um, yo = mk("RH"), mk("Um"), mk("yo")
                gC = mk("gC", (64, 8))
                pool = [p.ps("prs", [64, 8, 64], es=st) for _ in range(8)]
                pi = [0]

                def nps():
                    pi[0] += 1
                    return pool[pi[0] % 8]
                f2 = lambda b: b[:].rearrange("p h t -> p (h t)")
                bwd = [3, 2, 1, 0] + list(range(35, 3, -1))
                idb = ident[:64, :64].unsqueeze(1).broadcast_to([64, 8, 64])
                ei = [0]

                def exp_to(d, src, sgn):
                    e_ = ex[ei[0] % 2][d]; ei[0] += 1
                    p.A(lambda e: e.activation(out=e_[:], in_=src[:], func=AF.Exp, scale=sgn), w=[e_], r=[src])
                    return e_
                for step in range(36):
                    cd = (step, bwd[step])
                    for d in range(2):
                        t0 = cd[d] * 64
                        for qi, nm in enumerate(("R", "K", "V", "AN", "B", "LW")):
                            src = RWS["LW%d" % d] if nm == "LW" else RWS[nm]
                            p.dma(ld[nm][d][:], src[:, :, t0:t0 + 64], w=[ld[nm][d]], r=[src], q=("sp" if qi % 2 == 0 else "pool"))
                    for d in range(2):
                        lw = ld["LW"][d]; F_, E_, G_, H_ = Ft[d], Et[d], Gt[d], Ht[d]
                        p.V(lambda e: e.tensor_tensor_scan(out=f2(F_), data0=f2(rmask), data1=f2(lw), initial=0.0, op0=ALU.mult, op1=ALU.add), w=[F_], r=[rmask, lw])
                        p.G(lambda e: e.tensor_tensor(out=E_[:], in0=F_[:], in1=lw[:], op=ALU.subtract), w=[E_], r=[F_, lw])
                        fend = F_[:, :, 63:64].broadcast_to([64, 8, 64])
                        p.V(lambda e: e.tensor_tensor(out=G_[:], in0=fend, in1=F_[:], op=ALU.subtract), w=[G_], r=[F_])
                        p.A(lambda e: e.activation(out=gC[d][:], in_=F_[:, :, 63], func=AF.Exp), w=[gC[d]], r=[F_])
                        if d == 1:
                            p.V(lambda e: e.tensor_tensor(out=H_[:], in0=E_[:], in1=fend, op=ALU.subtract), w=[H_], r=[E_, F_])
                        R_, K_, AN_, B_ = ld["R"][d], ld["K"][d], ld["AN"][d], ld["B"][d]
                        specs = ((E_, 1.0), (F_, -1.0), (F_, 1.0), (G_, 1.0)) if d == 0 else ((G_, 1.0), (H_, 1.0), (H_, -1.0), (E_, 1.0))
                        e1 = exp_to(d, *specs[0])
                        p.V(lambda e: e.tensor_tensor(out=AR[d][:, :, 0, :], in0=AN_[:], in1=e1[:], op=ALU.mult), w=[AR[d]], r=[AN_, e1])
                        e2 = exp_to(d, *specs[1])
                        p.V(lambda e: e.tensor_tensor(out=bt[d][:], in0=B_[:], in1=e2[:], op=ALU.mult), w=[bt[d]], r=[B_, e2])
                        p.G(lambda e: e.tensor_tensor(out=kt_[d][:], in0=K_[:], in1=e2[:], op=ALU.mult), w=[kt_[d]], r=[K_, e2])
                        e3 = exp_to(d, *specs[2])
                        p.V(lambda e: e.tensor_tensor(out=AR[d][:, :, 1, :], in0=R_[:], in1=e3[:], op=ALU.mult), w=[AR[d]], r=[R_, e3])
                        e4 = exp_to(d, *specs[3])
                        p.V(lambda e: e.tensor_tensor(out=bh[d][:], in0=B_[:], in1=e4[:], op=ALU.mult), w=[bh[d]], r=[B_, e4])
                        p.G(lambda e: e.tensor_tensor(out=kh[d][:], in0=K_[:], in1=e4[:], op=ALU.mult), w=[kh[d]], r=[K_, e4])
                    for d in range(2):
                        for (src, dst) in ((ld["V"][d], Vtm[d]), (bh[d], Btm[d]), (kh[d], Ktm[d])):
                            ps_ = nps()
                            for h in range(8):
                                p.T(lambda e, h=h, ps_=ps_, src=src: e.transpose(ps_[:, h, :], src[:, h, :], ident[:64, :64]), w=[ps_], r=[src, ident])
                            p.A(lambda e, ps_=ps_, dst=dst: e.copy(out=dst[:], in_=ps_[:]), w=[dst], r=[ps_])
                    for d in range(2):
                        cm = masks[:, 2 * d:2 * d + 2, :].rearrange("p a b -> p (a b)").unsqueeze(1).broadcast_to([64, 4, 128])
                        for (lh, Wd) in ((bt[d], W1[d]), (kt_[d], W2[d])):
                            for half in range(2):
                                ps_ = nps()
                                pv = ps_[:].rearrange("p h t -> p (h t)").rearrange("p (a b) -> p a b", b=128)
                                for hh in range(4):
                                    h = half * 4 + hh
                                    p.T(lambda e, h=h, hh=hh, pv=pv, lh=lh: e.matmul(pv[:, hh, :], lhsT=lh[:, h, :], rhs=AR[d][:, h, :, :].rearrange("p a b -> p (a b)"),
                                                                                   start=True, stop=True), w=[ps_], r=[lh, AR[d]])
                                p.V(lambda e, pv=pv, Wd=Wd, half=half: e.tensor_tensor(out=Wd[:, half * 4:(half + 1) * 4, :], in0=pv, in1=cm, op=ALU.mult), w=[Wd], r=[ps_, masks])
                        ps_ = nps()
                        for h in range(8):
                            p.T(lambda e, h=h, ps_=ps_: e.matmul(ps_[:, h, :], lhsT=AR[d][:, h, 0, :], rhs=bt[d][:, h, :], start=True, stop=True), w=[ps_], r=[AR[d], bt[d]])
                        mnt = masks[:, 2 - 2 * d, :].unsqueeze(1).broadcast_to([64, 8, 64])
                        p.V(lambda e, ps_=ps_: e.tensor_tensor(out=Lm[d][:], in0=ps_[:], in1=mnt, op=ALU.mult), w=[Lm[d]], r=[ps_, masks])
                        p.G(lambda e: e.tensor_tensor(out=Xm[d][:], in0=W1[d][:, :, 0:64], in1=idb, op=ALU.add), w=[Xm[d]], r=[W1[d], ident])
                    cur = [(lambda h, d=d: W1[d][:, h, 0:64], W1[d], lambda h, d=d: Lm[d][:, h, :], Lm[d]) for d in range(2)]
                    for i in range(1, 6):
                        for d in range(2):
                            Pf, Pbuf, PTf, PTbuf = cur[d]
                            nP, nPT = Pb[i % 2][d], PTb[i % 2][d]
                            if i < 5:
                                ps_ = nps()
                                for h in range(8):
                                    p.T(lambda e, h=h, ps_=ps_: e.matmul(ps_[:, h, :], lhsT=PTf(h), rhs=Pf(h), start=True, stop=True), w=[ps_], r=[Pbuf, PTbuf])
                                p.A(lambda e, ps_=ps_, nP=nP: e.copy(out=nP[:], in_=ps_[:]), w=[nP], r=[ps_])
                            ps2 = nps()
                            for h in range(8):
                                p.T(lambda e, h=h, ps2=ps2: e.matmul(ps2[:, h, :], lhsT=Pf(h), rhs=PTf(h), start=True, stop=True), w=[ps2], r=[Pbuf, PTbuf])
                            p.V(lambda e, ps2=ps2, nPT=nPT: e.tensor_copy(out=nPT[:], in_=ps2[:]), w=[nPT], r=[ps2])
                            ps3 = nps()
                            for h in range(8):
                                p.T(lambda e, h=h, ps3=ps3, nPT=nPT: e.matmul(ps3[:, h, :], lhsT=nPT[:, h, :], rhs=Xm[d][:, h, :], start=True, stop=True), w=[ps3], r=[nPT, Xm[d]])
                            p.V(lambda e, ps3=ps3: e.tensor_tensor(out=Xm[d][:], in0=Xm[d][:], in1=ps3[:], op=ALU.add), w=[Xm[d]], r=[Xm[d], ps3])
                            cur[d] = (lambda h, nP=nP: nP[:, h, :], nP, lambda h, nPT=nPT: nPT[:, h, :], nPT)
                    for d in range(2):
                        ps_ = nps()
                        for h in range(8):
                            p.T(lambda e, h=h, ps_=ps_: e.matmul(ps_[:, h, :], lhsT=AR[d][:, h, 0, :], rhs=ST[d][:, h, :], start=True, stop=False), w=[ps_], r=[AR[d], ST[d]])
                            p.T(lambda e, h=h, ps_=ps_: e.matmul(ps_[:, h, :], lhsT=W2[d][:, h, 0:64], rhs=Vtm[d][:, h, :], start=False, stop=True), w=[ps_], r=[W2[d], Vtm[d]])
                        p.A(lambda e, ps_=ps_: e.copy(out=RH[d][:], in_=ps_[:]), w=[RH[d]], r=[ps_])
                        ps_ = nps()
                        for h in range(8):
                            p.T(lambda e, h=h, ps_=ps_: e.matmul(ps_[:, h, :], lhsT=Xm[d][:, h, :], rhs=RH[d][:, h, :], start=True, stop=True), w=[ps_], r=[Xm[d], RH[d]])
                        p.V(lambda e, ps_=ps_: e.tensor_copy(out=Um[d][:], in_=ps_[:]), w=[Um[d]], r=[ps_])
                        ps_ = nps()
                        for h in range(8):
                            p.T(lambda e, h=h, ps_=ps_: e.matmul(ps_[:, h, :], lhsT=ST[d][:, h, :], rhs=AR[d][:, h, 1, :], start=True, stop=False), w=[ps_], r=[ST[d], AR[d]])
                            p.T(lambda e, h=h, ps_=ps_: e.matmul(ps_[:, h, :], lhsT=Um[d][:, h, :], rhs=W1[d][:, h, 64:128], start=False, stop=False), w=[ps_], r=[Um[d], W1[d]])
                            p.T(lambda e, h=h, ps_=ps_: e.matmul(ps_[:, h, :], lhsT=Vtm[d][:, h, :], rhs=W2[d][:, h, 64:128], start=False, stop=True), w=[ps_], r=[Vtm[d], W2[d]])
                        p.A(lambda e, ps_=ps_: e.copy(out=yo[d][:], in_=ps_[:]), w=[yo[d]], r=[ps_])
                        t0 = cd[d] * 64
                        p.dma(YRW[d][:, :, t0:t0 + 64], yo[d][:], w=[YRW[d]], r=[yo[d]], q=("sp" if d == 0 else "pool"))
                        ps_ = nps()
                        for h in range(8):
                            p.T(lambda e, h=h, ps_=ps_: e.matmul(ps_[:, h, :], lhsT=Btm[d][:, h, :], rhs=Um[d][:, h, :], start=True, stop=False), w=[ps_], r=[Btm[d], Um[d]])
                            p.T(lambda e, h=h, ps_=ps_: e.matmul(ps_[:, h, :], lhsT=Ktm[d][:, h, :], rhs=Vtm[d][:, h, :], start=False, stop=True), w=[ps_], r=[Ktm[d], Vtm[d]])
                        p.V(lambda e: e.tensor_tensor(out=ST[d][:], in0=ST[d][:], in1=gC[d][:].unsqueeze(2).broadcast_to([64, 8, 64]), op=ALU.mult), w=[ST[d]], r=[ST[d], gC[d]])
                        p.V(lambda e, ps_=ps_: e.tensor_tensor(out=ST[d][:], in0=ST[d][:], in1=ps_[:], op=ALU.add), w=[ST[d]], r=[ST[d], ps_])
                p.barrier()

        def stage_rw_fin(l):
            with ExitStack() as st:
                vec = p.sb("rwvec", [64, 8, 8], es=st); p.dma(vec[:], I["rw_vecT"][l], w=[vec], r=[I["rw_vecT"]])
                wproj = load_w_bf(st, I["rw_proj"][l].rearrange("(h k) c -> k h c", k=64), 8, D, "rwp", pk=64)
                bt = alloc_branch_tiles(st)
                n = 256
                mk = lambda nm: p.sb(nm, [64, 8, n], es=st)
                ya, yb, bo, gg, sq2 = mk("ya"), mk("yb"), mk("bo"), mk("gg"), mk("sq2")
                zT = p.sb("zTr", [64, 8, 512], BF16, es=st)
                pool = [p.ps("prf", [64, 2, n], es=st) for _ in range(2)]
                bc = lambda i: vec[:, i, :].unsqueeze(2).broadcast_to([64, 8, n])
                pi = 0
                for blk in range(9):
                    b0 = blk * n
                    p.dma(ya[:], YRW[0][:, :, b0:b0 + n], w=[ya], r=[YRW[0]]); p.dma(yb[:], YRW[1][:, :, b0:b0 + n], w=[yb], r=[YRW[1]], q="pool")
                    p.dma(bo[:], RWS["BON"][:, :, b0:b0 + n], w=[bo], r=[RWS["BON"]]); p.dma(gg[:], RWS["G"][:, :, b0:b0 + n], w=[gg], r=[RWS["G"]], q="pool")
                    p.V(lambda e: e.tensor_tensor(out=ya[:], in0=ya[:], in1=yb[:], op=ALU.add), w=[ya], r=[ya, yb])
                    for hp in range(4):
                        ps_ = pool[pi % 2]; pi += 1
                        p.T(lambda e, hp=hp, ps_=ps_: e.matmul(ps_[:].rearrange("p a b -> p (a b)"), lhsT=ones_f[:64, :64],
                                                              rhs=ya[:, 2 * hp:2 * hp + 2, :].rearrange("p a b -> p (a b)"), start=True, stop=True), w=[ps_], r=[ones_f, ya])
                        p.V(lambda e, hp=hp, ps_=ps_: e.scalar_tensor_tensor(out=yb[:, 2 * hp:2 * hp + 2, :], in0=ps_[:], scalar=-1.0 / 64, in1=ya[:, 2 * hp:2 * hp + 2, :],
                                                                            op0=ALU.mult, op1=ALU.add), w=[yb], r=[ps_, ya])
                    p.G(lambda e: e.tensor_tensor(out=sq2[:], in0=yb[:], in1=yb[:], op=ALU.mult), w=[sq2], r=[yb])
                    for hp in range(4):
                        ps_ = pool[pi % 2]; pi += 1
                        p.T(lambda e, hp=hp, ps_=ps_: e.matmul(ps_[:].rearrange("p a b -> p (a b)"), lhsT=ones_f[:64, :64],
                                                              rhs=sq2[:, 2 * hp:2 * hp + 2, :].rearrange("p a b -> p (a b)"), start=True, stop=True), w=[ps_], r=[ones_f, sq2])
                        p.A(lambda e, hp=hp, ps_=ps_: e.activation(out=ya[:, 2 * hp:2 * hp + 2, :], in_=ps_[:], func=AF.Sqrt, bias=64e-5, scale=1.0 / 64), w=[ya], r=[ps_])
                    p.V(lambda e: e.reciprocal(out=ya[:], in_=ya[:]), w=[ya], r=[ya])
                    p.V(lambda e: e.tensor_tensor(out=yb[:], in0=yb[:], in1=ya[:], op=ALU.mult), w=[yb], r=[yb, ya])
                    p.V(lambda e: e.tensor_tensor(out=yb[:], in0=yb[:], in1=bc(4), op=ALU.mult), w=[yb], r=[yb, vec])
                    p.G(lambda e: e.tensor_tensor(out=yb[:], in0=yb[:], in1=bc(5), op=ALU.add), w=[yb], r=[yb, vec])
                    p.G(lambda e: e.tensor_tensor(out=yb[:], in0=yb[:], in1=bo[:], op=ALU.add), w=[yb], r=[yb, bo])
                    p.V(lambda e: e.tensor_tensor(out=zT[:, :, :n], in0=yb[:], in1=gg[:], op=ALU.mult), w=[zT], r=[yb, gg])
                    branch_out(bt, zT, 8, 64, wproj, 1, b0, n)
                p.barrier()


        def stage_merge(l):
            with ExitStack() as st:
                wo = load_w_bf(st, I["w_out"][l].rearrange("(k p) c -> p k c", p=128), 8, D, "wo")
                ya = [p.sb("mya", [128, 8, 512], es=st) for _ in range(3)]
                mT = p.sb("mT", [128, 8, 512], BF16, es=st)
                xb = p.sb("mxb", [128, 8, 512], es=st)
                po = [p.ps("mpo", [128, 512], es=st) for _ in range(2)]
                blks = BLKS if l == 0 else BLKS[1:]
                for (b0, n, seg) in blks:
                    for i in range(3):
                        p.dma(ya[i][:, :, :n], YG[i][:, b0:b0 + n].rearrange("(kc p) t -> p kc t", p=128), w=[ya[i]], r=[YG[i]], q=("sp" if i != 1 else "pool"))
                    p.dma(xb[:, :, :n], X[:, b0:b0 + n].rearrange("(kc p) t -> p kc t", p=128), w=[xb], r=[X], q="pool")
                    p.V(lambda e: e.tensor_tensor(out=ya[0][:, :, :n], in0=ya[0][:, :, :n], in1=ya[1][:, :, :n], op=ALU.add), w=[ya[0]], r=[ya[0], ya[1]])
                    p.V(lambda e: e.tensor_tensor(out=mT[:, :, :n], in0=ya[0][:, :, :n], in1=ya[2][:, :, :n], op=ALU.add), w=[mT], r=[ya[0], ya[2]])
                    for oc in range(8):
                        po_ = po[oc % 2]
                        for kc in range(8):
                            p.T(lambda e, kc=kc, oc=oc, po_=po_: e.matmul(po_[:, :n], lhsT=wo[:, kc, oc * 128:(oc + 1) * 128], rhs=mT[:, kc, :n],
                                                                         start=(kc == 0), stop=(kc == 7)), w=[po_], r=[wo, mT])
                        p.V(lambda e, oc=oc, po_=po_: e.scalar_tensor_tensor(out=xb[:, oc, :n], in0=po_[:, :n], scalar=mod[l][:, 16 + oc, seg:seg + 1], in1=xb[:, oc, :n],
                                                                            op0=ALU.mult, op1=ALU.add), w=[xb], r=[po_, mod[l], xb])
                    p.dma(X[:, b0:b0 + n].rearrange("(kc p) t -> p kc t", p=128), xb[:, :, :n], w=[X], r=[xb])
                p.barrier()

        def stage_ffn(l):
            moe = (l % 2 == 1)
            blks = BLKS[1:] if moe else BLKS
            nexp = NE if moe else 1
            UTs = p.dram("UT%d" % l, [nexp, FF, T], BF16)
            with ExitStack() as st:
                hT = p.sb("hTf", [128, 8, T], BF16, es=st)
                combT = p.sb("combT", [8, TL], es=st) if moe else None
                with ExitStack() as st2:
                    nt = alloc_norm_tiles(st2)
                    m = mod[l]
                    if moe:
                        h32 = p.sb("h32", [128, 8, 512], es=st2)
                        rt = p.sb("rt", [128, 8, NE], es=st2)
                        p.dma(rt[:], I["moe_router"][0].rearrange("(kc p) e -> p kc e", p=128), w=[rt], r=[I["moe_router"]])
                        lg = p.sb("lg", [128, 16, NE], es=st2)
                        plg = p.ps("plg", [128, NE], es=st2)
                    for bi, (b0, n, seg) in enumerate(blks):
                        norm_block(nt, b0, n, lambda kc, seg=seg: gsf[l][:, kc, seg:seg + 1], lambda kc, seg=seg: m[:, 24 + kc, seg:seg + 1], hT,
                                   h32=(h32 if moe else None))
                        if moe:
                            for j in range(4):
                                for kc in range(8):
                                    p.T(lambda e, kc=kc, j=j: e.matmul(plg[:], lhsT=h32[:, kc, j * 128:(j + 1) * 128], rhs=rt[:, kc, :], start=(kc == 0), stop=(kc == 7)),
                                        w=[plg], r=[h32, rt])
                                p.V(lambda e, j=j, bi=bi: e.tensor_copy(out=lg[:, bi * 4 + j, :], in_=plg[:]), w=[lg], r=[plg])
                    if moe:
                        m1 = p.sb("m1", [128, 16], es=st2); eq = p.sb("eq", [128, 16, NE], es=st2); l2 = p.sb("l2", [128, 16, NE], es=st2)
                        m2 = p.sb("m2", [128, 16], es=st2); ex = p.sb("exr", [128, 16, NE], es=st2); sm = p.sb("sm", [128, 16], es=st2)
                        b3 = lambda t_: t_[:].unsqueeze(2).broadcast_to([128, 16, NE])
                        p.V(lambda e: e.reduce_max(out=m1[:], in_=lg[:], axis=AX.X), w=[m1], r=[lg])
                        p.V(lambda e: e.tensor_tensor(out=eq[:], in0=lg[:], in1=b3(m1), op=ALU.is_equal), w=[eq], r=[lg, m1])
                        p.V(lambda e: e.scalar_tensor_tensor(out=l2[:], in0=eq[:], scalar=-1e30, in1=lg[:], op0=ALU.mult, op1=ALU.add), w=[l2], r=[eq, lg])
                        p.V(lambda e: e.reduce_max(out=m2[:], in_=l2[:], axis=AX.X), w=[m2], r=[l2])
                        p.V(lambda e: e.tensor_tensor(out=eq[:], in0=lg[:], in1=b3(m2), op=ALU.is_ge), w=[eq], r=[lg, m2])
                        p.V(lambda e: e.tensor_tensor(out=l2[:], in0=lg[:], in1=b3(m1), op=ALU.subtract), w=[l2], r=[lg, m1])
                        p.A(lambda e: e.activation(out=ex[:], in_=l2[:], func=AF.Exp), w=[ex], r=[l2])
                        p.V(lambda e: e.tensor_tensor(out=ex[:], in0=ex[:], in1=eq[:], op=ALU.mult), w=[ex], r=[ex, eq])
                        p.V(lambda e: e.reduce_sum(out=sm[:], in_=ex[:], axis=AX.X), w=[sm], r=[ex])
                        p.V(lambda e: e.reciprocal(out=sm[:], in_=sm[:]), w=[sm], r=[sm])
                        p.V(lambda e: e.tensor_tensor(out=ex[:], in0=ex[:], in1=b3(sm), op=ALU.mult), w=[ex], r=[ex, sm])
                        pct = p.ps("pct", [8, 512], es=st2)
                        for g4 in range(4):
                            for j in range(4):
                                p.T(lambda e, g4=g4, j=j: e.transpose(pct[:, j * 128:(j + 1) * 128], ex[:, g4 * 4 + j, :], ident[:]), w=[pct], r=[ex, ident])
                            p.V(lambda e, g4=g4: e.tensor_copy(out=combT[:, g4 * 512:(g4 + 1) * 512], in_=pct[:]), w=[combT], r=[pct])
                    p.barrier()
                if "comb" in dbg and moe:
                    cdd = p.dram("dbg_comb", [8, TL], F32, kind="ExternalOutput")
                    p.dma(cdd[:], combT[:], w=[cdd], r=[combT])
                with ExitStack() as st2:
                    wf1 = [p.sb("wf1", [128, 8, 512], es=st2) for _ in range(2)]; wf3 = [p.sb("wf3", [128, 8, 512], es=st2) for _ in range(2)]
                    wb1 = [p.sb("wb1", [128, 8, 512], BF16, es=st2) for _ in range(2)]; wb3 = [p.sb("wb3", [128, 8, 512], BF16, es=st2) for _ in range(2)]
                    pa = [p.ps("pa", [128, 512], es=st2) for _ in range(2)]; pb = [p.ps("pb", [128, 512], es=st2) for _ in range(2)]
                    pcb = p.ps("pcb", [128, 512], es=st2)
                    sl = [p.sb("sl", [128, 512], es=st2) for _ in range(2)]; ut = [p.sb("ut", [128, 512], BF16, es=st2) for _ in range(2)]
                    sel = p.sb("sel8", [8, 8, 128], es=st2)
                    p.dma(sel[:], I["sel8"][:].rearrange("e k m -> k e m"), w=[sel], r=[I["sel8"]])
                    cbc = p.sb("cbc", [128, TL], es=st2) if moe else None
                    gi = 0; ui = 0
                    for ex_ in range(nexp):
                        w1s = I["moe_w1"][0, ex_] if moe else I["ffn_w1"][0]
                        w3s = I["moe_w3"][0, ex_] if moe else I["ffn_w3"][0]
                        if moe:
                            for g4 in range(4):
                                p.T(lambda e, g4=g4, ex_=ex_: e.matmul(pcb[:], lhsT=sel[:, ex_, :], rhs=combT[:, g4 * 512:(g4 + 1) * 512], start=True, stop=True), w=[pcb], r=[sel, combT])
                                p.A(lambda e, g4=g4: e.copy(out=cbc[:, g4 * 512:(g4 + 1) * 512], in_=pcb[:]), w=[cbc], r=[pcb])
                        for fg in range(6):
                            f0 = fg * 512
                            nf = min(512, FF - f0)
                            a1, a3, c1, c3 = wf1[gi % 2], wf3[gi % 2], wb1[gi % 2], wb3[gi % 2]
                            gi += 1
                            p.dma(a1[:, :, :nf], w1s[:, f0:f0 + nf].rearrange("(kc p) c -> p kc c", p=128), w=[a1], r=[])
                            p.dma(a3[:, :, :nf], w3s[:, f0:f0 + nf].rearrange("(kc p) c -> p kc c", p=128), w=[a3], r=[], q="pool")
                            for kc in range(8):
                                p.V(lambda e, kc=kc: e.tensor_copy(out=c1[:, kc, :nf], in_=a1[:, kc, :nf]), w=[c1], r=[a1])
                                p.G(lambda e, kc=kc: e.tensor_copy(out=c3[:, kc, :nf], in_=a3[:, kc, :nf]), w=[c3], r=[a3])
                            for sub in range(nf // 128):
                                fb = fg * 4 + sub
                                for (b0, n, seg) in blks:
                                    pa_, pb_, sl_, ut_ = pa[ui % 2], pb[ui % 2], sl[ui % 2], ut[ui % 2]
                                    ui += 1
                                    for kc in range(8):
                                        p.T(lambda e, kc=kc, sub=sub, pa_=pa_: e.matmul(pa_[:, :n], lhsT=c1[:, kc, sub * 128:(sub + 1) * 128], rhs=hT[:, kc, b0:b0 + n],
                                                                                       start=(kc == 0), stop=(kc == 7)), w=[pa_], r=[c1, hT])
                                    for kc in range(8):
                                        p.T(lambda e, kc=kc, sub=sub, pb_=pb_: e.matmul(pb_[:, :n], lhsT=c3[:, kc, sub * 128:(sub + 1) * 128], rhs=hT[:, kc, b0:b0 + n],
                                                                                       start=(kc == 0), stop=(kc == 7)), w=[pb_], r=[c3, hT])
                                    p.A(lambda e, pa_=pa_, sl_=sl_: e.activation(out=sl_[:, :n], in_=pa_[:, :n], func=AF.Silu), w=[sl_], r=[pa_])
                                    if moe:
                                        p.G(lambda e, sl_=sl_, b0=b0: e.tensor_tensor(out=sl_[:, :n], in0=sl_[:, :n], in1=cbc[:, b0 - TC:b0 - TC + n], op=ALU.mult), w=[sl_], r=[sl_, cbc])
                                    p.V(lambda e, pb_=pb_, sl_=sl_, ut_=ut_: e.tensor_tensor(out=ut_[:, :n], in0=sl_[:, :n], in1=pb_[:, :n], op=ALU.mult), w=[ut_], r=[sl_, pb_])
                                    p.dma(UTs[ex_, fb * 128:(fb + 1) * 128, b0:b0 + n], ut_[:, :n], w=[UTs], r=[ut_], q=("sp" if ui % 2 == 0 else "pool"))
                    p.barrier()
            with ExitStack() as st:
                w2f = p.sb("w2f", [128, 2, D], es=st); w2b = p.sb("w2b", [128, 22, D], BF16, es=st)
                uu = [p.sb("uu", [128, 22, 512], BF16, es=st) for _ in range(2)]
                acc = p.sb("facc", [128, 8, 512], es=st); xb = p.sb("fxb", [128, 8, 512], es=st)
                po = [p.ps("fpo", [128, 512], es=st) for _ in range(4)]
                ui = 0; oi = 0
                for ex_ in range(nexp):
                    w2s = I["moe_w2"][0, ex_] if moe else I["ffn_w2"][0]
                    for f2_ in range(11):
                        p.dma(w2f[:], w2s[f2_ * 256:(f2_ + 1) * 256, :].rearrange("(k p) c -> p k c", p=128), w=[w2f], r=[], q=("sp" if f2_ % 2 == 0 else "pool"))
                        p.V(lambda e, f2_=f2_: e.tensor_copy(out=w2b[:, 2 * f2_, :], in_=w2f[:, 0, :]), w=[w2b], r=[w2f])
                        p.G(lambda e, f2_=f2_: e.tensor_copy(out=w2b[:, 2 * f2_ + 1, :], in_=w2f[:, 1, :]), w=[w2b], r=[w2f])
                    for (b0, n, seg) in blks:
                        u_ = uu[ui % 2]; ui += 1
                        p.dma(u_[:, :, :n], UTs[ex_, :, b0:b0 + n].rearrange("(k p) t -> p k t", p=128), w=[u_], r=[UTs])
                        if ex_ > 0:
                            p.dma(acc[:, :, :n], FACC[:, b0:b0 + n].rearrange("(kc p) t -> p kc t", p=128), w=[acc], r=[FACC], q="pool")
                        last = (ex_ == nexp - 1)
                        if last:
                            p.dma(xb[:, :, :n], X[:, b0:b0 + n].rearrange("(kc p) t -> p kc t", p=128), w=[xb], r=[X], q="pool")
                        for oc in range(8):
                            po_ = po[oi % 4]; oi += 1
                            for k in range(22):
                                p.T(lambda e, k=k, oc=oc, po_=po_, u_=u_: e.matmul(po_[:, :n], lhsT=w2b[:, k, oc * 128:(oc + 1) * 128], rhs=u_[:, k, :n],
                                                                                  start=(k == 0), stop=(k == 21)), w=[po_], r=[w2b, u_])
                            if ex_ == 0:
                                p.A(lambda e, oc=oc, po_=po_: e.copy(out=acc[:, oc, :n], in_=po_[:, :n]), w=[acc], r=[po_])
                            else:
                                p.V(lambda e, oc=oc, po_=po_: e.tensor_tensor(out=acc[:, oc, :n], in0=acc[:, oc, :n], in1=po_[:, :n], op=ALU.add), w=[acc], r=[acc, po_])
                            if last:
                                p.V(lambda e, oc=oc: e.scalar_tensor_tensor(out=xb[:, oc, :n], in0=acc[:, oc, :n], scalar=mod[l][:, 40 + oc, seg:seg + 1], in1=xb[:, oc, :n],
                                                                           op0=ALU.mult, op1=ALU.add), w=[xb], r=[acc, mod[l], xb])
                        if last:
                            p.dma(X[:, b0:b0 + n].rearrange("(kc p) t -> p kc t", p=128), xb[:, :, :n], w=[X], r=[xb])
                        else:
                            p.dma(FACC[:, b0:b0 + n].rearrange("(kc p) t -> p kc t", p=128), acc[:, :, :n], w=[FACC], r=[acc])
                p.barrier()

        def stage_final():
            with ExitStack() as st:
                nt = alloc_norm_tiles(st)
                xb, sq, pss, rstd, tmp = nt
                for (b0, n, seg) in BLKS[1:]:
                    norm_block(nt, b0, n, lambda kc: fng[:, kc:kc + 1], None, None)
                    p.dma(outT[:, b0 - TC:b0 - TC + n].rearrange("(kc p) t -> p kc t", p=128), tmp[:, :, :n], w=[outT], r=[tmp])
                p.barrier()

        for l in range(n_layers):
            stage_ada(l)
        if "mod" in dbg:
            md = p.dram("dbg_mod", [2, 128, 96], F32, kind="ExternalOutput")
            for l in range(2):
                p.dma(md[l], mod[l][:].rearrange("p a b -> p (a b)"), w=[md], r=[mod[l]])
        for l in range(n_layers):
            stage_proj(l)
            if stop_after == ("proj", l):
                break
            if "hg" not in skip:
                stage_hg(l)
            if stop_after == ("hg", l):
                break
            if "da" not in skip:
                stage_da(l)
            if stop_after == ("da", l):
                break
            if "rw" not in skip:
                stage_rw_prep(l)
                stage_rw_scan(l)
                stage_rw_fin(l)
            if stop_after == ("rw", l):
                break
            if "merge" not in skip:
                stage_merge(l)
            if stop_after == ("merge", l):
                break
            if "ffn" not in skip:
                stage_ffn(l)
            if stop_after == ("ffn", l):
                break
        if stop_after is None:
            stage_final()
        if "X" in dbg:
            xd = p.dram("dbg_X", [D, T], F32, kind="ExternalOutput")
            p.dma(xd[:], X[:], w=[xd], r=[X])
        if "YG1" in dbg:
            yd = p.dram("dbg_YG1", [D, T], F32, kind="ExternalOutput")
            p.dma(yd[:], YG[1][:], w=[yd], r=[YG[1]])
            for nm in RWN:
                dd = p.dram("dbg_RWS_" + nm, [64, 8, T], F32, kind="ExternalOutput")
                p.dma(dd[:], RWS[nm][:], w=[dd], r=[RWS[nm]])
            for d in range(2):
                dd = p.dram("dbg_YRW%d" % d, [64, 8, T], F32, kind="ExternalOutput")
                p.dma(dd[:], YRW[d][:], w=[dd], r=[YRW[d]])
        if "YG2" in dbg:
            yd = p.dram("dbg_YG2", [D, T], F32, kind="ExternalOutput")
            p.dma(yd[:], YG[2][:], w=[yd], r=[YG[2]])
        if "YG0" in dbg:
            yd = p.dram("dbg_YG0", [D, T], F32, kind="ExternalOutput")
            p.dma(yd[:], YG[0][:], w=[yd], r=[YG[0]])
            od = p.dram("dbg_OHG", [2, 512, T], F32, kind="ExternalOutput")
            p.dma(od[:], OHG[:], w=[od], r=[OHG])
        if "PT" in dbg:
            pd = p.dram("dbg_PT", [INC, T], F32, kind="ExternalOutput")
            p.dma(pd[:], PT[:], w=[pd], r=[PT])
            vd = p.dram("dbg_VHG", [T, 512], F32, kind="ExternalOutput")
            p.dma(vd[:], VHG[:], w=[vd], r=[VHG])
        p.barrier()
        print("instrs", p.ninstr, "waits", p.nwait)
    return nc


def host_inputs(inputs, b):
    f = np.float32
    g = {}
    x = np.asarray(inputs["x"][b], f); ctx = np.asarray(inputs["ctx"][b], f)
    g["xT"] = np.ascontiguousarray(np.concatenate([ctx.T, x.T], axis=1))
    c2 = np.stack([np.asarray(inputs["c"][b], f), np.asarray(inputs["c_ctx"], f)], axis=-1)
    g["c2"] = np.ascontiguousarray(c2.reshape(8, 128, 2).transpose(1, 0, 2))
    return g


def shared_inputs(inputs):
    f = np.float32
    g = {}
    A = lambda k: np.asarray(inputs[k], f)
    g["ada_w"] = A("ada_w")
    g["ada_bT"] = np.ascontiguousarray(A("ada_b").reshape(2, 48, 128).transpose(0, 2, 1))
    g["nmgT"] = np.ascontiguousarray(A("norm_mix_g").reshape(2, 8, 128).transpose(0, 2, 1))
    g["nfgT"] = np.ascontiguousarray(A("norm_ffn_g").reshape(2, 8, 128).transpose(0, 2, 1))
    g["fngT"] = np.ascontiguousarray(A("final_norm_g").reshape(8, 128).T)
    g["w_in"] = A("w_in")
    mu_full = np.zeros((2, 71 * 128), f)
    mu_full[:, RW0:DA0] = A("rw_mu")
    g["muT"] = np.ascontiguousarray(mu_full.reshape(2, 71, 128).transpose(0, 2, 1))
    g["ident"] = np.eye(128, dtype=f)
    i = np.arange(64)[:, None]; j = np.arange(64)[None, :]
    g["masks"] = np.ascontiguousarray(np.stack([(i < j), (i <= j), (i > j), (i >= j)], axis=1).astype(f))
    g["hg_lbT"] = np.ascontiguousarray(A("hg_lb_logits").reshape(2, 2, 4, 128).transpose(0, 1, 3, 2))
    g["hg_ngT"] = np.ascontiguousarray(A("hg_norm_g").reshape(2, 128, 1))
    g["hg_proj"] = A("hg_proj")
    g["w_out"] = A("w_out")
    for k in ("ffn_w1", "ffn_w3", "ffn_w2", "moe_router", "moe_w1", "moe_w3", "moe_w2", "da_proj", "rw_w2", "rw_a2", "rw_g2", "rw_proj"):
        g[k] = A(k)
    sel = np.zeros((8, 8, 128), f)
    for e in range(8):
        sel[e, e, :] = 1.0
    g["sel8"] = sel
    g["da_lambda"] = A("da_lambda").reshape(2, 256)
    g["da_sg"] = A("da_subln_g")
    t = np.arange(TL)
    rowi = (t // 64).astype(f); coli = (t % 64).astype(f)
    inv_freq = (1.0 / (10000.0 ** (np.arange(0, 32, 2, dtype=f) / f(32)))).astype(f)
    ang = np.zeros((64, TL), f)
    for d in range(64):
        jj = d % 16
        ang[d] = (rowi if d < 32 else coli) * inv_freq[jj]
    g["ropeCS"] = np.ascontiguousarray(np.stack([np.cos(ang), np.sin(ang)], axis=1).astype(f))
    R = np.zeros((64, 64), f)
    for base in (0, 32):
        for q in range(16):
            R[base + q, base + 16 + q] = -1.0
            R[base + 16 + q, base + q] = 1.0
    g["ropeR"] = np.ascontiguousarray(R.T)
    vec = np.zeros((2, 8, 512), f)
    vec[:, 0] = A("rw_k_k"); vec[:, 1] = A("rw_k_a"); vec[:, 2] = A("rw_a0"); vec[:, 3] = A("rw_r_k").reshape(2, 512)
    vec[:, 4] = A("rw_ln_g"); vec[:, 5] = A("rw_ln_b")
    g["rw_vecT"] = np.ascontiguousarray(vec.reshape(2, 8, 8, 64).transpose(0, 3, 1, 2))
    g["rw_w0T"] = np.ascontiguousarray(A("rw_w0").reshape(2, 2, 8, 64).transpose(0, 1, 3, 2))
    return g


_NC_CACHE = {}


def kernel(**inputs):
    if "nc" not in _NC_CACHE:
        _NC_CACHE["nc"] = build()
    nc = _NC_CACHE["nc"]
    sh = shared_inputs(inputs)
    in_maps = []
    for b in range(8):
        m = dict(sh)
        m.update(host_inputs(inputs, b))
        in_maps.append(m)
    res = run_bass_kernel_spmd(nc, in_maps, core_ids=list(range(8)))
    out = np.stack([np.ascontiguousarray(r["outT"].T) for r in res.results], axis=0)
    return out.astype(np.float32)
```

```python
import math
import numpy as np
from contextlib import ExitStack
import concourse.bass as bass
import concourse.mybir as mybir
from concourse.bass_utils import run_bass_kernel_spmd

F32 = mybir.dt.float32
BF16 = mybir.dt.bfloat16
AF = mybir.ActivationFunctionType
ALU = mybir.AluOpType
AX = mybir.AxisListType

SAME_ENGINE_SYNC = True
N_DMA_SEMS = 40
SEM_EPOCH = 16000

T = 2304
TC = 256
TL = 2048
D = 1024
INC = 9024
FF = 2816
NE = 8
BLKS = [(0, 256, 1), (256, 512, 0), (768, 512, 0), (1280, 512, 0), (1792, 512, 0)]
HG0 = 0
RW0 = 2560
DA0 = 4416
GT0 = 5952
RW_R, RW_K, RW_V, RW_WF, RW_WB, RW_AD, RW_GD = RW0, RW0 + 512, RW0 + 1024, RW0 + 1536, RW0 + 1600, RW0 + 1664, RW0 + 1728


class Buf:
    def __init__(self, name, h):
        self.name = name
        self.h = h
        self.w = None
        self.r = {}

    def __getitem__(self, idx):
        return self.h[idx]


class Prog:
    ENG = ("pe", "act", "dve", "pool", "sp")

    def __init__(self, nc, es):
        self.nc = nc
        self.es = es
        self.e = dict(pe=nc.tensor, act=nc.scalar, dve=nc.vector, pool=nc.gpsimd, sp=nc.sync)
        self.sem = {}
        self.ekey = {}
        for k in self.ENG:
            self.ekey[k] = (k, 0)
            self.sem[(k, 0)] = es.enter_context(nc.semaphore("s_" + k))
        self.cnt = {k: 0 for k in self.ENG}
        self.dsem = [es.enter_context(nc.semaphore("d%d" % i)) for i in range(N_DMA_SEMS)]
        for i in range(N_DMA_SEMS):
            self.sem[("d", i)] = self.dsem[i]
        self.dval = [0] * N_DMA_SEMS
        self.dnext = 0
        self.seen = {k: {} for k in self.ENG}
        self.ninstr = {k: 0 for k in self.ENG}
        self.nwait = 0
        self.uid = 0

    def sb(self, name, shape, dtype=F32, es=None):
        self.uid += 1
        h = (es or self.es).enter_context(self.nc.sbuf_tensor("%s_%d" % (name, self.uid), list(shape), dtype))
        return Buf(name, h)

    def ps(self, name, shape, dtype=F32, es=None):
        self.uid += 1
        h = (es or self.es).enter_context(self.nc.psum_tensor("%s_%d" % (name, self.uid), list(shape), dtype))
        return Buf(name, h)

    def dram(self, name, shape, dtype=F32, kind="Internal"):
        h = self.nc.dram_tensor(name, list(shape), dtype, kind=kind)
        return Buf(name, h.ap())

    def _wait(self, ek, k, v):
        if self.seen[ek].get(k, 0) >= v:
            return
        self.e[ek].wait_ge(self.sem[k], v)
        self.seen[ek][k] = v
        self.nwait += 1

    def _deps(self, ek, r, w):
        deps = {}

        def add(ev):
            if ev is None:
                return
            k, v = ev
            if deps.get(k, 0) < v:
                deps[k] = v

        for b in r:
            add(b.w)
        for b in w:
            add(b.w)
            for k, v in b.r.items():
                add((k, v))
        for k, v in deps.items():
            if k[0] == ek and (not SAME_ENGINE_SYNC or ek in ("pe", "sp")):
                continue
            self._wait(ek, k, v)

    def _commit(self, ev, r, w):
        k, v = ev
        for b in w:
            b.w = ev
            b.r = {}
        for b in r:
            if b.r.get(k, 0) < v:
                b.r[k] = v

    def op(self, ek, fn, w=(), r=()):
        if self.cnt[ek] >= SEM_EPOCH:
            ep = self.ekey[ek][1] + 1
            self.ekey[ek] = (ek, ep)
            self.sem[(ek, ep)] = self.es.enter_context(self.nc.semaphore("s_%s_%d" % (ek, ep)))
            self.cnt[ek] = 0
        self._deps(ek, r, w)
        ins = fn(self.e[ek])
        self.cnt[ek] += 1
        key = self.ekey[ek]
        ins.then_inc(self.sem[key], 1)
        self.ninstr[ek] += 1
        self._commit((key, self.cnt[ek]), r, w)
        return ins

    def V(self, fn, w=(), r=()):
        return self.op("dve", fn, w, r)

    def A(self, fn, w=(), r=()):
        return self.op("act", fn, w, r)

    def G(self, fn, w=(), r=()):
        return self.op("pool", fn, w, r)

    def T(self, fn, w=(), r=()):
        return self.op("pe", fn, w, r)

    def dma(self, out, in_, w=(), r=(), q="sp", **kw):
        i = self.dnext
        self.dnext = (self.dnext + 1) % N_DMA_SEMS
        key = ("d", i)
        if self.dval[i] > 0:
            self._wait(q, key, self.dval[i])
        self._deps(q, r, w)
        ins = self.e[q].dma_start(out=out, in_=in_, **kw)
        self.dval[i] += 16
        ins.then_inc(self.dsem[i], 16)
        self.ninstr[q] += 1
        self._commit((key, self.dval[i]), r, w)
        return ins

    def barrier(self):
        for ek in self.ENG:
            for k in self.ENG:
                if k == ek:
                    continue
                key = self.ekey[k]
                if key[1] > 0:
                    self._wait(ek, (k, key[1] - 1), SEM_EPOCH)
                if self.cnt[k] > 0:
                    self._wait(ek, key, self.cnt[k])
            for i in range(N_DMA_SEMS):
                if self.dval[i] > 0:
                    self._wait(ek, ("d", i), self.dval[i])


def build(n_layers=2, dbg=(), stop_after=None, skip=()):
    nc = bass.Bass("TRN2", target_bir_lowering=False)
    with ExitStack() as es:
        p = Prog(nc, es)
        I = {}

        def inp(name, shape, dt=F32):
            I[name] = p.dram(name, shape, dt, kind="ExternalInput")
            return I[name]

        inp("xT", [D, T]); inp("c2", [128, 8, 2])
        inp("ada_w", [2, D, 6 * D]); inp("ada_bT", [2, 128, 48])
        inp("nmgT", [2, 128, 8]); inp("nfgT", [2, 128, 8]); inp("fngT", [128, 8])
        inp("w_in", [2, D, INC]); inp("muT", [2, 128, 71])
        inp("ident", [128, 128]); inp("masks", [64, 4, 64])
        inp("hg_lbT", [2, 2, 128, 4]); inp("hg_ngT", [2, 128, 1]); inp("hg_proj", [2, 512, D])
        inp("w_out", [2, D, D])
        inp("ffn_w1", [1, D, FF]); inp("ffn_w3", [1, D, FF]); inp("ffn_w2", [1, FF, D])
        inp("moe_router", [1, D, NE]); inp("moe_w1", [1, NE, D, FF]); inp("moe_w3", [1, NE, D, FF]); inp("moe_w2", [1, NE, FF, D])
        inp("sel8", [8, 8, 128])
        inp("da_lambda", [2, 256]); inp("da_sg", [2, 128]); inp("da_proj", [2, 512, D])
        inp("ropeCS", [64, 2, TL]); inp("ropeR", [64, 64])
        inp("rw_vecT", [2, 64, 8, 8]); inp("rw_w0T", [2, 2, 64, 8]); inp("rw_w2", [2, 2, 64, 512]); inp("rw_a2", [2, 64, 512])
        inp("rw_g2", [2, 128, 512]); inp("rw_proj", [2, 512, D])
        outT = p.dram("outT", [D, TL], F32, kind="ExternalOutput")

        X = p.dram("Xs", [D, T])
        PT = p.dram("PT", [INC, T])
        VHG = p.dram("VHG", [T, 512])
        VDA = p.dram("VDA", [T, 512])
        OHG = p.dram("OHG", [2, 512, T])
        YG = [p.dram("YG%d" % i, [D, T]) for i in range(3)]
        UT = p.dram("UT", [FF, T], BF16)
        FACC = p.dram("FACC", [D, T])
        dbg_out = {}

        ident = p.sb("ident", [128, 128]); p.dma(ident[:], I["ident"][:], w=[ident], r=[I["ident"]])
        masks = p.sb("masks", [64, 4, 64]); p.dma(masks[:], I["masks"][:], w=[masks], r=[I["masks"]])
        ones_bf = p.sb("ones_bf", [128, 128], BF16); p.V(lambda e: e.memset(ones_bf[:], 1.0), w=[ones_bf])
        ones_f = p.sb("ones_f", [128, 128]); p.V(lambda e: e.memset(ones_f[:], 1.0), w=[ones_f])
        ident_bf = p.sb("ident_bf", [128, 128], BF16); p.V(lambda e: e.tensor_copy(out=ident_bf[:], in_=ident[:]), w=[ident_bf], r=[ident])
        sc = p.sb("sc", [128, 8, 2]); p.dma(sc[:], I["c2"][:], w=[sc], r=[I["c2"]])
        p.A(lambda e: e.activation(out=sc[:], in_=sc[:], func=AF.Silu), w=[sc], r=[sc])
        mod = [p.sb("mod%d" % l, [128, 48, 2]) for l in range(2)]
        gsm = [p.sb("gsm%d" % l, [128, 8, 2]) for l in range(2)]
        gsf = [p.sb("gsf%d" % l, [128, 8, 2]) for l in range(2)]
        fng = p.sb("fng", [128, 8]); p.dma(fng[:], I["fngT"][:], w=[fng], r=[I["fngT"]])

        p.dma(X[:], I["xT"][:], w=[X], r=[I["xT"]])

        def stage_ada(l):
            with ExitStack() as st:
                wt = [p.sb("adaw", [128, 8, 512], es=st) for _ in range(2)]
                adab = p.sb("adab", [128, 48], es=st)
                ng = p.sb("ng", [128, 8], es=st); nf = p.sb("nf", [128, 8], es=st)
                pm = p.ps("pmod", [128, 48, 2], es=st)
                p.dma(adab[:], I["ada_bT"][l], w=[adab], r=[I["ada_bT"]])
                p.dma(ng[:], I["nmgT"][l], w=[ng], r=[I["nmgT"]])
                p.dma(nf[:], I["nfgT"][l], w=[nf], r=[I["nfgT"]])
                for cb in range(12):
                    w_ = wt[cb % 2]
                    p.dma(w_[:], I["ada_w"][l, :, cb * 512:(cb + 1) * 512].rearrange("(kc p) c -> p kc c", p=128),
                          w=[w_], r=[I["ada_w"]], q=("sp" if cb % 2 == 0 else "pool"))
                    for sub in range(4):
                        cc = cb * 4 + sub
                        for kc in range(8):
                            p.T(lambda e, w_=w_, cc=cc, kc=kc, sub=sub: e.matmul(pm[:, cc, :], lhsT=w_[:, kc, sub * 128:(sub + 1) * 128],
                                                                                rhs=sc[:, kc, :], start=(kc == 0), stop=(kc == 7)),
                                w=[pm], r=[w_, sc])
                m = mod[l]
                p.V(lambda e: e.tensor_tensor(out=m[:], in0=pm[:], in1=adab[:].unsqueeze(2).broadcast_to([128, 48, 2]), op=ALU.add),
                    w=[m], r=[pm, adab])
                for (gs, g, j) in ((gsm[l], ng, 1), (gsf[l], nf, 4)):
                    p.V(lambda e, gs=gs, g=g, j=j: e.scalar_tensor_tensor(out=gs[:], in0=m[:, j * 8:(j + 1) * 8, :], scalar=1.0,
                                                                         in1=g[:].unsqueeze(2).broadcast_to([128, 8, 2]),
                                                                         op0=ALU.add, op1=ALU.mult), w=[gs], r=[m, g])
                p.barrier()

        def norm_block(st_tiles, b0, n, gs_ap, sh_ap, hT, h32=None):
            xb, sq, pss, rstd, tmp = st_tiles
            p.dma(xb[:, :, :n], X[:, b0:b0 + n].rearrange("(kc p) t -> p kc t", p=128), w=[xb], r=[X])
            for kc in range(8):
                p.A(lambda e, kc=kc: e.activation(out=sq[:, kc, :n], in_=xb[:, kc, :n], func=AF.Square), w=[sq], r=[xb])
            for kc in range(8):
                p.T(lambda e, kc=kc: e.matmul(pss[:, :n], lhsT=ones_bf[:], rhs=sq[:, kc, :n], start=(kc == 0), stop=(kc == 7)),
                    w=[pss], r=[ones_bf, sq])
            p.A(lambda e: e.activation(out=rstd[:, :n], in_=pss[:, :n], func=AF.Sqrt, bias=1e-6, scale=1.0 / D), w=[rstd], r=[pss])
            p.V(lambda e: e.reciprocal(out=rstd[:, :n], in_=rstd[:, :n]), w=[rstd], r=[rstd])
            for kc in range(8):
                p.V(lambda e, kc=kc: e.scalar_tensor_tensor(out=tmp[:, kc, :n], in0=xb[:, kc, :n], scalar=gs_ap(kc), in1=rstd[:, :n],
                                                           op0=ALU.mult, op1=ALU.mult), w=[tmp], r=[xb, rstd])
                if sh_ap is not None:
                    if h32 is not None:
                        p.A(lambda e, kc=kc: e.activation(out=h32[:, kc, :n], in_=tmp[:, kc, :n], func=AF.Identity, bias=sh_ap(kc), scale=1.0),
                            w=[h32], r=[tmp])
                        p.V(lambda e, kc=kc: e.tensor_copy(out=hT[:, kc, b0:b0 + n], in_=h32[:, kc, :n]), w=[hT], r=[h32])
                    else:
                        p.A(lambda e, kc=kc: e.activation(out=hT[:, kc, b0:b0 + n], in_=tmp[:, kc, :n], func=AF.Identity, bias=sh_ap(kc), scale=1.0),
                            w=[hT], r=[tmp])

        def alloc_norm_tiles(st):
            return (p.sb("xb", [128, 8, 512], es=st), p.sb("sq", [128, 8, 512], BF16, es=st), p.ps("pss", [128, 512], es=st),
                    p.sb("rstd", [128, 512], es=st), p.sb("ntmp", [128, 8, 512], es=st))

        def stage_proj(l):
            with ExitStack() as st:
                hT = p.sb("hT", [128, 8, T], BF16, es=st)
                with ExitStack() as st2:
                    nt = alloc_norm_tiles(st2)
                    m = mod[l]
                    for (b0, n, seg) in BLKS:
                        norm_block(nt, b0, n, lambda kc, seg=seg: gsm[l][:, kc, seg:seg + 1], lambda kc, seg=seg: m[:, kc, seg:seg + 1], hT)
                    p.barrier()
                if "h" in dbg and l == dbg["h"]:
                    hd = p.dram("dbg_h", [D, T], BF16, kind="ExternalOutput")
                    p.dma(hd[:].rearrange("(kc p) t -> p kc t", p=128), hT[:], w=[hd], r=[hT])
                wf = [p.sb("wf", [128, 8, 512], es=st) for _ in range(2)]
                wb = [p.sb("wb", [128, 8, 512], BF16, es=st) for _ in range(2)]
                row = [p.sb("row", [128, T + 4], es=st) for _ in range(2)]
                tsm = p.sb("tsm", [128, T], es=st)
                mu = p.sb("mu", [128, 71], es=st); om = p.sb("om", [128, 71], es=st)
                pc = [p.ps("pc", [128, 512], es=st) for _ in range(4)]
                p.dma(mu[:], I["muT"][l], w=[mu], r=[I["muT"]])
                p.V(lambda e: e.tensor_scalar(out=om[:], in0=mu[:], scalar1=-1.0, scalar2=1.0, op0=ALU.mult, op1=ALU.add), w=[om], r=[mu])
                p.V(lambda e: e.tensor_scalar(out=mu[:], in0=mu[:], scalar1=0.5, scalar2=None, op0=ALU.mult), w=[mu], r=[mu])
                for r_ in row:
                    p.G(lambda e, r_=r_: e.memset(r_[:], 0.0), w=[r_])
                def rcol(t0):
                    return t0 + 1 if t0 < TC else t0 + 3
                ngrp = (INC + 511) // 512
                ei = 0
                for g in range(ngrp):
                    c0 = g * 512
                    ncg = min(512, INC - c0)
                    wf_, wb_ = wf[g % 2], wb[g % 2]
                    p.dma(wf_[:, :, :ncg], I["w_in"][l, :, c0:c0 + ncg].rearrange("(kc p) c -> p kc c", p=128), w=[wf_], r=[I["w_in"]],
                          q=("sp" if g % 2 == 0 else "pool"))
                    for kc in range(8):
                        if kc % 2 == 0:
                            p.V(lambda e, kc=kc: e.tensor_copy(out=wb_[:, kc, :ncg], in_=wf_[:, kc, :ncg]), w=[wb_], r=[wf_])
                        else:
                            p.A(lambda e, kc=kc: e.copy(out=wb_[:, kc, :ncg], in_=wf_[:, kc, :ncg]), w=[wb_], r=[wf_])
                    for sub in range((ncg + 127) // 128):
                        cb = g * 4 + sub
                        ncol = min(128, ncg - sub * 128)
                        rw_ = row[cb % 2]
                        for bi, (b0, n, seg) in enumerate(BLKS):
                            ps_ = pc[ei % 4]
                            for kc in range(8):
                                p.T(lambda e, kc=kc, ps_=ps_, n=n, b0=b0, sub=sub, ncol=ncol: e.matmul(
                                    ps_[:ncol, :n], lhsT=wb_[:, kc, sub * 128:sub * 128 + ncol], rhs=hT[:, kc, b0:b0 + n],
                                    start=(kc == 0), stop=(kc == 7)), w=[ps_], r=[wb_, hT])
                            rc = rcol(b0)
                            if ei % 2 == 0:
                                p.A(lambda e, ps_=ps_, rc=rc, n=n, ncol=ncol: e.copy(out=rw_[:ncol, rc:rc + n], in_=ps_[:ncol, :n]), w=[rw_], r=[ps_])
                            else:
                                p.V(lambda e, ps_=ps_, rc=rc, n=n, ncol=ncol: e.tensor_copy(out=rw_[:ncol, rc:rc + n], in_=ps_[:ncol, :n]), w=[rw_], r=[ps_])
                            ei += 1
                        col0 = cb * 128
                        if RW0 // 128 <= cb <= (DA0 - 1) // 128:
                            for (t0, n) in ((0, TC), (TC, TL)):
                                rc = rcol(t0)
                                p.V(lambda e, rc=rc, n=n, t0=t0: e.tensor_tensor(out=tsm[:ncol, t0:t0 + n], in0=rw_[:ncol, rc - 1:rc - 1 + n],
                                                                                 in1=rw_[:ncol, rc + 1:rc + 1 + n], op=ALU.add), w=[tsm], r=[rw_])
                                p.V(lambda e, n=n, t0=t0, cb=cb: e.tensor_scalar(out=tsm[:ncol, t0:t0 + n], in0=tsm[:ncol, t0:t0 + n],
                                                                                 scalar1=mu[:ncol, cb:cb + 1], scalar2=None, op0=ALU.mult), w=[tsm], r=[tsm, mu])
                                p.V(lambda e, rc=rc, n=n, t0=t0, cb=cb: e.scalar_tensor_tensor(out=tsm[:ncol, t0:t0 + n], in0=rw_[:ncol, rc:rc + n],
                                                                                               scalar=om[:ncol, cb:cb + 1], in1=tsm[:ncol, t0:t0 + n],
                                                                                               op0=ALU.mult, op1=ALU.add), w=[tsm], r=[rw_, om, tsm])
                            p.dma(PT[col0:col0 + ncol, :], tsm[:ncol, :], w=[PT], r=[tsm])
                        else:
                            p.dma(PT[col0:col0 + ncol, 0:TC], rw_[:ncol, 1:1 + TC], w=[PT], r=[rw_])
                            p.dma(PT[col0:col0 + ncol, TC:T], rw_[:ncol, TC + 3:T + 3], w=[PT], r=[rw_], q="pool")
                for (cstart, dst) in ((HG0 + 1536, VHG), (DA0 + 1024, VDA)):
                    wf_, wb_ = wf[0], wb[0]
                    p.dma(wf_[:], I["w_in"][l, :, cstart:cstart + 512].rearrange("(kc p) c -> p kc c", p=128), w=[wf_], r=[I["w_in"]])
                    for kc in range(8):
                        p.V(lambda e, kc=kc: e.tensor_copy(out=wb_[:, kc, :], in_=wf_[:, kc, :]), w=[wb_], r=[wf_])
                    for tt in range(18):
                        ps_ = pc[tt % 4]
                        for kc in range(8):
                            p.T(lambda e, kc=kc, ps_=ps_, tt=tt: e.matmul(ps_[:, :], lhsT=hT[:, kc, tt * 128:(tt + 1) * 128], rhs=wb_[:, kc, :],
                                                                          start=(kc == 0), stop=(kc == 7)), w=[ps_], r=[wb_, hT])
                        o_ = row[tt % 2]
                        if tt % 2 == 0:
                            p.A(lambda e, ps_=ps_, o_=o_: e.copy(out=o_[:, 0:512], in_=ps_[:, :]), w=[o_], r=[ps_])
                        else:
                            p.V(lambda e, ps_=ps_, o_=o_: e.tensor_copy(out=o_[:, 0:512], in_=ps_[:, :]), w=[o_], r=[ps_])
                        p.dma(dst[tt * 128:(tt + 1) * 128, :], o_[:, 0:512], w=[dst], r=[o_])
                p.barrier()


        def load_w_bf(st, src_ap, nk, cols, name, pk=128):
            wf_ = p.sb(name + "_f", [pk, nk, cols], es=st)
            wb_ = p.sb(name + "_b", [pk, nk, cols], BF16, es=st)
            p.dma(wf_[:], src_ap, w=[wf_], r=[])
            for k in range(nk):
                p.V(lambda e, k=k: e.tensor_copy(out=wb_[:, k, :], in_=wf_[:, k, :]), w=[wb_], r=[wf_])
            return wb_

        def branch_out(bt, zT, nk, pk, wproj, gi, b0, n):
            gt, sgt, po, yo = bt
            for oc in range(8):
                p.dma(gt[:, :n], PT[GT0 + gi * 1024 + oc * 128:GT0 + gi * 1024 + (oc + 1) * 128, b0:b0 + n], w=[gt], r=[PT],
                      q=("sp" if oc % 2 == 0 else "pool"))
                p.A(lambda e: e.activation(out=sgt[:, :n], in_=gt[:, :n], func=AF.Sigmoid), w=[sgt], r=[gt])
                po_ = po[oc % 2]
                for k in range(nk):
                    p.T(lambda e, k=k, oc=oc, po_=po_: e.matmul(po_[:, :n], lhsT=wproj[:pk, k, oc * 128:(oc + 1) * 128], rhs=zT[:pk, k, :n],
                                                               start=(k == 0), stop=(k == nk - 1)), w=[po_], r=[wproj, zT])
                yo_ = yo[oc % 2]
                p.V(lambda e, po_=po_, yo_=yo_: e.tensor_tensor(out=yo_[:, :n], in0=po_[:, :n], in1=sgt[:, :n], op=ALU.mult), w=[yo_], r=[po_, sgt])
                p.dma(YG[gi][oc * 128:(oc + 1) * 128, b0:b0 + n], yo_[:, :n], w=[YG[gi]], r=[yo_], q=("pool" if oc % 2 == 0 else "sp"))

        def alloc_branch_tiles(st):
            return (p.sb("gt", [128, 512], es=st), p.sb("sgt", [128, 512], es=st),
                    [p.ps("po", [128, 512], es=st) for _ in range(2)], [p.sb("yo", [128, 512], es=st) for _ in range(2)])

        def stage_hg(l):
            with ExitStack() as st:
                lb = p.sb("lb", [128, 2, 4], es=st); oml = p.sb("oml", [128, 2, 4], es=st)
                if l == 0:
                    p.V(lambda e: e.memset(lb[:], 0.0), w=[lb])
                else:
                    lg0 = p.sb("lg0", [128, 2, 4], es=st)
                    p.dma(lg0[:], I["hg_lbT"][0].rearrange("d p h -> p d h"), w=[lg0], r=[I["hg_lbT"]])
                    p.dma(lb[:], I["hg_lbT"][1].rearrange("d p h -> p d h"), w=[lb], r=[I["hg_lbT"]])
                    p.V(lambda e: e.tensor_tensor(out=lb[:], in0=lb[:], in1=lg0[:], op=ALU.subtract), w=[lb], r=[lb, lg0])
                    p.A(lambda e: e.activation(out=lb[:], in_=lb[:], func=AF.Sigmoid), w=[lb], r=[lb])
                p.V(lambda e: e.tensor_scalar(out=oml[:], in0=lb[:], scalar1=-1.0, scalar2=1.0, op0=ALU.mult, op1=ALU.add), w=[oml], r=[lb])
                S = [p.sb("S", [128, 4, 128], es=st) for _ in range(2)]
                for d in range(2):
                    p.G(lambda e, d=d: e.memset(S[d][:], 0.0), w=[S[d]])
                names = ("qtl", "ktl", "qh", "kh")
                prep = [[{nm: p.sb(nm, [128, 4, 256], BF16, es=st) for nm in names} for _ in range(2)] for _ in range(2)]
                Sb = [p.sb("Sb", [128, 4, 128], BF16, es=st) for _ in range(2)]
                for d in range(2):
                    p.G(lambda e, d=d: e.memset(Sb[d][:], 0.0), w=[Sb[d]])
                Vgf = [p.sb("Vgf", [32, 8, 512], es=st) for _ in range(2)]
                ebC = [[p.sb("ebC", [128, 4, 8], es=st) for _ in range(2)] for _ in range(2)]
                Vg = [p.sb("Vg", [32, 8, 512], BF16, es=st) for _ in range(2)]
                og = [p.sb("og", [128, 4, 256], es=st) for _ in range(2)]
                tmp = {nm: [p.sb(nm, [128, 256], es=st) for _ in range(2)] for nm in ("zt", "qt", "lf", "F", "E", "kg", "X", "ex")}
                ones256 = p.sb("ones256", [128, 256], es=st); p.V(lambda e: e.memset(ones256[:], 1.0), w=[ones256])
                khT = [p.sb("khT", [32, 512], BF16, es=st) for _ in range(2)]
                AT = [p.sb("AT", [32, 4, 32], BF16, es=st) for _ in range(2)]
                pT = [p.ps("pT", [32, 512], BF16, es=st) for _ in range(2)]
                pA = [p.ps("pA", [32, 4, 32], es=st) for _ in range(2)]
                pO = [p.ps("pO", [128, 4, 32], es=st) for _ in range(2)]
                pS = [p.ps("pS", [128, 4, 128], es=st) for _ in range(2)]
                bwd_groups = [0, 8, 7, 6, 5, 4, 3, 2, 1]
                ti = 0
                for step in range(9):
                    for d in range(2):
                        g = step if d == 0 else bwd_groups[step]
                        t0 = g * 256
                        pr = prep[d][step % 2]
                        eb = ebC[d][step % 2]
                        p.dma(Vgf[d][:], VHG[t0:t0 + 256, :].rearrange("(c s) v -> s c v", s=32), w=[Vgf[d]], r=[VHG], q="pool")
                        p.A(lambda e, d=d: e.copy(out=Vg[d][:], in_=Vgf[d][:]), w=[Vg[d]], r=[Vgf[d]])
                        for h in range(4):
                            tt = {nm: tmp[nm][ti % 2] for nm in tmp}
                            ti += 1
                            zt, qt, lf, Ft, Et, kg, Xt, ex = (tt[nm] for nm in ("zt", "qt", "lf", "F", "E", "kg", "X", "ex"))
                            zr = 512 * (1 + d) + h * 128
                            p.dma(zt[:], PT[zr:zr + 128, t0:t0 + 256], w=[zt], r=[PT])
                            p.dma(qt[:], PT[h * 128:(h + 1) * 128, t0:t0 + 256], w=[qt], r=[PT])
                            p.A(lambda e: e.activation(out=zt[:], in_=zt[:], func=AF.Sigmoid), w=[zt], r=[zt])
                            p.V(lambda e, d=d, h=h: e.tensor_scalar(out=zt[:], in0=zt[:], scalar1=oml[:, d, h:h + 1], scalar2=lb[:, d, h:h + 1],
                                                                   op0=ALU.mult, op1=ALU.add), w=[zt], r=[zt, oml, lb])
                            p.A(lambda e: e.activation(out=lf[:], in_=zt[:], func=AF.Ln), w=[lf], r=[zt])
                            p.A(lambda e: e.activation(out=kg[:], in_=zt[:], func=AF.Identity, bias=1.0, scale=-1.0), w=[kg], r=[zt])
                            p.A(lambda e: e.activation(out=qt[:], in_=qt[:], func=AF.Silu), w=[qt], r=[qt])
                            p.V(lambda e: e.tensor_tensor_scan(out=Ft[:], data0=ones256[:], data1=lf[:], initial=0.0, op0=ALU.mult, op1=ALU.add),
                                w=[Ft], r=[ones256, lf])
                            p.V(lambda e: e.tensor_tensor(out=Et[:], in0=Ft[:], in1=lf[:], op=ALU.subtract), w=[Et], r=[Ft, lf])
                            F3 = Ft[:].rearrange("p (c t) -> p c t", t=32); E3 = Et[:].rearrange("p (c t) -> p c t", t=32)
                            X3 = Xt[:].rearrange("p (c t) -> p c t", t=32)
                            bc = lambda ap: ap.broadcast_to([128, 8, 32])
                            if d == 0:
                                plan = [(F3, F3[:, :, 15:16], [("qtl", qt, 1.0), ("ktl", kg, -1.0)]),
                                        (F3, E3[:, :, 0:1], [("qh", qt, 1.0)]),
                                        (F3, F3[:, :, 31:32], [("kh", kg, -1.0)])]
                            else:
                                plan = [(E3, E3[:, :, 16:17], [("qtl", qt, -1.0), ("ktl", kg, 1.0)]),
                                        (E3, F3[:, :, 31:32], [("qh", qt, -1.0)]),
                                        (E3, E3[:, :, 0:1], [("kh", kg, 1.0)])]
                            for (src3, ref, outs) in plan:
                                p.V(lambda e, src3=src3, ref=ref: e.tensor_tensor(out=X3, in0=src3, in1=bc(ref), op=ALU.subtract), w=[Xt], r=[Ft, Et])
                                for (nm, mul, sgn) in outs:
                                    p.A(lambda e, sgn=sgn: e.activation(out=ex[:], in_=Xt[:], func=AF.Exp, scale=sgn), w=[ex], r=[Xt])
                                    p.V(lambda e, nm=nm, mul=mul, h=h: e.tensor_tensor(out=pr[nm][:, h, :], in0=mul[:], in1=ex[:], op=ALU.mult),
                                        w=[pr[nm]], r=[mul, ex])
                            p.V(lambda e, h=h: e.tensor_tensor(out=eb[:, h, :], in0=F3[:, :, 31], in1=E3[:, :, 0], op=ALU.subtract), w=[eb], r=[Ft, Et])
                            p.A(lambda e, h=h: e.activation(out=eb[:, h, :], in_=eb[:, h, :], func=AF.Exp), w=[eb], r=[eb])
                        mi = 1 if d == 0 else 3
                        order = range(8) if d == 0 else range(7, -1, -1)
                        for c in order:
                            cs = c * 32
                            for h in range(4):
                                p.T(lambda e, h=h, cs=cs: e.transpose(pT[d][:32, h * 128:(h + 1) * 128], pr["kh"][:, h, cs:cs + 32], ident_bf[:]),
                                    w=[pT[d]], r=[pr["kh"], ident_bf])
                            p.A(lambda e: e.copy(out=khT[d][:], in_=pT[d][:]), w=[khT[d]], r=[pT[d]])
                            for h in range(4):
                                p.T(lambda e, h=h, cs=cs: e.matmul(pA[d][:, h, :], lhsT=pr["ktl"][:, h, cs:cs + 32], rhs=pr["qtl"][:, h, cs:cs + 32],
                                                                  start=True, stop=True), w=[pA[d]], r=[pr["ktl"], pr["qtl"]])
                            p.V(lambda e: e.tensor_tensor(out=AT[d][:], in0=pA[d][:], in1=masks[0:32, mi, 0:32].unsqueeze(1).broadcast_to([32, 4, 32]),
                                                          op=ALU.mult), w=[AT[d]], r=[pA[d], masks])
                            for h in range(4):
                                p.T(lambda e, h=h, c=c: e.matmul(pO[d][:, h, :], lhsT=Vg[d][:, c, h * 128:(h + 1) * 128], rhs=AT[d][:, h, :],
                                                                start=True, stop=False), w=[pO[d]], r=[Vg[d], AT[d]])
                                p.T(lambda e, h=h, cs=cs: e.matmul(pO[d][:, h, :], lhsT=Sb[d][:, h, :], rhs=pr["qh"][:, h, cs:cs + 32],
                                                                  start=False, stop=True), w=[pO[d]], r=[Sb[d], pr["qh"]])
                            p.A(lambda e, cs=cs: e.copy(out=og[d][:, :, cs:cs + 32], in_=pO[d][:]), w=[og[d]], r=[pO[d]])
                            for h in range(4):
                                p.T(lambda e, h=h, c=c: e.matmul(pS[d][:, h, :], lhsT=khT[d][:, h * 128:(h + 1) * 128], rhs=Vg[d][:, c, h * 128:(h + 1) * 128],
                                                                start=True, stop=True), w=[pS[d]], r=[khT[d], Vg[d]])
                            for h in range(4):
                                p.V(lambda e, h=h, c=c: e.scalar_tensor_tensor(out=S[d][:, h, :], in0=S[d][:, h, :], scalar=eb[:, h, c:c + 1], in1=pS[d][:, h, :],
                                                                              op0=ALU.mult, op1=ALU.add), w=[S[d]], r=[S[d], eb, pS[d]])
                            p.A(lambda e: e.copy(out=Sb[d][:], in_=S[d][:]), w=[Sb[d]], r=[S[d]])
                        p.dma(OHG[d, :, t0:t0 + 256].rearrange("(h v) t -> v h t", v=128), og[d][:], w=[OHG], r=[og[d]])
                p.barrier()
            with ExitStack() as st:
                wproj = load_w_bf(st, I["hg_proj"][l].rearrange("(k p) c -> p k c", p=128), 4, D, "hgp")
                ngv = p.sb("ngv", [128, 1], es=st); p.dma(ngv[:], I["hg_ngT"][l], w=[ngv], r=[I["hg_ngT"]])
                bt = alloc_branch_tiles(st)
                oa = p.sb("oa", [128, 4, 512], es=st); ob = p.sb("ob", [128, 4, 512], es=st); gg = p.sb("gg", [128, 4, 512], es=st)
                sq = p.sb("sqh", [128, 4, 512], BF16, es=st); zT = p.sb("zT", [128, 4, 512], BF16, es=st)
                pn = p.ps("pn", [128, 512], es=st); rs = p.sb("rs", [128, 512], es=st)
                for (b0, n, seg) in BLKS:
                    p.dma(oa[:, :, :n], OHG[0, :, b0:b0 + n].rearrange("(h v) t -> v h t", v=128), w=[oa], r=[OHG])
                    p.dma(ob[:, :, :n], OHG[1, :, b0:b0 + n].rearrange("(h v) t -> v h t", v=128), w=[ob], r=[OHG], q="pool")
                    p.dma(gg[:, :, :n], PT[2048:2560, b0:b0 + n].rearrange("(h v) t -> v h t", v=128), w=[gg], r=[PT])
                    p.V(lambda e: e.tensor_tensor(out=oa[:, :, :n], in0=oa[:, :, :n], in1=ob[:, :, :n], op=ALU.add), w=[oa], r=[oa, ob])
                    p.A(lambda e: e.activation(out=gg[:, :, :n], in_=gg[:, :, :n], func=AF.Silu), w=[gg], r=[gg])
                    p.V(lambda e: e.tensor_tensor(out=sq[:, :, :n], in0=oa[:, :, :n], in1=oa[:, :, :n], op=ALU.mult), w=[sq], r=[oa])
                    for h in range(4):
                        p.T(lambda e, h=h: e.matmul(pn[:, :n], lhsT=ones_bf[:], rhs=sq[:, h, :n], start=True, stop=True), w=[pn], r=[ones_bf, sq])
                        p.A(lambda e: e.activation(out=rs[:, :n], in_=pn[:, :n], func=AF.Sqrt, bias=1e-6, scale=1.0 / 128), w=[rs], r=[pn])
                        p.V(lambda e: e.reciprocal(out=rs[:, :n], in_=rs[:, :n]), w=[rs], r=[rs])
                        p.V(lambda e, h=h: e.scalar_tensor_tensor(out=oa[:, h, :n], in0=oa[:, h, :n], scalar=ngv[:, 0:1], in1=rs[:, :n],
                                                                 op0=ALU.mult, op1=ALU.mult), w=[oa], r=[oa, ngv, rs])
                        p.V(lambda e, h=h: e.tensor_tensor(out=zT[:, h, :n], in0=oa[:, h, :n], in1=gg[:, h, :n], op=ALU.mult), w=[zT], r=[oa, gg])
                    branch_out(bt, zT, 4, 128, wproj, 0, b0, n)
                p.barrier()


        def stage_da(l):
            lam_init = 0.8 - 0.6 * math.exp(-0.3 * l)
            scale = 64 ** -0.5
            with ExitStack() as st:
                lp = p.sb("lp", [128, 4, 64], es=st); s12 = p.sb("s12", [128, 2], es=st); nlam = p.sb("nlam", [128, 1], es=st)
                pr_ = p.sb("lpp", [128, 2, 64], es=st)
                p.dma(lp[:].rearrange("p a b -> p (a b)"), I["da_lambda"][l].partition_broadcast(128), w=[lp], r=[I["da_lambda"]])
                p.V(lambda e: e.tensor_tensor(out=pr_[:, 0, :], in0=lp[:, 0, :], in1=lp[:, 1, :], op=ALU.mult), w=[pr_], r=[lp])
                p.V(lambda e: e.tensor_tensor(out=pr_[:, 1, :], in0=lp[:, 2, :], in1=lp[:, 3, :], op=ALU.mult), w=[pr_], r=[lp])
                p.V(lambda e: e.reduce_sum(out=s12[:], in_=pr_[:], axis=AX.X), w=[s12], r=[pr_])
                p.A(lambda e: e.activation(out=s12[:], in_=s12[:], func=AF.Exp), w=[s12], r=[s12])
                p.V(lambda e: e.tensor_tensor(out=nlam[:], in0=s12[:, 1:2], in1=s12[:, 0:1], op=ALU.subtract), w=[nlam], r=[s12])
                p.V(lambda e: e.tensor_scalar(out=nlam[:], in0=nlam[:], scalar1=-lam_init, scalar2=None, op0=ALU.add), w=[nlam], r=[nlam])
                sg = p.sb("sg", [128, 128], es=st)
                p.dma(sg[:], I["da_sg"][l].partition_broadcast(128), w=[sg], r=[I["da_sg"]])
                p.V(lambda e: e.tensor_scalar(out=sg[:], in0=sg[:], scalar1=1.0 - lam_init, scalar2=None, op0=ALU.mult), w=[sg], r=[sg])
                KT = p.sb("KT", [64, 8, T], BF16, es=st); QT = p.sb("QT", [64, 8, T], BF16, es=st)
                Vb = p.sb("Vb", [128, 18, 512], BF16, es=st)
                with ExitStack() as st2:
                    cs = p.sb("cs", [64, 2, TL], es=st2); p.dma(cs[:], I["ropeCS"][:], w=[cs], r=[I["ropeCS"]])
                    rR = p.sb("rR", [64, 64], es=st2); p.dma(rR[:], I["ropeR"][:], w=[rR], r=[I["ropeR"]])
                    xr = [p.sb("xr", [64, T], es=st2) for _ in range(2)]
                    t1 = [p.sb("t1", [64, 512], es=st2) for _ in range(2)]; t2 = [p.sb("t2", [64, 512], es=st2) for _ in range(2)]
                    pr2 = [p.ps("prp", [64, 512], es=st2) for _ in range(2)]
                    vf = [p.sb("vf", [128, 512], es=st2) for _ in range(2)]
                    i2 = 0
                    for qi, (dst, roff) in enumerate(((QT, DA0), (KT, DA0 + 512))):
                        for hm in range(8):
                            x_ = xr[hm % 2]
                            p.dma(x_[:], PT[roff + hm * 64:roff + (hm + 1) * 64, :], w=[x_], r=[PT], q=("sp" if hm % 2 == 0 else "pool"))
                            p.A(lambda e, hm=hm, dst=dst, x_=x_: e.copy(out=dst[:, hm, 0:TC], in_=x_[:, 0:TC]), w=[dst], r=[x_])
                            for bi in range(4):
                                a0 = TC + bi * 512
                                pp = pr2[i2 % 2]; t1_ = t1[i2 % 2]; t2_ = t2[i2 % 2]; i2 += 1
                                p.T(lambda e, pp=pp, x_=x_, a0=a0: e.matmul(pp[:], lhsT=rR[:], rhs=x_[:, a0:a0 + 512], start=True, stop=True), w=[pp], r=[rR, x_])
                                p.V(lambda e, t1_=t1_, x_=x_, a0=a0, bi=bi: e.tensor_tensor(out=t1_[:], in0=x_[:, a0:a0 + 512], in1=cs[:, 0, bi * 512:(bi + 1) * 512], op=ALU.mult),
                                    w=[t1_], r=[x_, cs])
                                p.V(lambda e, t2_=t2_, pp=pp, bi=bi: e.tensor_tensor(out=t2_[:], in0=pp[:], in1=cs[:, 1, bi * 512:(bi + 1) * 512], op=ALU.mult),
                                    w=[t2_], r=[pp, cs])
                                p.V(lambda e, dst=dst, hm=hm, a0=a0, t1_=t1_, t2_=t2_: e.tensor_tensor(out=dst[:, hm, a0:a0 + 512], in0=t1_[:], in1=t2_[:], op=ALU.add),
                                    w=[dst], r=[t1_, t2_])
                    for kt in range(18):
                        v_ = vf[kt % 2]
                        p.dma(v_[:], VDA[kt * 128:(kt + 1) * 128, :], w=[v_], r=[VDA], q=("sp" if kt % 2 == 0 else "pool"))
                        p.V(lambda e, kt=kt, v_=v_: e.tensor_copy(out=Vb[:, kt, :], in_=v_[:]), w=[Vb], r=[v_])
                    p.barrier()
                wproj = load_w_bf(st, I["da_proj"][l].rearrange("(k p) c -> p k c", p=128), 4, D, "dap")
                bt = alloc_branch_tiles(st)
                pS = [p.ps("pSc", [128, 512], es=st) for _ in range(2)]
                Eb = [p.sb("Eb", [128, 18, 512], BF16, es=st) for _ in range(2)]
                acc = [p.ps("acc", [128, 4, 128], es=st) for _ in range(2)]
                den = p.ps("den", [128, 2, 4], es=st)
                pZ = p.ps("pZ", [128, 512], es=st)
                rden = p.sb("rden", [128, 2, 4], es=st); rl = p.sb("rl", [128, 4], es=st)
                o1 = p.sb("o1", [128, 4, 128], es=st); o2 = p.sb("o2", [128, 4, 128], es=st); ssq = p.sb("ssq", [128, 4], es=st)
                zT = p.sb("zTd", [128, 4, 512], BF16, es=st)
                qblocks = [(0, 256, [0, 1])] + [(TC + i * 512, 512, list(range(18))) for i in range(4)]
                ei = [0]

                def phase1(q0, nq, kts, hm):
                    Eb_ = Eb[hm % 2]
                    for kt in kts:
                        pS_ = pS[ei[0] % 2]; ei[0] += 1
                        p.T(lambda e, pS_=pS_, kt=kt: e.matmul(pS_[:, :nq], lhsT=KT[:, hm, kt * 128:(kt + 1) * 128], rhs=QT[:, hm, q0:q0 + nq],
                                                              start=True, stop=True), w=[pS_], r=[KT, QT])
                        p.A(lambda e, pS_=pS_, kt=kt: e.activation(out=Eb_[:, kt, :nq], in_=pS_[:, :nq], func=AF.Exp, scale=scale), w=[Eb_], r=[pS_])

                def phase2(q0, nq, kts, hm):
                    Eb_ = Eb[hm % 2]; h = hm // 2; m = hm % 2
                    for j in range(nq // 128):
                        for kt in kts:
                            p.T(lambda e, j=j, kt=kt: e.matmul(acc[m][:, j, :], lhsT=Eb_[:, kt, j * 128:(j + 1) * 128], rhs=Vb[:, kt, h * 128:(h + 1) * 128],
                                                              start=(kt == kts[0]), stop=(kt == kts[-1])), w=[acc[m]], r=[Eb_, Vb])
                        for kt in kts:
                            p.T(lambda e, j=j, kt=kt: e.matmul(den[:, m, j:j + 1], lhsT=Eb_[:, kt, j * 128:(j + 1) * 128], rhs=ones_bf[:, 0:1],
                                                              start=(kt == kts[0]), stop=(kt == kts[-1])), w=[den], r=[Eb_, ones_bf])

                for (q0, nq, kts) in qblocks:
                    nj = nq // 128
                    phase1(q0, nq, kts, 0)
                    for hm in range(8):
                        if hm + 1 < 8:
                            phase1(q0, nq, kts, hm + 1)
                        phase2(q0, nq, kts, hm)
                        if hm % 2 == 0:
                            continue
                        h = hm // 2
                        p.V(lambda e: e.reciprocal(out=rden[:, :, :nj], in_=den[:, :, :nj]), w=[rden], r=[den])
                        p.V(lambda e: e.tensor_scalar(out=rl[:, :nj], in0=rden[:, 1, :nj], scalar1=nlam[:, 0:1], scalar2=None, op0=ALU.mult), w=[rl], r=[rden, nlam])
                        p.V(lambda e: e.tensor_tensor(out=o1[:, :nj, :], in0=acc[0][:, :nj, :], in1=rden[:, 0, :nj].unsqueeze(2).broadcast_to([128, nj, 128]), op=ALU.mult),
                            w=[o1], r=[acc[0], rden])
                        p.V(lambda e: e.tensor_tensor(out=o2[:, :nj, :], in0=acc[1][:, :nj, :], in1=rl[:, :nj].unsqueeze(2).broadcast_to([128, nj, 128]), op=ALU.mult),
                            w=[o2], r=[acc[1], rl])
                        p.V(lambda e: e.tensor_tensor(out=o1[:, :nj, :], in0=o1[:, :nj, :], in1=o2[:, :nj, :], op=ALU.add), w=[o1], r=[o1, o2])
                        p.V(lambda e: e.tensor_tensor(out=o2[:, :nj, :], in0=o1[:, :nj, :], in1=o1[:, :nj, :], op=ALU.mult), w=[o2], r=[o1])
                        p.V(lambda e: e.reduce_sum(out=ssq[:, :nj], in_=o2[:, :nj, :], axis=AX.X), w=[ssq], r=[o2])
                        p.A(lambda e: e.activation(out=ssq[:, :nj], in_=ssq[:, :nj], func=AF.Sqrt, bias=1e-5, scale=1.0 / 128), w=[ssq], r=[ssq])
                        p.V(lambda e: e.reciprocal(out=ssq[:, :nj], in_=ssq[:, :nj]), w=[ssq], r=[ssq])
                        p.V(lambda e: e.tensor_tensor(out=o1[:, :nj, :], in0=o1[:, :nj, :], in1=ssq[:, :nj].unsqueeze(2).broadcast_to([128, nj, 128]), op=ALU.mult),
                            w=[o1], r=[o1, ssq])
                        p.V(lambda e: e.tensor_tensor(out=o1[:, :nj, :], in0=o1[:, :nj, :], in1=sg[:].unsqueeze(1).broadcast_to([128, nj, 128]), op=ALU.mult),
                            w=[o1], r=[o1, sg])
                        for j in range(nj):
                            p.T(lambda e, j=j: e.transpose(pZ[:, j * 128:(j + 1) * 128], o1[:, j, :], ident[:]), w=[pZ], r=[o1, ident])
                        p.A(lambda e, h=h: e.copy(out=zT[:, h, :nq], in_=pZ[:, :nq]), w=[zT], r=[pZ])
                    branch_out(bt, zT, 4, 128, wproj, 2, q0, nq)
                p.barrier()


        RWN = ("R", "K", "V", "AN", "B", "LW0", "LW1", "BON", "G")
        RWS = {nm: p.dram("RWS_" + nm, [64, 8, T]) for nm in RWN}
        YRW = [p.dram("YRW%d" % d, [64, 8, T]) for d in range(2)]

        def stage_rw_prep(l):
            with ExitStack() as st:
                vec = p.sb("rwvec", [64, 8, 8], es=st); p.dma(vec[:], I["rw_vecT"][l], w=[vec], r=[I["rw_vecT"]])
                w0 = p.sb("rww0", [64, 2, 8], es=st); p.dma(w0[:], I["rw_w0T"][l].rearrange("d k h -> k d h"), w=[w0], r=[I["rw_w0T"]])
                w2 = p.sb("rww2", [64, 2, 512], es=st); p.dma(w2[:], I["rw_w2"][l].rearrange("d j c -> j d c"), w=[w2], r=[I["rw_w2"]])
                a2 = p.sb("rwa2", [64, 512], es=st); p.dma(a2[:], I["rw_a2"][l], w=[a2], r=[I["rw_a2"]])
                g2 = p.sb("rwg2", [128, 512], es=st); p.dma(g2[:], I["rw_g2"][l], w=[g2], r=[I["rw_g2"]])
                n = 256
                mk = lambda nm: p.sb(nm, [64, 8, n], es=st)
                r_, kr, v_, a_, kk, sq, nrm, k_, b_, an, rk, bon, g_ = (mk(x) for x in ("r_", "kr", "v_", "a_", "kk", "sq", "nrm", "k_", "b_", "an", "rk", "bon", "g_"))
                lw = [mk("lw0"), mk("lw1")]
                adt = p.sb("adt", [64, n], es=st); wd = [p.sb("wdf", [64, n], es=st), p.sb("wdb", [64, n], es=st)]
                gdt = p.sb("gdt", [128, n], es=st); th = p.sb("th", [64, n], es=st)
                pool = [p.ps("prw", [64, 2, n], es=st) for _ in range(4)]
                pi = [0]

                def nps():
                    pi[0] += 1
                    return pool[pi[0] % 4]
                bc = lambda i: vec[:, i, :].unsqueeze(2).broadcast_to([64, 8, n])
                for blk in range(9):
                    b0 = blk * n
                    hk = lambda c0: PT[c0:c0 + 512, b0:b0 + n].rearrange("(h k) t -> k h t", k=64)
                    p.dma(r_[:], hk(RW_R), w=[r_], r=[PT]); p.dma(kr[:], hk(RW_K), w=[kr], r=[PT], q="pool"); p.dma(v_[:], hk(RW_V), w=[v_], r=[PT])
                    p.dma(adt[:], PT[RW_AD:RW_AD + 64, b0:b0 + n], w=[adt], r=[PT], q="pool")
                    p.dma(wd[0][:], PT[RW_WF:RW_WF + 64, b0:b0 + n], w=[wd[0]], r=[PT]); p.dma(wd[1][:], PT[RW_WB:RW_WB + 64, b0:b0 + n], w=[wd[1]], r=[PT], q="pool")
                    p.dma(gdt[:], PT[RW_GD:RW_GD + 128, b0:b0 + n], w=[gdt], r=[PT])
                    for hp in range(4):
                        ps_ = nps()
                        for hh in range(2):
                            h = hp * 2 + hh
                            p.T(lambda e, h=h, hh=hh, ps_=ps_: e.matmul(ps_[:, hh, :], lhsT=a2[:, h * 64:(h + 1) * 64], rhs=adt[:], start=True, stop=True), w=[ps_], r=[a2, adt])
                        for hh in range(2):
                            h = hp * 2 + hh
                            p.A(lambda e, h=h, hh=hh, ps_=ps_: e.activation(out=a_[:, h, :], in_=ps_[:, hh, :], func=AF.Sigmoid, bias=vec[:, 2, h:h + 1], scale=1.0),
                                w=[a_], r=[ps_, vec])
                    p.V(lambda e: e.tensor_tensor(out=kk[:], in0=kr[:], in1=bc(0), op=ALU.mult), w=[kk], r=[kr, vec])
                    p.A(lambda e: e.activation(out=sq[:], in_=kk[:], func=AF.Square), w=[sq], r=[kk])
                    for hp in range(4):
                        ps_ = nps()
                        p.T(lambda e, hp=hp, ps_=ps_: e.matmul(ps_[:].rearrange("p a b -> p (a b)"), lhsT=ones_f[:64, :64],
                                                              rhs=sq[:, 2 * hp:2 * hp + 2, :].rearrange("p a b -> p (a b)"), start=True, stop=True), w=[ps_], r=[ones_f, sq])
                        p.A(lambda e, hp=hp, ps_=ps_: e.activation(out=nrm[:, 2 * hp:2 * hp + 2, :], in_=ps_[:], func=AF.Sqrt), w=[nrm], r=[ps_])
                    p.V(lambda e: e.tensor_scalar(out=nrm[:], in0=nrm[:], scalar1=1e-12, scalar2=None, op0=ALU.max), w=[nrm], r=[nrm])
                    p.V(lambda e: e.reciprocal(out=nrm[:], in_=nrm[:]), w=[nrm], r=[nrm])
                    p.V(lambda e: e.tensor_tensor(out=kk[:], in0=kk[:], in1=nrm[:], op=ALU.mult), w=[kk], r=[kk, nrm])
                    p.V(lambda e: e.scalar_tensor_tensor(out=k_[:], in0=a_[:], scalar=-1.0, in1=bc(1), op0=ALU.add, op1=ALU.mult), w=[k_], r=[a_, vec])
                    p.V(lambda e: e.scalar_tensor_tensor(out=k_[:], in0=k_[:], scalar=1.0, in1=kr[:], op0=ALU.add, op1=ALU.mult), w=[k_], r=[k_, kr])
                    p.V(lambda e: e.tensor_tensor(out=b_[:], in0=kk[:], in1=a_[:], op=ALU.mult), w=[b_], r=[kk, a_])
                    p.A(lambda e: e.activation(out=an[:], in_=kk[:], func=AF.Identity, scale=-1.0), w=[an], r=[kk])
                    p.V(lambda e: e.tensor_tensor(out=rk[:], in0=r_[:], in1=k_[:], op=ALU.mult), w=[rk], r=[r_, k_])
                    p.V(lambda e: e.tensor_tensor(out=rk[:], in0=rk[:], in1=bc(3), op=ALU.mult), w=[rk], r=[rk, vec])
                    for hp in range(4):
                        ps_ = nps()
                        p.T(lambda e, hp=hp, ps_=ps_: e.matmul(ps_[:].rearrange("p a b -> p (a b)"), lhsT=ones_f[:64, :64],
                                                              rhs=rk[:, 2 * hp:2 * hp + 2, :].rearrange("p a b -> p (a b)"), start=True, stop=True), w=[ps_], r=[ones_f, rk])
                        p.V(lambda e, hp=hp, ps_=ps_: e.tensor_tensor(out=bon[:, 2 * hp:2 * hp + 2, :], in0=ps_[:], in1=v_[:, 2 * hp:2 * hp + 2, :], op=ALU.mult),
                            w=[bon], r=[ps_, v_])
                    p.A(lambda e: e.activation(out=gdt[:], in_=gdt[:], func=AF.Sigmoid), w=[gdt], r=[gdt])
                    for hp in range(4):
                        ps_ = nps()
                        for hh in range(2):
                            h = hp * 2 + hh
                            p.T(lambda e, h=h, hh=hh, ps_=ps_: e.matmul(ps_[:, hh, :], lhsT=g2[:, h * 64:(h + 1) * 64], rhs=gdt[:], start=True, stop=True), w=[ps_], r=[g2, gdt])
                        p.A(lambda e, hp=hp, ps_=ps_: e.copy(out=g_[:, 2 * hp:2 * hp + 2, :], in_=ps_[:]), w=[g_], r=[ps_])
                    for d in range(2):
                        p.A(lambda e, d=d: e.activation(out=th[:], in_=wd[d][:], func=AF.Tanh), w=[th], r=[wd[d]])
                        for hp in range(4):
                            ps_ = nps()
                            for hh in range(2):
                                h = hp * 2 + hh
                                p.T(lambda e, h=h, hh=hh, ps_=ps_, d=d: e.matmul(ps_[:, hh, :], lhsT=w2[:, d, h * 64:(h + 1) * 64], rhs=th[:], start=True, stop=True), w=[ps_], r=[w2, th])
                            for hh in range(2):
                                h = hp * 2 + hh
                                p.A(lambda e, h=h, hh=hh, ps_=ps_, d=d: e.activation(out=lw[d][:, h, :], in_=ps_[:, hh, :], func=AF.Sigmoid, bias=w0[:, d, h:h + 1], scale=1.0),
                                    w=[lw[d]], r=[ps_, w0])
                        p.V(lambda e, d=d: e.tensor_scalar(out=lw[d][:], in0=lw[d][:], scalar1=-math.exp(-0.5), scalar2=None, op0=ALU.mult), w=[lw[d]], r=[lw[d]])
                    for qi, (nm, tl) in enumerate((("R", r_), ("K", k_), ("V", v_), ("AN", an), ("B", b_), ("LW0", lw[0]), ("LW1", lw[1]), ("BON", bon), ("G", g_))):
                        p.dma(RWS[nm][:, :, b0:b0 + n], tl[:], w=[RWS[nm]], r=[tl], q=("sp" if qi % 2 == 0 else "pool"))
                p.barrier()

        def stage_rw_scan(l):
            with ExitStack() as st:
                mk = lambda nm, shp=(64, 8, 64), dt=F32: [p.sb(nm, list(shp), dt, es=st) for _ in range(2)]
                ST = mk("ST"); STb = mk("STb", dt=BF16); Vb = mk("Vb", dt=BF16); Xb = mk("Xb", dt=BF16)
                for d in range(2):
                    p.G(lambda e, d=d: e.memset(STb[d][:], 0.0), w=[STb[d]])
                for d in range(2):
                    p.G(lambda e, d=d: e.memset(ST[d][:], 0.0), w=[ST[d]])
                rmask = p.sb("rmask", [64, 8, 64], es=st)
                p.V(lambda e: e.memset(rmask[:], 1.0), w=[rmask]); p.V(lambda e: e.memset(rmask[:, :, 0:1], 0.0), w=[rmask])
                ld = {nm: mk("l" + nm) for nm in ("R", "K", "V", "AN", "B", "LW")}
                Ft, Et, Gt, Ht = mk("F"), mk("E"), mk("G"), mk("H")
                ex = [mk("ex0"), mk("ex1")]
                AR = mk("AR", (64, 8, 2, 64), BF16); bt, kt_, bh, kh = mk("bt", dt=BF16), mk("kt", dt=BF16), mk("bh", dt=BF16), mk("kh", dt=BF16)
                Vtm, Btm, Ktm = mk("Vtm", dt=BF16), mk("Btm", dt=BF16), mk("Ktm", dt=BF16)
                W1 = mk("W1", (64, 8, 128), BF16); W2 = mk("W2", (64, 8, 128), BF16); Lm = mk("Lm", dt=BF16); Xm = mk("Xm")
                Pb = [mk("Pb0", dt=BF16), mk("Pb1", dt=BF16)]; PTb = [mk("PTb0", dt=BF16), mk("PTb1", dt=BF16)]
                RH, Um, yo = mk("RH", dt=BF16), mk("Um", dt=BF16), mk("yo")
                gC = mk("gC", (64, 8))
                pool = [p.ps("prs", [64, 8, 64], es=st) for _ in range(6)]
                poolb = [p.ps("prsb", [64, 8, 64], BF16, es=st) for _ in range(2)]
                pi = [0, 0]

                def nps():
                    pi[0] += 1
                    return pool[pi[0] % 6]

                def npsb():
                    pi[1] += 1
                    return poolb[pi[1] % 2]
                f2 = lambda b: b[:].rearrange("p h t -> p (h t)")
                bwd = [3, 2, 1, 0] + list(range(35, 3, -1))
                idb = ident[:64, :64].unsqueeze(1).broadcast_to([64, 8, 64])
                ei = [0]

                def exp_to(d, src, sgn):
                    e_ = ex[ei[0] % 2][d]; ei[0] += 1
                    p.A(lambda e: e.activation(out=e_[:], in_=src[:], func=AF.Exp, scale=sgn), w=[e_], r=[src])
                    return e_
                for step in range(36):
                    cd = (step, bwd[step])
                    for d in range(2):
                        t0 = cd[d] * 64
                        for qi, nm in enumerate(("R", "K", "V", "AN", "B", "LW")):
                            src = RWS["LW%d" % d] if nm == "LW" else RWS[nm]
                            p.dma(ld[nm][d][:], src[:, :, t0:t0 + 64], w=[ld[nm][d]], r=[src], q=("sp" if qi % 2 == 0 else "pool"))
                    for d in range(2):
                        lw = ld["LW"][d]; F_, E_, G_, H_ = Ft[d], Et[d], Gt[d], Ht[d]
                        p.V(lambda e: e.tensor_tensor_scan(out=f2(F_), data0=f2(rmask), data1=f2(lw), initial=0.0, op0=ALU.mult, op1=ALU.add), w=[F_], r=[rmask, lw])
                        p.G(lambda e: e.tensor_tensor(out=E_[:], in0=F_[:], in1=lw[:], op=ALU.subtract), w=[E_], r=[F_, lw])
                        fend = F_[:, :, 63:64].broadcast_to([64, 8, 64])
                        p.V(lambda e: e.tensor_tensor(out=G_[:], in0=fend, in1=F_[:], op=ALU.subtract), w=[G_], r=[F_])
                        p.A(lambda e: e.activation(out=gC[d][:], in_=F_[:, :, 63], func=AF.Exp), w=[gC[d]], r=[F_])
                        if d == 1:
                            p.V(lambda e: e.tensor_tensor(out=H_[:], in0=E_[:], in1=fend, op=ALU.subtract), w=[H_], r=[E_, F_])
                        R_, K_, AN_, B_ = ld["R"][d], ld["K"][d], ld["AN"][d], ld["B"][d]
                        specs = ((E_, 1.0), (F_, -1.0), (F_, 1.0), (G_, 1.0)) if d == 0 else ((G_, 1.0), (H_, 1.0), (H_, -1.0), (E_, 1.0))
                        e1 = exp_to(d, *specs[0])
                        p.V(lambda e: e.tensor_tensor(out=AR[d][:, :, 0, :], in0=AN_[:], in1=e1[:], op=ALU.mult), w=[AR[d]], r=[AN_, e1])
                        e2 = exp_to(d, *specs[1])
                        p.V(lambda e: e.tensor_tensor(out=bt[d][:], in0=B_[:], in1=e2[:], op=ALU.mult), w=[bt[d]], r=[B_, e2])
                        p.G(lambda e: e.tensor_tensor(out=kt_[d][:], in0=K_[:], in1=e2[:], op=ALU.mult), w=[kt_[d]], r=[K_, e2])
                        e3 = exp_to(d, *specs[2])
                        p.V(lambda e: e.tensor_tensor(out=AR[d][:, :, 1, :], in0=R_[:], in1=e3[:], op=ALU.mult), w=[AR[d]], r=[R_, e3])
                        e4 = exp_to(d, *specs[3])
                        p.V(lambda e: e.tensor_tensor(out=bh[d][:], in0=B_[:], in1=e4[:], op=ALU.mult), w=[bh[d]], r=[B_, e4])
                        p.G(lambda e: e.tensor_tensor(out=kh[d][:], in0=K_[:], in1=e4[:], op=ALU.mult), w=[kh[d]], r=[K_, e4])
                    for d in range(2):
                        p.A(lambda e: e.copy(out=Vb[d][:], in_=ld["V"][d][:]), w=[Vb[d]], r=[ld["V"][d]])
                        for (src, dst) in ((Vb[d], Vtm[d]), (bh[d], Btm[d]), (kh[d], Ktm[d])):
                            ps_ = npsb()
                            for h in range(8):
                                p.T(lambda e, h=h, ps_=ps_, src=src: e.transpose(ps_[:, h, :], src[:, h, :], ident_bf[:64, :64]), w=[ps_], r=[src, ident_bf])
                            p.A(lambda e, ps_=ps_, dst=dst: e.copy(out=dst[:], in_=ps_[:]), w=[dst], r=[ps_])
                    for d in range(2):
                        cm = masks[:, 2 * d:2 * d + 2, :].rearrange("p a b -> p (a b)").unsqueeze(1).broadcast_to([64, 4, 128])
                        for (lh, Wd) in ((bt[d], W1[d]), (kt_[d], W2[d])):
                            for half in range(2):
                                ps_ = nps()
                                pv = ps_[:].rearrange("p h t -> p (h t)").rearrange("p (a b) -> p a b", b=128)
                                for hh in range(4):
                                    h = half * 4 + hh
                                    p.T(lambda e, h=h, hh=hh, pv=pv, lh=lh: e.matmul(pv[:, hh, :], lhsT=lh[:, h, :], rhs=AR[d][:, h, :, :].rearrange("p a b -> p (a b)"),
                                                                                   start=True, stop=True), w=[ps_], r=[lh, AR[d]])
                                p.V(lambda e, pv=pv, Wd=Wd, half=half: e.tensor_tensor(out=Wd[:, half * 4:(half + 1) * 4, :], in0=pv, in1=cm, op=ALU.mult), w=[Wd], r=[ps_, masks])
                        ps_ = nps()
                        for h in range(8):
                            p.T(lambda e, h=h, ps_=ps_: e.matmul(ps_[:, h, :], lhsT=AR[d][:, h, 0, :], rhs=bt[d][:, h, :], start=True, stop=True), w=[ps_], r=[AR[d], bt[d]])
                        mnt = masks[:, 2 - 2 * d, :].unsqueeze(1).broadcast_to([64, 8, 64])
                        p.V(lambda e, ps_=ps_: e.tensor_tensor(out=Lm[d][:], in0=ps_[:], in1=mnt, op=ALU.mult), w=[Lm[d]], r=[ps_, masks])
                        p.G(lambda e: e.tensor_tensor(out=Xm[d][:], in0=W1[d][:, :, 0:64], in1=idb, op=ALU.add), w=[Xm[d]], r=[W1[d], ident])
                        p.A(lambda e: e.copy(out=Xb[d][:], in_=Xm[d][:]), w=[Xb[d]], r=[Xm[d]])
                    cur = [(lambda h, d=d: W1[d][:, h, 0:64], W1[d], lambda h, d=d: Lm[d][:, h, :], Lm[d]) for d in range(2)]
                    for i in range(1, 6):
                        for d in range(2):
                            Pf, Pbuf, PTf, PTbuf = cur[d]
                            nP, nPT = Pb[i % 2][d], PTb[i % 2][d]
                            if i < 5:
                                ps_ = nps()
                                for h in range(8):
                                    p.T(lambda e, h=h, ps_=ps_: e.matmul(ps_[:, h, :], lhsT=PTf(h), rhs=Pf(h), start=True, stop=True), w=[ps_], r=[Pbuf, PTbuf])
                                p.A(lambda e, ps_=ps_, nP=nP: e.copy(out=nP[:], in_=ps_[:]), w=[nP], r=[ps_])
                            ps2 = nps()
                            for h in range(8):
                                p.T(lambda e, h=h, ps2=ps2: e.matmul(ps2[:, h, :], lhsT=Pf(h), rhs=PTf(h), start=True, stop=True), w=[ps2], r=[Pbuf, PTbuf])
                            p.V(lambda e, ps2=ps2, nPT=nPT: e.tensor_copy(out=nPT[:], in_=ps2[:]), w=[nPT], r=[ps2])
                            ps3 = nps()
                            for h in range(8):
                                p.T(lambda e, h=h, ps3=ps3, nPT=nPT: e.matmul(ps3[:, h, :], lhsT=nPT[:, h, :], rhs=Xb[d][:, h, :], start=True, stop=True), w=[ps3], r=[nPT, Xb[d]])
                            p.V(lambda e, ps3=ps3: e.tensor_tensor(out=Xm[d][:], in0=Xm[d][:], in1=ps3[:], op=ALU.add), w=[Xm[d]], r=[Xm[d], ps3])
                            p.A(lambda e: e.copy(out=Xb[d][:], in_=Xm[d][:]), w=[Xb[d]], r=[Xm[d]])
                            cur[d] = (lambda h, nP=nP: nP[:, h, :], nP, lambda h, nPT=nPT: nPT[:, h, :], nPT)
                    for d in range(2):
                        ps_ = nps()
                        for h in range(8):
                            p.T(lambda e, h=h, ps_=ps_: e.matmul(ps_[:, h, :], lhsT=AR[d][:, h, 0, :], rhs=STb[d][:, h, :], start=True, stop=False), w=[ps_], r=[AR[d], STb[d]])
                            p.T(lambda e, h=h, ps_=ps_: e.matmul(ps_[:, h, :], lhsT=W2[d][:, h, 0:64], rhs=Vtm[d][:, h, :], start=False, stop=True), w=[ps_], r=[W2[d], Vtm[d]])
                        p.A(lambda e, ps_=ps_: e.copy(out=RH[d][:], in_=ps_[:]), w=[RH[d]], r=[ps_])
                        ps_ = nps()
                        for h in range(8):
                            p.T(lambda e, h=h, ps_=ps_: e.matmul(ps_[:, h, :], lhsT=Xb[d][:, h, :], rhs=RH[d][:, h, :], start=True, stop=True), w=[ps_], r=[Xb[d], RH[d]])
                        p.V(lambda e, ps_=ps_: e.tensor_copy(out=Um[d][:], in_=ps_[:]), w=[Um[d]], r=[ps_])
                        ps_ = nps()
                        for h in range(8):
                            p.T(lambda e, h=h, ps_=ps_: e.matmul(ps_[:, h, :], lhsT=STb[d][:, h, :], rhs=AR[d][:, h, 1, :], start=True, stop=False), w=[ps_], r=[STb[d], AR[d]])
                            p.T(lambda e, h=h, ps_=ps_: e.matmul(ps_[:, h, :], lhsT=Um[d][:, h, :], rhs=W1[d][:, h, 64:128], start=False, stop=False), w=[ps_], r=[Um[d], W1[d]])
                            p.T(lambda e, h=h, ps_=ps_: e.matmul(ps_[:, h, :], lhsT=Vtm[d][:, h, :], rhs=W2[d][:, h, 64:128], start=False, stop=True), w=[ps_], r=[Vtm[d], W2[d]])
                        p.A(lambda e, ps_=ps_: e.copy(out=yo[d][:], in_=ps_[:]), w=[yo[d]], r=[ps_])
                        t0 = cd[d] * 64
                        p.dma(YRW[d][:, :, t0:t0 + 64], yo[d][:], w=[YRW[d]], r=[yo[d]], q=("sp" if d == 0 else "pool"))
                        ps_ = nps()
                        for h in range(8):
                            p.T(lambda e, h=h, ps_=ps_: e.matmul(ps_[:, h, :], lhsT=Btm[d][:, h, :], rhs=Um[d][:, h, :], start=True, stop=False), w=[ps_], r=[Btm[d], Um[d]])
                            p.T(lambda e, h=h, ps_=ps_: e.matmul(ps_[:, h, :], lhsT=Ktm[d][:, h, :], rhs=Vtm[d][:, h, :], start=False, stop=True), w=[ps_], r=[Ktm[d], Vtm[d]])
                        p.V(lambda e: e.tensor_tensor(out=ST[d][:], in0=ST[d][:], in1=gC[d][:].unsqueeze(2).broadcast_to([64, 8, 64]), op=ALU.mult), w=[ST[d]], r=[ST[d], gC[d]])
                        p.V(lambda e, ps_=ps_: e.tensor_tensor(out=ST[d][:], in0=ST[d][:], in1=ps_[:], op=ALU.add), w=[ST[d]], r=[ST[d], ps_])
                        p.A(lambda e: e.copy(out=STb[d][:], in_=ST[d][:]), w=[STb[d]], r=[ST[d]])
                p.barrier()

        def stage_rw_fin(l):
            with ExitStack() as st:
                vec = p.sb("rwvec", [64, 8, 8], es=st); p.dma(vec[:], I["rw_vecT"][l], w=[vec], r=[I["rw_vecT"]])
                wproj = load_w_bf(st, I["rw_proj"][l].rearrange("(h k) c -> k h c", k=64), 8, D, "rwp", pk=64)
                bt = alloc_branch_tiles(st)
                n = 256
                mk = lambda nm: p.sb(nm, [64, 8, n], es=st)
                ya, yb, bo, gg, sq2 = mk("ya"), mk("yb"), mk("bo"), mk("gg"), mk("sq2")
                zT = p.sb("zTr", [64, 8, 512], BF16, es=st)
                pool = [p.ps("prf", [64, 2, n], es=st) for _ in range(2)]
                bc = lambda i: vec[:, i, :].unsqueeze(2).broadcast_to([64, 8, n])
                pi = 0
                for blk in range(9):
                    b0 = blk * n
                    p.dma(ya[:], YRW[0][:, :, b0:b0 + n], w=[ya], r=[YRW[0]]); p.dma(yb[:], YRW[1][:, :, b0:b0 + n], w=[yb], r=[YRW[1]], q="pool")
                    p.dma(bo[:], RWS["BON"][:, :, b0:b0 + n], w=[bo], r=[RWS["BON"]]); p.dma(gg[:], RWS["G"][:, :, b0:b0 + n], w=[gg], r=[RWS["G"]], q="pool")
                    p.V(lambda e: e.tensor_tensor(out=ya[:], in0=ya[:], in1=yb[:], op=ALU.add), w=[ya], r=[ya, yb])
                    for hp in range(4):
                        ps_ = pool[pi % 2]; pi += 1
                        p.T(lambda e, hp=hp, ps_=ps_: e.matmul(ps_[:].rearrange("p a b -> p (a b)"), lhsT=ones_f[:64, :64],
                                                              rhs=ya[:, 2 * hp:2 * hp + 2, :].rearrange("p a b -> p (a b)"), start=True, stop=True), w=[ps_], r=[ones_f, ya])
                        p.V(lambda e, hp=hp, ps_=ps_: e.scalar_tensor_tensor(out=yb[:, 2 * hp:2 * hp + 2, :], in0=ps_[:], scalar=-1.0 / 64, in1=ya[:, 2 * hp:2 * hp + 2, :],
                                                                            op0=ALU.mult, op1=ALU.add), w=[yb], r=[ps_, ya])
                    p.A(lambda e: e.activation(out=sq2[:], in_=yb[:], func=AF.Square), w=[sq2], r=[yb])
                    for hp in range(4):
                        ps_ = pool[pi % 2]; pi += 1
                        p.T(lambda e, hp=hp, ps_=ps_: e.matmul(ps_[:].rearrange("p a b -> p (a b)"), lhsT=ones_f[:64, :64],
                                                              rhs=sq2[:, 2 * hp:2 * hp + 2, :].rearrange("p a b -> p (a b)"), start=True, stop=True), w=[ps_], r=[ones_f, sq2])
                        p.A(lambda e, hp=hp, ps_=ps_: e.activation(out=ya[:, 2 * hp:2 * hp + 2, :], in_=ps_[:], func=AF.Sqrt, bias=64e-5, scale=1.0 / 64), w=[ya], r=[ps_])
                    p.V(lambda e: e.reciprocal(out=ya[:], in_=ya[:]), w=[ya], r=[ya])
                    p.V(lambda e: e.tensor_tensor(out=yb[:], in0=yb[:], in1=ya[:], op=ALU.mult), w=[yb], r=[yb, ya])
                    p.V(lambda e: e.tensor_tensor(out=yb[:], in0=yb[:], in1=bc(4), op=ALU.mult), w=[yb], r=[yb, vec])
                    p.V(lambda e: e.tensor_tensor(out=yb[:], in0=yb[:], in1=bc(5), op=ALU.add), w=[yb], r=[yb, vec])
                    p.V(lambda e: e.tensor_tensor(out=yb[:], in0=yb[:], in1=bo[:], op=ALU.add), w=[yb], r=[yb, bo])
                    p.V(lambda e: e.tensor_tensor(out=zT[:, :, :n], in0=yb[:], in1=gg[:], op=ALU.mult), w=[zT], r=[yb, gg])
                    branch_out(bt, zT, 8, 64, wproj, 1, b0, n)
                p.barrier()


        def stage_merge(l):
            with ExitStack() as st:
                wo = load_w_bf(st, I["w_out"][l].rearrange("(k p) c -> p k c", p=128), 8, D, "wo")
                ya = [p.sb("mya", [128, 8, 512], es=st) for _ in range(3)]
                mT = p.sb("mT", [128, 8, 512], BF16, es=st)
                xb = p.sb("mxb", [128, 8, 512], es=st)
                po = [p.ps("mpo", [128, 512], es=st) for _ in range(2)]
                blks = BLKS if l == 0 else BLKS[1:]
                for (b0, n, seg) in blks:
                    for i in range(3):
                        p.dma(ya[i][:, :, :n], YG[i][:, b0:b0 + n].rearrange("(kc p) t -> p kc t", p=128), w=[ya[i]], r=[YG[i]], q=("sp" if i != 1 else "pool"))
                    p.dma(xb[:, :, :n], X[:, b0:b0 + n].rearrange("(kc p) t -> p kc t", p=128), w=[xb], r=[X], q="pool")
                    p.V(lambda e: e.tensor_tensor(out=ya[0][:, :, :n], in0=ya[0][:, :, :n], in1=ya[1][:, :, :n], op=ALU.add), w=[ya[0]], r=[ya[0], ya[1]])
                    p.V(lambda e: e.tensor_tensor(out=mT[:, :, :n], in0=ya[0][:, :, :n], in1=ya[2][:, :, :n], op=ALU.add), w=[mT], r=[ya[0], ya[2]])
                    for oc in range(8):
                        po_ = po[oc % 2]
                        for kc in range(8):
                            p.T(lambda e, kc=kc, oc=oc, po_=po_: e.matmul(po_[:, :n], lhsT=wo[:, kc, oc * 128:(oc + 1) * 128], rhs=mT[:, kc, :n],
                                                                         start=(kc == 0), stop=(kc == 7)), w=[po_], r=[wo, mT])
                        p.V(lambda e, oc=oc, po_=po_: e.scalar_tensor_tensor(out=xb[:, oc, :n], in0=po_[:, :n], scalar=mod[l][:, 16 + oc, seg:seg + 1], in1=xb[:, oc, :n],
                                                                            op0=ALU.mult, op1=ALU.add), w=[xb], r=[po_, mod[l], xb])
                    p.dma(X[:, b0:b0 + n].rearrange("(kc p) t -> p kc t", p=128), xb[:, :, :n], w=[X], r=[xb])
                p.barrier()

        def stage_ffn(l):
            moe = (l % 2 == 1)
            blks = BLKS[1:] if moe else BLKS
            nexp = NE if moe else 1
            UTs = p.dram("UT%d" % l, [nexp, FF, T], BF16)
            with ExitStack() as st:
                hT = p.sb("hTf", [128, 8, T], BF16, es=st)
                combT = p.sb("combT", [8, TL], es=st) if moe else None
                with ExitStack() as st2:
                    nt = alloc_norm_tiles(st2)
                    m = mod[l]
                    if moe:
                        h32 = p.sb("h32", [128, 8, 512], es=st2)
                        rt = p.sb("rt", [128, 8, NE], es=st2)
                        p.dma(rt[:], I["moe_router"][0].rearrange("(kc p) e -> p kc e", p=128), w=[rt], r=[I["moe_router"]])
                        lg = p.sb("lg", [128, 16, NE], es=st2)
                        plg = p.ps("plg", [128, NE], es=st2)
                    for bi, (b0, n, seg) in enumerate(blks):
                        norm_block(nt, b0, n, lambda kc, seg=seg: gsf[l][:, kc, seg:seg + 1], lambda kc, seg=seg: m[:, 24 + kc, seg:seg + 1], hT,
                                   h32=(h32 if moe else None))
                        if moe:
                            for j in range(4):
                                for kc in range(8):
                                    p.T(lambda e, kc=kc, j=j: e.matmul(plg[:], lhsT=h32[:, kc, j * 128:(j + 1) * 128], rhs=rt[:, kc, :], start=(kc == 0), stop=(kc == 7)),
                                        w=[plg], r=[h32, rt])
                                p.V(lambda e, j=j, bi=bi: e.tensor_copy(out=lg[:, bi * 4 + j, :], in_=plg[:]), w=[lg], r=[plg])
                    if moe:
                        m1 = p.sb("m1", [128, 16], es=st2); eq = p.sb("eq", [128, 16, NE], es=st2); l2 = p.sb("l2", [128, 16, NE], es=st2)
                        m2 = p.sb("m2", [128, 16], es=st2); ex = p.sb("exr", [128, 16, NE], es=st2); sm = p.sb("sm", [128, 16], es=st2)
                        b3 = lambda t_: t_[:].unsqueeze(2).broadcast_to([128, 16, NE])
                        p.V(lambda e: e.reduce_max(out=m1[:], in_=lg[:], axis=AX.X), w=[m1], r=[lg])
                        p.V(lambda e: e.tensor_tensor(out=eq[:], in0=lg[:], in1=b3(m1), op=ALU.is_equal), w=[eq], r=[lg, m1])
                        p.V(lambda e: e.scalar_tensor_tensor(out=l2[:], in0=eq[:], scalar=-1e30, in1=lg[:], op0=ALU.mult, op1=ALU.add), w=[l2], r=[eq, lg])
                        p.V(lambda e: e.reduce_max(out=m2[:], in_=l2[:], axis=AX.X), w=[m2], r=[l2])
                        p.V(lambda e: e.tensor_tensor(out=eq[:], in0=lg[:], in1=b3(m2), op=ALU.is_ge), w=[eq], r=[lg, m2])
                        p.V(lambda e: e.tensor_tensor(out=l2[:], in0=lg[:], in1=b3(m1), op=ALU.subtract), w=[l2], r=[lg, m1])
                        p.A(lambda e: e.activation(out=ex[:], in_=l2[:], func=AF.Exp), w=[ex], r=[l2])
                        p.V(lambda e: e.tensor_tensor(out=ex[:], in0=ex[:], in1=eq[:], op=ALU.mult), w=[ex], r=[ex, eq])
                        p.V(lambda e: e.reduce_sum(out=sm[:], in_=ex[:], axis=AX.X), w=[sm], r=[ex])
                        p.V(lambda e: e.reciprocal(out=sm[:], in_=sm[:]), w=[sm], r=[sm])
                        p.V(lambda e: e.tensor_tensor(out=ex[:], in0=ex[:], in1=b3(sm), op=ALU.mult), w=[ex], r=[ex, sm])
                        pct = p.ps("pct", [8, 512], es=st2)
                        for g4 in range(4):
                            for j in range(4):
                                p.T(lambda e, g4=g4, j=j: e.transpose(pct[:, j * 128:(j + 1) * 128], ex[:, g4 * 4 + j, :], ident[:]), w=[pct], r=[ex, ident])
                            p.V(lambda e, g4=g4: e.tensor_copy(out=combT[:, g4 * 512:(g4 + 1) * 512], in_=pct[:]), w=[combT], r=[pct])
                    p.barrier()
                if "comb" in dbg and moe:
                    cdd = p.dram("dbg_comb", [8, TL], F32, kind="ExternalOutput")
                    p.dma(cdd[:], combT[:], w=[cdd], r=[combT])
                with ExitStack() as st2:
                    wf1 = [p.sb("wf1", [128, 8, 512], es=st2) for _ in range(2)]; wf3 = [p.sb("wf3", [128, 8, 512], es=st2) for _ in range(2)]
                    wb1 = [p.sb("wb1", [128, 8, 512], BF16, es=st2) for _ in range(2)]; wb3 = [p.sb("wb3", [128, 8, 512], BF16, es=st2) for _ in range(2)]
                    pa = [p.ps("pa", [128, 512], es=st2) for _ in range(2)]; pb = [p.ps("pb", [128, 512], es=st2) for _ in range(2)]
                    pcb = p.ps("pcb", [128, 512], es=st2)
                    sl = [p.sb("sl", [128, 512], es=st2) for _ in range(2)]; ut = [p.sb("ut", [128, 512], BF16, es=st2) for _ in range(2)]
                    sel = p.sb("sel8", [8, 8, 128], es=st2)
                    p.dma(sel[:], I["sel8"][:].rearrange("e k m -> k e m"), w=[sel], r=[I["sel8"]])
                    cbc = p.sb("cbc", [128, TL], es=st2) if moe else None
                    gi = 0; ui = 0
                    for ex_ in range(nexp):
                        w1s = I["moe_w1"][0, ex_] if moe else I["ffn_w1"][0]
                        w3s = I["moe_w3"][0, ex_] if moe else I["ffn_w3"][0]
                        if moe:
                            for g4 in range(4):
                                p.T(lambda e, g4=g4, ex_=ex_: e.matmul(pcb[:], lhsT=sel[:, ex_, :], rhs=combT[:, g4 * 512:(g4 + 1) * 512], start=True, stop=True), w=[pcb], r=[sel, combT])
                                p.A(lambda e, g4=g4: e.copy(out=cbc[:, g4 * 512:(g4 + 1) * 512], in_=pcb[:]), w=[cbc], r=[pcb])
                        for fg in range(6):
                            f0 = fg * 512
                            nf = min(512, FF - f0)
                            a1, a3, c1, c3 = wf1[gi % 2], wf3[gi % 2], wb1[gi % 2], wb3[gi % 2]
                            gi += 1
                            p.dma(a1[:, :, :nf], w1s[:, f0:f0 + nf].rearrange("(kc p) c -> p kc c", p=128), w=[a1], r=[])
                            p.dma(a3[:, :, :nf], w3s[:, f0:f0 + nf].rearrange("(kc p) c -> p kc c", p=128), w=[a3], r=[], q="pool")
                            for kc in range(8):
                                p.V(lambda e, kc=kc: e.tensor_copy(out=c1[:, kc, :nf], in_=a1[:, kc, :nf]), w=[c1], r=[a1])
                                p.A(lambda e, kc=kc: e.copy(out=c3[:, kc, :nf], in_=a3[:, kc, :nf]), w=[c3], r=[a3])
                            for sub in range(nf // 128):
                                fb = fg * 4 + sub
                                for (b0, n, seg) in blks:
                                    pa_, pb_, sl_, ut_ = pa[ui % 2], pb[ui % 2], sl[ui % 2], ut[ui % 2]
                                    ui += 1
                                    for kc in range(8):
                                        p.T(lambda e, kc=kc, sub=sub, pa_=pa_: e.matmul(pa_[:, :n], lhsT=c1[:, kc, sub * 128:(sub + 1) * 128], rhs=hT[:, kc, b0:b0 + n],
                                                                                       start=(kc == 0), stop=(kc == 7)), w=[pa_], r=[c1, hT])
                                    for kc in range(8):
                                        p.T(lambda e, kc=kc, sub=sub, pb_=pb_: e.matmul(pb_[:, :n], lhsT=c3[:, kc, sub * 128:(sub + 1) * 128], rhs=hT[:, kc, b0:b0 + n],
                                                                                       start=(kc == 0), stop=(kc == 7)), w=[pb_], r=[c3, hT])
                                    p.A(lambda e, pa_=pa_, sl_=sl_: e.activation(out=sl_[:, :n], in_=pa_[:, :n], func=AF.Silu), w=[sl_], r=[pa_])
                                    if moe:
                                        p.V(lambda e, sl_=sl_, b0=b0: e.tensor_tensor(out=sl_[:, :n], in0=sl_[:, :n], in1=cbc[:, b0 - TC:b0 - TC + n], op=ALU.mult), w=[sl_], r=[sl_, cbc])
                                    p.V(lambda e, pb_=pb_, sl_=sl_, ut_=ut_: e.tensor_tensor(out=ut_[:, :n], in0=sl_[:, :n], in1=pb_[:, :n], op=ALU.mult), w=[ut_], r=[sl_, pb_])
                                    p.dma(UTs[ex_, fb * 128:(fb + 1) * 128, b0:b0 + n], ut_[:, :n], w=[UTs], r=[ut_], q=("sp" if ui % 2 == 0 else "pool"))
                    p.barrier()
            with ExitStack() as st:
                w2f = p.sb("w2f", [128, 2, D], es=st); w2b = p.sb("w2b", [128, 22, D], BF16, es=st)
                uu = [p.sb("uu", [128, 22, 512], BF16, es=st) for _ in range(2)]
                acc = p.sb("facc", [128, 8, 512], es=st); xb = p.sb("fxb", [128, 8, 512], es=st)
                po = [p.ps("fpo", [128, 512], es=st) for _ in range(4)]
                ui = 0; oi = 0
                for ex_ in range(nexp):
                    w2s = I["moe_w2"][0, ex_] if moe else I["ffn_w2"][0]
                    for f2_ in range(11):
                        p.dma(w2f[:], w2s[f2_ * 256:(f2_ + 1) * 256, :].rearrange("(k p) c -> p k c", p=128), w=[w2f], r=[], q=("sp" if f2_ % 2 == 0 else "pool"))
                        p.V(lambda e, f2_=f2_: e.tensor_copy(out=w2b[:, 2 * f2_, :], in_=w2f[:, 0, :]), w=[w2b], r=[w2f])
                        p.A(lambda e, f2_=f2_: e.copy(out=w2b[:, 2 * f2_ + 1, :], in_=w2f[:, 1, :]), w=[w2b], r=[w2f])
                    for (b0, n, seg) in blks:
                        u_ = uu[ui % 2]; ui += 1
                        p.dma(u_[:, :, :n], UTs[ex_, :, b0:b0 + n].rearrange("(k p) t -> p k t", p=128), w=[u_], r=[UTs])
                        if ex_ > 0:
                            p.dma(acc[:, :, :n], FACC[:, b0:b0 + n].rearrange("(kc p) t -> p kc t", p=128), w=[acc], r=[FACC], q="pool")
                        last = (ex_ == nexp - 1)
                        if last:
                            p.dma(xb[:, :, :n], X[:, b0:b0 + n].rearrange("(kc p) t -> p kc t", p=128), w=[xb], r=[X], q="pool")
                        for oc in range(8):
                            po_ = po[oi % 4]; oi += 1
                            for k in range(22):
                                p.T(lambda e, k=k, oc=oc, po_=po_, u_=u_: e.matmul(po_[:, :n], lhsT=w2b[:, k, oc * 128:(oc + 1) * 128], rhs=u_[:, k, :n],
                                                                                  start=(k == 0), stop=(k == 21)), w=[po_], r=[w2b, u_])
                            if ex_ == 0:
                                p.A(lambda e, oc=oc, po_=po_: e.copy(out=acc[:, oc, :n], in_=po_[:, :n]), w=[acc], r=[po_])
                            else:
                                p.V(lambda e, oc=oc, po_=po_: e.tensor_tensor(out=acc[:, oc, :n], in0=acc[:, oc, :n], in1=po_[:, :n], op=ALU.add), w=[acc], r=[acc, po_])
                            if last:
                                p.V(lambda e, oc=oc: e.scalar_tensor_tensor(out=xb[:, oc, :n], in0=acc[:, oc, :n], scalar=mod[l][:, 40 + oc, seg:seg + 1], in1=xb[:, oc, :n],
                                                                           op0=ALU.mult, op1=ALU.add), w=[xb], r=[acc, mod[l], xb])
                        if last:
                            p.dma(X[:, b0:b0 + n].rearrange("(kc p) t -> p kc t", p=128), xb[:, :, :n], w=[X], r=[xb])
                        else:
                            p.dma(FACC[:, b0:b0 + n].rearrange("(kc p) t -> p kc t", p=128), acc[:, :, :n], w=[FACC], r=[acc])
                p.barrier()

        def stage_final():
            with ExitStack() as st:
                nt = alloc_norm_tiles(st)
                xb, sq, pss, rstd, tmp = nt
                for (b0, n, seg) in BLKS[1:]:
                    norm_block(nt, b0, n, lambda kc: fng[:, kc:kc + 1], None, None)
                    p.dma(outT[:, b0 - TC:b0 - TC + n].rearrange("(kc p) t -> p kc t", p=128), tmp[:, :, :n], w=[outT], r=[tmp])
                p.barrier()

        for l in range(n_layers):
            stage_ada(l)
        if "mod" in dbg:
            md = p.dram("dbg_mod", [2, 128, 96], F32, kind="ExternalOutput")
            for l in range(2):
                p.dma(md[l], mod[l][:].rearrange("p a b -> p (a b)"), w=[md], r=[mod[l]])
        for l in range(n_layers):
            stage_proj(l)
            if stop_after == ("proj", l):
                break
            if "hg" not in skip:
                stage_hg(l)
            if stop_after == ("hg", l):
                break
            if "da" not in skip:
                stage_da(l)
            if stop_after == ("da", l):
                break
            if "rw" not in skip:
                stage_rw_prep(l)
                stage_rw_scan(l)
                stage_rw_fin(l)
            if stop_after == ("rw", l):
                break
            if "merge" not in skip:
                stage_merge(l)
            if stop_after == ("merge", l):
                break
            if "ffn" not in skip:
                stage_ffn(l)
            if stop_after == ("ffn", l):
                break
        if stop_after is None:
            stage_final()
        if "X" in dbg:
            xd = p.dram("dbg_X", [D, T], F32, kind="ExternalOutput")
            p.dma(xd[:], X[:], w=[xd], r=[X])
        if "YG1" in dbg:
            yd = p.dram("dbg_YG1", [D, T], F32, kind="ExternalOutput")
            p.dma(yd[:], YG[1][:], w=[yd], r=[YG[1]])
            for nm in RWN:
                dd = p.dram("dbg_RWS_" + nm, [64, 8, T], F32, kind="ExternalOutput")
                p.dma(dd[:], RWS[nm][:], w=[dd], r=[RWS[nm]])
            for d in range(2):
                dd = p.dram("dbg_YRW%d" % d, [64, 8, T], F32, kind="ExternalOutput")
                p.dma(dd[:], YRW[d][:], w=[dd], r=[YRW[d]])
        if "YG2" in dbg:
            yd = p.dram("dbg_YG2", [D, T], F32, kind="ExternalOutput")
            p.dma(yd[:], YG[2][:], w=[yd], r=[YG[2]])
        if "YG0" in dbg:
            yd = p.dram("dbg_YG0", [D, T], F32, kind="ExternalOutput")
            p.dma(yd[:], YG[0][:], w=[yd], r=[YG[0]])
            od = p.dram("dbg_OHG", [2, 512, T], F32, kind="ExternalOutput")
            p.dma(od[:], OHG[:], w=[od], r=[OHG])
        if "PT" in dbg:
            pd = p.dram("dbg_PT", [INC, T], F32, kind="ExternalOutput")
            p.dma(pd[:], PT[:], w=[pd], r=[PT])
            vd = p.dram("dbg_VHG", [T, 512], F32, kind="ExternalOutput")
            p.dma(vd[:], VHG[:], w=[vd], r=[VHG])
        p.barrier()
        print("instrs", p.ninstr, "waits", p.nwait)
    return nc


def host_inputs(inputs, b):
    f = np.float32
    g = {}
    x = np.asarray(inputs["x"][b], f); ctx = np.asarray(inputs["ctx"][b], f)
    g["xT"] = np.ascontiguousarray(np.concatenate([ctx.T, x.T], axis=1))
    c2 = np.stack([np.asarray(inputs["c"][b], f), np.asarray(inputs["c_ctx"], f)], axis=-1)
    g["c2"] = np.ascontiguousarray(c2.reshape(8, 128, 2).transpose(1, 0, 2))
    return g


def shared_inputs(inputs):
    f = np.float32
    g = {}
    A = lambda k: np.asarray(inputs[k], f)
    g["ada_w"] = A("ada_w")
    g["ada_bT"] = np.ascontiguousarray(A("ada_b").reshape(2, 48, 128).transpose(0, 2, 1))
    g["nmgT"] = np.ascontiguousarray(A("norm_mix_g").reshape(2, 8, 128).transpose(0, 2, 1))
    g["nfgT"] = np.ascontiguousarray(A("norm_ffn_g").reshape(2, 8, 128).transpose(0, 2, 1))
    g["fngT"] = np.ascontiguousarray(A("final_norm_g").reshape(8, 128).T)
    g["w_in"] = A("w_in")
    mu_full = np.zeros((2, 71 * 128), f)
    mu_full[:, RW0:DA0] = A("rw_mu")
    g["muT"] = np.ascontiguousarray(mu_full.reshape(2, 71, 128).transpose(0, 2, 1))
    g["ident"] = np.eye(128, dtype=f)
    i = np.arange(64)[:, None]; j = np.arange(64)[None, :]
    g["masks"] = np.ascontiguousarray(np.stack([(i < j), (i <= j), (i > j), (i >= j)], axis=1).astype(f))
    g["hg_lbT"] = np.ascontiguousarray(A("hg_lb_logits").reshape(2, 2, 4, 128).transpose(0, 1, 3, 2))
    g["hg_ngT"] = np.ascontiguousarray(A("hg_norm_g").reshape(2, 128, 1))
    g["hg_proj"] = A("hg_proj")
    g["w_out"] = A("w_out")
    for k in ("ffn_w1", "ffn_w3", "ffn_w2", "moe_router", "moe_w1", "moe_w3", "moe_w2", "da_proj", "rw_w2", "rw_a2", "rw_g2", "rw_proj"):
        g[k] = A(k)
    sel = np.zeros((8, 8, 128), f)
    for e in range(8):
        sel[e, e, :] = 1.0
    g["sel8"] = sel
    g["da_lambda"] = A("da_lambda").reshape(2, 256)
    g["da_sg"] = A("da_subln_g")
    t = np.arange(TL)
    rowi = (t // 64).astype(f); coli = (t % 64).astype(f)
    inv_freq = (1.0 / (10000.0 ** (np.arange(0, 32, 2, dtype=f) / f(32)))).astype(f)
    ang = np.zeros((64, TL), f)
    for d in range(64):
        jj = d % 16
        ang[d] = (rowi if d < 32 else coli) * inv_freq[jj]
    g["ropeCS"] = np.ascontiguousarray(np.stack([np.cos(ang), np.sin(ang)], axis=1).astype(f))
    R = np.zeros((64, 64), f)
    for base in (0, 32):
        for q in range(16):
            R[base + q, base + 16 + q] = -1.0
            R[base + 16 + q, base + q] = 1.0
    g["ropeR"] = np.ascontiguousarray(R.T)
    vec = np.zeros((2, 8, 512), f)
    vec[:, 0] = A("rw_k_k"); vec[:, 1] = A("rw_k_a"); vec[:, 2] = A("rw_a0"); vec[:, 3] = A("rw_r_k").reshape(2, 512)
    vec[:, 4] = A("rw_ln_g"); vec[:, 5] = A("rw_ln_b")
    g["rw_vecT"] = np.ascontiguousarray(vec.reshape(2, 8, 8, 64).transpose(0, 3, 1, 2))
    g["rw_w0T"] = np.ascontiguousarray(A("rw_w0").reshape(2, 2, 8, 64).transpose(0, 1, 3, 2))
    return g


_NC_CACHE = {}


def kernel(**inputs):
    if "nc" not in _NC_CACHE:
        _NC_CACHE["nc"] = build()
    nc = _NC_CACHE["nc"]
    sh = shared_inputs(inputs)
    in_maps = []
    for b in range(8):
        m = dict(sh)
        m.update(host_inputs(inputs, b))
        in_maps.append(m)
    res = run_bass_kernel_spmd(nc, in_maps, core_ids=list(range(8)))
    out = np.stack([np.ascontiguousarray(r["outT"].T) for r in res.results], axis=0)
    return out.astype(np.float32)
```

```python
import math
import numpy as np
from contextlib import ExitStack
import concourse.bass as bass
import concourse.mybir as mybir
from concourse.bass_utils import run_bass_kernel_spmd

F32 = mybir.dt.float32
BF16 = mybir.dt.bfloat16
AF = mybir.ActivationFunctionType
ALU = mybir.AluOpType
AX = mybir.AxisListType

SAME_ENGINE_SYNC = True
N_DMA_SEMS = 40
SEM_EPOCH = 16000

T = 2304
TC = 256
TL = 2048
D = 1024
INC = 9024
FF = 2816
NE = 8
BLKS = [(0, 256, 1), (256, 512, 0), (768, 512, 0), (1280, 512, 0), (1792, 512, 0)]
HG0 = 0
RW0 = 2560
DA0 = 4416
GT0 = 5952
RW_R, RW_K, RW_V, RW_WF, RW_WB, RW_AD, RW_GD = RW0, RW0 + 512, RW0 + 1024, RW0 + 1536, RW0 + 1600, RW0 + 1664, RW0 + 1728


class Buf:
    def __init__(self, name, h):
        self.name = name
        self.h = h
        self.w = None
        self.r = {}

    def __getitem__(self, idx):
        return self.h[idx]


class Prog:
    ENG = ("pe", "act", "dve", "pool", "sp")

    def __init__(self, nc, es):
        self.nc = nc
        self.es = es
        self.e = dict(pe=nc.tensor, act=nc.scalar, dve=nc.vector, pool=nc.gpsimd, sp=nc.sync)
        self.sem = {}
        self.ekey = {}
        for k in self.ENG:
            self.ekey[k] = (k, 0)
            self.sem[(k, 0)] = es.enter_context(nc.semaphore("s_" + k))
        self.cnt = {k: 0 for k in self.ENG}
        self.dsem = [es.enter_context(nc.semaphore("d%d" % i)) for i in range(N_DMA_SEMS)]
        for i in range(N_DMA_SEMS):
            self.sem[("d", i)] = self.dsem[i]
        self.dval = [0] * N_DMA_SEMS
        self.dnext = 0
        self.seen = {k: {} for k in self.ENG}
        self.ninstr = {k: 0 for k in self.ENG}
        self.nwait = 0
        self.uid = 0

    def sb(self, name, shape, dtype=F32, es=None):
        self.uid += 1
        h = (es or self.es).enter_context(self.nc.sbuf_tensor("%s_%d" % (name, self.uid), list(shape), dtype))
        return Buf(name, h)

    def ps(self, name, shape, dtype=F32, es=None):
        self.uid += 1
        h = (es or self.es).enter_context(self.nc.psum_tensor("%s_%d" % (name, self.uid), list(shape), dtype))
        return Buf(name, h)

    def dram(self, name, shape, dtype=F32, kind="Internal"):
        h = self.nc.dram_tensor(name, list(shape), dtype, kind=kind)
        return Buf(name, h.ap())

    def _wait(self, ek, k, v):
        if self.seen[ek].get(k, 0) >= v:
            return
        self.e[ek].wait_ge(self.sem[k], v)
        self.seen[ek][k] = v
        self.nwait += 1

    def _deps(self, ek, r, w):
        deps = {}

        def add(ev):
            if ev is None:
                return
            k, v = ev
            if deps.get(k, 0) < v:
                deps[k] = v

        for b in r:
            add(b.w)
        for b in w:
            add(b.w)
            for k, v in b.r.items():
                add((k, v))
        for k, v in deps.items():
            if k[0] == ek and (not SAME_ENGINE_SYNC or ek in ("pe", "sp")):
                continue
            self._wait(ek, k, v)

    def _commit(self, ev, r, w):
        k, v = ev
        for b in w:
            b.w = ev
            b.r = {}
        for b in r:
            if b.r.get(k, 0) < v:
                b.r[k] = v

    def op(self, ek, fn, w=(), r=()):
        if self.cnt[ek] >= SEM_EPOCH:
            ep = self.ekey[ek][1] + 1
            self.ekey[ek] = (ek, ep)
            self.sem[(ek, ep)] = self.es.enter_context(self.nc.semaphore("s_%s_%d" % (ek, ep)))
            self.cnt[ek] = 0
        self._deps(ek, r, w)
        ins = fn(self.e[ek])
        self.cnt[ek] += 1
        key = self.ekey[ek]
        ins.then_inc(self.sem[key], 1)
        self.ninstr[ek] += 1
        self._commit((key, self.cnt[ek]), r, w)
        return ins

    def V(self, fn, w=(), r=()):
        return self.op("dve", fn, w, r)

    def A(self, fn, w=(), r=()):
        return self.op("act", fn, w, r)

    def G(self, fn, w=(), r=()):
        return self.op("pool", fn, w, r)

    def T(self, fn, w=(), r=()):
        return self.op("pe", fn, w, r)

    def dma(self, out, in_, w=(), r=(), q="sp", **kw):
        i = self.dnext
        self.dnext = (self.dnext + 1) % N_DMA_SEMS
        key = ("d", i)
        if self.dval[i] > 0:
            self._wait(q, key, self.dval[i])
        self._deps(q, r, w)
        ins = self.e[q].dma_start(out=out, in_=in_, **kw)
        self.dval[i] += 16
        ins.then_inc(self.dsem[i], 16)
        self.ninstr[q] += 1
        self._commit((key, self.dval[i]), r, w)
        return ins

    def barrier(self):
        for ek in self.ENG:
            for k in self.ENG:
                if k == ek:
                    continue
                key = self.ekey[k]
                if key[1] > 0:
                    self._wait(ek, (k, key[1] - 1), SEM_EPOCH)
                if self.cnt[k] > 0:
                    self._wait(ek, key, self.cnt[k])
            for i in range(N_DMA_SEMS):
                if self.dval[i] > 0:
                    self._wait(ek, ("d", i), self.dval[i])


def build(n_layers=2, dbg=(), stop_after=None, skip=()):
    nc = bass.Bass("TRN2", target_bir_lowering=False)
    with ExitStack() as es:
        p = Prog(nc, es)
        I = {}

        def inp(name, shape, dt=F32):
            I[name] = p.dram(name, shape, dt, kind="ExternalInput")
            return I[name]

        inp("xT", [D, T]); inp("c2", [128, 8, 2])
        inp("ada_w", [2, D, 6 * D]); inp("ada_bT", [2, 128, 48])
        inp("nmgT", [2, 128, 8]); inp("nfgT", [2, 128, 8]); inp("fngT", [128, 8])
        inp("w_in", [2, D, INC]); inp("muT", [2, 128, 71])
        inp("ident", [128, 128]); inp("masks", [64, 4, 64])
        inp("hg_lbT", [2, 2, 128, 4]); inp("hg_ngT", [2, 128, 1]); inp("hg_proj", [2, 512, D])
        inp("w_out", [2, D, D])
        inp("ffn_w1", [1, D, FF]); inp("ffn_w3", [1, D, FF]); inp("ffn_w2", [1, FF, D])
        inp("moe_router", [1, D, NE]); inp("moe_w1", [1, NE, D, FF]); inp("moe_w3", [1, NE, D, FF]); inp("moe_w2", [1, NE, FF, D])
        inp("sel8", [8, 8, 128])
        inp("da_lambda", [2, 256]); inp("da_sg", [2, 128]); inp("da_proj", [2, 512, D])
        inp("ropeCS", [64, 2, TL]); inp("ropeR", [64, 64])
        inp("rw_vecT", [2, 64, 8, 8]); inp("rw_w0T", [2, 2, 64, 8]); inp("rw_w2", [2, 2, 64, 512]); inp("rw_a2", [2, 64, 512])
        inp("rw_g2", [2, 128, 512]); inp("rw_proj", [2, 512, D])
        outT = p.dram("outT", [D, TL], F32, kind="ExternalOutput")

        X = p.dram("Xs", [D, T])
        PT = p.dram("PT", [INC, T])
        VHG = p.dram("VHG", [T, 512])
        VDA = p.dram("VDA", [T, 512])
        OHG = p.dram("OHG", [2, 512, T])
        YG = [p.dram("YG%d" % i, [D, T]) for i in range(3)]
        UT = p.dram("UT", [FF, T], BF16)
        FACC = p.dram("FACC", [D, T])
        dbg_out = {}

        ident = p.sb("ident", [128, 128]); p.dma(ident[:], I["ident"][:], w=[ident], r=[I["ident"]])
        masks = p.sb("masks", [64, 4, 64]); p.dma(masks[:], I["masks"][:], w=[masks], r=[I["masks"]])
        ones_bf = p.sb("ones_bf", [128, 128], BF16); p.V(lambda e: e.memset(ones_bf[:], 1.0), w=[ones_bf])
        ones_f = p.sb("ones_f", [128, 128]); p.V(lambda e: e.memset(ones_f[:], 1.0), w=[ones_f])
        ident_bf = p.sb("ident_bf", [128, 128], BF16); p.V(lambda e: e.tensor_copy(out=ident_bf[:], in_=ident[:]), w=[ident_bf], r=[ident])
        sc = p.sb("sc", [128, 8, 2]); p.dma(sc[:], I["c2"][:], w=[sc], r=[I["c2"]])
        p.A(lambda e: e.activation(out=sc[:], in_=sc[:], func=AF.Silu), w=[sc], r=[sc])
        mod = [p.sb("mod%d" % l, [128, 48, 2]) for l in range(2)]
        gsm = [p.sb("gsm%d" % l, [128, 8, 2]) for l in range(2)]
        gsf = [p.sb("gsf%d" % l, [128, 8, 2]) for l in range(2)]
        fng = p.sb("fng", [128, 8]); p.dma(fng[:], I["fngT"][:], w=[fng], r=[I["fngT"]])

        p.dma(X[:], I["xT"][:], w=[X], r=[I["xT"]])

        def stage_ada(l):
            with ExitStack() as st:
                wt = [p.sb("adaw", [128, 8, 512], es=st) for _ in range(2)]
                adab = p.sb("adab", [128, 48], es=st)
                ng = p.sb("ng", [128, 8], es=st); nf = p.sb("nf", [128, 8], es=st)
                pm = p.ps("pmod", [128, 48, 2], es=st)
                p.dma(adab[:], I["ada_bT"][l], w=[adab], r=[I["ada_bT"]])
                p.dma(ng[:], I["nmgT"][l], w=[ng], r=[I["nmgT"]])
                p.dma(nf[:], I["nfgT"][l], w=[nf], r=[I["nfgT"]])
                for cb in range(12):
                    w_ = wt[cb % 2]
                    p.dma(w_[:], I["ada_w"][l, :, cb * 512:(cb + 1) * 512].rearrange("(kc p) c -> p kc c", p=128),
                          w=[w_], r=[I["ada_w"]], q=("sp" if cb % 2 == 0 else "pool"))
                    for sub in range(4):
                        cc = cb * 4 + sub
                        for kc in range(8):
                            p.T(lambda e, w_=w_, cc=cc, kc=kc, sub=sub: e.matmul(pm[:, cc, :], lhsT=w_[:, kc, sub * 128:(sub + 1) * 128],
                                                                                rhs=sc[:, kc, :], start=(kc == 0), stop=(kc == 7)),
                                w=[pm], r=[w_, sc])
                m = mod[l]
                p.V(lambda e: e.tensor_tensor(out=m[:], in0=pm[:], in1=adab[:].unsqueeze(2).broadcast_to([128, 48, 2]), op=ALU.add),
                    w=[m], r=[pm, adab])
                for (gs, g, j) in ((gsm[l], ng, 1), (gsf[l], nf, 4)):
                    p.V(lambda e, gs=gs, g=g, j=j: e.scalar_tensor_tensor(out=gs[:], in0=m[:, j * 8:(j + 1) * 8, :], scalar=1.0,
                                                                         in1=g[:].unsqueeze(2).broadcast_to([128, 8, 2]),
                                                                         op0=ALU.add, op1=ALU.mult), w=[gs], r=[m, g])
                p.barrier()

        def norm_block(st_tiles, b0, n, gs_ap, sh_ap, hT, h32=None):
            xb, sq, pss, rstd, tmp = st_tiles
            p.dma(xb[:, :, :n], X[:, b0:b0 + n].rearrange("(kc p) t -> p kc t", p=128), w=[xb], r=[X])
            for kc in range(8):
                p.A(lambda e, kc=kc: e.activation(out=sq[:, kc, :n], in_=xb[:, kc, :n], func=AF.Square), w=[sq], r=[xb])
            for kc in range(8):
                p.T(lambda e, kc=kc: e.matmul(pss[:, :n], lhsT=ones_bf[:], rhs=sq[:, kc, :n], start=(kc == 0), stop=(kc == 7)),
                    w=[pss], r=[ones_bf, sq])
            p.A(lambda e: e.activation(out=rstd[:, :n], in_=pss[:, :n], func=AF.Sqrt, bias=1e-6, scale=1.0 / D), w=[rstd], r=[pss])
            p.V(lambda e: e.reciprocal(out=rstd[:, :n], in_=rstd[:, :n]), w=[rstd], r=[rstd])
            for kc in range(8):
                p.V(lambda e, kc=kc: e.scalar_tensor_tensor(out=tmp[:, kc, :n], in0=xb[:, kc, :n], scalar=gs_ap(kc), in1=rstd[:, :n],
                                                           op0=ALU.mult, op1=ALU.mult), w=[tmp], r=[xb, rstd])
                if sh_ap is not None:
                    if h32 is not None:
                        p.A(lambda e, kc=kc: e.activation(out=h32[:, kc, :n], in_=tmp[:, kc, :n], func=AF.Identity, bias=sh_ap(kc), scale=1.0),
                            w=[h32], r=[tmp])
                        p.V(lambda e, kc=kc: e.tensor_copy(out=hT[:, kc, b0:b0 + n], in_=h32[:, kc, :n]), w=[hT], r=[h32])
                    else:
                        p.A(lambda e, kc=kc: e.activation(out=hT[:, kc, b0:b0 + n], in_=tmp[:, kc, :n], func=AF.Identity, bias=sh_ap(kc), scale=1.0),
                            w=[hT], r=[tmp])

        def alloc_norm_tiles(st):
            return (p.sb("xb", [128, 8, 512], es=st), p.sb("sq", [128, 8, 512], BF16, es=st), p.ps("pss", [128, 512], es=st),
                    p.sb("rstd", [128, 512], es=st), p.sb("ntmp", [128, 8, 512], es=st))

        def stage_proj(l):
            with ExitStack() as st:
                hT = p.sb("hT", [128, 8, T], BF16, es=st)
                with ExitStack() as st2:
                    nt = alloc_norm_tiles(st2)
                    m = mod[l]
                    for (b0, n, seg) in BLKS:
                        norm_block(nt, b0, n, lambda kc, seg=seg: gsm[l][:, kc, seg:seg + 1], lambda kc, seg=seg: m[:, kc, seg:seg + 1], hT)
                    p.barrier()
                if "h" in dbg and l == dbg["h"]:
                    hd = p.dram("dbg_h", [D, T], BF16, kind="ExternalOutput")
                    p.dma(hd[:].rearrange("(kc p) t -> p kc t", p=128), hT[:], w=[hd], r=[hT])
                wf = [p.sb("wf", [128, 8, 512], es=st) for _ in range(2)]
                wb = [p.sb("wb", [128, 8, 512], BF16, es=st) for _ in range(2)]
                row = [p.sb("row", [128, T + 4], es=st) for _ in range(4)]
                tsm = p.sb("tsm", [128, T], es=st)
                mu = p.sb("mu", [128, 71], es=st); om = p.sb("om", [128, 71], es=st)
                pc = [p.ps("pc", [128, 512], es=st) for _ in range(4)]
                p.dma(mu[:], I["muT"][l], w=[mu], r=[I["muT"]])
                p.V(lambda e: e.tensor_scalar(out=om[:], in0=mu[:], scalar1=-1.0, scalar2=1.0, op0=ALU.mult, op1=ALU.add), w=[om], r=[mu])
                p.V(lambda e: e.tensor_scalar(out=mu[:], in0=mu[:], scalar1=0.5, scalar2=None, op0=ALU.mult), w=[mu], r=[mu])
                for r_ in row:
                    p.G(lambda e, r_=r_: e.memset(r_[:], 0.0), w=[r_])
                def rcol(t0):
                    return t0 + 1 if t0 < TC else t0 + 3
                ngrp = (INC + 511) // 512
                ei = 0

                def load_wg(g):
                    c0 = g * 512
                    ncg = min(512, INC - c0)
                    p.dma(wf[g % 2][:, :, :ncg], I["w_in"][l, :, c0:c0 + ncg].rearrange("(kc p) c -> p kc c", p=128), w=[wf[g % 2]], r=[I["w_in"]], q="pool")
                load_wg(0)
                for g in range(ngrp):
                    c0 = g * 512
                    ncg = min(512, INC - c0)
                    wf_, wb_ = wf[g % 2], wb[g % 2]
                    for kc in range(8):
                        if kc % 2 == 0:
                            p.V(lambda e, kc=kc: e.tensor_copy(out=wb_[:, kc, :ncg], in_=wf_[:, kc, :ncg]), w=[wb_], r=[wf_])
                        else:
                            p.A(lambda e, kc=kc: e.copy(out=wb_[:, kc, :ncg], in_=wf_[:, kc, :ncg]), w=[wb_], r=[wf_])
                    if g + 1 < ngrp:
                        load_wg(g + 1)
                    for sub in range((ncg + 127) // 128):
                        cb = g * 4 + sub
                        ncol = min(128, ncg - sub * 128)
                        rw_ = row[cb % 4]
                        for bi, (b0, n, seg) in enumerate(BLKS):
                            ps_ = pc[ei % 4]
                            for kc in range(8):
                                p.T(lambda e, kc=kc, ps_=ps_, n=n, b0=b0, sub=sub, ncol=ncol: e.matmul(
                                    ps_[:ncol, :n], lhsT=wb_[:, kc, sub * 128:sub * 128 + ncol], rhs=hT[:, kc, b0:b0 + n],
                                    start=(kc == 0), stop=(kc == 7)), w=[ps_], r=[wb_, hT])
                            rc = rcol(b0)
                            if ei % 2 == 0:
                                p.A(lambda e, ps_=ps_, rc=rc, n=n, ncol=ncol: e.copy(out=rw_[:ncol, rc:rc + n], in_=ps_[:ncol, :n]), w=[rw_], r=[ps_])
                            else:
                                p.V(lambda e, ps_=ps_, rc=rc, n=n, ncol=ncol: e.tensor_copy(out=rw_[:ncol, rc:rc + n], in_=ps_[:ncol, :n]), w=[rw_], r=[ps_])
                            ei += 1
                        col0 = cb * 128
                        if RW0 // 128 <= cb <= (DA0 - 1) // 128:
                            for (t0, n) in ((0, TC), (TC, TL)):
                                rc = rcol(t0)
                                p.V(lambda e, rc=rc, n=n, t0=t0: e.tensor_tensor(out=tsm[:ncol, t0:t0 + n], in0=rw_[:ncol, rc - 1:rc - 1 + n],
                                                                                 in1=rw_[:ncol, rc + 1:rc + 1 + n], op=ALU.add), w=[tsm], r=[rw_])
                                p.V(lambda e, n=n, t0=t0, cb=cb: e.tensor_scalar(out=tsm[:ncol, t0:t0 + n], in0=tsm[:ncol, t0:t0 + n],
                                                                                 scalar1=mu[:ncol, cb:cb + 1], scalar2=None, op0=ALU.mult), w=[tsm], r=[tsm, mu])
                                p.V(lambda e, rc=rc, n=n, t0=t0, cb=cb: e.scalar_tensor_tensor(out=tsm[:ncol, t0:t0 + n], in0=rw_[:ncol, rc:rc + n],
                                                                                               scalar=om[:ncol, cb:cb + 1], in1=tsm[:ncol, t0:t0 + n],
                                                                                               op0=ALU.mult, op1=ALU.add), w=[tsm], r=[rw_, om, tsm])
                            p.dma(PT[col0:col0 + ncol, :], tsm[:ncol, :], w=[PT], r=[tsm])
                        else:
                            p.dma(PT[col0:col0 + ncol, 0:TC], rw_[:ncol, 1:1 + TC], w=[PT], r=[rw_])
                            p.dma(PT[col0:col0 + ncol, TC:T], rw_[:ncol, TC + 3:T + 3], w=[PT], r=[rw_])
                for (cstart, dst) in ((HG0 + 1536, VHG), (DA0 + 1024, VDA)):
                    wf_, wb_ = wf[0], wb[0]
                    p.dma(wf_[:], I["w_in"][l, :, cstart:cstart + 512].rearrange("(kc p) c -> p kc c", p=128), w=[wf_], r=[I["w_in"]])
                    for kc in range(8):
                        p.V(lambda e, kc=kc: e.tensor_copy(out=wb_[:, kc, :], in_=wf_[:, kc, :]), w=[wb_], r=[wf_])
                    for tt in range(18):
                        ps_ = pc[tt % 4]
                        for kc in range(8):
                            p.T(lambda e, kc=kc, ps_=ps_, tt=tt: e.matmul(ps_[:, :], lhsT=hT[:, kc, tt * 128:(tt + 1) * 128], rhs=wb_[:, kc, :],
                                                                          start=(kc == 0), stop=(kc == 7)), w=[ps_], r=[wb_, hT])
                        o_ = row[tt % 4]
                        if tt % 2 == 0:
                            p.A(lambda e, ps_=ps_, o_=o_: e.copy(out=o_[:, 0:512], in_=ps_[:, :]), w=[o_], r=[ps_])
                        else:
                            p.V(lambda e, ps_=ps_, o_=o_: e.tensor_copy(out=o_[:, 0:512], in_=ps_[:, :]), w=[o_], r=[ps_])
                        p.dma(dst[tt * 128:(tt + 1) * 128, :], o_[:, 0:512], w=[dst], r=[o_])
                p.barrier()


        def load_w_bf(st, src_ap, nk, cols, name, pk=128):
            wf_ = p.sb(name + "_f", [pk, nk, cols], es=st)
            wb_ = p.sb(name + "_b", [pk, nk, cols], BF16, es=st)
            p.dma(wf_[:], src_ap, w=[wf_], r=[])
            for k in range(nk):
                p.V(lambda e, k=k: e.tensor_copy(out=wb_[:, k, :], in_=wf_[:, k, :]), w=[wb_], r=[wf_])
            return wb_

        def branch_out(bt, zT, nk, pk, wproj, gi, b0, n):
            gt, sgt, po, yo = bt
            for oc in range(8):
                p.dma(gt[:, :n], PT[GT0 + gi * 1024 + oc * 128:GT0 + gi * 1024 + (oc + 1) * 128, b0:b0 + n], w=[gt], r=[PT],
                      q=("sp" if oc % 2 == 0 else "pool"))
                p.A(lambda e: e.activation(out=sgt[:, :n], in_=gt[:, :n], func=AF.Sigmoid), w=[sgt], r=[gt])
                po_ = po[oc % 2]
                for k in range(nk):
                    p.T(lambda e, k=k, oc=oc, po_=po_: e.matmul(po_[:, :n], lhsT=wproj[:pk, k, oc * 128:(oc + 1) * 128], rhs=zT[:pk, k, :n],
                                                               start=(k == 0), stop=(k == nk - 1)), w=[po_], r=[wproj, zT])
                yo_ = yo[oc % 2]
                p.V(lambda e, po_=po_, yo_=yo_: e.tensor_tensor(out=yo_[:, :n], in0=po_[:, :n], in1=sgt[:, :n], op=ALU.mult), w=[yo_], r=[po_, sgt])
                p.dma(YG[gi][oc * 128:(oc + 1) * 128, b0:b0 + n], yo_[:, :n], w=[YG[gi]], r=[yo_], q=("pool" if oc % 2 == 0 else "sp"))

        def alloc_branch_tiles(st):
            return (p.sb("gt", [128, 512], es=st), p.sb("sgt", [128, 512], es=st),
                    [p.ps("po", [128, 512], es=st) for _ in range(2)], [p.sb("yo", [128, 512], es=st) for _ in range(2)])

        def stage_hg(l):
            with ExitStack() as st:
                lb = p.sb("lb", [128, 2, 4], es=st); oml = p.sb("oml", [128, 2, 4], es=st)
                if l == 0:
                    p.V(lambda e: e.memset(lb[:], 0.0), w=[lb])
                else:
                    lg0 = p.sb("lg0", [128, 2, 4], es=st)
                    p.dma(lg0[:], I["hg_lbT"][0].rearrange("d p h -> p d h"), w=[lg0], r=[I["hg_lbT"]])
                    p.dma(lb[:], I["hg_lbT"][1].rearrange("d p h -> p d h"), w=[lb], r=[I["hg_lbT"]])
                    p.V(lambda e: e.tensor_tensor(out=lb[:], in0=lb[:], in1=lg0[:], op=ALU.subtract), w=[lb], r=[lb, lg0])
                    p.A(lambda e: e.activation(out=lb[:], in_=lb[:], func=AF.Sigmoid), w=[lb], r=[lb])
                p.V(lambda e: e.tensor_scalar(out=oml[:], in0=lb[:], scalar1=-1.0, scalar2=1.0, op0=ALU.mult, op1=ALU.add), w=[oml], r=[lb])
                S = [p.sb("S", [128, 4, 128], es=st) for _ in range(2)]
                for d in range(2):
                    p.G(lambda e, d=d: e.memset(S[d][:], 0.0), w=[S[d]])
                names = ("qtl", "ktl", "qh", "kh")
                prep = [[{nm: p.sb(nm, [128, 4, 256], BF16, es=st) for nm in names} for _ in range(2)] for _ in range(2)]
                Sb = [p.sb("Sb", [128, 4, 128], BF16, es=st) for _ in range(2)]
                for d in range(2):
                    p.G(lambda e, d=d: e.memset(Sb[d][:], 0.0), w=[Sb[d]])
                Vgf = [p.sb("Vgf", [32, 8, 512], es=st) for _ in range(2)]
                ebC = [[p.sb("ebC", [128, 4, 8], es=st) for _ in range(2)] for _ in range(2)]
                Vg = [p.sb("Vg", [32, 8, 512], BF16, es=st) for _ in range(2)]
                og = [p.sb("og", [128, 4, 256], es=st) for _ in range(2)]
                tmp = {nm: [p.sb(nm, [128, 256], es=st) for _ in range(2)] for nm in ("zt", "qt", "lf", "F", "E", "kg", "X", "ex")}
                ones256 = p.sb("ones256", [128, 256], es=st); p.V(lambda e: e.memset(ones256[:], 1.0), w=[ones256])
                khT = [p.sb("khT", [32, 512], BF16, es=st) for _ in range(2)]
                AT = [p.sb("AT", [32, 4, 32], BF16, es=st) for _ in range(2)]
                pT = [p.ps("pT", [32, 512], BF16, es=st) for _ in range(2)]
                pA = [p.ps("pA", [32, 4, 32], es=st) for _ in range(2)]
                pO = [p.ps("pO", [128, 4, 32], es=st) for _ in range(2)]
                pS = [p.ps("pS", [128, 4, 128], es=st) for _ in range(2)]
                bwd_groups = [0, 8, 7, 6, 5, 4, 3, 2, 1]
                ti = 0
                for step in range(9):
                    ctx_ = {}
                    for d in range(2):
                        g = step if d == 0 else bwd_groups[step]
                        t0 = g * 256
                        pr = prep[d][step % 2]
                        eb = ebC[d][step % 2]
                        ctx_[d] = (t0, pr, eb)
                        p.dma(Vgf[d][:], VHG[t0:t0 + 256, :].rearrange("(c s) v -> s c v", s=32), w=[Vgf[d]], r=[VHG], q="pool")
                        p.A(lambda e, d=d: e.copy(out=Vg[d][:], in_=Vgf[d][:]), w=[Vg[d]], r=[Vgf[d]])
                        for h in range(4):
                            tt = {nm: tmp[nm][ti % 2] for nm in tmp}
                            ti += 1
                            zt, qt, lf, Ft, Et, kg, Xt, ex = (tt[nm] for nm in ("zt", "qt", "lf", "F", "E", "kg", "X", "ex"))
                            zr = 512 * (1 + d) + h * 128
                            p.dma(zt[:], PT[zr:zr + 128, t0:t0 + 256], w=[zt], r=[PT])
                            p.dma(qt[:], PT[h * 128:(h + 1) * 128, t0:t0 + 256], w=[qt], r=[PT])
                            p.A(lambda e: e.activation(out=zt[:], in_=zt[:], func=AF.Sigmoid), w=[zt], r=[zt])
                            p.V(lambda e, d=d, h=h: e.tensor_scalar(out=zt[:], in0=zt[:], scalar1=oml[:, d, h:h + 1], scalar2=lb[:, d, h:h + 1],
                                                                   op0=ALU.mult, op1=ALU.add), w=[zt], r=[zt, oml, lb])
                            p.A(lambda e: e.activation(out=lf[:], in_=zt[:], func=AF.Ln), w=[lf], r=[zt])
                            p.A(lambda e: e.activation(out=kg[:], in_=zt[:], func=AF.Identity, bias=1.0, scale=-1.0), w=[kg], r=[zt])
                            p.A(lambda e: e.activation(out=qt[:], in_=qt[:], func=AF.Silu), w=[qt], r=[qt])
                            p.V(lambda e: e.tensor_tensor_scan(out=Ft[:], data0=ones256[:], data1=lf[:], initial=0.0, op0=ALU.mult, op1=ALU.add),
                                w=[Ft], r=[ones256, lf])
                            p.V(lambda e: e.tensor_tensor(out=Et[:], in0=Ft[:], in1=lf[:], op=ALU.subtract), w=[Et], r=[Ft, lf])
                            F3 = Ft[:].rearrange("p (c t) -> p c t", t=32); E3 = Et[:].rearrange("p (c t) -> p c t", t=32)
                            X3 = Xt[:].rearrange("p (c t) -> p c t", t=32)
                            bc = lambda ap: ap.broadcast_to([128, 8, 32])
                            if d == 0:
                                plan = [(F3, F3[:, :, 15:16], [("qtl", qt, 1.0), ("ktl", kg, -1.0)]),
                                        (F3, E3[:, :, 0:1], [("qh", qt, 1.0)]),
                                        (F3, F3[:, :, 31:32], [("kh", kg, -1.0)])]
                            else:
                                plan = [(E3, E3[:, :, 16:17], [("qtl", qt, -1.0), ("ktl", kg, 1.0)]),
                                        (E3, F3[:, :, 31:32], [("qh", qt, -1.0)]),
                                        (E3, E3[:, :, 0:1], [("kh", kg, 1.0)])]
                            for (src3, ref, outs) in plan:
                                p.V(lambda e, src3=src3, ref=ref: e.tensor_tensor(out=X3, in0=src3, in1=bc(ref), op=ALU.subtract), w=[Xt], r=[Ft, Et])
                                for (nm, mul, sgn) in outs:
                                    p.A(lambda e, sgn=sgn: e.activation(out=ex[:], in_=Xt[:], func=AF.Exp, scale=sgn), w=[ex], r=[Xt])
                                    p.V(lambda e, nm=nm, mul=mul, h=h: e.tensor_tensor(out=pr[nm][:, h, :], in0=mul[:], in1=ex[:], op=ALU.mult),
                                        w=[pr[nm]], r=[mul, ex])
                            p.V(lambda e, h=h: e.tensor_tensor(out=eb[:, h, :], in0=F3[:, :, 31], in1=E3[:, :, 0], op=ALU.subtract), w=[eb], r=[Ft, Et])
                            p.A(lambda e, h=h: e.activation(out=eb[:, h, :], in_=eb[:, h, :], func=AF.Exp), w=[eb], r=[eb])
                    for ci in range(8):
                        for d in range(2):
                            t0, pr, eb = ctx_[d]
                            mi = 1 if d == 0 else 3
                            c = ci if d == 0 else 7 - ci
                            cs = c * 32
                            for h in range(4):
                                p.T(lambda e, h=h, cs=cs: e.transpose(pT[d][:32, h * 128:(h + 1) * 128], pr["kh"][:, h, cs:cs + 32], ident_bf[:]),
                                    w=[pT[d]], r=[pr["kh"], ident_bf])
                            p.A(lambda e: e.copy(out=khT[d][:], in_=pT[d][:]), w=[khT[d]], r=[pT[d]])
                            for h in range(4):
                                p.T(lambda e, h=h, cs=cs: e.matmul(pA[d][:, h, :], lhsT=pr["ktl"][:, h, cs:cs + 32], rhs=pr["qtl"][:, h, cs:cs + 32],
                                                                  start=True, stop=True), w=[pA[d]], r=[pr["ktl"], pr["qtl"]])
                            p.V(lambda e: e.tensor_tensor(out=AT[d][:], in0=pA[d][:], in1=masks[0:32, mi, 0:32].unsqueeze(1).broadcast_to([32, 4, 32]),
                                                          op=ALU.mult), w=[AT[d]], r=[pA[d], masks])
                            for h in range(4):
                                p.T(lambda e, h=h, c=c: e.matmul(pO[d][:, h, :], lhsT=Vg[d][:, c, h * 128:(h + 1) * 128], rhs=AT[d][:, h, :],
                                                                start=True, stop=False), w=[pO[d]], r=[Vg[d], AT[d]])
                                p.T(lambda e, h=h, cs=cs: e.matmul(pO[d][:, h, :], lhsT=Sb[d][:, h, :], rhs=pr["qh"][:, h, cs:cs + 32],
                                                                  start=False, stop=True), w=[pO[d]], r=[Sb[d], pr["qh"]])
                            p.A(lambda e, cs=cs: e.copy(out=og[d][:, :, cs:cs + 32], in_=pO[d][:]), w=[og[d]], r=[pO[d]])
                            for h in range(4):
                                p.T(lambda e, h=h, c=c: e.matmul(pS[d][:, h, :], lhsT=khT[d][:, h * 128:(h + 1) * 128], rhs=Vg[d][:, c, h * 128:(h + 1) * 128],
                                                                start=True, stop=True), w=[pS[d]], r=[khT[d], Vg[d]])
                            p.V(lambda e, c=c: e.tensor_tensor(out=S[d][:], in0=S[d][:], in1=eb[:, :, c:c + 1].broadcast_to([128, 4, 128]), op=ALU.mult),
                                w=[S[d]], r=[S[d], eb])
                            p.V(lambda e: e.tensor_tensor(out=S[d][:], in0=S[d][:], in1=pS[d][:], op=ALU.add), w=[S[d]], r=[S[d], pS[d]])
                            p.A(lambda e: e.copy(out=Sb[d][:], in_=S[d][:]), w=[Sb[d]], r=[S[d]])
                    for d in range(2):
                        t0, pr, eb = ctx_[d]
                        p.dma(OHG[d, :, t0:t0 + 256].rearrange("(h v) t -> v h t", v=128), og[d][:], w=[OHG], r=[og[d]])
                p.barrier()
            with ExitStack() as st:
                wproj = load_w_bf(st, I["hg_proj"][l].rearrange("(k p) c -> p k c", p=128), 4, D, "hgp")
                ngv = p.sb("ngv", [128, 1], es=st); p.dma(ngv[:], I["hg_ngT"][l], w=[ngv], r=[I["hg_ngT"]])
                bt = alloc_branch_tiles(st)
                oa = p.sb("oa", [128, 4, 512], es=st); ob = p.sb("ob", [128, 4, 512], es=st); gg = p.sb("gg", [128, 4, 512], es=st)
                sq = p.sb("sqh", [128, 4, 512], BF16, es=st); zT = p.sb("zT", [128, 4, 512], BF16, es=st)
                pn = p.ps("pn", [128, 512], es=st); rs = p.sb("rs", [128, 512], es=st)
                for (b0, n, seg) in BLKS:
                    p.dma(oa[:, :, :n], OHG[0, :, b0:b0 + n].rearrange("(h v) t -> v h t", v=128), w=[oa], r=[OHG])
                    p.dma(ob[:, :, :n], OHG[1, :, b0:b0 + n].rearrange("(h v) t -> v h t", v=128), w=[ob], r=[OHG], q="pool")
                    p.dma(gg[:, :, :n], PT[2048:2560, b0:b0 + n].rearrange("(h v) t -> v h t", v=128), w=[gg], r=[PT])
                    p.V(lambda e: e.tensor_tensor(out=oa[:, :, :n], in0=oa[:, :, :n], in1=ob[:, :, :n], op=ALU.add), w=[oa], r=[oa, ob])
                    p.A(lambda e: e.activation(out=gg[:, :, :n], in_=gg[:, :, :n], func=AF.Silu), w=[gg], r=[gg])
                    p.V(lambda e: e.tensor_tensor(out=sq[:, :, :n], in0=oa[:, :, :n], in1=oa[:, :, :n], op=ALU.mult), w=[sq], r=[oa])
                    for h in range(4):
                        p.T(lambda e, h=h: e.matmul(pn[:, :n], lhsT=ones_bf[:], rhs=sq[:, h, :n], start=True, stop=True), w=[pn], r=[ones_bf, sq])
                        p.A(lambda e: e.activation(out=rs[:, :n], in_=pn[:, :n], func=AF.Sqrt, bias=1e-6, scale=1.0 / 128), w=[rs], r=[pn])
                        p.V(lambda e: e.reciprocal(out=rs[:, :n], in_=rs[:, :n]), w=[rs], r=[rs])
                        p.V(lambda e, h=h: e.scalar_tensor_tensor(out=oa[:, h, :n], in0=oa[:, h, :n], scalar=ngv[:, 0:1], in1=rs[:, :n],
                                                                 op0=ALU.mult, op1=ALU.mult), w=[oa], r=[oa, ngv, rs])
                        p.V(lambda e, h=h: e.tensor_tensor(out=zT[:, h, :n], in0=oa[:, h, :n], in1=gg[:, h, :n], op=ALU.mult), w=[zT], r=[oa, gg])
                    branch_out(bt, zT, 4, 128, wproj, 0, b0, n)
                p.barrier()


        def stage_da(l):
            lam_init = 0.8 - 0.6 * math.exp(-0.3 * l)
            scale = 64 ** -0.5
            with ExitStack() as st:
                lp = p.sb("lp", [128, 4, 64], es=st); s12 = p.sb("s12", [128, 2], es=st); nlam = p.sb("nlam", [128, 1], es=st)
                pr_ = p.sb("lpp", [128, 2, 64], es=st)
                p.dma(lp[:].rearrange("p a b -> p (a b)"), I["da_lambda"][l].partition_broadcast(128), w=[lp], r=[I["da_lambda"]])
                p.V(lambda e: e.tensor_tensor(out=pr_[:, 0, :], in0=lp[:, 0, :], in1=lp[:, 1, :], op=ALU.mult), w=[pr_], r=[lp])
                p.V(lambda e: e.tensor_tensor(out=pr_[:, 1, :], in0=lp[:, 2, :], in1=lp[:, 3, :], op=ALU.mult), w=[pr_], r=[lp])
                p.V(lambda e: e.reduce_sum(out=s12[:], in_=pr_[:], axis=AX.X), w=[s12], r=[pr_])
                p.A(lambda e: e.activation(out=s12[:], in_=s12[:], func=AF.Exp), w=[s12], r=[s12])
                p.V(lambda e: e.tensor_tensor(out=nlam[:], in0=s12[:, 1:2], in1=s12[:, 0:1], op=ALU.subtract), w=[nlam], r=[s12])
                p.V(lambda e: e.tensor_scalar(out=nlam[:], in0=nlam[:], scalar1=-lam_init, scalar2=None, op0=ALU.add), w=[nlam], r=[nlam])
                sg = p.sb("sg", [128, 128], es=st)
                p.dma(sg[:], I["da_sg"][l].partition_broadcast(128), w=[sg], r=[I["da_sg"]])
                p.V(lambda e: e.tensor_scalar(out=sg[:], in0=sg[:], scalar1=1.0 - lam_init, scalar2=None, op0=ALU.mult), w=[sg], r=[sg])
                KT = p.sb("KT", [64, 8, T], BF16, es=st); QT = p.sb("QT", [64, 8, T], BF16, es=st)
                Vb = p.sb("Vb", [128, 18, 512], BF16, es=st)
                with ExitStack() as st2:
                    cs = p.sb("cs", [64, 2, TL], es=st2); p.dma(cs[:], I["ropeCS"][:], w=[cs], r=[I["ropeCS"]])
                    rR = p.sb("rR", [64, 64], es=st2); p.dma(rR[:], I["ropeR"][:], w=[rR], r=[I["ropeR"]])
                    xr = [p.sb("xr", [64, T], es=st2) for _ in range(2)]
                    t1 = [p.sb("t1", [64, 512], es=st2) for _ in range(2)]; t2 = [p.sb("t2", [64, 512], es=st2) for _ in range(2)]
                    pr2 = [p.ps("prp", [64, 512], es=st2) for _ in range(2)]
                    vf = [p.sb("vf", [128, 512], es=st2) for _ in range(2)]
                    i2 = 0
                    for qi, (dst, roff) in enumerate(((QT, DA0), (KT, DA0 + 512))):
                        for hm in range(8):
                            x_ = xr[hm % 2]
                            p.dma(x_[:], PT[roff + hm * 64:roff + (hm + 1) * 64, :], w=[x_], r=[PT], q=("sp" if hm % 2 == 0 else "pool"))
                            p.A(lambda e, hm=hm, dst=dst, x_=x_: e.copy(out=dst[:, hm, 0:TC], in_=x_[:, 0:TC]), w=[dst], r=[x_])
                            for bi in range(4):
                                a0 = TC + bi * 512
                                pp = pr2[i2 % 2]; t1_ = t1[i2 % 2]; t2_ = t2[i2 % 2]; i2 += 1
                                p.T(lambda e, pp=pp, x_=x_, a0=a0: e.matmul(pp[:], lhsT=rR[:], rhs=x_[:, a0:a0 + 512], start=True, stop=True), w=[pp], r=[rR, x_])
                                p.V(lambda e, t1_=t1_, x_=x_, a0=a0, bi=bi: e.tensor_tensor(out=t1_[:], in0=x_[:, a0:a0 + 512], in1=cs[:, 0, bi * 512:(bi + 1) * 512], op=ALU.mult),
                                    w=[t1_], r=[x_, cs])
                                p.V(lambda e, t2_=t2_, pp=pp, bi=bi: e.tensor_tensor(out=t2_[:], in0=pp[:], in1=cs[:, 1, bi * 512:(bi + 1) * 512], op=ALU.mult),
                                    w=[t2_], r=[pp, cs])
                                p.V(lambda e, dst=dst, hm=hm, a0=a0, t1_=t1_, t2_=t2_: e.tensor_tensor(out=dst[:, hm, a0:a0 + 512], in0=t1_[:], in1=t2_[:], op=ALU.add),
                                    w=[dst], r=[t1_, t2_])
                    for kt in range(18):
                        v_ = vf[kt % 2]
                        p.dma(v_[:], VDA[kt * 128:(kt + 1) * 128, :], w=[v_], r=[VDA], q=("sp" if kt % 2 == 0 else "pool"))
                        p.V(lambda e, kt=kt, v_=v_: e.tensor_copy(out=Vb[:, kt, :], in_=v_[:]), w=[Vb], r=[v_])
                    p.barrier()
                wproj = load_w_bf(st, I["da_proj"][l].rearrange("(k p) c -> p k c", p=128), 4, D, "dap")
                bt = alloc_branch_tiles(st)
                pS = [p.ps("pSc", [128, 512], es=st) for _ in range(2)]
                Eb = [p.sb("Eb", [128, 18, 512], BF16, es=st) for _ in range(2)]
                acc = [p.ps("acc", [128, 4, 128], es=st) for _ in range(2)]
                den = p.ps("den", [128, 2, 4], es=st)
                pZ = p.ps("pZ", [128, 512], es=st)
                rden = p.sb("rden", [128, 2, 4], es=st); rl = p.sb("rl", [128, 4], es=st)
                o1 = p.sb("o1", [128, 4, 128], es=st); o2 = p.sb("o2", [128, 4, 128], es=st); ssq = p.sb("ssq", [128, 4], es=st)
                zT = p.sb("zTd", [128, 4, 512], BF16, es=st)
                qblocks = [(0, 256, [0, 1])] + [(TC + i * 512, 512, list(range(18))) for i in range(4)]
                ei = [0]

                def phase1(q0, nq, kts, hm):
                    Eb_ = Eb[hm % 2]
                    for kt in kts:
                        pS_ = pS[ei[0] % 2]; ei[0] += 1
                        p.T(lambda e, pS_=pS_, kt=kt: e.matmul(pS_[:, :nq], lhsT=KT[:, hm, kt * 128:(kt + 1) * 128], rhs=QT[:, hm, q0:q0 + nq],
                                                              start=True, stop=True), w=[pS_], r=[KT, QT])
                        p.A(lambda e, pS_=pS_, kt=kt: e.activation(out=Eb_[:, kt, :nq], in_=pS_[:, :nq], func=AF.Exp, scale=scale), w=[Eb_], r=[pS_])

                def phase2(q0, nq, kts, hm):
                    Eb_ = Eb[hm % 2]; h = hm // 2; m = hm % 2
                    for j in range(nq // 128):
                        for kt in kts:
                            p.T(lambda e, j=j, kt=kt: e.matmul(acc[m][:, j, :], lhsT=Eb_[:, kt, j * 128:(j + 1) * 128], rhs=Vb[:, kt, h * 128:(h + 1) * 128],
                                                              start=(kt == kts[0]), stop=(kt == kts[-1])), w=[acc[m]], r=[Eb_, Vb])
                        for kt in kts:
                            p.T(lambda e, j=j, kt=kt: e.matmul(den[:, m, j:j + 1], lhsT=Eb_[:, kt, j * 128:(j + 1) * 128], rhs=ones_bf[:, 0:1],
                                                              start=(kt == kts[0]), stop=(kt == kts[-1])), w=[den], r=[Eb_, ones_bf])

                for (q0, nq, kts) in qblocks:
                    nj = nq // 128
                    phase1(q0, nq, kts, 0)
                    for hm in range(8):
                        if hm + 1 < 8:
                            phase1(q0, nq, kts, hm + 1)
                        phase2(q0, nq, kts, hm)
                        if hm % 2 == 0:
                            continue
                        h = hm // 2
                        p.V(lambda e: e.reciprocal(out=rden[:, :, :nj], in_=den[:, :, :nj]), w=[rden], r=[den])
                        p.V(lambda e: e.tensor_scalar(out=rl[:, :nj], in0=rden[:, 1, :nj], scalar1=nlam[:, 0:1], scalar2=None, op0=ALU.mult), w=[rl], r=[rden, nlam])
                        p.V(lambda e: e.tensor_tensor(out=o1[:, :nj, :], in0=acc[0][:, :nj, :], in1=rden[:, 0, :nj].unsqueeze(2).broadcast_to([128, nj, 128]), op=ALU.mult),
                            w=[o1], r=[acc[0], rden])
                        p.V(lambda e: e.tensor_tensor(out=o2[:, :nj, :], in0=acc[1][:, :nj, :], in1=rl[:, :nj].unsqueeze(2).broadcast_to([128, nj, 128]), op=ALU.mult),
                            w=[o2], r=[acc[1], rl])
                        p.V(lambda e: e.tensor_tensor(out=o1[:, :nj, :], in0=o1[:, :nj, :], in1=o2[:, :nj, :], op=ALU.add), w=[o1], r=[o1, o2])
                        p.V(lambda e: e.tensor_tensor(out=o2[:, :nj, :], in0=o1[:, :nj, :], in1=o1[:, :nj, :], op=ALU.mult), w=[o2], r=[o1])
                        p.V(lambda e: e.reduce_sum(out=ssq[:, :nj], in_=o2[:, :nj, :], axis=AX.X), w=[ssq], r=[o2])
                        p.A(lambda e: e.activation(out=ssq[:, :nj], in_=ssq[:, :nj], func=AF.Sqrt, bias=1e-5, scale=1.0 / 128), w=[ssq], r=[ssq])
                        p.V(lambda e: e.reciprocal(out=ssq[:, :nj], in_=ssq[:, :nj]), w=[ssq], r=[ssq])
                        p.V(lambda e: e.tensor_tensor(out=o1[:, :nj, :], in0=o1[:, :nj, :], in1=ssq[:, :nj].unsqueeze(2).broadcast_to([128, nj, 128]), op=ALU.mult),
                            w=[o1], r=[o1, ssq])
                        p.V(lambda e: e.tensor_tensor(out=o1[:, :nj, :], in0=o1[:, :nj, :], in1=sg[:].unsqueeze(1).broadcast_to([128, nj, 128]), op=ALU.mult),
                            w=[o1], r=[o1, sg])
                        for j in range(nj):
                            p.T(lambda e, j=j: e.transpose(pZ[:, j * 128:(j + 1) * 128], o1[:, j, :], ident[:]), w=[pZ], r=[o1, ident])
                        p.A(lambda e, h=h: e.copy(out=zT[:, h, :nq], in_=pZ[:, :nq]), w=[zT], r=[pZ])
                    branch_out(bt, zT, 4, 128, wproj, 2, q0, nq)
                p.barrier()


        RWN = ("R", "K", "V", "AN", "B", "LW0", "LW1", "BON", "G")
        RWS = {nm: p.dram("RWS_" + nm, [64, 8, T]) for nm in RWN}
        YRW = [p.dram("YRW%d" % d, [64, 8, T]) for d in range(2)]

        def stage_rw_prep(l):
            with ExitStack() as st:
                vec = p.sb("rwvec", [64, 8, 8], es=st); p.dma(vec[:], I["rw_vecT"][l], w=[vec], r=[I["rw_vecT"]])
                w0 = p.sb("rww0", [64, 2, 8], es=st); p.dma(w0[:], I["rw_w0T"][l].rearrange("d k h -> k d h"), w=[w0], r=[I["rw_w0T"]])
                w2 = p.sb("rww2", [64, 2, 512], es=st); p.dma(w2[:], I["rw_w2"][l].rearrange("d j c -> j d c"), w=[w2], r=[I["rw_w2"]])
                a2 = p.sb("rwa2", [64, 512], es=st); p.dma(a2[:], I["rw_a2"][l], w=[a2], r=[I["rw_a2"]])
                g2 = p.sb("rwg2", [128, 512], es=st); p.dma(g2[:], I["rw_g2"][l], w=[g2], r=[I["rw_g2"]])
                n = 256
                mk = lambda nm: p.sb(nm, [64, 8, n], es=st)
                r_, kr, v_, a_, kk, sq, nrm, k_, b_, an, rk, bon, g_ = (mk(x) for x in ("r_", "kr", "v_", "a_", "kk", "sq", "nrm", "k_", "b_", "an", "rk", "bon", "g_"))
                lw = [mk("lw0"), mk("lw1")]
                adt = p.sb("adt", [64, n], es=st); wd = [p.sb("wdf", [64, n], es=st), p.sb("wdb", [64, n], es=st)]
                gdt = p.sb("gdt", [128, n], es=st); th = p.sb("th", [64, n], es=st)
                pool = [p.ps("prw", [64, 2, n], es=st) for _ in range(4)]
                pi = [0]

                def nps():
                    pi[0] += 1
                    return pool[pi[0] % 4]
                bc = lambda i: vec[:, i, :].unsqueeze(2).broadcast_to([64, 8, n])
                for blk in range(9):
                    b0 = blk * n
                    hk = lambda c0: PT[c0:c0 + 512, b0:b0 + n].rearrange("(h k) t -> k h t", k=64)
                    p.dma(r_[:], hk(RW_R), w=[r_], r=[PT]); p.dma(kr[:], hk(RW_K), w=[kr], r=[PT], q="pool"); p.dma(v_[:], hk(RW_V), w=[v_], r=[PT])
                    p.dma(adt[:], PT[RW_AD:RW_AD + 64, b0:b0 + n], w=[adt], r=[PT], q="pool")
                    p.dma(wd[0][:], PT[RW_WF:RW_WF + 64, b0:b0 + n], w=[wd[0]], r=[PT]); p.dma(wd[1][:], PT[RW_WB:RW_WB + 64, b0:b0 + n], w=[wd[1]], r=[PT], q="pool")
                    p.dma(gdt[:], PT[RW_GD:RW_GD + 128, b0:b0 + n], w=[gdt], r=[PT])
                    for hp in range(4):
                        ps_ = nps()
                        for hh in range(2):
                            h = hp * 2 + hh
                            p.T(lambda e, h=h, hh=hh, ps_=ps_: e.matmul(ps_[:, hh, :], lhsT=a2[:, h * 64:(h + 1) * 64], rhs=adt[:], start=True, stop=True), w=[ps_], r=[a2, adt])
                        for hh in range(2):
                            h = hp * 2 + hh
                            p.A(lambda e, h=h, hh=hh, ps_=ps_: e.activation(out=a_[:, h, :], in_=ps_[:, hh, :], func=AF.Sigmoid, bias=vec[:, 2, h:h + 1], scale=1.0),
                                w=[a_], r=[ps_, vec])
                    p.V(lambda e: e.tensor_tensor(out=kk[:], in0=kr[:], in1=bc(0), op=ALU.mult), w=[kk], r=[kr, vec])
                    p.A(lambda e: e.activation(out=sq[:], in_=kk[:], func=AF.Square), w=[sq], r=[kk])
                    for hp in range(4):
                        ps_ = nps()
                        p.T(lambda e, hp=hp, ps_=ps_: e.matmul(ps_[:].rearrange("p a b -> p (a b)"), lhsT=ones_f[:64, :64],
                                                              rhs=sq[:, 2 * hp:2 * hp + 2, :].rearrange("p a b -> p (a b)"), start=True, stop=True), w=[ps_], r=[ones_f, sq])
                        p.A(lambda e, hp=hp, ps_=ps_: e.activation(out=nrm[:, 2 * hp:2 * hp + 2, :], in_=ps_[:], func=AF.Sqrt), w=[nrm], r=[ps_])
                    p.V(lambda e: e.tensor_scalar(out=nrm[:], in0=nrm[:], scalar1=1e-12, scalar2=None, op0=ALU.max), w=[nrm], r=[nrm])
                    p.V(lambda e: e.reciprocal(out=nrm[:], in_=nrm[:]), w=[nrm], r=[nrm])
                    p.V(lambda e: e.tensor_tensor(out=kk[:], in0=kk[:], in1=nrm[:], op=ALU.mult), w=[kk], r=[kk, nrm])
                    p.V(lambda e: e.scalar_tensor_tensor(out=k_[:], in0=a_[:], scalar=-1.0, in1=bc(1), op0=ALU.add, op1=ALU.mult), w=[k_], r=[a_, vec])
                    p.V(lambda e: e.scalar_tensor_tensor(out=k_[:], in0=k_[:], scalar=1.0, in1=kr[:], op0=ALU.add, op1=ALU.mult), w=[k_], r=[k_, kr])
                    p.V(lambda e: e.tensor_tensor(out=b_[:], in0=kk[:], in1=a_[:], op=ALU.mult), w=[b_], r=[kk, a_])
                    p.A(lambda e: e.activation(out=an[:], in_=kk[:], func=AF.Identity, scale=-1.0), w=[an], r=[kk])
                    p.V(lambda e: e.tensor_tensor(out=rk[:], in0=r_[:], in1=k_[:], op=ALU.mult), w=[rk], r=[r_, k_])
                    p.V(lambda e: e.tensor_tensor(out=rk[:], in0=rk[:], in1=bc(3), op=ALU.mult), w=[rk], r=[rk, vec])
                    for hp in range(4):
                        ps_ = nps()
                        p.T(lambda e, hp=hp, ps_=ps_: e.matmul(ps_[:].rearrange("p a b -> p (a b)"), lhsT=ones_f[:64, :64],
                                                              rhs=rk[:, 2 * hp:2 * hp + 2, :].rearrange("p a b -> p (a b)"), start=True, stop=True), w=[ps_], r=[ones_f, rk])
                        p.V(lambda e, hp=hp, ps_=ps_: e.tensor_tensor(out=bon[:, 2 * hp:2 * hp + 2, :], in0=ps_[:], in1=v_[:, 2 * hp:2 * hp + 2, :], op=ALU.mult),
                            w=[bon], r=[ps_, v_])
                    p.A(lambda e: e.activation(out=gdt[:], in_=gdt[:], func=AF.Sigmoid), w=[gdt], r=[gdt])
                    for hp in range(4):
                        ps_ = nps()
                        for hh in range(2):
                            h = hp * 2 + hh
                            p.T(lambda e, h=h, hh=hh, ps_=ps_: e.matmul(ps_[:, hh, :], lhsT=g2[:, h * 64:(h + 1) * 64], rhs=gdt[:], start=True, stop=True), w=[ps_], r=[g2, gdt])
                        p.A(lambda e, hp=hp, ps_=ps_: e.copy(out=g_[:, 2 * hp:2 * hp + 2, :], in_=ps_[:]), w=[g_], r=[ps_])
                    for d in range(2):
                        p.A(lambda e, d=d: e.activation(out=th[:], in_=wd[d][:], func=AF.Tanh), w=[th], r=[wd[d]])
                        for hp in range(4):
                            ps_ = nps()
                            for hh in range(2):
                                h = hp * 2 + hh
                                p.T(lambda e, h=h, hh=hh, ps_=ps_, d=d: e.matmul(ps_[:, hh, :], lhsT=w2[:, d, h * 64:(h + 1) * 64], rhs=th[:], start=True, stop=True), w=[ps_], r=[w2, th])
                            for hh in range(2):
                                h = hp * 2 + hh
                                p.A(lambda e, h=h, hh=hh, ps_=ps_, d=d: e.activation(out=lw[d][:, h, :], in_=ps_[:, hh, :], func=AF.Sigmoid, bias=w0[:, d, h:h + 1], scale=1.0),
                                    w=[lw[d]], r=[ps_, w0])
                        p.V(lambda e, d=d: e.tensor_scalar(out=lw[d][:], in0=lw[d][:], scalar1=-math.exp(-0.5), scalar2=None, op0=ALU.mult), w=[lw[d]], r=[lw[d]])
                    for qi, (nm, tl) in enumerate((("R", r_), ("K", k_), ("V", v_), ("AN", an), ("B", b_), ("LW0", lw[0]), ("LW1", lw[1]), ("BON", bon), ("G", g_))):
                        p.dma(RWS[nm][:, :, b0:b0 + n], tl[:], w=[RWS[nm]], r=[tl], q=("sp" if qi % 2 == 0 else "pool"))
                p.barrier()

        def stage_rw_scan(l):
            with ExitStack() as st:
                mk = lambda nm, shp=(64, 8, 64), dt=F32: [p.sb(nm, list(shp), dt, es=st) for _ in range(2)]
                ST = mk("ST"); STb = mk("STb", dt=BF16); Vb = mk("Vb", dt=BF16); Xb = mk("Xb", dt=BF16)
                for d in range(2):
                    p.G(lambda e, d=d: e.memset(STb[d][:], 0.0), w=[STb[d]])
                for d in range(2):
                    p.G(lambda e, d=d: e.memset(ST[d][:], 0.0), w=[ST[d]])
                rmask = p.sb("rmask", [64, 8, 64], es=st)
                p.V(lambda e: e.memset(rmask[:], 1.0), w=[rmask]); p.V(lambda e: e.memset(rmask[:, :, 0:1], 0.0), w=[rmask])
                ld = {nm: mk("l" + nm) for nm in ("R", "K", "V", "AN", "B", "LW")}
                Ft, Et, Gt, Ht = mk("F"), mk("E"), mk("G"), mk("H")
                ex = [mk("ex0"), mk("ex1")]
                AR = mk("AR", (64, 8, 2, 64), BF16); bt, kt_, bh, kh = mk("bt", dt=BF16), mk("kt", dt=BF16), mk("bh", dt=BF16), mk("kh", dt=BF16)
                Vtm, Btm, Ktm = mk("Vtm", dt=BF16), mk("Btm", dt=BF16), mk("Ktm", dt=BF16)
                W1 = mk("W1", (64, 8, 128), BF16); W2 = mk("W2", (64, 8, 128), BF16); Lm = mk("Lm", dt=BF16); Xm = mk("Xm")
                Pb = [mk("Pb0", dt=BF16), mk("Pb1", dt=BF16)]; PTb = [mk("PTb0", dt=BF16), mk("PTb1", dt=BF16)]
                RH, Um, yo = mk("RH", dt=BF16), mk("Um", dt=BF16), mk("yo")
                gC = mk("gC", (64, 8))
                pool = [p.ps("prs", [64, 8, 64], es=st) for _ in range(6)]
                poolb = [p.ps("prsb", [64, 8, 64], BF16, es=st) for _ in range(2)]
                pi = [0, 0]

                def nps():
                    pi[0] += 1
                    return pool[pi[0] % 6]

                def npsb():
                    pi[1] += 1
                    return poolb[pi[1] % 2]
                f2 = lambda b: b[:].rearrange("p h t -> p (h t)")
                bwd = [3, 2, 1, 0] + list(range(35, 3, -1))
                idb = ident[:64, :64].unsqueeze(1).broadcast_to([64, 8, 64])
                ei = [0]

                def exp_to(d, src, sgn):
                    e_ = ex[ei[0] % 2][d]; ei[0] += 1
                    p.A(lambda e: e.activation(out=e_[:], in_=src[:], func=AF.Exp, scale=sgn), w=[e_], r=[src])
                    return e_
                for step in range(36):
                    cd = (step, bwd[step])
                    for d in range(2):
                        t0 = cd[d] * 64
                        for qi, nm in enumerate(("R", "K", "V", "AN", "B", "LW")):
                            src = RWS["LW%d" % d] if nm == "LW" else RWS[nm]
                            p.dma(ld[nm][d][:], src[:, :, t0:t0 + 64], w=[ld[nm][d]], r=[src], q=("sp" if qi % 2 == 0 else "pool"))
                    for d in range(2):
                        lw = ld["LW"][d]; F_, E_, G_, H_ = Ft[d], Et[d], Gt[d], Ht[d]
                        p.V(lambda e: e.tensor_tensor_scan(out=f2(F_), data0=f2(rmask), data1=f2(lw), initial=0.0, op0=ALU.mult, op1=ALU.add), w=[F_], r=[rmask, lw])
                        p.G(lambda e: e.tensor_tensor(out=E_[:], in0=F_[:], in1=lw[:], op=ALU.subtract), w=[E_], r=[F_, lw])
                        fend = F_[:, :, 63:64].broadcast_to([64, 8, 64])
                        p.V(lambda e: e.tensor_tensor(out=G_[:], in0=fend, in1=F_[:], op=ALU.subtract), w=[G_], r=[F_])
                        p.A(lambda e: e.activation(out=gC[d][:], in_=F_[:, :, 63], func=AF.Exp), w=[gC[d]], r=[F_])
                        if d == 1:
                            p.V(lambda e: e.tensor_tensor(out=H_[:], in0=E_[:], in1=fend, op=ALU.subtract), w=[H_], r=[E_, F_])
                        R_, K_, AN_, B_ = ld["R"][d], ld["K"][d], ld["AN"][d], ld["B"][d]
                        specs = ((E_, 1.0), (F_, -1.0), (F_, 1.0), (G_, 1.0)) if d == 0 else ((G_, 1.0), (H_, 1.0), (H_, -1.0), (E_, 1.0))
                        e1 = exp_to(d, *specs[0])
                        p.V(lambda e: e.tensor_tensor(out=AR[d][:, :, 0, :], in0=AN_[:], in1=e1[:], op=ALU.mult), w=[AR[d]], r=[AN_, e1])
                        e2 = exp_to(d, *specs[1])
                        p.V(lambda e: e.tensor_tensor(out=bt[d][:], in0=B_[:], in1=e2[:], op=ALU.mult), w=[bt[d]], r=[B_, e2])
                        p.G(lambda e: e.tensor_tensor(out=kt_[d][:], in0=K_[:], in1=e2[:], op=ALU.mult), w=[kt_[d]], r=[K_, e2])
                        e3 = exp_to(d, *specs[2])
                        p.V(lambda e: e.tensor_tensor(out=AR[d][:, :, 1, :], in0=R_[:], in1=e3[:], op=ALU.mult), w=[AR[d]], r=[R_, e3])
                        e4 = exp_to(d, *specs[3])
                        p.V(lambda e: e.tensor_tensor(out=bh[d][:], in0=B_[:], in1=e4[:], op=ALU.mult), w=[bh[d]], r=[B_, e4])
                        p.G(lambda e: e.tensor_tensor(out=kh[d][:], in0=K_[:], in1=e4[:], op=ALU.mult), w=[kh[d]], r=[K_, e4])
                    for d in range(2):
                        p.A(lambda e: e.copy(out=Vb[d][:], in_=ld["V"][d][:]), w=[Vb[d]], r=[ld["V"][d]])
                        for (src, dst) in ((Vb[d], Vtm[d]), (bh[d], Btm[d]), (kh[d], Ktm[d])):
                            ps_ = npsb()
                            for h in range(8):
                                p.T(lambda e, h=h, ps_=ps_, src=src: e.transpose(ps_[:, h, :], src[:, h, :], ident_bf[:64, :64]), w=[ps_], r=[src, ident_bf])
                            p.A(lambda e, ps_=ps_, dst=dst: e.copy(out=dst[:], in_=ps_[:]), w=[dst], r=[ps_])
                    for d in range(2):
                        cm = masks[:, 2 * d:2 * d + 2, :].rearrange("p a b -> p (a b)").unsqueeze(1).broadcast_to([64, 4, 128])
                        for (lh, Wd) in ((bt[d], W1[d]), (kt_[d], W2[d])):
                            for half in range(2):
                                ps_ = nps()
                                pv = ps_[:].rearrange("p h t -> p (h t)").rearrange("p (a b) -> p a b", b=128)
                                for hh in range(4):
                                    h = half * 4 + hh
                                    p.T(lambda e, h=h, hh=hh, pv=pv, lh=lh: e.matmul(pv[:, hh, :], lhsT=lh[:, h, :], rhs=AR[d][:, h, :, :].rearrange("p a b -> p (a b)"),
                                                                                   start=True, stop=True), w=[ps_], r=[lh, AR[d]])
                                p.V(lambda e, pv=pv, Wd=Wd, half=half: e.tensor_tensor(out=Wd[:, half * 4:(half + 1) * 4, :], in0=pv, in1=cm, op=ALU.mult), w=[Wd], r=[ps_, masks])
                        ps_ = nps()
                        for h in range(8):
                            p.T(lambda e, h=h, ps_=ps_: e.matmul(ps_[:, h, :], lhsT=AR[d][:, h, 0, :], rhs=bt[d][:, h, :], start=True, stop=True), w=[ps_], r=[AR[d], bt[d]])
                        mnt = masks[:, 2 - 2 * d, :].unsqueeze(1).broadcast_to([64, 8, 64])
                        p.V(lambda e, ps_=ps_: e.tensor_tensor(out=Lm[d][:], in0=ps_[:], in1=mnt, op=ALU.mult), w=[Lm[d]], r=[ps_, masks])
                        p.G(lambda e: e.tensor_tensor(out=Xm[d][:], in0=W1[d][:, :, 0:64], in1=idb, op=ALU.add), w=[Xm[d]], r=[W1[d], ident])
                        p.A(lambda e: e.copy(out=Xb[d][:], in_=Xm[d][:]), w=[Xb[d]], r=[Xm[d]])
                    cur = [(lambda h, d=d: W1[d][:, h, 0:64], W1[d], lambda h, d=d: Lm[d][:, h, :], Lm[d]) for d in range(2)]
                    for i in range(1, 6):
                        for d in range(2):
                            Pf, Pbuf, PTf, PTbuf = cur[d]
                            nP, nPT = Pb[i % 2][d], PTb[i % 2][d]
                            if i < 5:
                                ps_ = nps()
                                for h in range(8):
                                    p.T(lambda e, h=h, ps_=ps_: e.matmul(ps_[:, h, :], lhsT=PTf(h), rhs=Pf(h), start=True, stop=True), w=[ps_], r=[Pbuf, PTbuf])
                                p.A(lambda e, ps_=ps_, nP=nP: e.copy(out=nP[:], in_=ps_[:]), w=[nP], r=[ps_])
                            ps2 = nps()
                            for h in range(8):
                                p.T(lambda e, h=h, ps2=ps2: e.matmul(ps2[:, h, :], lhsT=Pf(h), rhs=PTf(h), start=True, stop=True), w=[ps2], r=[Pbuf, PTbuf])
                            p.V(lambda e, ps2=ps2, nPT=nPT: e.tensor_copy(out=nPT[:], in_=ps2[:]), w=[nPT], r=[ps2])
                            ps3 = nps()
                            for h in range(8):
                                p.T(lambda e, h=h, ps3=ps3, nPT=nPT: e.matmul(ps3[:, h, :], lhsT=nPT[:, h, :], rhs=Xb[d][:, h, :], start=True, stop=True), w=[ps3], r=[nPT, Xb[d]])
                            p.V(lambda e, ps3=ps3: e.tensor_tensor(out=Xm[d][:], in0=Xm[d][:], in1=ps3[:], op=ALU.add), w=[Xm[d]], r=[Xm[d], ps3])
                            p.A(lambda e: e.copy(out=Xb[d][:], in_=Xm[d][:]), w=[Xb[d]], r=[Xm[d]])
                            cur[d] = (lambda h, nP=nP: nP[:, h, :], nP, lambda h, nPT=nPT: nPT[:, h, :], nPT)
                    for d in range(2):
                        ps_ = nps()
                        for h in range(8):
                            p.T(lambda e, h=h, ps_=ps_: e.matmul(ps_[:, h, :], lhsT=AR[d][:, h, 0, :], rhs=STb[d][:, h, :], start=True, stop=False), w=[ps_], r=[AR[d], STb[d]])
                            p.T(lambda e, h=h, ps_=ps_: e.matmul(ps_[:, h, :], lhsT=W2[d][:, h, 0:64], rhs=Vtm[d][:, h, :], start=False, stop=True), w=[ps_], r=[W2[d], Vtm[d]])
                        p.A(lambda e, ps_=ps_: e.copy(out=RH[d][:], in_=ps_[:]), w=[RH[d]], r=[ps_])
                        ps_ = nps()
                        for h in range(8):
                            p.T(lambda e, h=h, ps_=ps_: e.matmul(ps_[:, h, :], lhsT=Xb[d][:, h, :], rhs=RH[d][:, h, :], start=True, stop=True), w=[ps_], r=[Xb[d], RH[d]])
                        p.V(lambda e, ps_=ps_: e.tensor_copy(out=Um[d][:], in_=ps_[:]), w=[Um[d]], r=[ps_])
                        ps_ = nps()
                        for h in range(8):
                            p.T(lambda e, h=h, ps_=ps_: e.matmul(ps_[:, h, :], lhsT=STb[d][:, h, :], rhs=AR[d][:, h, 1, :], start=True, stop=False), w=[ps_], r=[STb[d], AR[d]])
                            p.T(lambda e, h=h, ps_=ps_: e.matmul(ps_[:, h, :], lhsT=Um[d][:, h, :], rhs=W1[d][:, h, 64:128], start=False, stop=False), w=[ps_], r=[Um[d], W1[d]])
                            p.T(lambda e, h=h, ps_=ps_: e.matmul(ps_[:, h, :], lhsT=Vtm[d][:, h, :], rhs=W2[d][:, h, 64:128], start=False, stop=True), w=[ps_], r=[Vtm[d], W2[d]])
                        p.A(lambda e, ps_=ps_: e.copy(out=yo[d][:], in_=ps_[:]), w=[yo[d]], r=[ps_])
                        t0 = cd[d] * 64
                        p.dma(YRW[d][:, :, t0:t0 + 64], yo[d][:], w=[YRW[d]], r=[yo[d]], q=("sp" if d == 0 else "pool"))
                        ps_ = nps()
                        for h in range(8):
                            p.T(lambda e, h=h, ps_=ps_: e.matmul(ps_[:, h, :], lhsT=Btm[d][:, h, :], rhs=Um[d][:, h, :], start=True, stop=False), w=[ps_], r=[Btm[d], Um[d]])
                            p.T(lambda e, h=h, ps_=ps_: e.matmul(ps_[:, h, :], lhsT=Ktm[d][:, h, :], rhs=Vtm[d][:, h, :], start=False, stop=True), w=[ps_], r=[Ktm[d], Vtm[d]])
                        p.V(lambda e: e.tensor_tensor(out=ST[d][:], in0=ST[d][:], in1=gC[d][:].unsqueeze(2).broadcast_to([64, 8, 64]), op=ALU.mult), w=[ST[d]], r=[ST[d], gC[d]])
                        p.V(lambda e, ps_=ps_: e.tensor_tensor(out=ST[d][:], in0=ST[d][:], in1=ps_[:], op=ALU.add), w=[ST[d]], r=[ST[d], ps_])
                        p.A(lambda e: e.copy(out=STb[d][:], in_=ST[d][:]), w=[STb[d]], r=[ST[d]])
                p.barrier()

        def stage_rw_fin(l):
            with ExitStack() as st:
                vec = p.sb("rwvec", [64, 8, 8], es=st); p.dma(vec[:], I["rw_vecT"][l], w=[vec], r=[I["rw_vecT"]])
                wproj = load_w_bf(st, I["rw_proj"][l].rearrange("(h k) c -> k h c", k=64), 8, D, "rwp", pk=64)
                bt = alloc_branch_tiles(st)
                n = 256
                mk = lambda nm: p.sb(nm, [64, 8, n], es=st)
                ya, yb, bo, gg, sq2 = mk("ya"), mk("yb"), mk("bo"), mk("gg"), mk("sq2")
                zT = p.sb("zTr", [64, 8, 512], BF16, es=st)
                pool = [p.ps("prf", [64, 2, n], es=st) for _ in range(2)]
                bc = lambda i: vec[:, i, :].unsqueeze(2).broadcast_to([64, 8, n])
                pi = 0
                for blk in range(9):
                    b0 = blk * n
                    p.dma(ya[:], YRW[0][:, :, b0:b0 + n], w=[ya], r=[YRW[0]]); p.dma(yb[:], YRW[1][:, :, b0:b0 + n], w=[yb], r=[YRW[1]], q="pool")
                    p.dma(bo[:], RWS["BON"][:, :, b0:b0 + n], w=[bo], r=[RWS["BON"]]); p.dma(gg[:], RWS["G"][:, :, b0:b0 + n], w=[gg], r=[RWS["G"]], q="pool")
                    p.V(lambda e: e.tensor_tensor(out=ya[:], in0=ya[:], in1=yb[:], op=ALU.add), w=[ya], r=[ya, yb])
                    for hp in range(4):
                        ps_ = pool[pi % 2]; pi += 1
                        p.T(lambda e, hp=hp, ps_=ps_: e.matmul(ps_[:].rearrange("p a b -> p (a b)"), lhsT=ones_f[:64, :64],
                                                              rhs=ya[:, 2 * hp:2 * hp + 2, :].rearrange("p a b -> p (a b)"), start=True, stop=True), w=[ps_], r=[ones_f, ya])
                        p.V(lambda e, hp=hp, ps_=ps_: e.scalar_tensor_tensor(out=yb[:, 2 * hp:2 * hp + 2, :], in0=ps_[:], scalar=-1.0 / 64, in1=ya[:, 2 * hp:2 * hp + 2, :],
                                                                            op0=ALU.mult, op1=ALU.add), w=[yb], r=[ps_, ya])
                    p.A(lambda e: e.activation(out=sq2[:], in_=yb[:], func=AF.Square), w=[sq2], r=[yb])
                    for hp in range(4):
                        ps_ = pool[pi % 2]; pi += 1
                        p.T(lambda e, hp=hp, ps_=ps_: e.matmul(ps_[:].rearrange("p a b -> p (a b)"), lhsT=ones_f[:64, :64],
                                                              rhs=sq2[:, 2 * hp:2 * hp + 2, :].rearrange("p a b -> p (a b)"), start=True, stop=True), w=[ps_], r=[ones_f, sq2])
                        p.A(lambda e, hp=hp, ps_=ps_: e.activation(out=ya[:, 2 * hp:2 * hp + 2, :], in_=ps_[:], func=AF.Sqrt, bias=64e-5, scale=1.0 / 64), w=[ya], r=[ps_])
                    p.V(lambda e: e.reciprocal(out=ya[:], in_=ya[:]), w=[ya], r=[ya])
                    p.V(lambda e: e.tensor_tensor(out=yb[:], in0=yb[:], in1=ya[:], op=ALU.mult), w=[yb], r=[yb, ya])
                    p.V(lambda e: e.tensor_tensor(out=yb[:], in0=yb[:], in1=bc(4), op=ALU.mult), w=[yb], r=[yb, vec])
                    p.V(lambda e: e.tensor_tensor(out=yb[:], in0=yb[:], in1=bc(5), op=ALU.add), w=[yb], r=[yb, vec])
                    p.V(lambda e: e.tensor_tensor(out=yb[:], in0=yb[:], in1=bo[:], op=ALU.add), w=[yb], r=[yb, bo])
                    p.V(lambda e: e.tensor_tensor(out=zT[:, :, :n], in0=yb[:], in1=gg[:], op=ALU.mult), w=[zT], r=[yb, gg])
                    branch_out(bt, zT, 8, 64, wproj, 1, b0, n)
                p.barrier()


        def stage_merge(l):
            with ExitStack() as st:
                wo = load_w_bf(st, I["w_out"][l].rearrange("(k p) c -> p k c", p=128), 8, D, "wo")
                ya = [p.sb("mya", [128, 8, 512], es=st) for _ in range(3)]
                mT = p.sb("mT", [128, 8, 512], BF16, es=st)
                xb = p.sb("mxb", [128, 8, 512], es=st)
                po = [p.ps("mpo", [128, 512], es=st) for _ in range(2)]
                blks = BLKS if l == 0 else BLKS[1:]
                for (b0, n, seg) in blks:
                    for i in range(3):
                        p.dma(ya[i][:, :, :n], YG[i][:, b0:b0 + n].rearrange("(kc p) t -> p kc t", p=128), w=[ya[i]], r=[YG[i]], q=("sp" if i != 1 else "pool"))
                    p.dma(xb[:, :, :n], X[:, b0:b0 + n].rearrange("(kc p) t -> p kc t", p=128), w=[xb], r=[X], q="pool")
                    p.V(lambda e: e.tensor_tensor(out=ya[0][:, :, :n], in0=ya[0][:, :, :n], in1=ya[1][:, :, :n], op=ALU.add), w=[ya[0]], r=[ya[0], ya[1]])
                    p.V(lambda e: e.tensor_tensor(out=mT[:, :, :n], in0=ya[0][:, :, :n], in1=ya[2][:, :, :n], op=ALU.add), w=[mT], r=[ya[0], ya[2]])
                    for oc in range(8):
                        po_ = po[oc % 2]
                        for kc in range(8):
                            p.T(lambda e, kc=kc, oc=oc, po_=po_: e.matmul(po_[:, :n], lhsT=wo[:, kc, oc * 128:(oc + 1) * 128], rhs=mT[:, kc, :n],
                                                                         start=(kc == 0), stop=(kc == 7)), w=[po_], r=[wo, mT])
                        p.V(lambda e, oc=oc, po_=po_: e.scalar_tensor_tensor(out=xb[:, oc, :n], in0=po_[:, :n], scalar=mod[l][:, 16 + oc, seg:seg + 1], in1=xb[:, oc, :n],
                                                                            op0=ALU.mult, op1=ALU.add), w=[xb], r=[po_, mod[l], xb])
                    p.dma(X[:, b0:b0 + n].rearrange("(kc p) t -> p kc t", p=128), xb[:, :, :n], w=[X], r=[xb])
                p.barrier()

        def stage_ffn(l):
            moe = (l % 2 == 1)
            blks = BLKS[1:] if moe else BLKS
            nexp = NE if moe else 1
            UTs = p.dram("UT%d" % l, [nexp, FF, T], BF16)
            with ExitStack() as st:
                hT = p.sb("hTf", [128, 8, T], BF16, es=st)
                combT = p.sb("combT", [8, TL], es=st) if moe else None
                with ExitStack() as st2:
                    nt = alloc_norm_tiles(st2)
                    m = mod[l]
                    if moe:
                        h32 = p.sb("h32", [128, 8, 512], es=st2)
                        rt = p.sb("rt", [128, 8, NE], es=st2)
                        p.dma(rt[:], I["moe_router"][0].rearrange("(kc p) e -> p kc e", p=128), w=[rt], r=[I["moe_router"]])
                        lg = p.sb("lg", [128, 16, NE], es=st2)
                        plg = p.ps("plg", [128, NE], es=st2)
                    for bi, (b0, n, seg) in enumerate(blks):
                        norm_block(nt, b0, n, lambda kc, seg=seg: gsf[l][:, kc, seg:seg + 1], lambda kc, seg=seg: m[:, 24 + kc, seg:seg + 1], hT,
                                   h32=(h32 if moe else None))
                        if moe:
                            for j in range(4):
                                for kc in range(8):
                                    p.T(lambda e, kc=kc, j=j: e.matmul(plg[:], lhsT=h32[:, kc, j * 128:(j + 1) * 128], rhs=rt[:, kc, :], start=(kc == 0), stop=(kc == 7)),
                                        w=[plg], r=[h32, rt])
                                p.V(lambda e, j=j, bi=bi: e.tensor_copy(out=lg[:, bi * 4 + j, :], in_=plg[:]), w=[lg], r=[plg])
                    if moe:
                        m1 = p.sb("m1", [128, 16], es=st2); eq = p.sb("eq", [128, 16, NE], es=st2); l2 = p.sb("l2", [128, 16, NE], es=st2)
                        m2 = p.sb("m2", [128, 16], es=st2); ex = p.sb("exr", [128, 16, NE], es=st2); sm = p.sb("sm", [128, 16], es=st2)
                        b3 = lambda t_: t_[:].unsqueeze(2).broadcast_to([128, 16, NE])
                        p.V(lambda e: e.reduce_max(out=m1[:], in_=lg[:], axis=AX.X), w=[m1], r=[lg])
                        p.V(lambda e: e.tensor_tensor(out=eq[:], in0=lg[:], in1=b3(m1), op=ALU.is_equal), w=[eq], r=[lg, m1])
                        p.V(lambda e: e.scalar_tensor_tensor(out=l2[:], in0=eq[:], scalar=-1e30, in1=lg[:], op0=ALU.mult, op1=ALU.add), w=[l2], r=[eq, lg])
                        p.V(lambda e: e.reduce_max(out=m2[:], in_=l2[:], axis=AX.X), w=[m2], r=[l2])
                        p.V(lambda e: e.tensor_tensor(out=eq[:], in0=lg[:], in1=b3(m2), op=ALU.is_ge), w=[eq], r=[lg, m2])
                        p.V(lambda e: e.tensor_tensor(out=l2[:], in0=lg[:], in1=b3(m1), op=ALU.subtract), w=[l2], r=[lg, m1])
                        p.A(lambda e: e.activation(out=ex[:], in_=l2[:], func=AF.Exp), w=[ex], r=[l2])
                        p.V(lambda e: e.tensor_tensor(out=ex[:], in0=ex[:], in1=eq[:], op=ALU.mult), w=[ex], r=[ex, eq])
                        p.V(lambda e: e.reduce_sum(out=sm[:], in_=ex[:], axis=AX.X), w=[sm], r=[ex])
                        p.V(lambda e: e.reciprocal(out=sm[:], in_=sm[:]), w=[sm], r=[sm])
                        p.V(lambda e: e.tensor_tensor(out=ex[:], in0=ex[:], in1=b3(sm), op=ALU.mult), w=[ex], r=[ex, sm])
                        pct = p.ps("pct", [8, 512], es=st2)
                        for g4 in range(4):
                            for j in range(4):
                                p.T(lambda e, g4=g4, j=j: e.transpose(pct[:, j * 128:(j + 1) * 128], ex[:, g4 * 4 + j, :], ident[:]), w=[pct], r=[ex, ident])
                            p.V(lambda e, g4=g4: e.tensor_copy(out=combT[:, g4 * 512:(g4 + 1) * 512], in_=pct[:]), w=[combT], r=[pct])
                    p.barrier()
                if "comb" in dbg and moe:
                    cdd = p.dram("dbg_comb", [8, TL], F32, kind="ExternalOutput")
                    p.dma(cdd[:], combT[:], w=[cdd], r=[combT])
                with ExitStack() as st2:
                    wf1 = [p.sb("wf1", [128, 8, 512], es=st2) for _ in range(2)]; wf3 = [p.sb("wf3", [128, 8, 512], es=st2) for _ in range(2)]
                    wb1 = [p.sb("wb1", [128, 8, 512], BF16, es=st2) for _ in range(2)]; wb3 = [p.sb("wb3", [128, 8, 512], BF16, es=st2) for _ in range(2)]
                    pa = [p.ps("pa", [128, 512], es=st2) for _ in range(2)]; pb = [p.ps("pb", [128, 512], es=st2) for _ in range(2)]
                    pcb = p.ps("pcb", [128, 512], es=st2)
                    sl = [p.sb("sl", [128, 512], es=st2) for _ in range(2)]; ut = [p.sb("ut", [128, 512], BF16, es=st2) for _ in range(2)]
                    sel = p.sb("sel8", [8, 8, 128], es=st2)
                    p.dma(sel[:], I["sel8"][:].rearrange("e k m -> k e m"), w=[sel], r=[I["sel8"]])
                    cbc = p.sb("cbc", [128, TL], es=st2) if moe else None
                    gi = 0; ui = 0
                    for ex_ in range(nexp):
                        w1s = I["moe_w1"][0, ex_] if moe else I["ffn_w1"][0]
                        w3s = I["moe_w3"][0, ex_] if moe else I["ffn_w3"][0]
                        if moe:
                            for g4 in range(4):
                                p.T(lambda e, g4=g4, ex_=ex_: e.matmul(pcb[:], lhsT=sel[:, ex_, :], rhs=combT[:, g4 * 512:(g4 + 1) * 512], start=True, stop=True), w=[pcb], r=[sel, combT])
                                p.A(lambda e, g4=g4: e.copy(out=cbc[:, g4 * 512:(g4 + 1) * 512], in_=pcb[:]), w=[cbc], r=[pcb])
                        for fg in range(6):
                            f0 = fg * 512
                            nf = min(512, FF - f0)
                            a1, a3, c1, c3 = wf1[gi % 2], wf3[gi % 2], wb1[gi % 2], wb3[gi % 2]
                            gi += 1
                            p.dma(a1[:, :, :nf], w1s[:, f0:f0 + nf].rearrange("(kc p) c -> p kc c", p=128), w=[a1], r=[], q="pool")
                            p.dma(a3[:, :, :nf], w3s[:, f0:f0 + nf].rearrange("(kc p) c -> p kc c", p=128), w=[a3], r=[], q="pool")
                            for kc in range(8):
                                p.V(lambda e, kc=kc: e.tensor_copy(out=c1[:, kc, :nf], in_=a1[:, kc, :nf]), w=[c1], r=[a1])
                                p.A(lambda e, kc=kc: e.copy(out=c3[:, kc, :nf], in_=a3[:, kc, :nf]), w=[c3], r=[a3])
                            for sub in range(nf // 128):
                                fb = fg * 4 + sub
                                for (b0, n, seg) in blks:
                                    pa_, pb_, sl_, ut_ = pa[ui % 2], pb[ui % 2], sl[ui % 2], ut[ui % 2]
                                    ui += 1
                                    for kc in range(8):
                                        p.T(lambda e, kc=kc, sub=sub, pa_=pa_: e.matmul(pa_[:, :n], lhsT=c1[:, kc, sub * 128:(sub + 1) * 128], rhs=hT[:, kc, b0:b0 + n],
                                                                                       start=(kc == 0), stop=(kc == 7)), w=[pa_], r=[c1, hT])
                                    for kc in range(8):
                                        p.T(lambda e, kc=kc, sub=sub, pb_=pb_: e.matmul(pb_[:, :n], lhsT=c3[:, kc, sub * 128:(sub + 1) * 128], rhs=hT[:, kc, b0:b0 + n],
                                                                                       start=(kc == 0), stop=(kc == 7)), w=[pb_], r=[c3, hT])
                                    p.A(lambda e, pa_=pa_, sl_=sl_: e.activation(out=sl_[:, :n], in_=pa_[:, :n], func=AF.Silu), w=[sl_], r=[pa_])
                                    if moe:
                                        p.V(lambda e, sl_=sl_, b0=b0: e.tensor_tensor(out=sl_[:, :n], in0=sl_[:, :n], in1=cbc[:, b0 - TC:b0 - TC + n], op=ALU.mult), w=[sl_], r=[sl_, cbc])
                                    p.V(lambda e, pb_=pb_, sl_=sl_, ut_=ut_: e.tensor_tensor(out=ut_[:, :n], in0=sl_[:, :n], in1=pb_[:, :n], op=ALU.mult), w=[ut_], r=[sl_, pb_])
                                    p.dma(UTs[ex_, fb * 128:(fb + 1) * 128, b0:b0 + n], ut_[:, :n], w=[UTs], r=[ut_])
                    p.barrier()
            with ExitStack() as st:
                w2f = [p.sb("w2f", [128, 2, D], es=st) for _ in range(2)]
                w2bs = [p.sb("w2b", [128, 22, D], BF16, es=st) for _ in range(2 if nexp > 1 else 1)]
                uu = [p.sb("uu", [128, 22, 512], BF16, es=st) for _ in range(2)]
                acc = p.sb("facc", [128, 8, 512], es=st); xb = p.sb("fxb", [128, 8, 512], es=st)
                po = [p.ps("fpo", [128, 512], es=st) for _ in range(4)]
                ui = 0; oi = 0

                def load_w2(ex_):
                    w2s = I["moe_w2"][0, ex_] if moe else I["ffn_w2"][0]
                    w2b_ = w2bs[ex_ % len(w2bs)]
                    for f2_ in range(11):
                        wf_ = w2f[f2_ % 2]
                        p.dma(wf_[:], w2s[f2_ * 256:(f2_ + 1) * 256, :].rearrange("(k p) c -> p k c", p=128), w=[wf_], r=[], q="pool")
                        p.V(lambda e, f2_=f2_, wf_=wf_: e.tensor_copy(out=w2b_[:, 2 * f2_, :], in_=wf_[:, 0, :]), w=[w2b_], r=[wf_])
                        p.A(lambda e, f2_=f2_, wf_=wf_: e.copy(out=w2b_[:, 2 * f2_ + 1, :], in_=wf_[:, 1, :]), w=[w2b_], r=[wf_])
                load_w2(0)
                for ex_ in range(nexp):
                    w2b = w2bs[ex_ % len(w2bs)]
                    if ex_ + 1 < nexp:
                        load_w2(ex_ + 1)
                    for (b0, n, seg) in blks:
                        u_ = uu[ui % 2]; ui += 1
                        p.dma(u_[:, :, :n], UTs[ex_, :, b0:b0 + n].rearrange("(k p) t -> p k t", p=128), w=[u_], r=[UTs], q="pool")
                        if ex_ > 0:
                            p.dma(acc[:, :, :n], FACC[:, b0:b0 + n].rearrange("(kc p) t -> p kc t", p=128), w=[acc], r=[FACC])
                        last = (ex_ == nexp - 1)
                        if last:
                            p.dma(xb[:, :, :n], X[:, b0:b0 + n].rearrange("(kc p) t -> p kc t", p=128), w=[xb], r=[X])
                        for oc in range(8):
                            po_ = po[oi % 4]; oi += 1
                            for k in range(22):
                                p.T(lambda e, k=k, oc=oc, po_=po_, u_=u_: e.matmul(po_[:, :n], lhsT=w2b[:, k, oc * 128:(oc + 1) * 128], rhs=u_[:, k, :n],
                                                                                  start=(k == 0), stop=(k == 21)), w=[po_], r=[w2b, u_])
                            if ex_ == 0:
                                p.A(lambda e, oc=oc, po_=po_: e.copy(out=acc[:, oc, :n], in_=po_[:, :n]), w=[acc], r=[po_])
                            else:
                                p.V(lambda e, oc=oc, po_=po_: e.tensor_tensor(out=acc[:, oc, :n], in0=acc[:, oc, :n], in1=po_[:, :n], op=ALU.add), w=[acc], r=[acc, po_])
                            if last:
                                p.V(lambda e, oc=oc: e.scalar_tensor_tensor(out=xb[:, oc, :n], in0=acc[:, oc, :n], scalar=mod[l][:, 40 + oc, seg:seg + 1], in1=xb[:, oc, :n],
                                                                           op0=ALU.mult, op1=ALU.add), w=[xb], r=[acc, mod[l], xb])
                        if last:
                            p.dma(X[:, b0:b0 + n].rearrange("(kc p) t -> p kc t", p=128), xb[:, :, :n], w=[X], r=[xb])
                        else:
                            p.dma(FACC[:, b0:b0 + n].rearrange("(kc p) t -> p kc t", p=128), acc[:, :, :n], w=[FACC], r=[acc])
                p.barrier()

        def stage_final():
            with ExitStack() as st:
                nt = alloc_norm_tiles(st)
                xb, sq, pss, rstd, tmp = nt
                for (b0, n, seg) in BLKS[1:]:
                    norm_block(nt, b0, n, lambda kc: fng[:, kc:kc + 1], None, None)
                    p.dma(outT[:, b0 - TC:b0 - TC + n].rearrange("(kc p) t -> p kc t", p=128), tmp[:, :, :n], w=[outT], r=[tmp])
                p.barrier()

        for l in range(n_layers):
            stage_ada(l)
        if "mod" in dbg:
            md = p.dram("dbg_mod", [2, 128, 96], F32, kind="ExternalOutput")
            for l in range(2):
                p.dma(md[l], mod[l][:].rearrange("p a b -> p (a b)"), w=[md], r=[mod[l]])
        for l in range(n_layers):
            stage_proj(l)
            if stop_after == ("proj", l):
                break
            if "hg" not in skip:
                stage_hg(l)
            if stop_after == ("hg", l):
                break
            if "da" not in skip:
                stage_da(l)
            if stop_after == ("da", l):
                break
            if "rw" not in skip:
                stage_rw_prep(l)
                stage_rw_scan(l)
                stage_rw_fin(l)
            if stop_after == ("rw", l):
                break
            if "merge" not in skip:
                stage_merge(l)
            if stop_after == ("merge", l):
                break
            if "ffn" not in skip:
                stage_ffn(l)
            if stop_after == ("ffn", l):
                break
        if stop_after is None:
            stage_final()
        if "X" in dbg:
            xd = p.dram("dbg_X", [D, T], F32, kind="ExternalOutput")
            p.dma(xd[:], X[:], w=[xd], r=[X])
        if "YG1" in dbg:
            yd = p.dram("dbg_YG1", [D, T], F32, kind="ExternalOutput")
            p.dma(yd[:], YG[1][:], w=[yd], r=[YG[1]])
            for nm in RWN:
                dd = p.dram("dbg_RWS_" + nm, [64, 8, T], F32, kind="ExternalOutput")
                p.dma(dd[:], RWS[nm][:], w=[dd], r=[RWS[nm]])
            for d in range(2):
                dd = p.dram("dbg_YRW%d" % d, [64, 8, T], F32, kind="ExternalOutput")
                p.dma(dd[:], YRW[d][:], w=[dd], r=[YRW[d]])
        if "YG2" in dbg:
            yd = p.dram("dbg_YG2", [D, T], F32, kind="ExternalOutput")
            p.dma(yd[:], YG[2][:], w=[yd], r=[YG[2]])
        if "YG0" in dbg:
            yd = p.dram("dbg_YG0", [D, T], F32, kind="ExternalOutput")
            p.dma(yd[:], YG[0][:], w=[yd], r=[YG[0]])
            od = p.dram("dbg_OHG", [2, 512, T], F32, kind="ExternalOutput")
            p.dma(od[:], OHG[:], w=[od], r=[OHG])
        if "PT" in dbg:
            pd = p.dram("dbg_PT", [INC, T], F32, kind="ExternalOutput")
            p.dma(pd[:], PT[:], w=[pd], r=[PT])
            vd = p.dram("dbg_VHG", [T, 512], F32, kind="ExternalOutput")
            p.dma(vd[:], VHG[:], w=[vd], r=[VHG])
        p.barrier()
        print("instrs", p.ninstr, "waits", p.nwait)
    return nc


def host_inputs(inputs, b):
    f = np.float32
    g = {}
    x = np.asarray(inputs["x"][b], f); ctx = np.asarray(inputs["ctx"][b], f)
    g["xT"] = np.ascontiguousarray(np.concatenate([ctx.T, x.T], axis=1))
    c2 = np.stack([np.asarray(inputs["c"][b], f), np.asarray(inputs["c_ctx"], f)], axis=-1)
    g["c2"] = np.ascontiguousarray(c2.reshape(8, 128, 2).transpose(1, 0, 2))
    return g


def shared_inputs(inputs):
    f = np.float32
    g = {}
    A = lambda k: np.asarray(inputs[k], f)
    g["ada_w"] = A("ada_w")
    g["ada_bT"] = np.ascontiguousarray(A("ada_b").reshape(2, 48, 128).transpose(0, 2, 1))
    g["nmgT"] = np.ascontiguousarray(A("norm_mix_g").reshape(2, 8, 128).transpose(0, 2, 1))
    g["nfgT"] = np.ascontiguousarray(A("norm_ffn_g").reshape(2, 8, 128).transpose(0, 2, 1))
    g["fngT"] = np.ascontiguousarray(A("final_norm_g").reshape(8, 128).T)
    g["w_in"] = A("w_in")
    mu_full = np.zeros((2, 71 * 128), f)
    mu_full[:, RW0:DA0] = A("rw_mu")
    g["muT"] = np.ascontiguousarray(mu_full.reshape(2, 71, 128).transpose(0, 2, 1))
    g["ident"] = np.eye(128, dtype=f)
    i = np.arange(64)[:, None]; j = np.arange(64)[None, :]
    g["masks"] = np.ascontiguousarray(np.stack([(i < j), (i <= j), (i > j), (i >= j)], axis=1).astype(f))
    g["hg_lbT"] = np.ascontiguousarray(A("hg_lb_logits").reshape(2, 2, 4, 128).transpose(0, 1, 3, 2))
    g["hg_ngT"] = np.ascontiguousarray(A("hg_norm_g").reshape(2, 128, 1))
    g["hg_proj"] = A("hg_proj")
    g["w_out"] = A("w_out")
    for k in ("ffn_w1", "ffn_w3", "ffn_w2", "moe_router", "moe_w1", "moe_w3", "moe_w2", "da_proj", "rw_w2", "rw_a2", "rw_g2", "rw_proj"):
        g[k] = A(k)
    sel = np.zeros((8, 8, 128), f)
    for e in range(8):
        sel[e, e, :] = 1.0
    g["sel8"] = sel
    g["da_lambda"] = A("da_lambda").reshape(2, 256)
    g["da_sg"] = A("da_subln_g")
    t = np.arange(TL)
    rowi = (t // 64).astype(f); coli = (t % 64).astype(f)
    inv_freq = (1.0 / (10000.0 ** (np.arange(0, 32, 2, dtype=f) / f(32)))).astype(f)
    ang = np.zeros((64, TL), f)
    for d in range(64):
        jj = d % 16
        ang[d] = (rowi if d < 32 else coli) * inv_freq[jj]
    g["ropeCS"] = np.ascontiguousarray(np.stack([np.cos(ang), np.sin(ang)], axis=1).astype(f))
    R = np.zeros((64, 64), f)
    for base in (0, 32):
        for q in range(16):
            R[base + q, base + 16 + q] = -1.0
            R[base + 16 + q, base + q] = 1.0
    g["ropeR"] = np.ascontiguousarray(R.T)
    vec = np.zeros((2, 8, 512), f)
    vec[:, 0] = A("rw_k_k"); vec[:, 1] = A("rw_k_a"); vec[:, 2] = A("rw_a0"); vec[:, 3] = A("rw_r_k").reshape(2, 512)
    vec[:, 4] = A("rw_ln_g"); vec[:, 5] = A("rw_ln_b")
    g["rw_vecT"] = np.ascontiguousarray(vec.reshape(2, 8, 8, 64).transpose(0, 3, 1, 2))
    g["rw_w0T"] = np.ascontiguousarray(A("rw_w0").reshape(2, 2, 8, 64).transpose(0, 1, 3, 2))
    return g


_NC_CACHE = {}


def kernel(**inputs):
    if "nc" not in _NC_CACHE:
        _NC_CACHE["nc"] = build()
    nc = _NC_CACHE["nc"]
    sh = shared_inputs(inputs)
    in_maps = []
    for b in range(8):
        m = dict(sh)
        m.update(host_inputs(inputs, b))
        in_maps.append(m)
    res = run_bass_kernel_spmd(nc, in_maps, core_ids=list(range(8)))
    out = np.stack([np.ascontiguousarray(r["outT"].T) for r in res.results], axis=0)
    return out.astype(np.float32)
```

```python
import math
import numpy as np
from contextlib import ExitStack
import concourse.bass as bass
import concourse.mybir as mybir
from concourse.bass_utils import run_bass_kernel_spmd

F32 = mybir.dt.float32
BF16 = mybir.dt.bfloat16
AF = mybir.ActivationFunctionType
ALU = mybir.AluOpType
AX = mybir.AxisListType

SAME_ENGINE_SYNC = True
N_DMA_SEMS = 40
SEM_EPOCH = 16000

T = 2304
TC = 256
TL = 2048
D = 1024
INC = 9024
FF = 2816
NE = 8
BLKS = [(0, 256, 1), (256, 512, 0), (768, 512, 0), (1280, 512, 0), (1792, 512, 0)]
HG0 = 0
RW0 = 2560
DA0 = 4416
GT0 = 5952
RW_R, RW_K, RW_V, RW_WF, RW_WB, RW_AD, RW_GD = RW0, RW0 + 512, RW0 + 1024, RW0 + 1536, RW0 + 1600, RW0 + 1664, RW0 + 1728


class Buf:
    def __init__(self, name, h):
        self.name = name
        self.h = h
        self.w = None
        self.r = {}

    def __getitem__(self, idx):
        return self.h[idx]


class Prog:
    ENG = ("pe", "act", "dve", "pool", "sp")

    def __init__(self, nc, es):
        self.nc = nc
        self.es = es
        self.e = dict(pe=nc.tensor, act=nc.scalar, dve=nc.vector, pool=nc.gpsimd, sp=nc.sync)
        self.sem = {}
        self.ekey = {}
        for k in self.ENG:
            self.ekey[k] = (k, 0)
            self.sem[(k, 0)] = es.enter_context(nc.semaphore("s_" + k))
        self.cnt = {k: 0 for k in self.ENG}
        self.dsem = [es.enter_context(nc.semaphore("d%d" % i)) for i in range(N_DMA_SEMS)]
        for i in range(N_DMA_SEMS):
            self.sem[("d", i)] = self.dsem[i]
        self.dval = [0] * N_DMA_SEMS
        self.dnext = 0
        self.seen = {k: {} for k in self.ENG}
        self.ninstr = {k: 0 for k in self.ENG}
        self.nwait = 0
        self.uid = 0

    def sb(self, name, shape, dtype=F32, es=None):
        self.uid += 1
        h = (es or self.es).enter_context(self.nc.sbuf_tensor("%s_%d" % (name, self.uid), list(shape), dtype))
        return Buf(name, h)

    def ps(self, name, shape, dtype=F32, es=None):
        self.uid += 1
        h = (es or self.es).enter_context(self.nc.psum_tensor("%s_%d" % (name, self.uid), list(shape), dtype))
        return Buf(name, h)

    def dram(self, name, shape, dtype=F32, kind="Internal"):
        h = self.nc.dram_tensor(name, list(shape), dtype, kind=kind)
        return Buf(name, h.ap())

    def _wait(self, ek, k, v):
        if self.seen[ek].get(k, 0) >= v:
            return
        self.e[ek].wait_ge(self.sem[k], v)
        self.seen[ek][k] = v
        self.nwait += 1

    def _deps(self, ek, r, w):
        deps = {}

        def add(ev):
            if ev is None:
                return
            k, v = ev
            if deps.get(k, 0) < v:
                deps[k] = v

        for b in r:
            add(b.w)
        for b in w:
            add(b.w)
            for k, v in b.r.items():
                add((k, v))
        for k, v in deps.items():
            if k[0] == ek and (not SAME_ENGINE_SYNC or ek in ("pe", "sp")):
                continue
            self._wait(ek, k, v)

    def _commit(self, ev, r, w):
        k, v = ev
        for b in w:
            b.w = ev
            b.r = {}
        for b in r:
            if b.r.get(k, 0) < v:
                b.r[k] = v

    def op(self, ek, fn, w=(), r=()):
        if self.cnt[ek] >= SEM_EPOCH:
            ep = self.ekey[ek][1] + 1
            self.ekey[ek] = (ek, ep)
            self.sem[(ek, ep)] = self.es.enter_context(self.nc.semaphore("s_%s_%d" % (ek, ep)))
            self.cnt[ek] = 0
        self._deps(ek, r, w)
        ins = fn(self.e[ek])
        self.cnt[ek] += 1
        key = self.ekey[ek]
        ins.then_inc(self.sem[key], 1)
        self.ninstr[ek] += 1
        self._commit((key, self.cnt[ek]), r, w)
        return ins

    def V(self, fn, w=(), r=()):
        return self.op("dve", fn, w, r)

    def A(self, fn, w=(), r=()):
        return self.op("act", fn, w, r)

    def G(self, fn, w=(), r=()):
        return self.op("pool", fn, w, r)

    def T(self, fn, w=(), r=()):
        return self.op("pe", fn, w, r)

    def dma(self, out, in_, w=(), r=(), q="sp", **kw):
        i = self.dnext
        self.dnext = (self.dnext + 1) % N_DMA_SEMS
        key = ("d", i)
        if self.dval[i] > 0:
            self._wait(q, key, self.dval[i])
        self._deps(q, r, w)
        ins = self.e[q].dma_start(out=out, in_=in_, **kw)
        self.dval[i] += 16
        ins.then_inc(self.dsem[i], 16)
        self.ninstr[q] += 1
        self._commit((key, self.dval[i]), r, w)
        return ins

    def barrier(self):
        for ek in self.ENG:
            for k in self.ENG:
                if k == ek:
                    continue
                key = self.ekey[k]
                if key[1] > 0:
                    self._wait(ek, (k, key[1] - 1), SEM_EPOCH)
                if self.cnt[k] > 0:
                    self._wait(ek, key, self.cnt[k])
            for i in range(N_DMA_SEMS):
                if self.dval[i] > 0:
                    self._wait(ek, ("d", i), self.dval[i])


def build(n_layers=2, dbg=(), stop_after=None, skip=()):
    nc = bass.Bass("TRN2", target_bir_lowering=False)
    with ExitStack() as es:
        p = Prog(nc, es)
        I = {}

        def inp(name, shape, dt=F32):
            I[name] = p.dram(name, shape, dt, kind="ExternalInput")
            return I[name]

        inp("xT", [D, T]); inp("c2", [128, 8, 2])
        inp("ada_w", [2, D, 6 * D]); inp("ada_bT", [2, 128, 48])
        inp("nmgT", [2, 128, 8]); inp("nfgT", [2, 128, 8]); inp("fngT", [128, 8])
        inp("w_in", [2, D, INC]); inp("muT", [2, 128, 71])
        inp("ident", [128, 128]); inp("masks", [64, 4, 64])
        inp("hg_lbT", [2, 2, 128, 4]); inp("hg_ngT", [2, 128, 1]); inp("hg_proj", [2, 512, D])
        inp("w_out", [2, D, D])
        inp("ffn_w1", [1, D, FF]); inp("ffn_w3", [1, D, FF]); inp("ffn_w2", [1, FF, D])
        inp("moe_router", [1, D, NE]); inp("moe_w1", [1, NE, D, FF]); inp("moe_w3", [1, NE, D, FF]); inp("moe_w2", [1, NE, FF, D])
        inp("sel8", [8, 8, 128])
        inp("da_lambda", [2, 256]); inp("da_sg", [2, 128]); inp("da_proj", [2, 512, D])
        inp("ropeCS", [64, 2, TL]); inp("ropeR", [64, 64])
        inp("rw_vecT", [2, 64, 8, 8]); inp("rw_w0T", [2, 2, 64, 8]); inp("rw_w2", [2, 2, 64, 512]); inp("rw_a2", [2, 64, 512])
        inp("rw_g2", [2, 128, 512]); inp("rw_proj", [2, 512, D])
        outT = p.dram("outT", [D, TL], F32, kind="ExternalOutput")

        X = p.dram("Xs", [D, T])
        PT = p.dram("PT", [INC, T])
        VHG = p.dram("VHG", [T, 512])
        VDA = p.dram("VDA", [T, 512])
        OHG = p.dram("OHG", [2, 512, T])
        YG = [p.dram("YG%d" % i, [D, T]) for i in range(3)]
        UT = p.dram("UT", [FF, T], BF16)
        FACC = p.dram("FACC", [D, T])
        dbg_out = {}

        ident = p.sb("ident", [128, 128]); p.dma(ident[:], I["ident"][:], w=[ident], r=[I["ident"]])
        masks = p.sb("masks", [64, 4, 64]); p.dma(masks[:], I["masks"][:], w=[masks], r=[I["masks"]])
        ones_bf = p.sb("ones_bf", [128, 128], BF16); p.V(lambda e: e.memset(ones_bf[:], 1.0), w=[ones_bf])
        ones_f = p.sb("ones_f", [128, 128]); p.V(lambda e: e.memset(ones_f[:], 1.0), w=[ones_f])
        ident_bf = p.sb("ident_bf", [128, 128], BF16); p.V(lambda e: e.tensor_copy(out=ident_bf[:], in_=ident[:]), w=[ident_bf], r=[ident])
        sc = p.sb("sc", [128, 8, 2]); p.dma(sc[:], I["c2"][:], w=[sc], r=[I["c2"]])
        p.A(lambda e: e.activation(out=sc[:], in_=sc[:], func=AF.Silu), w=[sc], r=[sc])
        mod = [p.sb("mod%d" % l, [128, 48, 2]) for l in range(2)]
        gsm = [p.sb("gsm%d" % l, [128, 8, 2]) for l in range(2)]
        gsf = [p.sb("gsf%d" % l, [128, 8, 2]) for l in range(2)]
        fng = p.sb("fng", [128, 8]); p.dma(fng[:], I["fngT"][:], w=[fng], r=[I["fngT"]])

        p.dma(X[:], I["xT"][:], w=[X], r=[I["xT"]])

        def stage_ada(l):
            with ExitStack() as st:
                wt = [p.sb("adaw", [128, 8, 512], es=st) for _ in range(2)]
                adab = p.sb("adab", [128, 48], es=st)
                ng = p.sb("ng", [128, 8], es=st); nf = p.sb("nf", [128, 8], es=st)
                pm = p.ps("pmod", [128, 48, 2], es=st)
                p.dma(adab[:], I["ada_bT"][l], w=[adab], r=[I["ada_bT"]])
                p.dma(ng[:], I["nmgT"][l], w=[ng], r=[I["nmgT"]])
                p.dma(nf[:], I["nfgT"][l], w=[nf], r=[I["nfgT"]])
                for cb in range(12):
                    w_ = wt[cb % 2]
                    p.dma(w_[:], I["ada_w"][l, :, cb * 512:(cb + 1) * 512].rearrange("(kc p) c -> p kc c", p=128),
                          w=[w_], r=[I["ada_w"]], q=("sp" if cb % 2 == 0 else "pool"))
                    for sub in range(4):
                        cc = cb * 4 + sub
                        for kc in range(8):
                            p.T(lambda e, w_=w_, cc=cc, kc=kc, sub=sub: e.matmul(pm[:, cc, :], lhsT=w_[:, kc, sub * 128:(sub + 1) * 128],
                                                                                rhs=sc[:, kc, :], start=(kc == 0), stop=(kc == 7)),
                                w=[pm], r=[w_, sc])
                m = mod[l]
                p.V(lambda e: e.tensor_tensor(out=m[:], in0=pm[:], in1=adab[:].unsqueeze(2).broadcast_to([128, 48, 2]), op=ALU.add),
                    w=[m], r=[pm, adab])
                for (gs, g, j) in ((gsm[l], ng, 1), (gsf[l], nf, 4)):
                    p.V(lambda e, gs=gs, g=g, j=j: e.scalar_tensor_tensor(out=gs[:], in0=m[:, j * 8:(j + 1) * 8, :], scalar=1.0,
                                                                         in1=g[:].unsqueeze(2).broadcast_to([128, 8, 2]),
                                                                         op0=ALU.add, op1=ALU.mult), w=[gs], r=[m, g])
                p.barrier()

        def norm_block(st_tiles, b0, n, gs_ap, sh_ap, hT, h32=None):
            xb, sq, pss, rstd, tmp = st_tiles
            p.dma(xb[:, :, :n], X[:, b0:b0 + n].rearrange("(kc p) t -> p kc t", p=128), w=[xb], r=[X])
            for kc in range(8):
                p.A(lambda e, kc=kc: e.activation(out=sq[:, kc, :n], in_=xb[:, kc, :n], func=AF.Square), w=[sq], r=[xb])
            for kc in range(8):
                p.T(lambda e, kc=kc: e.matmul(pss[:, :n], lhsT=ones_bf[:], rhs=sq[:, kc, :n], start=(kc == 0), stop=(kc == 7)),
                    w=[pss], r=[ones_bf, sq])
            p.A(lambda e: e.activation(out=rstd[:, :n], in_=pss[:, :n], func=AF.Sqrt, bias=1e-6, scale=1.0 / D), w=[rstd], r=[pss])
            p.V(lambda e: e.reciprocal(out=rstd[:, :n], in_=rstd[:, :n]), w=[rstd], r=[rstd])
            for kc in range(8):
                p.V(lambda e, kc=kc: e.scalar_tensor_tensor(out=tmp[:, kc, :n], in0=xb[:, kc, :n], scalar=gs_ap(kc), in1=rstd[:, :n],
                                                           op0=ALU.mult, op1=ALU.mult), w=[tmp], r=[xb, rstd])
                if sh_ap is not None:
                    if h32 is not None:
                        p.A(lambda e, kc=kc: e.activation(out=h32[:, kc, :n], in_=tmp[:, kc, :n], func=AF.Identity, bias=sh_ap(kc), scale=1.0),
                            w=[h32], r=[tmp])
                        p.V(lambda e, kc=kc: e.tensor_copy(out=hT[:, kc, b0:b0 + n], in_=h32[:, kc, :n]), w=[hT], r=[h32])
                    else:
                        p.A(lambda e, kc=kc: e.activation(out=hT[:, kc, b0:b0 + n], in_=tmp[:, kc, :n], func=AF.Identity, bias=sh_ap(kc), scale=1.0),
                            w=[hT], r=[tmp])

        def alloc_norm_tiles(st):
            return (p.sb("xb", [128, 8, 512], es=st), p.sb("sq", [128, 8, 512], BF16, es=st), p.ps("pss", [128, 512], es=st),
                    p.sb("rstd", [128, 512], es=st), p.sb("ntmp", [128, 8, 512], es=st))

        def stage_proj(l):
            with ExitStack() as st:
                hT = p.sb("hT", [128, 8, T], BF16, es=st)
                with ExitStack() as st2:
                    nt = alloc_norm_tiles(st2)
                    m = mod[l]
                    for (b0, n, seg) in BLKS:
                        norm_block(nt, b0, n, lambda kc, seg=seg: gsm[l][:, kc, seg:seg + 1], lambda kc, seg=seg: m[:, kc, seg:seg + 1], hT)
                    p.barrier()
                if "h" in dbg and l == dbg["h"]:
                    hd = p.dram("dbg_h", [D, T], BF16, kind="ExternalOutput")
                    p.dma(hd[:].rearrange("(kc p) t -> p kc t", p=128), hT[:], w=[hd], r=[hT])
                wf = [p.sb("wf", [128, 8, 512], es=st) for _ in range(2)]
                wb = [p.sb("wb", [128, 8, 512], BF16, es=st) for _ in range(2)]
                row = [p.sb("row", [128, T + 4], es=st) for _ in range(4)]
                tsm = p.sb("tsm", [128, T], es=st)
                mu = p.sb("mu", [128, 71], es=st); om = p.sb("om", [128, 71], es=st)
                pc = [p.ps("pc", [128, 512], es=st) for _ in range(4)]
                p.dma(mu[:], I["muT"][l], w=[mu], r=[I["muT"]])
                p.V(lambda e: e.tensor_scalar(out=om[:], in0=mu[:], scalar1=-1.0, scalar2=1.0, op0=ALU.mult, op1=ALU.add), w=[om], r=[mu])
                p.V(lambda e: e.tensor_scalar(out=mu[:], in0=mu[:], scalar1=0.5, scalar2=None, op0=ALU.mult), w=[mu], r=[mu])
                for r_ in row:
                    p.G(lambda e, r_=r_: e.memset(r_[:], 0.0), w=[r_])
                def rcol(t0):
                    return t0 + 1 if t0 < TC else t0 + 3
                ngrp = (INC + 511) // 512
                ei = 0

                def load_wg(g):
                    c0 = g * 512
                    ncg = min(512, INC - c0)
                    p.dma(wf[g % 2][:, :, :ncg], I["w_in"][l, :, c0:c0 + ncg].rearrange("(kc p) c -> p kc c", p=128), w=[wf[g % 2]], r=[I["w_in"]], q="pool")
                load_wg(0)
                for g in range(ngrp):
                    c0 = g * 512
                    ncg = min(512, INC - c0)
                    wf_, wb_ = wf[g % 2], wb[g % 2]
                    for kc in range(8):
                        if kc % 2 == 0:
                            p.V(lambda e, kc=kc: e.tensor_copy(out=wb_[:, kc, :ncg], in_=wf_[:, kc, :ncg]), w=[wb_], r=[wf_])
                        else:
                            p.A(lambda e, kc=kc: e.copy(out=wb_[:, kc, :ncg], in_=wf_[:, kc, :ncg]), w=[wb_], r=[wf_])
                    if g + 1 < ngrp:
                        load_wg(g + 1)
                    for sub in range((ncg + 127) // 128):
                        cb = g * 4 + sub
                        ncol = min(128, ncg - sub * 128)
                        rw_ = row[cb % 4]
                        for bi, (b0, n, seg) in enumerate(BLKS):
                            ps_ = pc[ei % 4]
                            for kc in range(8):
                                p.T(lambda e, kc=kc, ps_=ps_, n=n, b0=b0, sub=sub, ncol=ncol: e.matmul(
                                    ps_[:ncol, :n], lhsT=wb_[:, kc, sub * 128:sub * 128 + ncol], rhs=hT[:, kc, b0:b0 + n],
                                    start=(kc == 0), stop=(kc == 7)), w=[ps_], r=[wb_, hT])
                            rc = rcol(b0)
                            if ei % 2 == 0:
                                p.A(lambda e, ps_=ps_, rc=rc, n=n, ncol=ncol: e.copy(out=rw_[:ncol, rc:rc + n], in_=ps_[:ncol, :n]), w=[rw_], r=[ps_])
                            else:
                                p.V(lambda e, ps_=ps_, rc=rc, n=n, ncol=ncol: e.tensor_copy(out=rw_[:ncol, rc:rc + n], in_=ps_[:ncol, :n]), w=[rw_], r=[ps_])
                            ei += 1
                        col0 = cb * 128
                        if RW0 // 128 <= cb <= (DA0 - 1) // 128:
                            for (t0, n) in ((0, TC), (TC, TL)):
                                rc = rcol(t0)
                                p.V(lambda e, rc=rc, n=n, t0=t0: e.tensor_tensor(out=tsm[:ncol, t0:t0 + n], in0=rw_[:ncol, rc - 1:rc - 1 + n],
                                                                                 in1=rw_[:ncol, rc + 1:rc + 1 + n], op=ALU.add), w=[tsm], r=[rw_])
                                p.V(lambda e, n=n, t0=t0, cb=cb: e.tensor_scalar(out=tsm[:ncol, t0:t0 + n], in0=tsm[:ncol, t0:t0 + n],
                                                                                 scalar1=mu[:ncol, cb:cb + 1], scalar2=None, op0=ALU.mult), w=[tsm], r=[tsm, mu])
                                p.V(lambda e, rc=rc, n=n, t0=t0, cb=cb: e.scalar_tensor_tensor(out=tsm[:ncol, t0:t0 + n], in0=rw_[:ncol, rc:rc + n],
                                                                                               scalar=om[:ncol, cb:cb + 1], in1=tsm[:ncol, t0:t0 + n],
                                                                                               op0=ALU.mult, op1=ALU.add), w=[tsm], r=[rw_, om, tsm])
                            p.dma(PT[col0:col0 + ncol, :], tsm[:ncol, :], w=[PT], r=[tsm])
                        else:
                            p.dma(PT[col0:col0 + ncol, 0:TC], rw_[:ncol, 1:1 + TC], w=[PT], r=[rw_])
                            p.dma(PT[col0:col0 + ncol, TC:T], rw_[:ncol, TC + 3:T + 3], w=[PT], r=[rw_])
                for (cstart, dst) in ((HG0 + 1536, VHG), (DA0 + 1024, VDA)):
                    wf_, wb_ = wf[0], wb[0]
                    p.dma(wf_[:], I["w_in"][l, :, cstart:cstart + 512].rearrange("(kc p) c -> p kc c", p=128), w=[wf_], r=[I["w_in"]])
                    for kc in range(8):
                        p.V(lambda e, kc=kc: e.tensor_copy(out=wb_[:, kc, :], in_=wf_[:, kc, :]), w=[wb_], r=[wf_])
                    for tt in range(18):
                        ps_ = pc[tt % 4]
                        for kc in range(8):
                            p.T(lambda e, kc=kc, ps_=ps_, tt=tt: e.matmul(ps_[:, :], lhsT=hT[:, kc, tt * 128:(tt + 1) * 128], rhs=wb_[:, kc, :],
                                                                          start=(kc == 0), stop=(kc == 7)), w=[ps_], r=[wb_, hT])
                        o_ = row[tt % 4]
                        if tt % 2 == 0:
                            p.A(lambda e, ps_=ps_, o_=o_: e.copy(out=o_[:, 0:512], in_=ps_[:, :]), w=[o_], r=[ps_])
                        else:
                            p.V(lambda e, ps_=ps_, o_=o_: e.tensor_copy(out=o_[:, 0:512], in_=ps_[:, :]), w=[o_], r=[ps_])
                        p.dma(dst[tt * 128:(tt + 1) * 128, :], o_[:, 0:512], w=[dst], r=[o_])
                p.barrier()


        def load_w_bf(st, src_ap, nk, cols, name, pk=128):
            wf_ = p.sb(name + "_f", [pk, nk, cols], es=st)
            wb_ = p.sb(name + "_b", [pk, nk, cols], BF16, es=st)
            p.dma(wf_[:], src_ap, w=[wf_], r=[])
            for k in range(nk):
                p.V(lambda e, k=k: e.tensor_copy(out=wb_[:, k, :], in_=wf_[:, k, :]), w=[wb_], r=[wf_])
            return wb_

        def branch_out(bt, zT, nk, pk, wproj, gi, b0, n):
            gt, sgt, po, yo = bt
            for oc in range(8):
                p.dma(gt[:, :n], PT[GT0 + gi * 1024 + oc * 128:GT0 + gi * 1024 + (oc + 1) * 128, b0:b0 + n], w=[gt], r=[PT],
                      q=("sp" if oc % 2 == 0 else "pool"))
                p.A(lambda e: e.activation(out=sgt[:, :n], in_=gt[:, :n], func=AF.Sigmoid), w=[sgt], r=[gt])
                po_ = po[oc % 2]
                for k in range(nk):
                    p.T(lambda e, k=k, oc=oc, po_=po_: e.matmul(po_[:, :n], lhsT=wproj[:pk, k, oc * 128:(oc + 1) * 128], rhs=zT[:pk, k, :n],
                                                               start=(k == 0), stop=(k == nk - 1)), w=[po_], r=[wproj, zT])
                yo_ = yo[oc % 2]
                p.V(lambda e, po_=po_, yo_=yo_: e.tensor_tensor(out=yo_[:, :n], in0=po_[:, :n], in1=sgt[:, :n], op=ALU.mult), w=[yo_], r=[po_, sgt])
                p.dma(YG[gi][oc * 128:(oc + 1) * 128, b0:b0 + n], yo_[:, :n], w=[YG[gi]], r=[yo_], q=("pool" if oc % 2 == 0 else "sp"))

        def alloc_branch_tiles(st, npo=2):
            po = [p.ps("po", [128, 512], es=st) for _ in range(npo)]
            return (p.sb("gt", [128, 512], es=st), p.sb("sgt", [128, 512], es=st),
                    [po[i % npo] for i in range(2)], [p.sb("yo", [128, 512], es=st) for _ in range(2)])

        def stage_hg(l):
            with ExitStack() as st:
                lb = p.sb("lb", [128, 2, 4], es=st); oml = p.sb("oml", [128, 2, 4], es=st)
                if l == 0:
                    p.V(lambda e: e.memset(lb[:], 0.0), w=[lb])
                else:
                    lg0 = p.sb("lg0", [128, 2, 4], es=st)
                    p.dma(lg0[:], I["hg_lbT"][0].rearrange("d p h -> p d h"), w=[lg0], r=[I["hg_lbT"]])
                    p.dma(lb[:], I["hg_lbT"][1].rearrange("d p h -> p d h"), w=[lb], r=[I["hg_lbT"]])
                    p.V(lambda e: e.tensor_tensor(out=lb[:], in0=lb[:], in1=lg0[:], op=ALU.subtract), w=[lb], r=[lb, lg0])
                    p.A(lambda e: e.activation(out=lb[:], in_=lb[:], func=AF.Sigmoid), w=[lb], r=[lb])
                p.V(lambda e: e.tensor_scalar(out=oml[:], in0=lb[:], scalar1=-1.0, scalar2=1.0, op0=ALU.mult, op1=ALU.add), w=[oml], r=[lb])
                S = [p.sb("S", [128, 4, 128], es=st) for _ in range(2)]
                for d in range(2):
                    p.G(lambda e, d=d: e.memset(S[d][:], 0.0), w=[S[d]])
                names = ("qtl", "ktl", "qh", "kh")
                prep = [[{nm: p.sb(nm, [128, 4, 256], BF16, es=st) for nm in names} for _ in range(2)] for _ in range(2)]
                Sb = [p.sb("Sb", [128, 4, 128], BF16, es=st) for _ in range(2)]
                for d in range(2):
                    p.G(lambda e, d=d: e.memset(Sb[d][:], 0.0), w=[Sb[d]])
                Vgf = [p.sb("Vgf", [32, 8, 512], es=st) for _ in range(2)]
                ebC = [[p.sb("ebC", [128, 4, 8], es=st) for _ in range(2)] for _ in range(2)]
                Vg = [p.sb("Vg", [32, 8, 512], BF16, es=st) for _ in range(2)]
                og = [p.sb("og", [128, 4, 256], es=st) for _ in range(2)]
                tmp4 = {nm: p.sb(nm, [128, 4, 256], es=st) for nm in ("zt", "qt", "lf", "F", "E", "kg", "X", "ex")}
                ones256 = p.sb("ones256", [128, 256], es=st); p.V(lambda e: e.memset(ones256[:], 1.0), w=[ones256])
                khT = [p.sb("khT", [32, 512], BF16, es=st) for _ in range(2)]
                AT = [p.sb("AT", [32, 4, 32], BF16, es=st) for _ in range(2)]
                pT = [p.ps("pT", [32, 512], BF16, es=st) for _ in range(2)]
                pA = [p.ps("pA", [32, 4, 32], es=st) for _ in range(2)]
                pO = [p.ps("pO", [128, 4, 32], es=st) for _ in range(2)]
                pS = [p.ps("pS", [128, 4, 128], es=st) for _ in range(2)]
                bwd_groups = [0, 8, 7, 6, 5, 4, 3, 2, 1]
                ti = [0]
                Vg2 = [[Vg[d], p.sb("Vg2", [32, 8, 512], BF16, es=st)] for d in range(2)]
                og2 = [[og[d], p.sb("og2", [128, 4, 256], es=st)] for d in range(2)]

                def step_ctx(step, d):
                    g = step if d == 0 else bwd_groups[step]
                    return (g * 256, prep[d][step % 2], ebC[d][step % 2])

                def group_load(step, d):
                    t0, pr, eb = step_ctx(step, d)
                    p.dma(Vgf[d][:], VHG[t0:t0 + 256, :].rearrange("(c s) v -> s c v", s=32), w=[Vgf[d]], r=[VHG], q="pool")
                    p.A(lambda e: e.copy(out=Vg2[d][step % 2][:], in_=Vgf[d][:]), w=[Vg2[d][step % 2]], r=[Vgf[d]])

                rmask4 = p.sb("rmask4", [128, 4, 256], es=st)
                p.V(lambda e: e.memset(rmask4[:], 1.0), w=[rmask4]); p.V(lambda e: e.memset(rmask4[:, :, 0:1], 0.0), w=[rmask4])

                def prep_group(step, d):
                    t0, pr, eb = step_ctx(step, d)
                    zt, qt, lf, Ft, Et, kg, Xt, ex = (tmp4[nm] for nm in ("zt", "qt", "lf", "F", "E", "kg", "X", "ex"))
                    fl = lambda t_: t_[:].rearrange("p h t -> p (h t)")
                    p.dma(zt[:], PT[512 * (1 + d):512 * (2 + d), t0:t0 + 256].rearrange("(h k) t -> k h t", k=128), w=[zt], r=[PT], q="pool")
                    p.dma(qt[:], PT[0:512, t0:t0 + 256].rearrange("(h k) t -> k h t", k=128), w=[qt], r=[PT], q="pool")
                    p.A(lambda e: e.activation(out=zt[:], in_=zt[:], func=AF.Sigmoid), w=[zt], r=[zt])
                    p.A(lambda e: e.activation(out=qt[:], in_=qt[:], func=AF.Silu), w=[qt], r=[qt])
                    p.V(lambda e: e.tensor_tensor(out=zt[:], in0=zt[:], in1=oml[:, d, :].unsqueeze(2).broadcast_to([128, 4, 256]), op=ALU.mult), w=[zt], r=[zt, oml])
                    p.V(lambda e: e.tensor_tensor(out=zt[:], in0=zt[:], in1=lb[:, d, :].unsqueeze(2).broadcast_to([128, 4, 256]), op=ALU.add), w=[zt], r=[zt, lb])
                    p.A(lambda e: e.activation(out=lf[:], in_=zt[:], func=AF.Ln), w=[lf], r=[zt])
                    p.V(lambda e: e.tensor_scalar(out=kg[:], in0=zt[:], scalar1=-1.0, scalar2=1.0, op0=ALU.mult, op1=ALU.add), w=[kg], r=[zt])
                    p.V(lambda e: e.tensor_tensor_scan(out=fl(Ft), data0=fl(rmask4), data1=fl(lf), initial=0.0, op0=ALU.mult, op1=ALU.add),
                        w=[Ft], r=[rmask4, lf])
                    p.V(lambda e: e.tensor_tensor(out=Et[:], in0=Ft[:], in1=lf[:], op=ALU.subtract), w=[Et], r=[Ft, lf])
                    v3 = lambda t_: t_[:].rearrange("p h (c t) -> p (h c) t", t=32)
                    F3, E3, X3 = v3(Ft), v3(Et), v3(Xt)
                    bc = lambda ap: ap.broadcast_to([128, 32, 32])
                    if d == 0:
                        plan = [(F3, F3[:, :, 15:16], [("qtl", qt, 1.0), ("ktl", kg, -1.0)]),
                                (F3, E3[:, :, 0:1], [("qh", qt, 1.0)]),
                                (F3, F3[:, :, 31:32], [("kh", kg, -1.0)])]
                    else:
                        plan = [(E3, E3[:, :, 16:17], [("qtl", qt, -1.0), ("ktl", kg, 1.0)]),
                                (E3, F3[:, :, 31:32], [("qh", qt, -1.0)]),
                                (E3, E3[:, :, 0:1], [("kh", kg, 1.0)])]
                    for (src3, ref, outs) in plan:
                        p.V(lambda e, src3=src3, ref=ref: e.tensor_tensor(out=X3, in0=src3, in1=bc(ref), op=ALU.subtract), w=[Xt], r=[Ft, Et])
                        for (nm, mul, sgn) in outs:
                            p.A(lambda e, sgn=sgn: e.activation(out=ex[:], in_=Xt[:], func=AF.Exp, scale=sgn), w=[ex], r=[Xt])
                            p.V(lambda e, nm=nm, mul=mul: e.tensor_tensor(out=pr[nm][:], in0=mul[:], in1=ex[:], op=ALU.mult),
                                w=[pr[nm]], r=[mul, ex])
                    p.V(lambda e: e.tensor_tensor(out=eb[:].rearrange("p h c -> p (h c)"), in0=F3[:, :, 31], in1=E3[:, :, 0], op=ALU.subtract), w=[eb], r=[Ft, Et])
                    p.A(lambda e: e.activation(out=eb[:], in_=eb[:], func=AF.Exp), w=[eb], r=[eb])

                for d in range(2):
                    group_load(0, d)
                    prep_group(0, d)
                for step in range(9):
                    ctx_ = {d: step_ctx(step, d) for d in range(2)}
                    Vg = [Vg2[d][step % 2] for d in range(2)]
                    og = [og2[d][step % 2] for d in range(2)]
                    for ci in range(8):
                        for d in range(2):
                            t0, pr, eb = ctx_[d]
                            mi = 1 if d == 0 else 3
                            c = ci if d == 0 else 7 - ci
                            cs = c * 32
                            for h in range(4):
                                p.T(lambda e, h=h, cs=cs: e.transpose(pT[d][:32, h * 128:(h + 1) * 128], pr["kh"][:, h, cs:cs + 32], ident_bf[:]),
                                    w=[pT[d]], r=[pr["kh"], ident_bf])
                            p.A(lambda e: e.copy(out=khT[d][:], in_=pT[d][:]), w=[khT[d]], r=[pT[d]])
                            for h in range(4):
                                p.T(lambda e, h=h, cs=cs: e.matmul(pA[d][:, h, :], lhsT=pr["ktl"][:, h, cs:cs + 32], rhs=pr["qtl"][:, h, cs:cs + 32],
                                                                  start=True, stop=True), w=[pA[d]], r=[pr["ktl"], pr["qtl"]])
                            p.V(lambda e: e.tensor_tensor(out=AT[d][:], in0=pA[d][:], in1=masks[0:32, mi, 0:32].unsqueeze(1).broadcast_to([32, 4, 32]),
                                                          op=ALU.mult), w=[AT[d]], r=[pA[d], masks])
                            for h in range(4):
                                p.T(lambda e, h=h, c=c: e.matmul(pO[d][:, h, :], lhsT=Vg[d][:, c, h * 128:(h + 1) * 128], rhs=AT[d][:, h, :],
                                                                start=True, stop=False), w=[pO[d]], r=[Vg[d], AT[d]])
                                p.T(lambda e, h=h, cs=cs: e.matmul(pO[d][:, h, :], lhsT=Sb[d][:, h, :], rhs=pr["qh"][:, h, cs:cs + 32],
                                                                  start=False, stop=True), w=[pO[d]], r=[Sb[d], pr["qh"]])
                            p.A(lambda e, cs=cs: e.copy(out=og[d][:, :, cs:cs + 32], in_=pO[d][:]), w=[og[d]], r=[pO[d]])
                            for h in range(4):
                                p.T(lambda e, h=h, c=c: e.matmul(pS[d][:, h, :], lhsT=khT[d][:, h * 128:(h + 1) * 128], rhs=Vg[d][:, c, h * 128:(h + 1) * 128],
                                                                start=True, stop=True), w=[pS[d]], r=[khT[d], Vg[d]])
                            p.V(lambda e, c=c: e.tensor_tensor(out=S[d][:], in0=S[d][:], in1=eb[:, :, c:c + 1].broadcast_to([128, 4, 128]), op=ALU.mult),
                                w=[S[d]], r=[S[d], eb])
                            p.V(lambda e: e.tensor_tensor(out=S[d][:], in0=S[d][:], in1=pS[d][:], op=ALU.add), w=[S[d]], r=[S[d], pS[d]])
                            p.A(lambda e: e.copy(out=Sb[d][:], in_=S[d][:]), w=[Sb[d]], r=[S[d]])
                        if step + 1 < 9 and ci in (0, 4):
                            group_load(step + 1, ci // 4)
                            prep_group(step + 1, ci // 4)
                    for d in range(2):
                        t0, pr, eb = ctx_[d]
                        p.dma(OHG[d, :, t0:t0 + 256].rearrange("(h v) t -> v h t", v=128), og[d][:], w=[OHG], r=[og[d]])
                p.barrier()
            with ExitStack() as st:
                wproj = load_w_bf(st, I["hg_proj"][l].rearrange("(k p) c -> p k c", p=128), 4, D, "hgp")
                ngv = p.sb("ngv", [128, 1], es=st); p.dma(ngv[:], I["hg_ngT"][l], w=[ngv], r=[I["hg_ngT"]])
                bt = alloc_branch_tiles(st)
                oa = p.sb("oa", [128, 4, 512], es=st); ob = p.sb("ob", [128, 4, 512], es=st); gg = p.sb("gg", [128, 4, 512], es=st)
                sq = p.sb("sqh", [128, 4, 512], BF16, es=st); zT = p.sb("zT", [128, 4, 512], BF16, es=st)
                pn = p.ps("pn", [128, 512], es=st); rs = p.sb("rs", [128, 512], es=st)
                for (b0, n, seg) in BLKS:
                    p.dma(oa[:, :, :n], OHG[0, :, b0:b0 + n].rearrange("(h v) t -> v h t", v=128), w=[oa], r=[OHG])
                    p.dma(ob[:, :, :n], OHG[1, :, b0:b0 + n].rearrange("(h v) t -> v h t", v=128), w=[ob], r=[OHG], q="pool")
                    p.dma(gg[:, :, :n], PT[2048:2560, b0:b0 + n].rearrange("(h v) t -> v h t", v=128), w=[gg], r=[PT])
                    p.V(lambda e: e.tensor_tensor(out=oa[:, :, :n], in0=oa[:, :, :n], in1=ob[:, :, :n], op=ALU.add), w=[oa], r=[oa, ob])
                    p.A(lambda e: e.activation(out=gg[:, :, :n], in_=gg[:, :, :n], func=AF.Silu), w=[gg], r=[gg])
                    p.V(lambda e: e.tensor_tensor(out=sq[:, :, :n], in0=oa[:, :, :n], in1=oa[:, :, :n], op=ALU.mult), w=[sq], r=[oa])
                    for h in range(4):
                        p.T(lambda e, h=h: e.matmul(pn[:, :n], lhsT=ones_bf[:], rhs=sq[:, h, :n], start=True, stop=True), w=[pn], r=[ones_bf, sq])
                        p.A(lambda e: e.activation(out=rs[:, :n], in_=pn[:, :n], func=AF.Sqrt, bias=1e-6, scale=1.0 / 128), w=[rs], r=[pn])
                        p.V(lambda e: e.reciprocal(out=rs[:, :n], in_=rs[:, :n]), w=[rs], r=[rs])
                        p.V(lambda e, h=h: e.scalar_tensor_tensor(out=oa[:, h, :n], in0=oa[:, h, :n], scalar=ngv[:, 0:1], in1=rs[:, :n],
                                                                 op0=ALU.mult, op1=ALU.mult), w=[oa], r=[oa, ngv, rs])
                        p.V(lambda e, h=h: e.tensor_tensor(out=zT[:, h, :n], in0=oa[:, h, :n], in1=gg[:, h, :n], op=ALU.mult), w=[zT], r=[oa, gg])
                    branch_out(bt, zT, 4, 128, wproj, 0, b0, n)
                p.barrier()


        def stage_da(l):
            lam_init = 0.8 - 0.6 * math.exp(-0.3 * l)
            scale = 64 ** -0.5
            with ExitStack() as st:
                lp = p.sb("lp", [128, 4, 64], es=st); s12 = p.sb("s12", [128, 2], es=st); nlam = p.sb("nlam", [128, 1], es=st)
                pr_ = p.sb("lpp", [128, 2, 64], es=st)
                p.dma(lp[:].rearrange("p a b -> p (a b)"), I["da_lambda"][l].partition_broadcast(128), w=[lp], r=[I["da_lambda"]])
                p.V(lambda e: e.tensor_tensor(out=pr_[:, 0, :], in0=lp[:, 0, :], in1=lp[:, 1, :], op=ALU.mult), w=[pr_], r=[lp])
                p.V(lambda e: e.tensor_tensor(out=pr_[:, 1, :], in0=lp[:, 2, :], in1=lp[:, 3, :], op=ALU.mult), w=[pr_], r=[lp])
                p.V(lambda e: e.reduce_sum(out=s12[:], in_=pr_[:], axis=AX.X), w=[s12], r=[pr_])
                p.A(lambda e: e.activation(out=s12[:], in_=s12[:], func=AF.Exp), w=[s12], r=[s12])
                p.V(lambda e: e.tensor_tensor(out=nlam[:], in0=s12[:, 1:2], in1=s12[:, 0:1], op=ALU.subtract), w=[nlam], r=[s12])
                p.V(lambda e: e.tensor_scalar(out=nlam[:], in0=nlam[:], scalar1=-lam_init, scalar2=None, op0=ALU.add), w=[nlam], r=[nlam])
                sg = p.sb("sg", [128, 128], es=st)
                p.dma(sg[:], I["da_sg"][l].partition_broadcast(128), w=[sg], r=[I["da_sg"]])
                p.V(lambda e: e.tensor_scalar(out=sg[:], in0=sg[:], scalar1=1.0 - lam_init, scalar2=None, op0=ALU.mult), w=[sg], r=[sg])
                KT = p.sb("KT", [64, 8, T], BF16, es=st); QT = p.sb("QT", [64, 8, T], BF16, es=st)
                Vb = p.sb("Vb", [128, 18, 512], BF16, es=st)
                with ExitStack() as st2:
                    cs = p.sb("cs", [64, 2, TL], es=st2); p.dma(cs[:], I["ropeCS"][:], w=[cs], r=[I["ropeCS"]])
                    rR = p.sb("rR", [64, 64], es=st2); p.dma(rR[:], I["ropeR"][:], w=[rR], r=[I["ropeR"]])
                    xr = [p.sb("xr", [64, T], es=st2) for _ in range(2)]
                    t1 = [p.sb("t1", [64, 512], es=st2) for _ in range(2)]; t2 = [p.sb("t2", [64, 512], es=st2) for _ in range(2)]
                    pr2 = [p.ps("prp", [64, 512], es=st2) for _ in range(2)]
                    vf = [p.sb("vf", [128, 512], es=st2) for _ in range(2)]
                    i2 = 0
                    for qi, (dst, roff) in enumerate(((QT, DA0), (KT, DA0 + 512))):
                        for hm in range(8):
                            x_ = xr[hm % 2]
                            p.dma(x_[:], PT[roff + hm * 64:roff + (hm + 1) * 64, :], w=[x_], r=[PT], q=("sp" if hm % 2 == 0 else "pool"))
                            p.A(lambda e, hm=hm, dst=dst, x_=x_: e.copy(out=dst[:, hm, 0:TC], in_=x_[:, 0:TC]), w=[dst], r=[x_])
                            for bi in range(4):
                                a0 = TC + bi * 512
                                pp = pr2[i2 % 2]; t1_ = t1[i2 % 2]; t2_ = t2[i2 % 2]; i2 += 1
                                p.T(lambda e, pp=pp, x_=x_, a0=a0: e.matmul(pp[:], lhsT=rR[:], rhs=x_[:, a0:a0 + 512], start=True, stop=True), w=[pp], r=[rR, x_])
                                p.V(lambda e, t1_=t1_, x_=x_, a0=a0, bi=bi: e.tensor_tensor(out=t1_[:], in0=x_[:, a0:a0 + 512], in1=cs[:, 0, bi * 512:(bi + 1) * 512], op=ALU.mult),
                                    w=[t1_], r=[x_, cs])
                                p.V(lambda e, t2_=t2_, pp=pp, bi=bi: e.tensor_tensor(out=t2_[:], in0=pp[:], in1=cs[:, 1, bi * 512:(bi + 1) * 512], op=ALU.mult),
                                    w=[t2_], r=[pp, cs])
                                p.V(lambda e, dst=dst, hm=hm, a0=a0, t1_=t1_, t2_=t2_: e.tensor_tensor(out=dst[:, hm, a0:a0 + 512], in0=t1_[:], in1=t2_[:], op=ALU.add),
                                    w=[dst], r=[t1_, t2_])
                    for kt in range(18):
                        v_ = vf[kt % 2]
                        p.dma(v_[:], VDA[kt * 128:(kt + 1) * 128, :], w=[v_], r=[VDA], q=("sp" if kt % 2 == 0 else "pool"))
                        p.V(lambda e, kt=kt, v_=v_: e.tensor_copy(out=Vb[:, kt, :], in_=v_[:]), w=[Vb], r=[v_])
                    p.barrier()
                wproj = load_w_bf(st, I["da_proj"][l].rearrange("(k p) c -> p k c", p=128), 4, D, "dap")
                bt = alloc_branch_tiles(st, npo=1)
                pS = [p.ps("pSc", [128, 512], es=st) for _ in range(2)]
                Eb = [p.sb("Eb", [128, 18, 512], BF16, es=st) for _ in range(2)]
                acc = [p.ps("acc", [128, 4, 128], es=st) for _ in range(2)]
                den = p.ps("den", [128, 2, 4], es=st)
                pden = p.ps("pden", [1, 512], es=st); denT = p.sb("denT", [1, 512], es=st)
                pZ = p.ps("pZ", [128, 512], es=st)
                rden = p.sb("rden", [128, 2, 4], es=st); rl = p.sb("rl", [128, 4], es=st)
                o1 = p.sb("o1", [128, 4, 128], es=st); o2 = p.sb("o2", [128, 4, 128], es=st); ssq = p.sb("ssq", [128, 4], es=st)
                zT = p.sb("zTd", [128, 4, 512], BF16, es=st)
                qblocks = [(0, 256, [0, 1])] + [(TC + i * 512, 512, list(range(18))) for i in range(4)]
                ei = [0]

                def phase1(q0, nq, kts, hm):
                    Eb_ = Eb[hm % 2]
                    for kt in kts:
                        pS_ = pS[ei[0] % 2]; ei[0] += 1
                        p.T(lambda e, pS_=pS_, kt=kt: e.matmul(pS_[:, :nq], lhsT=KT[:, hm, kt * 128:(kt + 1) * 128], rhs=QT[:, hm, q0:q0 + nq],
                                                              start=True, stop=True), w=[pS_], r=[KT, QT])
                        p.A(lambda e, pS_=pS_, kt=kt: e.activation(out=Eb_[:, kt, :nq], in_=pS_[:, :nq], func=AF.Exp, scale=scale), w=[Eb_], r=[pS_])

                def phase2(q0, nq, kts, hm):
                    Eb_ = Eb[hm % 2]; h = hm // 2; m = hm % 2
                    for kt in kts:
                        p.T(lambda e, kt=kt: e.matmul(pden[0:1, :nq], lhsT=ones_bf[:, 0:1], rhs=Eb_[:, kt, :nq],
                                                      start=(kt == kts[0]), stop=(kt == kts[-1])), w=[pden], r=[Eb_, ones_bf])
                    p.A(lambda e: e.copy(out=denT[0:1, :nq], in_=pden[0:1, :nq]), w=[denT], r=[pden])
                    for j in range(nq // 128):
                        p.T(lambda e, j=j: e.matmul(den[:, m, j:j + 1], lhsT=denT[0:1, j * 128:(j + 1) * 128], rhs=ones_f[0:1, 0:1],
                                                    start=True, stop=True), w=[den], r=[denT, ones_f])
                    for j in range(nq // 128):
                        for kt in kts:
                            p.T(lambda e, j=j, kt=kt: e.matmul(acc[m][:, j, :], lhsT=Eb_[:, kt, j * 128:(j + 1) * 128], rhs=Vb[:, kt, h * 128:(h + 1) * 128],
                                                              start=(kt == kts[0]), stop=(kt == kts[-1])), w=[acc[m]], r=[Eb_, Vb])

                for (q0, nq, kts) in qblocks:
                    nj = nq // 128
                    phase1(q0, nq, kts, 0)
                    for hm in range(8):
                        if hm + 1 < 8:
                            phase1(q0, nq, kts, hm + 1)
                        phase2(q0, nq, kts, hm)
                        if hm % 2 == 0:
                            continue
                        h = hm // 2
                        p.V(lambda e: e.reciprocal(out=rden[:, :, :nj], in_=den[:, :, :nj]), w=[rden], r=[den])
                        p.V(lambda e: e.tensor_scalar(out=rl[:, :nj], in0=rden[:, 1, :nj], scalar1=nlam[:, 0:1], scalar2=None, op0=ALU.mult), w=[rl], r=[rden, nlam])
                        p.V(lambda e: e.tensor_tensor(out=o1[:, :nj, :], in0=acc[0][:, :nj, :], in1=rden[:, 0, :nj].unsqueeze(2).broadcast_to([128, nj, 128]), op=ALU.mult),
                            w=[o1], r=[acc[0], rden])
                        p.V(lambda e: e.tensor_tensor(out=o2[:, :nj, :], in0=acc[1][:, :nj, :], in1=rl[:, :nj].unsqueeze(2).broadcast_to([128, nj, 128]), op=ALU.mult),
                            w=[o2], r=[acc[1], rl])
                        p.V(lambda e: e.tensor_tensor(out=o1[:, :nj, :], in0=o1[:, :nj, :], in1=o2[:, :nj, :], op=ALU.add), w=[o1], r=[o1, o2])
                        p.V(lambda e: e.tensor_tensor(out=o2[:, :nj, :], in0=o1[:, :nj, :], in1=o1[:, :nj, :], op=ALU.mult), w=[o2], r=[o1])
                        p.V(lambda e: e.reduce_sum(out=ssq[:, :nj], in_=o2[:, :nj, :], axis=AX.X), w=[ssq], r=[o2])
                        p.A(lambda e: e.activation(out=ssq[:, :nj], in_=ssq[:, :nj], func=AF.Sqrt, bias=1e-5, scale=1.0 / 128), w=[ssq], r=[ssq])
                        p.V(lambda e: e.reciprocal(out=ssq[:, :nj], in_=ssq[:, :nj]), w=[ssq], r=[ssq])
                        p.V(lambda e: e.tensor_tensor(out=o1[:, :nj, :], in0=o1[:, :nj, :], in1=ssq[:, :nj].unsqueeze(2).broadcast_to([128, nj, 128]), op=ALU.mult),
                            w=[o1], r=[o1, ssq])
                        p.V(lambda e: e.tensor_tensor(out=o1[:, :nj, :], in0=o1[:, :nj, :], in1=sg[:].unsqueeze(1).broadcast_to([128, nj, 128]), op=ALU.mult),
                            w=[o1], r=[o1, sg])
                        for j in range(nj):
                            p.T(lambda e, j=j: e.transpose(pZ[:, j * 128:(j + 1) * 128], o1[:, j, :], ident[:]), w=[pZ], r=[o1, ident])
                        p.A(lambda e, h=h: e.copy(out=zT[:, h, :nq], in_=pZ[:, :nq]), w=[zT], r=[pZ])
                    branch_out(bt, zT, 4, 128, wproj, 2, q0, nq)
                p.barrier()


        RWN = ("R", "K", "V", "AN", "B", "LW0", "LW1", "BON", "G")
        RWS = {nm: p.dram("RWS_" + nm, [64, 8, T]) for nm in RWN}
        YRW = [p.dram("YRW%d" % d, [64, 8, T]) for d in range(2)]

        def stage_rw_prep(l):
            with ExitStack() as st:
                vec = p.sb("rwvec", [64, 8, 8], es=st); p.dma(vec[:], I["rw_vecT"][l], w=[vec], r=[I["rw_vecT"]])
                w0 = p.sb("rww0", [64, 2, 8], es=st); p.dma(w0[:], I["rw_w0T"][l].rearrange("d k h -> k d h"), w=[w0], r=[I["rw_w0T"]])
                w2 = p.sb("rww2", [64, 2, 512], es=st); p.dma(w2[:], I["rw_w2"][l].rearrange("d j c -> j d c"), w=[w2], r=[I["rw_w2"]])
                a2 = p.sb("rwa2", [64, 512], es=st); p.dma(a2[:], I["rw_a2"][l], w=[a2], r=[I["rw_a2"]])
                g2 = p.sb("rwg2", [128, 512], es=st); p.dma(g2[:], I["rw_g2"][l], w=[g2], r=[I["rw_g2"]])
                n = 256
                mk = lambda nm: p.sb(nm, [64, 8, n], es=st)
                r_, kr, v_, a_, kk, sq, nrm, k_, b_, an, rk, bon, g_ = (mk(x) for x in ("r_", "kr", "v_", "a_", "kk", "sq", "nrm", "k_", "b_", "an", "rk", "bon", "g_"))
                lw = [mk("lw0"), mk("lw1")]
                adt = p.sb("adt", [64, n], es=st); wd = [p.sb("wdf", [64, n], es=st), p.sb("wdb", [64, n], es=st)]
                gdt = p.sb("gdt", [128, n], es=st); th = p.sb("th", [64, n], es=st)
                pool = [p.ps("prw", [64, 2, n], es=st) for _ in range(4)]
                pi = [0]

                def nps():
                    pi[0] += 1
                    return pool[pi[0] % 4]
                bc = lambda i: vec[:, i, :].unsqueeze(2).broadcast_to([64, 8, n])
                for blk in range(9):
                    b0 = blk * n
                    hk = lambda c0: PT[c0:c0 + 512, b0:b0 + n].rearrange("(h k) t -> k h t", k=64)
                    p.dma(r_[:], hk(RW_R), w=[r_], r=[PT]); p.dma(kr[:], hk(RW_K), w=[kr], r=[PT], q="pool"); p.dma(v_[:], hk(RW_V), w=[v_], r=[PT])
                    p.dma(adt[:], PT[RW_AD:RW_AD + 64, b0:b0 + n], w=[adt], r=[PT], q="pool")
                    p.dma(wd[0][:], PT[RW_WF:RW_WF + 64, b0:b0 + n], w=[wd[0]], r=[PT]); p.dma(wd[1][:], PT[RW_WB:RW_WB + 64, b0:b0 + n], w=[wd[1]], r=[PT], q="pool")
                    p.dma(gdt[:], PT[RW_GD:RW_GD + 128, b0:b0 + n], w=[gdt], r=[PT])
                    for hp in range(4):
                        ps_ = nps()
                        for hh in range(2):
                            h = hp * 2 + hh
                            p.T(lambda e, h=h, hh=hh, ps_=ps_: e.matmul(ps_[:, hh, :], lhsT=a2[:, h * 64:(h + 1) * 64], rhs=adt[:], start=True, stop=True), w=[ps_], r=[a2, adt])
                        for hh in range(2):
                            h = hp * 2 + hh
                            p.A(lambda e, h=h, hh=hh, ps_=ps_: e.activation(out=a_[:, h, :], in_=ps_[:, hh, :], func=AF.Sigmoid, bias=vec[:, 2, h:h + 1], scale=1.0),
                                w=[a_], r=[ps_, vec])
                    p.V(lambda e: e.tensor_tensor(out=kk[:], in0=kr[:], in1=bc(0), op=ALU.mult), w=[kk], r=[kr, vec])
                    p.A(lambda e: e.activation(out=sq[:], in_=kk[:], func=AF.Square), w=[sq], r=[kk])
                    for hp in range(4):
                        ps_ = nps()
                        p.T(lambda e, hp=hp, ps_=ps_: e.matmul(ps_[:].rearrange("p a b -> p (a b)"), lhsT=ones_f[:64, :64],
                                                              rhs=sq[:, 2 * hp:2 * hp + 2, :].rearrange("p a b -> p (a b)"), start=True, stop=True), w=[ps_], r=[ones_f, sq])
                        p.A(lambda e, hp=hp, ps_=ps_: e.activation(out=nrm[:, 2 * hp:2 * hp + 2, :], in_=ps_[:], func=AF.Sqrt), w=[nrm], r=[ps_])
                    p.V(lambda e: e.tensor_scalar(out=nrm[:], in0=nrm[:], scalar1=1e-12, scalar2=None, op0=ALU.max), w=[nrm], r=[nrm])
                    p.V(lambda e: e.reciprocal(out=nrm[:], in_=nrm[:]), w=[nrm], r=[nrm])
                    p.V(lambda e: e.tensor_tensor(out=kk[:], in0=kk[:], in1=nrm[:], op=ALU.mult), w=[kk], r=[kk, nrm])
                    p.V(lambda e: e.scalar_tensor_tensor(out=k_[:], in0=a_[:], scalar=-1.0, in1=bc(1), op0=ALU.add, op1=ALU.mult), w=[k_], r=[a_, vec])
                    p.V(lambda e: e.scalar_tensor_tensor(out=k_[:], in0=k_[:], scalar=1.0, in1=kr[:], op0=ALU.add, op1=ALU.mult), w=[k_], r=[k_, kr])
                    p.V(lambda e: e.tensor_tensor(out=b_[:], in0=kk[:], in1=a_[:], op=ALU.mult), w=[b_], r=[kk, a_])
                    p.A(lambda e: e.activation(out=an[:], in_=kk[:], func=AF.Identity, scale=-1.0), w=[an], r=[kk])
                    p.V(lambda e: e.tensor_tensor(out=rk[:], in0=r_[:], in1=k_[:], op=ALU.mult), w=[rk], r=[r_, k_])
                    p.V(lambda e: e.tensor_tensor(out=rk[:], in0=rk[:], in1=bc(3), op=ALU.mult), w=[rk], r=[rk, vec])
                    for hp in range(4):
                        ps_ = nps()
                        p.T(lambda e, hp=hp, ps_=ps_: e.matmul(ps_[:].rearrange("p a b -> p (a b)"), lhsT=ones_f[:64, :64],
                                                              rhs=rk[:, 2 * hp:2 * hp + 2, :].rearrange("p a b -> p (a b)"), start=True, stop=True), w=[ps_], r=[ones_f, rk])
                        p.V(lambda e, hp=hp, ps_=ps_: e.tensor_tensor(out=bon[:, 2 * hp:2 * hp + 2, :], in0=ps_[:], in1=v_[:, 2 * hp:2 * hp + 2, :], op=ALU.mult),
                            w=[bon], r=[ps_, v_])
                    p.A(lambda e: e.activation(out=gdt[:], in_=gdt[:], func=AF.Sigmoid), w=[gdt], r=[gdt])
                    for hp in range(4):
                        ps_ = nps()
                        for hh in range(2):
                            h = hp * 2 + hh
                            p.T(lambda e, h=h, hh=hh, ps_=ps_: e.matmul(ps_[:, hh, :], lhsT=g2[:, h * 64:(h + 1) * 64], rhs=gdt[:], start=True, stop=True), w=[ps_], r=[g2, gdt])
                        p.A(lambda e, hp=hp, ps_=ps_: e.copy(out=g_[:, 2 * hp:2 * hp + 2, :], in_=ps_[:]), w=[g_], r=[ps_])
                    for d in range(2):
                        p.A(lambda e, d=d: e.activation(out=th[:], in_=wd[d][:], func=AF.Tanh), w=[th], r=[wd[d]])
                        for hp in range(4):
                            ps_ = nps()
                            for hh in range(2):
                                h = hp * 2 + hh
                                p.T(lambda e, h=h, hh=hh, ps_=ps_, d=d: e.matmul(ps_[:, hh, :], lhsT=w2[:, d, h * 64:(h + 1) * 64], rhs=th[:], start=True, stop=True), w=[ps_], r=[w2, th])
                            for hh in range(2):
                                h = hp * 2 + hh
                                p.A(lambda e, h=h, hh=hh, ps_=ps_, d=d: e.activation(out=lw[d][:, h, :], in_=ps_[:, hh, :], func=AF.Sigmoid, bias=w0[:, d, h:h + 1], scale=1.0),
                                    w=[lw[d]], r=[ps_, w0])
                        p.V(lambda e, d=d: e.tensor_scalar(out=lw[d][:], in0=lw[d][:], scalar1=-math.exp(-0.5), scalar2=None, op0=ALU.mult), w=[lw[d]], r=[lw[d]])
                    for qi, (nm, tl) in enumerate((("R", r_), ("K", k_), ("V", v_), ("AN", an), ("B", b_), ("LW0", lw[0]), ("LW1", lw[1]), ("BON", bon), ("G", g_))):
                        p.dma(RWS[nm][:, :, b0:b0 + n], tl[:], w=[RWS[nm]], r=[tl], q=("sp" if qi % 2 == 0 else "pool"))
                p.barrier()

        def stage_rw_scan(l):
            with ExitStack() as st:
                mk = lambda nm, shp=(64, 8, 64), dt=F32: [p.sb(nm, list(shp), dt, es=st) for _ in range(2)]
                ST = mk("ST"); STb = mk("STb", dt=BF16); Vb = mk("Vb", dt=BF16); Xb = mk("Xb", dt=BF16)
                for d in range(2):
                    p.G(lambda e, d=d: e.memset(STb[d][:], 0.0), w=[STb[d]])
                for d in range(2):
                    p.G(lambda e, d=d: e.memset(ST[d][:], 0.0), w=[ST[d]])
                rmask = p.sb("rmask", [64, 8, 64], es=st)
                p.V(lambda e: e.memset(rmask[:], 1.0), w=[rmask]); p.V(lambda e: e.memset(rmask[:, :, 0:1], 0.0), w=[rmask])
                ld = {nm: mk("l" + nm) for nm in ("R", "K", "V", "AN", "B", "LW")}
                Ft, Et, Gt, Ht = mk("F"), mk("E"), mk("G"), mk("H")
                ex = [mk("ex0"), mk("ex1")]
                AR = mk("AR", (64, 8, 2, 64), BF16); bt, kt_, bh, kh = mk("bt", dt=BF16), mk("kt", dt=BF16), mk("bh", dt=BF16), mk("kh", dt=BF16)
                Vtm, Btm, Ktm = mk("Vtm", dt=BF16), mk("Btm", dt=BF16), mk("Ktm", dt=BF16)
                W1 = mk("W1", (64, 8, 128), BF16); W2 = mk("W2", (64, 8, 128), BF16); Lm = mk("Lm", dt=BF16); Xm = mk("Xm")
                Pb = [mk("Pb0", dt=BF16), mk("Pb1", dt=BF16)]; PTb = [mk("PTb0", dt=BF16), mk("PTb1", dt=BF16)]
                RH, Um, yo = mk("RH", dt=BF16), mk("Um", dt=BF16), mk("yo")
                gC = mk("gC", (64, 8))
                pool = [p.ps("prs", [64, 8, 64], es=st) for _ in range(6)]
                poolb = [p.ps("prsb", [64, 8, 64], BF16, es=st) for _ in range(2)]
                pi = [0, 0]

                def nps():
                    pi[0] += 1
                    return pool[pi[0] % 6]

                def npsb():
                    pi[1] += 1
                    return poolb[pi[1] % 2]
                f2 = lambda b: b[:].rearrange("p h t -> p (h t)")
                bwd = [3, 2, 1, 0] + list(range(35, 3, -1))
                idb = ident[:64, :64].unsqueeze(1).broadcast_to([64, 8, 64])
                ei = [0]

                def exp_to(d, src, sgn):
                    e_ = ex[ei[0] % 2][d]; ei[0] += 1
                    p.A(lambda e: e.activation(out=e_[:], in_=src[:], func=AF.Exp, scale=sgn), w=[e_], r=[src])
                    return e_
                for step in range(36):
                    cd = (step, bwd[step])
                    for d in range(2):
                        t0 = cd[d] * 64
                        for qi, nm in enumerate(("R", "K", "V", "AN", "B", "LW")):
                            src = RWS["LW%d" % d] if nm == "LW" else RWS[nm]
                            p.dma(ld[nm][d][:], src[:, :, t0:t0 + 64], w=[ld[nm][d]], r=[src], q=("sp" if qi % 2 == 0 else "pool"))
                    for d in range(2):
                        lw = ld["LW"][d]; F_, E_, G_, H_ = Ft[d], Et[d], Gt[d], Ht[d]
                        p.V(lambda e: e.tensor_tensor_scan(out=f2(F_), data0=f2(rmask), data1=f2(lw), initial=0.0, op0=ALU.mult, op1=ALU.add), w=[F_], r=[rmask, lw])
                        p.G(lambda e: e.tensor_tensor(out=E_[:], in0=F_[:], in1=lw[:], op=ALU.subtract), w=[E_], r=[F_, lw])
                        fend = F_[:, :, 63:64].broadcast_to([64, 8, 64])
                        p.V(lambda e: e.tensor_tensor(out=G_[:], in0=fend, in1=F_[:], op=ALU.subtract), w=[G_], r=[F_])
                        p.A(lambda e: e.activation(out=gC[d][:], in_=F_[:, :, 63], func=AF.Exp), w=[gC[d]], r=[F_])
                        if d == 1:
                            p.V(lambda e: e.tensor_tensor(out=H_[:], in0=E_[:], in1=fend, op=ALU.subtract), w=[H_], r=[E_, F_])
                        R_, K_, AN_, B_ = ld["R"][d], ld["K"][d], ld["AN"][d], ld["B"][d]
                        specs = ((E_, 1.0), (F_, -1.0), (F_, 1.0), (G_, 1.0)) if d == 0 else ((G_, 1.0), (H_, 1.0), (H_, -1.0), (E_, 1.0))
                        e1 = exp_to(d, *specs[0])
                        p.V(lambda e: e.tensor_tensor(out=AR[d][:, :, 0, :], in0=AN_[:], in1=e1[:], op=ALU.mult), w=[AR[d]], r=[AN_, e1])
                        e2 = exp_to(d, *specs[1])
                        p.V(lambda e: e.tensor_tensor(out=bt[d][:], in0=B_[:], in1=e2[:], op=ALU.mult), w=[bt[d]], r=[B_, e2])
                        p.G(lambda e: e.tensor_tensor(out=kt_[d][:], in0=K_[:], in1=e2[:], op=ALU.mult), w=[kt_[d]], r=[K_, e2])
                        e3 = exp_to(d, *specs[2])
                        p.V(lambda e: e.tensor_tensor(out=AR[d][:, :, 1, :], in0=R_[:], in1=e3[:], op=ALU.mult), w=[AR[d]], r=[R_, e3])
                        e4 = exp_to(d, *specs[3])
                        p.V(lambda e: e.tensor_tensor(out=bh[d][:], in0=B_[:], in1=e4[:], op=ALU.mult), w=[bh[d]], r=[B_, e4])
                        p.G(lambda e: e.tensor_tensor(out=kh[d][:], in0=K_[:], in1=e4[:], op=ALU.mult), w=[kh[d]], r=[K_, e4])
                    for d in range(2):
                        p.A(lambda e: e.copy(out=Vb[d][:], in_=ld["V"][d][:]), w=[Vb[d]], r=[ld["V"][d]])
                        for (src, dst) in ((Vb[d], Vtm[d]), (bh[d], Btm[d]), (kh[d], Ktm[d])):
                            ps_ = npsb()
                            for h in range(8):
                                p.T(lambda e, h=h, ps_=ps_, src=src: e.transpose(ps_[:, h, :], src[:, h, :], ident_bf[:64, :64]), w=[ps_], r=[src, ident_bf])
                            p.A(lambda e, ps_=ps_, dst=dst: e.copy(out=dst[:], in_=ps_[:]), w=[dst], r=[ps_])
                    for d in range(2):
                        cm = masks[:, 2 * d:2 * d + 2, :].rearrange("p a b -> p (a b)").unsqueeze(1).broadcast_to([64, 4, 128])
                        for (lh, Wd) in ((bt[d], W1[d]), (kt_[d], W2[d])):
                            for half in range(2):
                                ps_ = nps()
                                pv = ps_[:].rearrange("p h t -> p (h t)").rearrange("p (a b) -> p a b", b=128)
                                for hh in range(4):
                                    h = half * 4 + hh
                                    p.T(lambda e, h=h, hh=hh, pv=pv, lh=lh: e.matmul(pv[:, hh, :], lhsT=lh[:, h, :], rhs=AR[d][:, h, :, :].rearrange("p a b -> p (a b)"),
                                                                                   start=True, stop=True), w=[ps_], r=[lh, AR[d]])
                                p.V(lambda e, pv=pv, Wd=Wd, half=half: e.tensor_tensor(out=Wd[:, half * 4:(half + 1) * 4, :], in0=pv, in1=cm, op=ALU.mult), w=[Wd], r=[ps_, masks])
                        ps_ = nps()
                        for h in range(8):
                            p.T(lambda e, h=h, ps_=ps_: e.matmul(ps_[:, h, :], lhsT=AR[d][:, h, 0, :], rhs=bt[d][:, h, :], start=True, stop=True), w=[ps_], r=[AR[d], bt[d]])
                        mnt = masks[:, 2 - 2 * d, :].unsqueeze(1).broadcast_to([64, 8, 64])
                        p.V(lambda e, ps_=ps_: e.tensor_tensor(out=Lm[d][:], in0=ps_[:], in1=mnt, op=ALU.mult), w=[Lm[d]], r=[ps_, masks])
                        p.G(lambda e: e.tensor_tensor(out=Xm[d][:], in0=W1[d][:, :, 0:64], in1=idb, op=ALU.add), w=[Xm[d]], r=[W1[d], ident])
                        p.A(lambda e: e.copy(out=Xb[d][:], in_=Xm[d][:]), w=[Xb[d]], r=[Xm[d]])
                    cur = [(lambda h, d=d: W1[d][:, h, 0:64], W1[d], lambda h, d=d: Lm[d][:, h, :], Lm[d]) for d in range(2)]
                    for i in range(1, 6):
                        for d in range(2):
                            Pf, Pbuf, PTf, PTbuf = cur[d]
                            nP, nPT = Pb[i % 2][d], PTb[i % 2][d]
                            if i < 5:
                                ps_ = nps()
                                for h in range(8):
                                    p.T(lambda e, h=h, ps_=ps_: e.matmul(ps_[:, h, :], lhsT=PTf(h), rhs=Pf(h), start=True, stop=True), w=[ps_], r=[Pbuf, PTbuf])
                                p.A(lambda e, ps_=ps_, nP=nP: e.copy(out=nP[:], in_=ps_[:]), w=[nP], r=[ps_])
                            ps2 = nps()
                            for h in range(8):
                                p.T(lambda e, h=h, ps2=ps2: e.matmul(ps2[:, h, :], lhsT=Pf(h), rhs=PTf(h), start=True, stop=True), w=[ps2], r=[Pbuf, PTbuf])
                            p.V(lambda e, ps2=ps2, nPT=nPT: e.tensor_copy(out=nPT[:], in_=ps2[:]), w=[nPT], r=[ps2])
                            ps3 = nps()
                            for h in range(8):
                                p.T(lambda e, h=h, ps3=ps3, nPT=nPT: e.matmul(ps3[:, h, :], lhsT=nPT[:, h, :], rhs=Xb[d][:, h, :], start=True, stop=True), w=[ps3], r=[nPT, Xb[d]])
                            p.V(lambda e, ps3=ps3: e.tensor_tensor(out=Xm[d][:], in0=Xm[d][:], in1=ps3[:], op=ALU.add), w=[Xm[d]], r=[Xm[d], ps3])
                            p.A(lambda e: e.copy(out=Xb[d][:], in_=Xm[d][:]), w=[Xb[d]], r=[Xm[d]])
                            cur[d] = (lambda h, nP=nP: nP[:, h, :], nP, lambda h, nPT=nPT: nPT[:, h, :], nPT)
                    for d in range(2):
                        ps_ = nps()
                        for h in range(8):
                            p.T(lambda e, h=h, ps_=ps_: e.matmul(ps_[:, h, :], lhsT=AR[d][:, h, 0, :], rhs=STb[d][:, h, :], start=True, stop=False), w=[ps_], r=[AR[d], STb[d]])
                            p.T(lambda e, h=h, ps_=ps_: e.matmul(ps_[:, h, :], lhsT=W2[d][:, h, 0:64], rhs=Vtm[d][:, h, :], start=False, stop=True), w=[ps_], r=[W2[d], Vtm[d]])
                        p.A(lambda e, ps_=ps_: e.copy(out=RH[d][:], in_=ps_[:]), w=[RH[d]], r=[ps_])
                        ps_ = nps()
                        for h in range(8):
                            p.T(lambda e, h=h, ps_=ps_: e.matmul(ps_[:, h, :], lhsT=Xb[d][:, h, :], rhs=RH[d][:, h, :], start=True, stop=True), w=[ps_], r=[Xb[d], RH[d]])
                        p.V(lambda e, ps_=ps_: e.tensor_copy(out=Um[d][:], in_=ps_[:]), w=[Um[d]], r=[ps_])
                        ps_ = nps()
                        for h in range(8):
                            p.T(lambda e, h=h, ps_=ps_: e.matmul(ps_[:, h, :], lhsT=STb[d][:, h, :], rhs=AR[d][:, h, 1, :], start=True, stop=False), w=[ps_], r=[STb[d], AR[d]])
                            p.T(lambda e, h=h, ps_=ps_: e.matmul(ps_[:, h, :], lhsT=Um[d][:, h, :], rhs=W1[d][:, h, 64:128], start=False, stop=False), w=[ps_], r=[Um[d], W1[d]])
                            p.T(lambda e, h=h, ps_=ps_: e.matmul(ps_[:, h, :], lhsT=Vtm[d][:, h, :], rhs=W2[d][:, h, 64:128], start=False, stop=True), w=[ps_], r=[Vtm[d], W2[d]])
                        p.A(lambda e, ps_=ps_: e.copy(out=yo[d][:], in_=ps_[:]), w=[yo[d]], r=[ps_])
                        t0 = cd[d] * 64
                        p.dma(YRW[d][:, :, t0:t0 + 64], yo[d][:], w=[YRW[d]], r=[yo[d]], q=("sp" if d == 0 else "pool"))
                        ps_ = nps()
                        for h in range(8):
                            p.T(lambda e, h=h, ps_=ps_: e.matmul(ps_[:, h, :], lhsT=Btm[d][:, h, :], rhs=Um[d][:, h, :], start=True, stop=False), w=[ps_], r=[Btm[d], Um[d]])
                            p.T(lambda e, h=h, ps_=ps_: e.matmul(ps_[:, h, :], lhsT=Ktm[d][:, h, :], rhs=Vtm[d][:, h, :], start=False, stop=True), w=[ps_], r=[Ktm[d], Vtm[d]])
                        p.V(lambda e: e.tensor_tensor(out=ST[d][:], in0=ST[d][:], in1=gC[d][:].unsqueeze(2).broadcast_to([64, 8, 64]), op=ALU.mult), w=[ST[d]], r=[ST[d], gC[d]])
                        p.V(lambda e, ps_=ps_: e.tensor_tensor(out=ST[d][:], in0=ST[d][:], in1=ps_[:], op=ALU.add), w=[ST[d]], r=[ST[d], ps_])
                        p.A(lambda e: e.copy(out=STb[d][:], in_=ST[d][:]), w=[STb[d]], r=[ST[d]])
                p.barrier()

        def stage_rw_fin(l):
            with ExitStack() as st:
                vec = p.sb("rwvec", [64, 8, 8], es=st); p.dma(vec[:], I["rw_vecT"][l], w=[vec], r=[I["rw_vecT"]])
                wproj = load_w_bf(st, I["rw_proj"][l].rearrange("(h k) c -> k h c", k=64), 8, D, "rwp", pk=64)
                bt = alloc_branch_tiles(st)
                n = 256
                mk = lambda nm: p.sb(nm, [64, 8, n], es=st)
                ya, yb, bo, gg, sq2 = mk("ya"), mk("yb"), mk("bo"), mk("gg"), mk("sq2")
                zT = p.sb("zTr", [64, 8, 512], BF16, es=st)
                pool = [p.ps("prf", [64, 2, n], es=st) for _ in range(2)]
                bc = lambda i: vec[:, i, :].unsqueeze(2).broadcast_to([64, 8, n])
                pi = 0
                for blk in range(9):
                    b0 = blk * n
                    p.dma(ya[:], YRW[0][:, :, b0:b0 + n], w=[ya], r=[YRW[0]]); p.dma(yb[:], YRW[1][:, :, b0:b0 + n], w=[yb], r=[YRW[1]], q="pool")
                    p.dma(bo[:], RWS["BON"][:, :, b0:b0 + n], w=[bo], r=[RWS["BON"]]); p.dma(gg[:], RWS["G"][:, :, b0:b0 + n], w=[gg], r=[RWS["G"]], q="pool")
                    p.V(lambda e: e.tensor_tensor(out=ya[:], in0=ya[:], in1=yb[:], op=ALU.add), w=[ya], r=[ya, yb])
                    for hp in range(4):
                        ps_ = pool[pi % 2]; pi += 1
                        p.T(lambda e, hp=hp, ps_=ps_: e.matmul(ps_[:].rearrange("p a b -> p (a b)"), lhsT=ones_f[:64, :64],
                                                              rhs=ya[:, 2 * hp:2 * hp + 2, :].rearrange("p a b -> p (a b)"), start=True, stop=True), w=[ps_], r=[ones_f, ya])
                        p.V(lambda e, hp=hp, ps_=ps_: e.scalar_tensor_tensor(out=yb[:, 2 * hp:2 * hp + 2, :], in0=ps_[:], scalar=-1.0 / 64, in1=ya[:, 2 * hp:2 * hp + 2, :],
                                                                            op0=ALU.mult, op1=ALU.add), w=[yb], r=[ps_, ya])
                    p.A(lambda e: e.activation(out=sq2[:], in_=yb[:], func=AF.Square), w=[sq2], r=[yb])
                    for hp in range(4):
                        ps_ = pool[pi % 2]; pi += 1
                        p.T(lambda e, hp=hp, ps_=ps_: e.matmul(ps_[:].rearrange("p a b -> p (a b)"), lhsT=ones_f[:64, :64],
                                                              rhs=sq2[:, 2 * hp:2 * hp + 2, :].rearrange("p a b -> p (a b)"), start=True, stop=True), w=[ps_], r=[ones_f, sq2])
                        p.A(lambda e, hp=hp, ps_=ps_: e.activation(out=ya[:, 2 * hp:2 * hp + 2, :], in_=ps_[:], func=AF.Sqrt, bias=64e-5, scale=1.0 / 64), w=[ya], r=[ps_])
                    p.V(lambda e: e.reciprocal(out=ya[:], in_=ya[:]), w=[ya], r=[ya])
                    p.V(lambda e: e.tensor_tensor(out=yb[:], in0=yb[:], in1=ya[:], op=ALU.mult), w=[yb], r=[yb, ya])
                    p.V(lambda e: e.tensor_tensor(out=yb[:], in0=yb[:], in1=bc(4), op=ALU.mult), w=[yb], r=[yb, vec])
                    p.V(lambda e: e.tensor_tensor(out=yb[:], in0=yb[:], in1=bc(5), op=ALU.add), w=[yb], r=[yb, vec])
                    p.V(lambda e: e.tensor_tensor(out=yb[:], in0=yb[:], in1=bo[:], op=ALU.add), w=[yb], r=[yb, bo])
                    p.V(lambda e: e.tensor_tensor(out=zT[:, :, :n], in0=yb[:], in1=gg[:], op=ALU.mult), w=[zT], r=[yb, gg])
                    branch_out(bt, zT, 8, 64, wproj, 1, b0, n)
                p.barrier()


        def stage_merge(l):
            with ExitStack() as st:
                wo = load_w_bf(st, I["w_out"][l].rearrange("(k p) c -> p k c", p=128), 8, D, "wo")
                ya = [p.sb("mya", [128, 8, 512], es=st) for _ in range(3)]
                mT = p.sb("mT", [128, 8, 512], BF16, es=st)
                xb = p.sb("mxb", [128, 8, 512], es=st)
                po = [p.ps("mpo", [128, 512], es=st) for _ in range(2)]
                blks = BLKS if l == 0 else BLKS[1:]
                for (b0, n, seg) in blks:
                    for i in range(3):
                        p.dma(ya[i][:, :, :n], YG[i][:, b0:b0 + n].rearrange("(kc p) t -> p kc t", p=128), w=[ya[i]], r=[YG[i]], q=("sp" if i != 1 else "pool"))
                    p.dma(xb[:, :, :n], X[:, b0:b0 + n].rearrange("(kc p) t -> p kc t", p=128), w=[xb], r=[X], q="pool")
                    p.V(lambda e: e.tensor_tensor(out=ya[0][:, :, :n], in0=ya[0][:, :, :n], in1=ya[1][:, :, :n], op=ALU.add), w=[ya[0]], r=[ya[0], ya[1]])
                    p.V(lambda e: e.tensor_tensor(out=mT[:, :, :n], in0=ya[0][:, :, :n], in1=ya[2][:, :, :n], op=ALU.add), w=[mT], r=[ya[0], ya[2]])
                    for oc in range(8):
                        po_ = po[oc % 2]
                        for kc in range(8):
                            p.T(lambda e, kc=kc, oc=oc, po_=po_: e.matmul(po_[:, :n], lhsT=wo[:, kc, oc * 128:(oc + 1) * 128], rhs=mT[:, kc, :n],
                                                                         start=(kc == 0), stop=(kc == 7)), w=[po_], r=[wo, mT])
                        p.V(lambda e, oc=oc, po_=po_: e.scalar_tensor_tensor(out=xb[:, oc, :n], in0=po_[:, :n], scalar=mod[l][:, 16 + oc, seg:seg + 1], in1=xb[:, oc, :n],
                                                                            op0=ALU.mult, op1=ALU.add), w=[xb], r=[po_, mod[l], xb])
                    p.dma(X[:, b0:b0 + n].rearrange("(kc p) t -> p kc t", p=128), xb[:, :, :n], w=[X], r=[xb])
                p.barrier()

        def stage_ffn(l):
            moe = (l % 2 == 1)
            blks = BLKS[1:] if moe else BLKS
            nexp = NE if moe else 1
            UTs = p.dram("UT%d" % l, [nexp, FF, T], BF16)
            with ExitStack() as st:
                hT = p.sb("hTf", [128, 8, T], BF16, es=st)
                combT = p.sb("combT", [8, TL], es=st) if moe else None
                with ExitStack() as st2:
                    nt = alloc_norm_tiles(st2)
                    m = mod[l]
                    if moe:
                        h32 = p.sb("h32", [128, 8, 512], es=st2)
                        rt = p.sb("rt", [128, 8, NE], es=st2)
                        p.dma(rt[:], I["moe_router"][0].rearrange("(kc p) e -> p kc e", p=128), w=[rt], r=[I["moe_router"]])
                        lg = p.sb("lg", [128, 16, NE], es=st2)
                        plg = p.ps("plg", [128, NE], es=st2)
                    for bi, (b0, n, seg) in enumerate(blks):
                        norm_block(nt, b0, n, lambda kc, seg=seg: gsf[l][:, kc, seg:seg + 1], lambda kc, seg=seg: m[:, 24 + kc, seg:seg + 1], hT,
                                   h32=(h32 if moe else None))
                        if moe:
                            for j in range(4):
                                for kc in range(8):
                                    p.T(lambda e, kc=kc, j=j: e.matmul(plg[:], lhsT=h32[:, kc, j * 128:(j + 1) * 128], rhs=rt[:, kc, :], start=(kc == 0), stop=(kc == 7)),
                                        w=[plg], r=[h32, rt])
                                p.V(lambda e, j=j, bi=bi: e.tensor_copy(out=lg[:, bi * 4 + j, :], in_=plg[:]), w=[lg], r=[plg])
                    if moe:
                        m1 = p.sb("m1", [128, 16], es=st2); eq = p.sb("eq", [128, 16, NE], es=st2); l2 = p.sb("l2", [128, 16, NE], es=st2)
                        m2 = p.sb("m2", [128, 16], es=st2); ex = p.sb("exr", [128, 16, NE], es=st2); sm = p.sb("sm", [128, 16], es=st2)
                        b3 = lambda t_: t_[:].unsqueeze(2).broadcast_to([128, 16, NE])
                        p.V(lambda e: e.reduce_max(out=m1[:], in_=lg[:], axis=AX.X), w=[m1], r=[lg])
                        p.V(lambda e: e.tensor_tensor(out=eq[:], in0=lg[:], in1=b3(m1), op=ALU.is_equal), w=[eq], r=[lg, m1])
                        p.V(lambda e: e.scalar_tensor_tensor(out=l2[:], in0=eq[:], scalar=-1e30, in1=lg[:], op0=ALU.mult, op1=ALU.add), w=[l2], r=[eq, lg])
                        p.V(lambda e: e.reduce_max(out=m2[:], in_=l2[:], axis=AX.X), w=[m2], r=[l2])
                        p.V(lambda e: e.tensor_tensor(out=eq[:], in0=lg[:], in1=b3(m2), op=ALU.is_ge), w=[eq], r=[lg, m2])
                        p.V(lambda e: e.tensor_tensor(out=l2[:], in0=lg[:], in1=b3(m1), op=ALU.subtract), w=[l2], r=[lg, m1])
                        p.A(lambda e: e.activation(out=ex[:], in_=l2[:], func=AF.Exp), w=[ex], r=[l2])
                        p.V(lambda e: e.tensor_tensor(out=ex[:], in0=ex[:], in1=eq[:], op=ALU.mult), w=[ex], r=[ex, eq])
                        p.V(lambda e: e.reduce_sum(out=sm[:], in_=ex[:], axis=AX.X), w=[sm], r=[ex])
                        p.V(lambda e: e.reciprocal(out=sm[:], in_=sm[:]), w=[sm], r=[sm])
                        p.V(lambda e: e.tensor_tensor(out=ex[:], in0=ex[:], in1=b3(sm), op=ALU.mult), w=[ex], r=[ex, sm])
                        pct = p.ps("pct", [8, 512], es=st2)
                        for g4 in range(4):
                            for j in range(4):
                                p.T(lambda e, g4=g4, j=j: e.transpose(pct[:, j * 128:(j + 1) * 128], ex[:, g4 * 4 + j, :], ident[:]), w=[pct], r=[ex, ident])
                            p.V(lambda e, g4=g4: e.tensor_copy(out=combT[:, g4 * 512:(g4 + 1) * 512], in_=pct[:]), w=[combT], r=[pct])
                    p.barrier()
                if "comb" in dbg and moe:
                    cdd = p.dram("dbg_comb", [8, TL], F32, kind="ExternalOutput")
                    p.dma(cdd[:], combT[:], w=[cdd], r=[combT])
                with ExitStack() as st2:
                    wf1 = [p.sb("wf1", [128, 8, 512], es=st2) for _ in range(2)]; wf3 = [p.sb("wf3", [128, 8, 512], es=st2) for _ in range(2)]
                    wb1 = [p.sb("wb1", [128, 8, 512], BF16, es=st2) for _ in range(2)]; wb3 = [p.sb("wb3", [128, 8, 512], BF16, es=st2) for _ in range(2)]
                    pa = [p.ps("pa", [128, 512], es=st2) for _ in range(2)]; pb = [p.ps("pb", [128, 512], es=st2) for _ in range(2)]
                    pcb = p.ps("pcb", [128, 512], es=st2)
                    sl = [p.sb("sl", [128, 512], es=st2) for _ in range(2)]; ut = [p.sb("ut", [128, 512], BF16, es=st2) for _ in range(2)]
                    sel = p.sb("sel8", [8, 8, 128], es=st2)
                    p.dma(sel[:], I["sel8"][:].rearrange("e k m -> k e m"), w=[sel], r=[I["sel8"]])
                    cbc = p.sb("cbc", [128, TL], es=st2) if moe else None
                    gi = 0; ui = 0
                    for ex_ in range(nexp):
                        w1s = I["moe_w1"][0, ex_] if moe else I["ffn_w1"][0]
                        w3s = I["moe_w3"][0, ex_] if moe else I["ffn_w3"][0]
                        if moe:
                            for g4 in range(4):
                                p.T(lambda e, g4=g4, ex_=ex_: e.matmul(pcb[:], lhsT=sel[:, ex_, :], rhs=combT[:, g4 * 512:(g4 + 1) * 512], start=True, stop=True), w=[pcb], r=[sel, combT])
                                p.A(lambda e, g4=g4: e.copy(out=cbc[:, g4 * 512:(g4 + 1) * 512], in_=pcb[:]), w=[cbc], r=[pcb])
                        for fg in range(6):
                            f0 = fg * 512
                            nf = min(512, FF - f0)
                            a1, a3, c1, c3 = wf1[gi % 2], wf3[gi % 2], wb1[gi % 2], wb3[gi % 2]
                            gi += 1
                            p.dma(a1[:, :, :nf], w1s[:, f0:f0 + nf].rearrange("(kc p) c -> p kc c", p=128), w=[a1], r=[], q="pool")
                            p.dma(a3[:, :, :nf], w3s[:, f0:f0 + nf].rearrange("(kc p) c -> p kc c", p=128), w=[a3], r=[], q="pool")
                            for kc in range(8):
                                p.V(lambda e, kc=kc: e.tensor_copy(out=c1[:, kc, :nf], in_=a1[:, kc, :nf]), w=[c1], r=[a1])
                                p.A(lambda e, kc=kc: e.copy(out=c3[:, kc, :nf], in_=a3[:, kc, :nf]), w=[c3], r=[a3])
                            for sub in range(nf // 128):
                                fb = fg * 4 + sub
                                for (b0, n, seg) in blks:
                                    pa_, pb_, sl_, ut_ = pa[ui % 2], pb[ui % 2], sl[ui % 2], ut[ui % 2]
                                    ui += 1
                                    for kc in range(8):
                                        p.T(lambda e, kc=kc, sub=sub, pa_=pa_: e.matmul(pa_[:, :n], lhsT=c1[:, kc, sub * 128:(sub + 1) * 128], rhs=hT[:, kc, b0:b0 + n],
                                                                                       start=(kc == 0), stop=(kc == 7)), w=[pa_], r=[c1, hT])
                                    for kc in range(8):
                                        p.T(lambda e, kc=kc, sub=sub, pb_=pb_: e.matmul(pb_[:, :n], lhsT=c3[:, kc, sub * 128:(sub + 1) * 128], rhs=hT[:, kc, b0:b0 + n],
                                                                                       start=(kc == 0), stop=(kc == 7)), w=[pb_], r=[c3, hT])
                                    p.A(lambda e, pa_=pa_, sl_=sl_: e.activation(out=sl_[:, :n], in_=pa_[:, :n], func=AF.Silu), w=[sl_], r=[pa_])
                                    if moe:
                                        p.V(lambda e, sl_=sl_, b0=b0: e.tensor_tensor(out=sl_[:, :n], in0=sl_[:, :n], in1=cbc[:, b0 - TC:b0 - TC + n], op=ALU.mult), w=[sl_], r=[sl_, cbc])
                                    p.V(lambda e, pb_=pb_, sl_=sl_, ut_=ut_: e.tensor_tensor(out=ut_[:, :n], in0=sl_[:, :n], in1=pb_[:, :n], op=ALU.mult), w=[ut_], r=[sl_, pb_])
                                    p.dma(UTs[ex_, fb * 128:(fb + 1) * 128, b0:b0 + n], ut_[:, :n], w=[UTs], r=[ut_])
                    p.barrier()
            with ExitStack() as st:
                w2f = [p.sb("w2f", [128, 2, D], es=st) for _ in range(2)]
                w2bs = [p.sb("w2b", [128, 22, D], BF16, es=st) for _ in range(2 if nexp > 1 else 1)]
                uu = [p.sb("uu", [128, 22, 512], BF16, es=st) for _ in range(2)]
                acc = p.sb("facc", [128, 8, 512], es=st); xb = p.sb("fxb", [128, 8, 512], es=st)
                po = [p.ps("fpo", [128, 512], es=st) for _ in range(4)]
                ui = 0; oi = 0

                def load_w2(ex_):
                    w2s = I["moe_w2"][0, ex_] if moe else I["ffn_w2"][0]
                    w2b_ = w2bs[ex_ % len(w2bs)]
                    for f2_ in range(11):
                        wf_ = w2f[f2_ % 2]
                        p.dma(wf_[:], w2s[f2_ * 256:(f2_ + 1) * 256, :].rearrange("(k p) c -> p k c", p=128), w=[wf_], r=[], q="pool")
                        p.V(lambda e, f2_=f2_, wf_=wf_: e.tensor_copy(out=w2b_[:, 2 * f2_, :], in_=wf_[:, 0, :]), w=[w2b_], r=[wf_])
                        p.A(lambda e, f2_=f2_, wf_=wf_: e.copy(out=w2b_[:, 2 * f2_ + 1, :], in_=wf_[:, 1, :]), w=[w2b_], r=[wf_])
                load_w2(0)
                for ex_ in range(nexp):
                    w2b = w2bs[ex_ % len(w2bs)]
                    if ex_ + 1 < nexp:
                        load_w2(ex_ + 1)
                    for (b0, n, seg) in blks:
                        u_ = uu[ui % 2]; ui += 1
                        p.dma(u_[:, :, :n], UTs[ex_, :, b0:b0 + n].rearrange("(k p) t -> p k t", p=128), w=[u_], r=[UTs], q="pool")
                        if ex_ > 0:
                            p.dma(acc[:, :, :n], FACC[:, b0:b0 + n].rearrange("(kc p) t -> p kc t", p=128), w=[acc], r=[FACC])
                        last = (ex_ == nexp - 1)
                        if last:
                            p.dma(xb[:, :, :n], X[:, b0:b0 + n].rearrange("(kc p) t -> p kc t", p=128), w=[xb], r=[X])
                        for oc in range(8):
                            po_ = po[oi % 4]; oi += 1
                            for k in range(22):
                                p.T(lambda e, k=k, oc=oc, po_=po_, u_=u_: e.matmul(po_[:, :n], lhsT=w2b[:, k, oc * 128:(oc + 1) * 128], rhs=u_[:, k, :n],
                                                                                  start=(k == 0), stop=(k == 21)), w=[po_], r=[w2b, u_])
                            if ex_ == 0:
                                p.A(lambda e, oc=oc, po_=po_: e.copy(out=acc[:, oc, :n], in_=po_[:, :n]), w=[acc], r=[po_])
                            else:
                                p.V(lambda e, oc=oc, po_=po_: e.tensor_tensor(out=acc[:, oc, :n], in0=acc[:, oc, :n], in1=po_[:, :n], op=ALU.add), w=[acc], r=[acc, po_])
                            if last:
                                p.V(lambda e, oc=oc: e.scalar_tensor_tensor(out=xb[:, oc, :n], in0=acc[:, oc, :n], scalar=mod[l][:, 40 + oc, seg:seg + 1], in1=xb[:, oc, :n],
                                                                           op0=ALU.mult, op1=ALU.add), w=[xb], r=[acc, mod[l], xb])
                        if last:
                            p.dma(X[:, b0:b0 + n].rearrange("(kc p) t -> p kc t", p=128), xb[:, :, :n], w=[X], r=[xb])
                        else:
                            p.dma(FACC[:, b0:b0 + n].rearrange("(kc p) t -> p kc t", p=128), acc[:, :, :n], w=[FACC], r=[acc])
                p.barrier()

        def stage_final():
            with ExitStack() as st:
                nt = alloc_norm_tiles(st)
                xb, sq, pss, rstd, tmp = nt
                for (b0, n, seg) in BLKS[1:]:
                    norm_block(nt, b0, n, lambda kc: fng[:, kc:kc + 1], None, None)
                    p.dma(outT[:, b0 - TC:b0 - TC + n].rearrange("(kc p) t -> p kc t", p=128), tmp[:, :, :n], w=[outT], r=[tmp])
                p.barrier()

        for l in range(n_layers):
            stage_ada(l)
        if "mod" in dbg:
            md = p.dram("dbg_mod", [2, 128, 96], F32, kind="ExternalOutput")
            for l in range(2):
                p.dma(md[l], mod[l][:].rearrange("p a b -> p (a b)"), w=[md], r=[mod[l]])
        for l in range(n_layers):
            stage_proj(l)
            if stop_after == ("proj", l):
                break
            if "hg" not in skip:
                stage_hg(l)
            if stop_after == ("hg", l):
                break
            if "da" not in skip:
                stage_da(l)
            if stop_after == ("da", l):
                break
            if "rw" not in skip:
                stage_rw_prep(l)
                stage_rw_scan(l)
                stage_rw_fin(l)
            if stop_after == ("rw", l):
                break
            if "merge" not in skip:
                stage_merge(l)
            if stop_after == ("merge", l):
                break
            if "ffn" not in skip:
                stage_ffn(l)
            if stop_after == ("ffn", l):
                break
        if stop_after is None:
            stage_final()
        if "X" in dbg:
            xd = p.dram("dbg_X", [D, T], F32, kind="ExternalOutput")
            p.dma(xd[:], X[:], w=[xd], r=[X])
        if "YG1" in dbg:
            yd = p.dram("dbg_YG1", [D, T], F32, kind="ExternalOutput")
            p.dma(yd[:], YG[1][:], w=[yd], r=[YG[1]])
            for nm in RWN:
                dd = p.dram("dbg_RWS_" + nm, [64, 8, T], F32, kind="ExternalOutput")
                p.dma(dd[:], RWS[nm][:], w=[dd], r=[RWS[nm]])
            for d in range(2):
                dd = p.dram("dbg_YRW%d" % d, [64, 8, T], F32, kind="ExternalOutput")
                p.dma(dd[:], YRW[d][:], w=[dd], r=[YRW[d]])
        if "YG2" in dbg:
            yd = p.dram("dbg_YG2", [D, T], F32, kind="ExternalOutput")
            p.dma(yd[:], YG[2][:], w=[yd], r=[YG[2]])
        if "YG0" in dbg:
            yd = p.dram("dbg_YG0", [D, T], F32, kind="ExternalOutput")
            p.dma(yd[:], YG[0][:], w=[yd], r=[YG[0]])
            od = p.dram("dbg_OHG", [2, 512, T], F32, kind="ExternalOutput")
            p.dma(od[:], OHG[:], w=[od], r=[OHG])
        if "PT" in dbg:
            pd = p.dram("dbg_PT", [INC, T], F32, kind="ExternalOutput")
            p.dma(pd[:], PT[:], w=[pd], r=[PT])
            vd = p.dram("dbg_VHG", [T, 512], F32, kind="ExternalOutput")
            p.dma(vd[:], VHG[:], w=[vd], r=[VHG])
        p.barrier()
        print("instrs", p.ninstr, "waits", p.nwait)
    return nc


def host_inputs(inputs, b):
    f = np.float32
    g = {}
    x = np.asarray(inputs["x"][b], f); ctx = np.asarray(inputs["ctx"][b], f)
    g["xT"] = np.ascontiguousarray(np.concatenate([ctx.T, x.T], axis=1))
    c2 = np.stack([np.asarray(inputs["c"][b], f), np.asarray(inputs["c_ctx"], f)], axis=-1)
    g["c2"] = np.ascontiguousarray(c2.reshape(8, 128, 2).transpose(1, 0, 2))
    return g


def shared_inputs(inputs):
    f = np.float32
    g = {}
    A = lambda k: np.asarray(inputs[k], f)
    g["ada_w"] = A("ada_w")
    g["ada_bT"] = np.ascontiguousarray(A("ada_b").reshape(2, 48, 128).transpose(0, 2, 1))
    g["nmgT"] = np.ascontiguousarray(A("norm_mix_g").reshape(2, 8, 128).transpose(0, 2, 1))
    g["nfgT"] = np.ascontiguousarray(A("norm_ffn_g").reshape(2, 8, 128).transpose(0, 2, 1))
    g["fngT"] = np.ascontiguousarray(A("final_norm_g").reshape(8, 128).T)
    g["w_in"] = A("w_in")
    mu_full = np.zeros((2, 71 * 128), f)
    mu_full[:, RW0:DA0] = A("rw_mu")
    g["muT"] = np.ascontiguousarray(mu_full.reshape(2, 71, 128).transpose(0, 2, 1))
    g["ident"] = np.eye(128, dtype=f)
    i = np.arange(64)[:, None]; j = np.arange(64)[None, :]
    g["masks"] = np.ascontiguousarray(np.stack([(i < j), (i <= j), (i > j), (i >= j)], axis=1).astype(f))
    g["hg_lbT"] = np.ascontiguousarray(A("hg_lb_logits").reshape(2, 2, 4, 128).transpose(0, 1, 3, 2))
    g["hg_ngT"] = np.ascontiguousarray(A("hg_norm_g").reshape(2, 128, 1))
    g["hg_proj"] = A("hg_proj")
    g["w_out"] = A("w_out")
    for k in ("ffn_w1", "ffn_w3", "ffn_w2", "moe_router", "moe_w1", "moe_w3", "moe_w2", "da_proj", "rw_w2", "rw_a2", "rw_g2", "rw_proj"):
        g[k] = A(k)
    sel = np.zeros((8, 8, 128), f)
    for e in range(8):
        sel[e, e, :] = 1.0
    g["sel8"] = sel
    g["da_lambda"] = A("da_lambda").reshape(2, 256)
    g["da_sg"] = A("da_subln_g")
    t = np.arange(TL)
    rowi = (t // 64).astype(f); coli = (t % 64).astype(f)
    inv_freq = (1.0 / (10000.0 ** (np.arange(0, 32, 2, dtype=f) / f(32)))).astype(f)
    ang = np.zeros((64, TL), f)
    for d in range(64):
        jj = d % 16
        ang[d] = (rowi if d < 32 else coli) * inv_freq[jj]
    g["ropeCS"] = np.ascontiguousarray(np.stack([np.cos(ang), np.sin(ang)], axis=1).astype(f))
    R = np.zeros((64, 64), f)
    for base in (0, 32):
        for q in range(16):
            R[base + q, base + 16 + q] = -1.0
            R[base + 16 + q, base + q] = 1.0
    g["ropeR"] = np.ascontiguousarray(R.T)
    vec = np.zeros((2, 8, 512), f)
    vec[:, 0] = A("rw_k_k"); vec[:, 1] = A("rw_k_a"); vec[:, 2] = A("rw_a0"); vec[:, 3] = A("rw_r_k").reshape(2, 512)
    vec[:, 4] = A("rw_ln_g"); vec[:, 5] = A("rw_ln_b")
    g["rw_vecT"] = np.ascontiguousarray(vec.reshape(2, 8, 8, 64).transpose(0, 3, 1, 2))
    g["rw_w0T"] = np.ascontiguousarray(A("rw_w0").reshape(2, 2, 8, 64).transpose(0, 1, 3, 2))
    return g


_NC_CACHE = {}


def kernel(**inputs):
    if "nc" not in _NC_CACHE:
        _NC_CACHE["nc"] = build()
    nc = _NC_CACHE["nc"]
    sh = shared_inputs(inputs)
    in_maps = []
    for b in range(8):
        m = dict(sh)
        m.update(host_inputs(inputs, b))
        in_maps.append(m)
    res = run_bass_kernel_spmd(nc, in_maps, core_ids=list(range(8)))
    out = np.stack([np.ascontiguousarray(r["outT"].T) for r in res.results], axis=0)
    return out.astype(np.float32)
```

```python
import math
import numpy as np
from contextlib import ExitStack
import concourse.bass as bass
import concourse.mybir as mybir
from concourse.bass_utils import run_bass_kernel_spmd

F32 = mybir.dt.float32
BF16 = mybir.dt.bfloat16
AF = mybir.ActivationFunctionType
ALU = mybir.AluOpType
AX = mybir.AxisListType

SAME_ENGINE_SYNC = True
N_DMA_SEMS = 40
SEM_EPOCH = 16000

T = 2304
TC = 256
TL = 2048
D = 1024
INC = 9024
FF = 2816
NE = 8
BLKS = [(0, 256, 1), (256, 512, 0), (768, 512, 0), (1280, 512, 0), (1792, 512, 0)]
HG0 = 0
RW0 = 2560
DA0 = 4416
GT0 = 5952
RW_R, RW_K, RW_V, RW_WF, RW_WB, RW_AD, RW_GD = RW0, RW0 + 512, RW0 + 1024, RW0 + 1536, RW0 + 1600, RW0 + 1664, RW0 + 1728


class Buf:
    def __init__(self, name, h):
        self.name = name
        self.h = h
        self.w = None
        self.r = {}

    def __getitem__(self, idx):
        return self.h[idx]


class Prog:
    ENG = ("pe", "act", "dve", "pool", "sp")

    def __init__(self, nc, es):
        self.nc = nc
        self.es = es
        self.e = dict(pe=nc.tensor, act=nc.scalar, dve=nc.vector, pool=nc.gpsimd, sp=nc.sync)
        self.sem = {}
        self.ekey = {}
        for k in self.ENG:
            self.ekey[k] = (k, 0)
            self.sem[(k, 0)] = es.enter_context(nc.semaphore("s_" + k))
        self.cnt = {k: 0 for k in self.ENG}
        self.dsem = [es.enter_context(nc.semaphore("d%d" % i)) for i in range(N_DMA_SEMS)]
        for i in range(N_DMA_SEMS):
            self.sem[("d", i)] = self.dsem[i]
        self.dval = [0] * N_DMA_SEMS
        self.dnext = 0
        self.seen = {k: {} for k in self.ENG}
        self.ninstr = {k: 0 for k in self.ENG}
        self.nwait = 0
        self.uid = 0

    def sb(self, name, shape, dtype=F32, es=None):
        self.uid += 1
        h = (es or self.es).enter_context(self.nc.sbuf_tensor("%s_%d" % (name, self.uid), list(shape), dtype))
        return Buf(name, h)

    def ps(self, name, shape, dtype=F32, es=None):
        self.uid += 1
        h = (es or self.es).enter_context(self.nc.psum_tensor("%s_%d" % (name, self.uid), list(shape), dtype))
        return Buf(name, h)

    def dram(self, name, shape, dtype=F32, kind="Internal"):
        h = self.nc.dram_tensor(name, list(shape), dtype, kind=kind)
        return Buf(name, h.ap())

    def _wait(self, ek, k, v):
        if self.seen[ek].get(k, 0) >= v:
            return
        self.e[ek].wait_ge(self.sem[k], v)
        self.seen[ek][k] = v
        self.nwait += 1

    def _deps(self, ek, r, w):
        deps = {}

        def add(ev):
            if ev is None:
                return
            k, v = ev
            if deps.get(k, 0) < v:
                deps[k] = v

        for b in r:
            add(b.w)
        for b in w:
            add(b.w)
            for k, v in b.r.items():
                add((k, v))
        for k, v in deps.items():
            if k[0] == ek and (not SAME_ENGINE_SYNC or ek in ("pe", "sp")):
                continue
            self._wait(ek, k, v)

    def _commit(self, ev, r, w):
        k, v = ev
        for b in w:
            b.w = ev
            b.r = {}
        for b in r:
            if b.r.get(k, 0) < v:
                b.r[k] = v

    def op(self, ek, fn, w=(), r=()):
        if self.cnt[ek] >= SEM_EPOCH:
            ep = self.ekey[ek][1] + 1
            self.ekey[ek] = (ek, ep)
            self.sem[(ek, ep)] = self.es.enter_context(self.nc.semaphore("s_%s_%d" % (ek, ep)))
            self.cnt[ek] = 0
        self._deps(ek, r, w)
        ins = fn(self.e[ek])
        self.cnt[ek] += 1
        key = self.ekey[ek]
        ins.then_inc(self.sem[key], 1)
        self.ninstr[ek] += 1
        self._commit((key, self.cnt[ek]), r, w)
        return ins

    def V(self, fn, w=(), r=()):
        return self.op("dve", fn, w, r)

    def A(self, fn, w=(), r=()):
        return self.op("act", fn, w, r)

    def G(self, fn, w=(), r=()):
        return self.op("pool", fn, w, r)

    def T(self, fn, w=(), r=()):
        return self.op("pe", fn, w, r)

    def dma(self, out, in_, w=(), r=(), q="sp", **kw):
        i = self.dnext
        self.dnext = (self.dnext + 1) % N_DMA_SEMS
        key = ("d", i)
        if self.dval[i] > 0:
            self._wait(q, key, self.dval[i])
        self._deps(q, r, w)
        ins = self.e[q].dma_start(out=out, in_=in_, **kw)
        self.dval[i] += 16
        ins.then_inc(self.dsem[i], 16)
        self.ninstr[q] += 1
        self._commit((key, self.dval[i]), r, w)
        return ins

    def barrier(self):
        for ek in self.ENG:
            for k in self.ENG:
                if k == ek:
                    continue
                key = self.ekey[k]
                if key[1] > 0:
                    self._wait(ek, (k, key[1] - 1), SEM_EPOCH)
                if self.cnt[k] > 0:
                    self._wait(ek, key, self.cnt[k])
            for i in range(N_DMA_SEMS):
                if self.dval[i] > 0:
                    self._wait(ek, ("d", i), self.dval[i])


def build(n_layers=2, dbg=(), stop_after=None, skip=()):
    nc = bass.Bass("TRN2", target_bir_lowering=False)
    with ExitStack() as es:
        p = Prog(nc, es)
        I = {}

        def inp(name, shape, dt=F32):
            I[name] = p.dram(name, shape, dt, kind="ExternalInput")
            return I[name]

        inp("xT", [D, T]); inp("c2", [128, 8, 2])
        inp("ada_w", [2, D, 6 * D]); inp("ada_bT", [2, 128, 48])
        inp("nmgT", [2, 128, 8]); inp("nfgT", [2, 128, 8]); inp("fngT", [128, 8])
        inp("w_in", [2, D, INC]); inp("muT", [2, 128, 71])
        inp("ident", [128, 128]); inp("masks", [64, 4, 64])
        inp("hg_lbT", [2, 2, 128, 4]); inp("hg_ngT", [2, 128, 1]); inp("hg_proj", [2, 512, D])
        inp("w_out", [2, D, D])
        inp("ffn_w1", [1, D, FF]); inp("ffn_w3", [1, D, FF]); inp("ffn_w2", [1, FF, D])
        inp("moe_router", [1, D, NE]); inp("moe_w1", [1, NE, D, FF]); inp("moe_w3", [1, NE, D, FF]); inp("moe_w2", [1, NE, FF, D])
        inp("sel8", [8, 8, 128])
        inp("da_lambda", [2, 256]); inp("da_sg", [2, 128]); inp("da_proj", [2, 512, D])
        inp("ropeCS", [64, 2, TL]); inp("ropeR", [64, 64])
        inp("rw_vecT", [2, 64, 8, 8]); inp("rw_w0T", [2, 2, 64, 8]); inp("rw_w2", [2, 2, 64, 512]); inp("rw_a2", [2, 64, 512])
        inp("rw_g2", [2, 128, 512]); inp("rw_proj", [2, 512, D])
        outT = p.dram("outT", [D, TL], F32, kind="ExternalOutput")

        X = p.dram("Xs", [D, T])
        PT = p.dram("PT", [INC, T])
        VHG = p.dram("VHG", [T, 512])
        VDA = p.dram("VDA", [T, 512])
        OHG = p.dram("OHG", [2, 512, T])
        YG = [p.dram("YG%d" % i, [D, T]) for i in range(3)]
        UT = p.dram("UT", [FF, T], BF16)
        FACC = p.dram("FACC", [D, T])
        dbg_out = {}

        ident = p.sb("ident", [128, 128]); p.dma(ident[:], I["ident"][:], w=[ident], r=[I["ident"]])
        masks = p.sb("masks", [64, 4, 64]); p.dma(masks[:], I["masks"][:], w=[masks], r=[I["masks"]])
        ones_bf = p.sb("ones_bf", [128, 128], BF16); p.V(lambda e: e.memset(ones_bf[:], 1.0), w=[ones_bf])
        ones_f = p.sb("ones_f", [128, 128]); p.V(lambda e: e.memset(ones_f[:], 1.0), w=[ones_f])
        ident_bf = p.sb("ident_bf", [128, 128], BF16); p.V(lambda e: e.tensor_copy(out=ident_bf[:], in_=ident[:]), w=[ident_bf], r=[ident])
        sc = p.sb("sc", [128, 8, 2]); p.dma(sc[:], I["c2"][:], w=[sc], r=[I["c2"]])
        p.A(lambda e: e.activation(out=sc[:], in_=sc[:], func=AF.Silu), w=[sc], r=[sc])
        mod = [p.sb("mod%d" % l, [128, 48, 2]) for l in range(2)]
        gsm = [p.sb("gsm%d" % l, [128, 8, 2]) for l in range(2)]
        gsf = [p.sb("gsf%d" % l, [128, 8, 2]) for l in range(2)]
        fng = p.sb("fng", [128, 8]); p.dma(fng[:], I["fngT"][:], w=[fng], r=[I["fngT"]])

        p.dma(X[:], I["xT"][:], w=[X], r=[I["xT"]])

        def stage_ada(l):
            with ExitStack() as st:
                wt = [p.sb("adaw", [128, 8, 512], es=st) for _ in range(2)]
                adab = p.sb("adab", [128, 48], es=st)
                ng = p.sb("ng", [128, 8], es=st); nf = p.sb("nf", [128, 8], es=st)
                pm = p.ps("pmod", [128, 48, 2], es=st)
                p.dma(adab[:], I["ada_bT"][l], w=[adab], r=[I["ada_bT"]])
                p.dma(ng[:], I["nmgT"][l], w=[ng], r=[I["nmgT"]])
                p.dma(nf[:], I["nfgT"][l], w=[nf], r=[I["nfgT"]])
                for cb in range(12):
                    w_ = wt[cb % 2]
                    p.dma(w_[:], I["ada_w"][l, :, cb * 512:(cb + 1) * 512].rearrange("(kc p) c -> p kc c", p=128),
                          w=[w_], r=[I["ada_w"]], q=("sp" if cb % 2 == 0 else "pool"))
                    for sub in range(4):
                        cc = cb * 4 + sub
                        for kc in range(8):
                            p.T(lambda e, w_=w_, cc=cc, kc=kc, sub=sub: e.matmul(pm[:, cc, :], lhsT=w_[:, kc, sub * 128:(sub + 1) * 128],
                                                                                rhs=sc[:, kc, :], start=(kc == 0), stop=(kc == 7)),
                                w=[pm], r=[w_, sc])
                m = mod[l]
                p.V(lambda e: e.tensor_tensor(out=m[:], in0=pm[:], in1=adab[:].unsqueeze(2).broadcast_to([128, 48, 2]), op=ALU.add),
                    w=[m], r=[pm, adab])
                for (gs, g, j) in ((gsm[l], ng, 1), (gsf[l], nf, 4)):
                    p.V(lambda e, gs=gs, g=g, j=j: e.scalar_tensor_tensor(out=gs[:], in0=m[:, j * 8:(j + 1) * 8, :], scalar=1.0,
                                                                         in1=g[:].unsqueeze(2).broadcast_to([128, 8, 2]),
                                                                         op0=ALU.add, op1=ALU.mult), w=[gs], r=[m, g])
                p.barrier()

        def norm_block(st_tiles, b0, n, gs_ap, sh_ap, hT, h32=None):
            xb, sq, pss, rstd, tmp = st_tiles
            p.dma(xb[:, :, :n], X[:, b0:b0 + n].rearrange("(kc p) t -> p kc t", p=128), w=[xb], r=[X])
            for kc in range(8):
                p.A(lambda e, kc=kc: e.activation(out=sq[:, kc, :n], in_=xb[:, kc, :n], func=AF.Square), w=[sq], r=[xb])
            for kc in range(8):
                p.T(lambda e, kc=kc: e.matmul(pss[:, :n], lhsT=ones_bf[:], rhs=sq[:, kc, :n], start=(kc == 0), stop=(kc == 7)),
                    w=[pss], r=[ones_bf, sq])
            p.A(lambda e: e.activation(out=rstd[:, :n], in_=pss[:, :n], func=AF.Sqrt, bias=1e-6, scale=1.0 / D), w=[rstd], r=[pss])
            p.V(lambda e: e.reciprocal(out=rstd[:, :n], in_=rstd[:, :n]), w=[rstd], r=[rstd])
            for kc in range(8):
                p.V(lambda e, kc=kc: e.scalar_tensor_tensor(out=tmp[:, kc, :n], in0=xb[:, kc, :n], scalar=gs_ap(kc), in1=rstd[:, :n],
                                                           op0=ALU.mult, op1=ALU.mult), w=[tmp], r=[xb, rstd])
                if sh_ap is not None:
                    if h32 is not None:
                        p.A(lambda e, kc=kc: e.activation(out=h32[:, kc, :n], in_=tmp[:, kc, :n], func=AF.Identity, bias=sh_ap(kc), scale=1.0),
                            w=[h32], r=[tmp])
                        p.V(lambda e, kc=kc: e.tensor_copy(out=hT[:, kc, b0:b0 + n], in_=h32[:, kc, :n]), w=[hT], r=[h32])
                    else:
                        p.A(lambda e, kc=kc: e.activation(out=hT[:, kc, b0:b0 + n], in_=tmp[:, kc, :n], func=AF.Identity, bias=sh_ap(kc), scale=1.0),
                            w=[hT], r=[tmp])

        def alloc_norm_tiles(st):
            return (p.sb("xb", [128, 8, 512], es=st), p.sb("sq", [128, 8, 512], BF16, es=st), p.ps("pss", [128, 512], es=st),
                    p.sb("rstd", [128, 512], es=st), p.sb("ntmp", [128, 8, 512], es=st))

        def stage_proj(l):
            with ExitStack() as st:
                hT = p.sb("hT", [128, 8, T], BF16, es=st)
                with ExitStack() as st2:
                    nt = alloc_norm_tiles(st2)
                    m = mod[l]
                    for (b0, n, seg) in BLKS:
                        norm_block(nt, b0, n, lambda kc, seg=seg: gsm[l][:, kc, seg:seg + 1], lambda kc, seg=seg: m[:, kc, seg:seg + 1], hT)
                    p.barrier()
                if "h" in dbg and l == dbg["h"]:
                    hd = p.dram("dbg_h", [D, T], BF16, kind="ExternalOutput")
                    p.dma(hd[:].rearrange("(kc p) t -> p kc t", p=128), hT[:], w=[hd], r=[hT])
                wf = [p.sb("wf", [128, 8, 512], es=st) for _ in range(2)]
                wb = [p.sb("wb", [128, 8, 512], BF16, es=st) for _ in range(2)]
                row = [p.sb("row", [128, T + 4], es=st) for _ in range(4)]
                tsm = p.sb("tsm", [128, T], es=st)
                mu = p.sb("mu", [128, 71], es=st); om = p.sb("om", [128, 71], es=st)
                pc = [p.ps("pc", [128, 512], es=st) for _ in range(4)]
                p.dma(mu[:], I["muT"][l], w=[mu], r=[I["muT"]])
                p.V(lambda e: e.tensor_scalar(out=om[:], in0=mu[:], scalar1=-1.0, scalar2=1.0, op0=ALU.mult, op1=ALU.add), w=[om], r=[mu])
                p.V(lambda e: e.tensor_scalar(out=mu[:], in0=mu[:], scalar1=0.5, scalar2=None, op0=ALU.mult), w=[mu], r=[mu])
                for r_ in row:
                    p.G(lambda e, r_=r_: e.memset(r_[:], 0.0), w=[r_])
                def rcol(t0):
                    return t0 + 1 if t0 < TC else t0 + 3
                ngrp = (INC + 511) // 512
                ei = 0

                def load_wg(g):
                    c0 = g * 512
                    ncg = min(512, INC - c0)
                    p.dma(wf[g % 2][:, :, :ncg], I["w_in"][l, :, c0:c0 + ncg].rearrange("(kc p) c -> p kc c", p=128), w=[wf[g % 2]], r=[I["w_in"]], q="pool")
                load_wg(0)
                for g in range(ngrp):
                    c0 = g * 512
                    ncg = min(512, INC - c0)
                    wf_, wb_ = wf[g % 2], wb[g % 2]
                    for kc in range(8):
                        if kc % 2 == 0:
                            p.V(lambda e, kc=kc: e.tensor_copy(out=wb_[:, kc, :ncg], in_=wf_[:, kc, :ncg]), w=[wb_], r=[wf_])
                        else:
                            p.A(lambda e, kc=kc: e.copy(out=wb_[:, kc, :ncg], in_=wf_[:, kc, :ncg]), w=[wb_], r=[wf_])
                    if g + 1 < ngrp:
                        load_wg(g + 1)
                    for sub in range((ncg + 127) // 128):
                        cb = g * 4 + sub
                        ncol = min(128, ncg - sub * 128)
                        rw_ = row[cb % 4]
                        for bi, (b0, n, seg) in enumerate(BLKS):
                            ps_ = pc[ei % 4]
                            for kc in range(8):
                                p.T(lambda e, kc=kc, ps_=ps_, n=n, b0=b0, sub=sub, ncol=ncol: e.matmul(
                                    ps_[:ncol, :n], lhsT=wb_[:, kc, sub * 128:sub * 128 + ncol], rhs=hT[:, kc, b0:b0 + n],
                                    start=(kc == 0), stop=(kc == 7)), w=[ps_], r=[wb_, hT])
                            rc = rcol(b0)
                            if ei % 2 == 0:
                                p.A(lambda e, ps_=ps_, rc=rc, n=n, ncol=ncol: e.copy(out=rw_[:ncol, rc:rc + n], in_=ps_[:ncol, :n]), w=[rw_], r=[ps_])
                            else:
                                p.V(lambda e, ps_=ps_, rc=rc, n=n, ncol=ncol: e.tensor_copy(out=rw_[:ncol, rc:rc + n], in_=ps_[:ncol, :n]), w=[rw_], r=[ps_])
                            ei += 1
                        col0 = cb * 128
                        if RW0 // 128 <= cb <= (DA0 - 1) // 128:
                            for (t0, n) in ((0, TC), (TC, TL)):
                                rc = rcol(t0)
                                p.V(lambda e, rc=rc, n=n, t0=t0: e.tensor_tensor(out=tsm[:ncol, t0:t0 + n], in0=rw_[:ncol, rc - 1:rc - 1 + n],
                                                                                 in1=rw_[:ncol, rc + 1:rc + 1 + n], op=ALU.add), w=[tsm], r=[rw_])
                                p.A(lambda e, n=n, t0=t0, cb=cb: e.activation(out=tsm[:ncol, t0:t0 + n], in_=tsm[:ncol, t0:t0 + n], func=AF.Copy,
                                                                               scale=mu[:ncol, cb:cb + 1]), w=[tsm], r=[tsm, mu])
                                p.V(lambda e, rc=rc, n=n, t0=t0, cb=cb: e.scalar_tensor_tensor(out=tsm[:ncol, t0:t0 + n], in0=rw_[:ncol, rc:rc + n],
                                                                                               scalar=om[:ncol, cb:cb + 1], in1=tsm[:ncol, t0:t0 + n],
                                                                                               op0=ALU.mult, op1=ALU.add), w=[tsm], r=[rw_, om, tsm])
                            p.dma(PT[col0:col0 + ncol, :], tsm[:ncol, :], w=[PT], r=[tsm])
                        else:
                            p.dma(PT[col0:col0 + ncol, 0:TC], rw_[:ncol, 1:1 + TC], w=[PT], r=[rw_])
                            p.dma(PT[col0:col0 + ncol, TC:T], rw_[:ncol, TC + 3:T + 3], w=[PT], r=[rw_])
                for (cstart, dst) in ((HG0 + 1536, VHG), (DA0 + 1024, VDA)):
                    wf_, wb_ = wf[0], wb[0]
                    p.dma(wf_[:], I["w_in"][l, :, cstart:cstart + 512].rearrange("(kc p) c -> p kc c", p=128), w=[wf_], r=[I["w_in"]])
                    for kc in range(8):
                        p.V(lambda e, kc=kc: e.tensor_copy(out=wb_[:, kc, :], in_=wf_[:, kc, :]), w=[wb_], r=[wf_])
                    for tt in range(18):
                        ps_ = pc[tt % 4]
                        for kc in range(8):
                            p.T(lambda e, kc=kc, ps_=ps_, tt=tt: e.matmul(ps_[:, :], lhsT=hT[:, kc, tt * 128:(tt + 1) * 128], rhs=wb_[:, kc, :],
                                                                          start=(kc == 0), stop=(kc == 7)), w=[ps_], r=[wb_, hT])
                        o_ = row[tt % 4]
                        if tt % 2 == 0:
                            p.A(lambda e, ps_=ps_, o_=o_: e.copy(out=o_[:, 0:512], in_=ps_[:, :]), w=[o_], r=[ps_])
                        else:
                            p.V(lambda e, ps_=ps_, o_=o_: e.tensor_copy(out=o_[:, 0:512], in_=ps_[:, :]), w=[o_], r=[ps_])
                        p.dma(dst[tt * 128:(tt + 1) * 128, :], o_[:, 0:512], w=[dst], r=[o_])
                p.barrier()


        def load_w_bf(st, src_ap, nk, cols, name, pk=128):
            wf_ = p.sb(name + "_f", [pk, nk, cols], es=st)
            wb_ = p.sb(name + "_b", [pk, nk, cols], BF16, es=st)
            p.dma(wf_[:], src_ap, w=[wf_], r=[])
            for k in range(nk):
                p.V(lambda e, k=k: e.tensor_copy(out=wb_[:, k, :], in_=wf_[:, k, :]), w=[wb_], r=[wf_])
            return wb_

        def branch_out(bt, zT, nk, pk, wproj, gi, b0, n):
            gt, sgt, po, yo = bt
            for oc in range(8):
                p.dma(gt[:, :n], PT[GT0 + gi * 1024 + oc * 128:GT0 + gi * 1024 + (oc + 1) * 128, b0:b0 + n], w=[gt], r=[PT],
                      q=("sp" if oc % 2 == 0 else "pool"))
                p.A(lambda e: e.activation(out=sgt[:, :n], in_=gt[:, :n], func=AF.Sigmoid), w=[sgt], r=[gt])
                po_ = po[oc % 2]
                for k in range(nk):
                    p.T(lambda e, k=k, oc=oc, po_=po_: e.matmul(po_[:, :n], lhsT=wproj[:pk, k, oc * 128:(oc + 1) * 128], rhs=zT[:pk, k, :n],
                                                               start=(k == 0), stop=(k == nk - 1)), w=[po_], r=[wproj, zT])
                yo_ = yo[oc % 2]
                p.V(lambda e, po_=po_, yo_=yo_: e.tensor_tensor(out=yo_[:, :n], in0=po_[:, :n], in1=sgt[:, :n], op=ALU.mult), w=[yo_], r=[po_, sgt])
                p.dma(YG[gi][oc * 128:(oc + 1) * 128, b0:b0 + n], yo_[:, :n], w=[YG[gi]], r=[yo_], q=("pool" if oc % 2 == 0 else "sp"))

        def alloc_branch_tiles(st, npo=2):
            po = [p.ps("po", [128, 512], es=st) for _ in range(npo)]
            return (p.sb("gt", [128, 512], es=st), p.sb("sgt", [128, 512], es=st),
                    [po[i % npo] for i in range(2)], [p.sb("yo", [128, 512], es=st) for _ in range(2)])

        def stage_hg(l):
            with ExitStack() as st:
                lb = p.sb("lb", [128, 2, 4], es=st); oml = p.sb("oml", [128, 2, 4], es=st)
                if l == 0:
                    p.V(lambda e: e.memset(lb[:], 0.0), w=[lb])
                else:
                    lg0 = p.sb("lg0", [128, 2, 4], es=st)
                    p.dma(lg0[:], I["hg_lbT"][0].rearrange("d p h -> p d h"), w=[lg0], r=[I["hg_lbT"]])
                    p.dma(lb[:], I["hg_lbT"][1].rearrange("d p h -> p d h"), w=[lb], r=[I["hg_lbT"]])
                    p.V(lambda e: e.tensor_tensor(out=lb[:], in0=lb[:], in1=lg0[:], op=ALU.subtract), w=[lb], r=[lb, lg0])
                    p.A(lambda e: e.activation(out=lb[:], in_=lb[:], func=AF.Sigmoid), w=[lb], r=[lb])
                p.V(lambda e: e.tensor_scalar(out=oml[:], in0=lb[:], scalar1=-1.0, scalar2=1.0, op0=ALU.mult, op1=ALU.add), w=[oml], r=[lb])
                S = [p.sb("S", [128, 4, 128], es=st) for _ in range(2)]
                for d in range(2):
                    p.G(lambda e, d=d: e.memset(S[d][:], 0.0), w=[S[d]])
                names = ("qtl", "ktl", "qh", "kh")
                prep = [[{nm: p.sb(nm, [128, 4, 256], BF16, es=st) for nm in names} for _ in range(2)] for _ in range(2)]
                Sb = [p.sb("Sb", [128, 4, 128], BF16, es=st) for _ in range(2)]
                for d in range(2):
                    p.G(lambda e, d=d: e.memset(Sb[d][:], 0.0), w=[Sb[d]])
                Vgf = [p.sb("Vgf", [32, 8, 512], es=st) for _ in range(2)]
                ebC = [[p.sb("ebC", [128, 4, 8], es=st) for _ in range(2)] for _ in range(2)]
                Vg = [p.sb("Vg", [32, 8, 512], BF16, es=st) for _ in range(2)]
                og = [p.sb("og", [128, 4, 256], es=st) for _ in range(2)]
                tmp4 = {nm: p.sb(nm, [128, 4, 256], es=st) for nm in ("zt", "qt", "lf", "F", "E", "kg", "X", "ex")}
                ones256 = p.sb("ones256", [128, 256], es=st); p.V(lambda e: e.memset(ones256[:], 1.0), w=[ones256])
                khT = [p.sb("khT", [32, 512], BF16, es=st) for _ in range(2)]
                AT = [p.sb("AT", [32, 4, 32], BF16, es=st) for _ in range(2)]
                pT = [p.ps("pT", [32, 512], BF16, es=st) for _ in range(2)]
                pA = [p.ps("pA", [32, 4, 32], es=st) for _ in range(2)]
                pO = [p.ps("pO", [128, 4, 32], es=st) for _ in range(2)]
                pS = [p.ps("pS", [128, 4, 128], es=st) for _ in range(2)]
                bwd_groups = [0, 8, 7, 6, 5, 4, 3, 2, 1]
                ti = [0]
                Vg2 = [[Vg[d], p.sb("Vg2", [32, 8, 512], BF16, es=st)] for d in range(2)]
                og2 = [[og[d], p.sb("og2", [128, 4, 256], es=st)] for d in range(2)]

                def step_ctx(step, d):
                    g = step if d == 0 else bwd_groups[step]
                    return (g * 256, prep[d][step % 2], ebC[d][step % 2])

                def group_load(step, d):
                    t0, pr, eb = step_ctx(step, d)
                    p.dma(Vgf[d][:], VHG[t0:t0 + 256, :].rearrange("(c s) v -> s c v", s=32), w=[Vgf[d]], r=[VHG], q="pool")
                    p.A(lambda e: e.copy(out=Vg2[d][step % 2][:], in_=Vgf[d][:]), w=[Vg2[d][step % 2]], r=[Vgf[d]])

                rmask4 = p.sb("rmask4", [128, 4, 256], es=st)
                p.V(lambda e: e.memset(rmask4[:], 1.0), w=[rmask4]); p.V(lambda e: e.memset(rmask4[:, :, 0:1], 0.0), w=[rmask4])

                def prep_group(step, d):
                    t0, pr, eb = step_ctx(step, d)
                    zt, qt, lf, Ft, Et, kg, Xt, ex = (tmp4[nm] for nm in ("zt", "qt", "lf", "F", "E", "kg", "X", "ex"))
                    fl = lambda t_: t_[:].rearrange("p h t -> p (h t)")
                    p.dma(zt[:], PT[512 * (1 + d):512 * (2 + d), t0:t0 + 256].rearrange("(h k) t -> k h t", k=128), w=[zt], r=[PT], q="pool")
                    p.dma(qt[:], PT[0:512, t0:t0 + 256].rearrange("(h k) t -> k h t", k=128), w=[qt], r=[PT], q="pool")
                    p.A(lambda e: e.activation(out=zt[:], in_=zt[:], func=AF.Sigmoid), w=[zt], r=[zt])
                    p.A(lambda e: e.activation(out=qt[:], in_=qt[:], func=AF.Silu), w=[qt], r=[qt])
                    p.V(lambda e: e.tensor_tensor(out=zt[:], in0=zt[:], in1=oml[:, d, :].unsqueeze(2).broadcast_to([128, 4, 256]), op=ALU.mult), w=[zt], r=[zt, oml])
                    p.V(lambda e: e.tensor_tensor(out=zt[:], in0=zt[:], in1=lb[:, d, :].unsqueeze(2).broadcast_to([128, 4, 256]), op=ALU.add), w=[zt], r=[zt, lb])
                    p.A(lambda e: e.activation(out=lf[:], in_=zt[:], func=AF.Ln), w=[lf], r=[zt])
                    p.V(lambda e: e.tensor_scalar(out=kg[:], in0=zt[:], scalar1=-1.0, scalar2=1.0, op0=ALU.mult, op1=ALU.add), w=[kg], r=[zt])
                    p.V(lambda e: e.tensor_tensor_scan(out=fl(Ft), data0=fl(rmask4), data1=fl(lf), initial=0.0, op0=ALU.mult, op1=ALU.add),
                        w=[Ft], r=[rmask4, lf])
                    p.V(lambda e: e.tensor_tensor(out=Et[:], in0=Ft[:], in1=lf[:], op=ALU.subtract), w=[Et], r=[Ft, lf])
                    v3 = lambda t_: t_[:].rearrange("p h (c t) -> p (h c) t", t=32)
                    F3, E3, X3 = v3(Ft), v3(Et), v3(Xt)
                    bc = lambda ap: ap.broadcast_to([128, 32, 32])
                    if d == 0:
                        plan = [(F3, F3[:, :, 15:16], [("qtl", qt, 1.0), ("ktl", kg, -1.0)]),
                                (F3, E3[:, :, 0:1], [("qh", qt, 1.0)]),
                                (F3, F3[:, :, 31:32], [("kh", kg, -1.0)])]
                    else:
                        plan = [(E3, E3[:, :, 16:17], [("qtl", qt, -1.0), ("ktl", kg, 1.0)]),
                                (E3, F3[:, :, 31:32], [("qh", qt, -1.0)]),
                                (E3, E3[:, :, 0:1], [("kh", kg, 1.0)])]
                    for (src3, ref, outs) in plan:
                        p.V(lambda e, src3=src3, ref=ref: e.tensor_tensor(out=X3, in0=src3, in1=bc(ref), op=ALU.subtract), w=[Xt], r=[Ft, Et])
                        for (nm, mul, sgn) in outs:
                            p.A(lambda e, sgn=sgn: e.activation(out=ex[:], in_=Xt[:], func=AF.Exp, scale=sgn), w=[ex], r=[Xt])
                            p.V(lambda e, nm=nm, mul=mul: e.tensor_tensor(out=pr[nm][:], in0=mul[:], in1=ex[:], op=ALU.mult),
                                w=[pr[nm]], r=[mul, ex])
                    p.V(lambda e: e.tensor_tensor(out=eb[:].rearrange("p h c -> p (h c)"), in0=F3[:, :, 31], in1=E3[:, :, 0], op=ALU.subtract), w=[eb], r=[Ft, Et])
                    p.A(lambda e: e.activation(out=eb[:], in_=eb[:], func=AF.Exp), w=[eb], r=[eb])

                for d in range(2):
                    group_load(0, d)
                    prep_group(0, d)
                for step in range(9):
                    ctx_ = {d: step_ctx(step, d) for d in range(2)}
                    Vg = [Vg2[d][step % 2] for d in range(2)]
                    og = [og2[d][step % 2] for d in range(2)]
                    for ci in range(8):
                        for d in range(2):
                            t0, pr, eb = ctx_[d]
                            mi = 1 if d == 0 else 3
                            c = ci if d == 0 else 7 - ci
                            cs = c * 32
                            for h in range(4):
                                p.T(lambda e, h=h, cs=cs: e.transpose(pT[d][:32, h * 128:(h + 1) * 128], pr["kh"][:, h, cs:cs + 32], ident_bf[:]),
                                    w=[pT[d]], r=[pr["kh"], ident_bf])
                            p.A(lambda e: e.copy(out=khT[d][:], in_=pT[d][:]), w=[khT[d]], r=[pT[d]])
                            for h in range(4):
                                p.T(lambda e, h=h, cs=cs: e.matmul(pA[d][:, h, :], lhsT=pr["ktl"][:, h, cs:cs + 32], rhs=pr["qtl"][:, h, cs:cs + 32],
                                                                  start=True, stop=True), w=[pA[d]], r=[pr["ktl"], pr["qtl"]])
                            p.V(lambda e: e.tensor_tensor(out=AT[d][:], in0=pA[d][:], in1=masks[0:32, mi, 0:32].unsqueeze(1).broadcast_to([32, 4, 32]),
                                                          op=ALU.mult), w=[AT[d]], r=[pA[d], masks])
                            for h in range(4):
                                p.T(lambda e, h=h, c=c: e.matmul(pO[d][:, h, :], lhsT=Vg[d][:, c, h * 128:(h + 1) * 128], rhs=AT[d][:, h, :],
                                                                start=True, stop=False), w=[pO[d]], r=[Vg[d], AT[d]])
                                p.T(lambda e, h=h, cs=cs: e.matmul(pO[d][:, h, :], lhsT=Sb[d][:, h, :], rhs=pr["qh"][:, h, cs:cs + 32],
                                                                  start=False, stop=True), w=[pO[d]], r=[Sb[d], pr["qh"]])
                            p.A(lambda e, cs=cs: e.copy(out=og[d][:, :, cs:cs + 32], in_=pO[d][:]), w=[og[d]], r=[pO[d]])
                            for h in range(4):
                                p.T(lambda e, h=h, c=c: e.matmul(pS[d][:, h, :], lhsT=khT[d][:, h * 128:(h + 1) * 128], rhs=Vg[d][:, c, h * 128:(h + 1) * 128],
                                                                start=True, stop=True), w=[pS[d]], r=[khT[d], Vg[d]])
                            p.V(lambda e, c=c: e.tensor_tensor(out=S[d][:], in0=S[d][:], in1=eb[:, :, c:c + 1].broadcast_to([128, 4, 128]), op=ALU.mult),
                                w=[S[d]], r=[S[d], eb])
                            p.V(lambda e: e.tensor_tensor(out=S[d][:], in0=S[d][:], in1=pS[d][:], op=ALU.add), w=[S[d]], r=[S[d], pS[d]])
                            p.A(lambda e: e.copy(out=Sb[d][:], in_=S[d][:]), w=[Sb[d]], r=[S[d]])
                        if step + 1 < 9 and ci in (0, 4):
                            group_load(step + 1, ci // 4)
                            prep_group(step + 1, ci // 4)
                    for d in range(2):
                        t0, pr, eb = ctx_[d]
                        p.dma(OHG[d, :, t0:t0 + 256].rearrange("(h v) t -> v h t", v=128), og[d][:], w=[OHG], r=[og[d]])
                p.barrier()
            with ExitStack() as st:
                wproj = load_w_bf(st, I["hg_proj"][l].rearrange("(k p) c -> p k c", p=128), 4, D, "hgp")
                ngv = p.sb("ngv", [128, 1], es=st); p.dma(ngv[:], I["hg_ngT"][l], w=[ngv], r=[I["hg_ngT"]])
                bt = alloc_branch_tiles(st)
                oa = p.sb("oa", [128, 4, 512], es=st); ob = p.sb("ob", [128, 4, 512], es=st); gg = p.sb("gg", [128, 4, 512], es=st)
                sq = p.sb("sqh", [128, 4, 512], BF16, es=st); zT = p.sb("zT", [128, 4, 512], BF16, es=st)
                pn = p.ps("pn", [128, 512], es=st); rs = p.sb("rs", [128, 512], es=st)
                for (b0, n, seg) in BLKS:
                    p.dma(oa[:, :, :n], OHG[0, :, b0:b0 + n].rearrange("(h v) t -> v h t", v=128), w=[oa], r=[OHG])
                    p.dma(ob[:, :, :n], OHG[1, :, b0:b0 + n].rearrange("(h v) t -> v h t", v=128), w=[ob], r=[OHG], q="pool")
                    p.dma(gg[:, :, :n], PT[2048:2560, b0:b0 + n].rearrange("(h v) t -> v h t", v=128), w=[gg], r=[PT])
                    p.V(lambda e: e.tensor_tensor(out=oa[:, :, :n], in0=oa[:, :, :n], in1=ob[:, :, :n], op=ALU.add), w=[oa], r=[oa, ob])
                    p.A(lambda e: e.activation(out=gg[:, :, :n], in_=gg[:, :, :n], func=AF.Silu), w=[gg], r=[gg])
                    p.V(lambda e: e.tensor_tensor(out=sq[:, :, :n], in0=oa[:, :, :n], in1=oa[:, :, :n], op=ALU.mult), w=[sq], r=[oa])
                    for h in range(4):
                        p.T(lambda e, h=h: e.matmul(pn[:, :n], lhsT=ones_bf[:], rhs=sq[:, h, :n], start=True, stop=True), w=[pn], r=[ones_bf, sq])
                        p.A(lambda e: e.activation(out=rs[:, :n], in_=pn[:, :n], func=AF.Sqrt, bias=1e-6, scale=1.0 / 128), w=[rs], r=[pn])
                        p.V(lambda e: e.reciprocal(out=rs[:, :n], in_=rs[:, :n]), w=[rs], r=[rs])
                        p.V(lambda e, h=h: e.scalar_tensor_tensor(out=oa[:, h, :n], in0=oa[:, h, :n], scalar=ngv[:, 0:1], in1=rs[:, :n],
                                                                 op0=ALU.mult, op1=ALU.mult), w=[oa], r=[oa, ngv, rs])
                        p.V(lambda e, h=h: e.tensor_tensor(out=zT[:, h, :n], in0=oa[:, h, :n], in1=gg[:, h, :n], op=ALU.mult), w=[zT], r=[oa, gg])
                    branch_out(bt, zT, 4, 128, wproj, 0, b0, n)
                p.barrier()


        def stage_da(l):
            lam_init = 0.8 - 0.6 * math.exp(-0.3 * l)
            scale = 64 ** -0.5
            with ExitStack() as st:
                lp = p.sb("lp", [128, 4, 64], es=st); s12 = p.sb("s12", [128, 2], es=st); nlam = p.sb("nlam", [128, 1], es=st)
                pr_ = p.sb("lpp", [128, 2, 64], es=st)
                p.dma(lp[:].rearrange("p a b -> p (a b)"), I["da_lambda"][l].partition_broadcast(128), w=[lp], r=[I["da_lambda"]])
                p.V(lambda e: e.tensor_tensor(out=pr_[:, 0, :], in0=lp[:, 0, :], in1=lp[:, 1, :], op=ALU.mult), w=[pr_], r=[lp])
                p.V(lambda e: e.tensor_tensor(out=pr_[:, 1, :], in0=lp[:, 2, :], in1=lp[:, 3, :], op=ALU.mult), w=[pr_], r=[lp])
                p.V(lambda e: e.reduce_sum(out=s12[:], in_=pr_[:], axis=AX.X), w=[s12], r=[pr_])
                p.A(lambda e: e.activation(out=s12[:], in_=s12[:], func=AF.Exp), w=[s12], r=[s12])
                p.V(lambda e: e.tensor_tensor(out=nlam[:], in0=s12[:, 1:2], in1=s12[:, 0:1], op=ALU.subtract), w=[nlam], r=[s12])
                p.V(lambda e: e.tensor_scalar(out=nlam[:], in0=nlam[:], scalar1=-lam_init, scalar2=None, op0=ALU.add), w=[nlam], r=[nlam])
                sg = p.sb("sg", [128, 128], es=st)
                p.dma(sg[:], I["da_sg"][l].partition_broadcast(128), w=[sg], r=[I["da_sg"]])
                p.V(lambda e: e.tensor_scalar(out=sg[:], in0=sg[:], scalar1=1.0 - lam_init, scalar2=None, op0=ALU.mult), w=[sg], r=[sg])
                KT = p.sb("KT", [64, 8, T], BF16, es=st); QT = p.sb("QT", [64, 8, T], BF16, es=st)
                Vb = p.sb("Vb", [128, 18, 512], BF16, es=st)
                with ExitStack() as st2:
                    cs = p.sb("cs", [64, 2, TL], es=st2); p.dma(cs[:], I["ropeCS"][:], w=[cs], r=[I["ropeCS"]])
                    rR = p.sb("rR", [64, 64], es=st2); p.dma(rR[:], I["ropeR"][:], w=[rR], r=[I["ropeR"]])
                    xr = [p.sb("xr", [64, T], es=st2) for _ in range(2)]
                    t1 = [p.sb("t1", [64, 512], es=st2) for _ in range(2)]; t2 = [p.sb("t2", [64, 512], es=st2) for _ in range(2)]
                    pr2 = [p.ps("prp", [64, 512], es=st2) for _ in range(2)]
                    vf = [p.sb("vf", [128, 512], es=st2) for _ in range(2)]
                    i2 = 0
                    for qi, (dst, roff) in enumerate(((QT, DA0), (KT, DA0 + 512))):
                        for hm in range(8):
                            x_ = xr[hm % 2]
                            p.dma(x_[:], PT[roff + hm * 64:roff + (hm + 1) * 64, :], w=[x_], r=[PT], q=("sp" if hm % 2 == 0 else "pool"))
                            p.A(lambda e, hm=hm, dst=dst, x_=x_: e.copy(out=dst[:, hm, 0:TC], in_=x_[:, 0:TC]), w=[dst], r=[x_])
                            for bi in range(4):
                                a0 = TC + bi * 512
                                pp = pr2[i2 % 2]; t1_ = t1[i2 % 2]; t2_ = t2[i2 % 2]; i2 += 1
                                p.T(lambda e, pp=pp, x_=x_, a0=a0: e.matmul(pp[:], lhsT=rR[:], rhs=x_[:, a0:a0 + 512], start=True, stop=True), w=[pp], r=[rR, x_])
                                p.V(lambda e, t1_=t1_, x_=x_, a0=a0, bi=bi: e.tensor_tensor(out=t1_[:], in0=x_[:, a0:a0 + 512], in1=cs[:, 0, bi * 512:(bi + 1) * 512], op=ALU.mult),
                                    w=[t1_], r=[x_, cs])
                                p.V(lambda e, t2_=t2_, pp=pp, bi=bi: e.tensor_tensor(out=t2_[:], in0=pp[:], in1=cs[:, 1, bi * 512:(bi + 1) * 512], op=ALU.mult),
                                    w=[t2_], r=[pp, cs])
                                p.V(lambda e, dst=dst, hm=hm, a0=a0, t1_=t1_, t2_=t2_: e.tensor_tensor(out=dst[:, hm, a0:a0 + 512], in0=t1_[:], in1=t2_[:], op=ALU.add),
                                    w=[dst], r=[t1_, t2_])
                    for kt in range(18):
                        v_ = vf[kt % 2]
                        p.dma(v_[:], VDA[kt * 128:(kt + 1) * 128, :], w=[v_], r=[VDA], q=("sp" if kt % 2 == 0 else "pool"))
                        p.V(lambda e, kt=kt, v_=v_: e.tensor_copy(out=Vb[:, kt, :], in_=v_[:]), w=[Vb], r=[v_])
                    p.barrier()
                wproj = load_w_bf(st, I["da_proj"][l].rearrange("(k p) c -> p k c", p=128), 4, D, "dap")
                bt = alloc_branch_tiles(st, npo=1)
                pS = [p.ps("pSc", [128, 512], es=st) for _ in range(2)]
                Eb = [p.sb("Eb", [128, 18, 512], BF16, es=st) for _ in range(2)]
                acc = [p.ps("acc", [128, 4, 128], es=st) for _ in range(2)]
                den = p.ps("den", [128, 2, 4], es=st)
                pden = p.ps("pden", [1, 512], es=st); denT = p.sb("denT", [1, 512], es=st)
                pZ = p.ps("pZ", [128, 512], es=st)
                rden = p.sb("rden", [128, 2, 4], es=st); rl = p.sb("rl", [128, 4], es=st)
                o1 = p.sb("o1", [128, 4, 128], es=st); o2 = p.sb("o2", [128, 4, 128], es=st); ssq = p.sb("ssq", [128, 4], es=st)
                zT = p.sb("zTd", [128, 4, 512], BF16, es=st)
                qblocks = [(0, 256, [0, 1])] + [(TC + i * 512, 512, list(range(18))) for i in range(4)]
                ei = [0]

                def phase1(q0, nq, kts, hm):
                    Eb_ = Eb[hm % 2]
                    for kt in kts:
                        pS_ = pS[ei[0] % 2]; ei[0] += 1
                        p.T(lambda e, pS_=pS_, kt=kt: e.matmul(pS_[:, :nq], lhsT=KT[:, hm, kt * 128:(kt + 1) * 128], rhs=QT[:, hm, q0:q0 + nq],
                                                              start=True, stop=True), w=[pS_], r=[KT, QT])
                        p.A(lambda e, pS_=pS_, kt=kt: e.activation(out=Eb_[:, kt, :nq], in_=pS_[:, :nq], func=AF.Exp, scale=scale), w=[Eb_], r=[pS_])

                def phase2(q0, nq, kts, hm):
                    Eb_ = Eb[hm % 2]; h = hm // 2; m = hm % 2
                    for j in range(nq // 128):
                        for kt in kts:
                            p.T(lambda e, j=j, kt=kt: e.matmul(acc[m][:, j, :], lhsT=Eb_[:, kt, j * 128:(j + 1) * 128], rhs=Vb[:, kt, h * 128:(h + 1) * 128],
                                                              start=(kt == kts[0]), stop=(kt == kts[-1])), w=[acc[m]], r=[Eb_, Vb])
                        for kt in kts:
                            p.T(lambda e, j=j, kt=kt: e.matmul(den[:, m, j:j + 1], lhsT=Eb_[:, kt, j * 128:(j + 1) * 128], rhs=ones_bf[:, 0:1],
                                                              start=(kt == kts[0]), stop=(kt == kts[-1])), w=[den], r=[Eb_, ones_bf])

                for (q0, nq, kts) in qblocks:
                    nj = nq // 128
                    phase1(q0, nq, kts, 0)
                    for hm in range(8):
                        if hm + 1 < 8:
                            phase1(q0, nq, kts, hm + 1)
                        phase2(q0, nq, kts, hm)
                        if hm % 2 == 0:
                            continue
                        h = hm // 2
                        p.V(lambda e: e.reciprocal(out=rden[:, :, :nj], in_=den[:, :, :nj]), w=[rden], r=[den])
                        p.V(lambda e: e.tensor_scalar(out=rl[:, :nj], in0=rden[:, 1, :nj], scalar1=nlam[:, 0:1], scalar2=None, op0=ALU.mult), w=[rl], r=[rden, nlam])
                        p.V(lambda e: e.tensor_tensor(out=o1[:, :nj, :], in0=acc[0][:, :nj, :], in1=rden[:, 0, :nj].unsqueeze(2).broadcast_to([128, nj, 128]), op=ALU.mult),
                            w=[o1], r=[acc[0], rden])
                        p.V(lambda e: e.tensor_tensor(out=o2[:, :nj, :], in0=acc[1][:, :nj, :], in1=rl[:, :nj].unsqueeze(2).broadcast_to([128, nj, 128]), op=ALU.mult),
                            w=[o2], r=[acc[1], rl])
                        p.V(lambda e: e.tensor_tensor(out=o1[:, :nj, :], in0=o1[:, :nj, :], in1=o2[:, :nj, :], op=ALU.add), w=[o1], r=[o1, o2])
                        p.V(lambda e: e.tensor_tensor(out=o2[:, :nj, :], in0=o1[:, :nj, :], in1=o1[:, :nj, :], op=ALU.mult), w=[o2], r=[o1])
                        p.V(lambda e: e.reduce_sum(out=ssq[:, :nj], in_=o2[:, :nj, :], axis=AX.X), w=[ssq], r=[o2])
                        p.A(lambda e: e.activation(out=ssq[:, :nj], in_=ssq[:, :nj], func=AF.Sqrt, bias=1e-5, scale=1.0 / 128), w=[ssq], r=[ssq])
                        p.V(lambda e: e.reciprocal(out=ssq[:, :nj], in_=ssq[:, :nj]), w=[ssq], r=[ssq])
                        p.V(lambda e: e.tensor_tensor(out=o1[:, :nj, :], in0=o1[:, :nj, :], in1=ssq[:, :nj].unsqueeze(2).broadcast_to([128, nj, 128]), op=ALU.mult),
                            w=[o1], r=[o1, ssq])
                        p.V(lambda e: e.tensor_tensor(out=o1[:, :nj, :], in0=o1[:, :nj, :], in1=sg[:].unsqueeze(1).broadcast_to([128, nj, 128]), op=ALU.mult),
                            w=[o1], r=[o1, sg])
                        for j in range(nj):
                            p.T(lambda e, j=j: e.transpose(pZ[:, j * 128:(j + 1) * 128], o1[:, j, :], ident[:]), w=[pZ], r=[o1, ident])
                        p.A(lambda e, h=h: e.copy(out=zT[:, h, :nq], in_=pZ[:, :nq]), w=[zT], r=[pZ])
                    branch_out(bt, zT, 4, 128, wproj, 2, q0, nq)
                p.barrier()


        RWN = ("R", "K", "V", "AN", "B", "LW0", "LW1", "BON", "G")
        RWS = {nm: p.dram("RWS_" + nm, [64, 8, T]) for nm in RWN}
        YRW = [p.dram("YRW%d" % d, [64, 8, T]) for d in range(2)]

        def stage_rw_prep(l):
            with ExitStack() as st:
                vec = p.sb("rwvec", [64, 8, 8], es=st); p.dma(vec[:], I["rw_vecT"][l], w=[vec], r=[I["rw_vecT"]])
                w0 = p.sb("rww0", [64, 2, 8], es=st); p.dma(w0[:], I["rw_w0T"][l].rearrange("d k h -> k d h"), w=[w0], r=[I["rw_w0T"]])
                w2 = p.sb("rww2", [64, 2, 512], es=st); p.dma(w2[:], I["rw_w2"][l].rearrange("d j c -> j d c"), w=[w2], r=[I["rw_w2"]])
                a2 = p.sb("rwa2", [64, 512], es=st); p.dma(a2[:], I["rw_a2"][l], w=[a2], r=[I["rw_a2"]])
                g2 = p.sb("rwg2", [128, 512], es=st); p.dma(g2[:], I["rw_g2"][l], w=[g2], r=[I["rw_g2"]])
                n = 256
                mk = lambda nm: p.sb(nm, [64, 8, n], es=st)
                r_, kr, v_, a_, kk, sq, nrm, k_, b_, an, rk, bon, g_ = (mk(x) for x in ("r_", "kr", "v_", "a_", "kk", "sq", "nrm", "k_", "b_", "an", "rk", "bon", "g_"))
                lw = [mk("lw0"), mk("lw1")]
                adt = p.sb("adt", [64, n], es=st); wd = [p.sb("wdf", [64, n], es=st), p.sb("wdb", [64, n], es=st)]
                gdt = p.sb("gdt", [128, n], es=st); th = p.sb("th", [64, n], es=st)
                pool = [p.ps("prw", [64, 2, n], es=st) for _ in range(4)]
                pi = [0]

                def nps():
                    pi[0] += 1
                    return pool[pi[0] % 4]
                bc = lambda i: vec[:, i, :].unsqueeze(2).broadcast_to([64, 8, n])
                for blk in range(9):
                    b0 = blk * n
                    hk = lambda c0: PT[c0:c0 + 512, b0:b0 + n].rearrange("(h k) t -> k h t", k=64)
                    p.dma(r_[:], hk(RW_R), w=[r_], r=[PT]); p.dma(kr[:], hk(RW_K), w=[kr], r=[PT], q="pool"); p.dma(v_[:], hk(RW_V), w=[v_], r=[PT])
                    p.dma(adt[:], PT[RW_AD:RW_AD + 64, b0:b0 + n], w=[adt], r=[PT], q="pool")
                    p.dma(wd[0][:], PT[RW_WF:RW_WF + 64, b0:b0 + n], w=[wd[0]], r=[PT]); p.dma(wd[1][:], PT[RW_WB:RW_WB + 64, b0:b0 + n], w=[wd[1]], r=[PT], q="pool")
                    p.dma(gdt[:], PT[RW_GD:RW_GD + 128, b0:b0 + n], w=[gdt], r=[PT])
                    for hp in range(4):
                        ps_ = nps()
                        for hh in range(2):
                            h = hp * 2 + hh
                            p.T(lambda e, h=h, hh=hh, ps_=ps_: e.matmul(ps_[:, hh, :], lhsT=a2[:, h * 64:(h + 1) * 64], rhs=adt[:], start=True, stop=True), w=[ps_], r=[a2, adt])
                        for hh in range(2):
                            h = hp * 2 + hh
                            p.A(lambda e, h=h, hh=hh, ps_=ps_: e.activation(out=a_[:, h, :], in_=ps_[:, hh, :], func=AF.Sigmoid, bias=vec[:, 2, h:h + 1], scale=1.0),
                                w=[a_], r=[ps_, vec])
                    p.V(lambda e: e.tensor_tensor(out=kk[:], in0=kr[:], in1=bc(0), op=ALU.mult), w=[kk], r=[kr, vec])
                    p.A(lambda e: e.activation(out=sq[:], in_=kk[:], func=AF.Square), w=[sq], r=[kk])
                    for hp in range(4):
                        ps_ = nps()
                        p.T(lambda e, hp=hp, ps_=ps_: e.matmul(ps_[:].rearrange("p a b -> p (a b)"), lhsT=ones_f[:64, :64],
                                                              rhs=sq[:, 2 * hp:2 * hp + 2, :].rearrange("p a b -> p (a b)"), start=True, stop=True), w=[ps_], r=[ones_f, sq])
                        p.A(lambda e, hp=hp, ps_=ps_: e.activation(out=nrm[:, 2 * hp:2 * hp + 2, :], in_=ps_[:], func=AF.Sqrt), w=[nrm], r=[ps_])
                    p.V(lambda e: e.tensor_scalar(out=nrm[:], in0=nrm[:], scalar1=1e-12, scalar2=None, op0=ALU.max), w=[nrm], r=[nrm])
                    p.V(lambda e: e.reciprocal(out=nrm[:], in_=nrm[:]), w=[nrm], r=[nrm])
                    p.V(lambda e: e.tensor_tensor(out=kk[:], in0=kk[:], in1=nrm[:], op=ALU.mult), w=[kk], r=[kk, nrm])
                    p.V(lambda e: e.scalar_tensor_tensor(out=k_[:], in0=a_[:], scalar=-1.0, in1=bc(1), op0=ALU.add, op1=ALU.mult), w=[k_], r=[a_, vec])
                    p.V(lambda e: e.scalar_tensor_tensor(out=k_[:], in0=k_[:], scalar=1.0, in1=kr[:], op0=ALU.add, op1=ALU.mult), w=[k_], r=[k_, kr])
                    p.V(lambda e: e.tensor_tensor(out=b_[:], in0=kk[:], in1=a_[:], op=ALU.mult), w=[b_], r=[kk, a_])
                    p.A(lambda e: e.activation(out=an[:], in_=kk[:], func=AF.Identity, scale=-1.0), w=[an], r=[kk])
                    p.V(lambda e: e.tensor_tensor(out=rk[:], in0=r_[:], in1=k_[:], op=ALU.mult), w=[rk], r=[r_, k_])
                    p.V(lambda e: e.tensor_tensor(out=rk[:], in0=rk[:], in1=bc(3), op=ALU.mult), w=[rk], r=[rk, vec])
                    for hp in range(4):
                        ps_ = nps()
                        p.T(lambda e, hp=hp, ps_=ps_: e.matmul(ps_[:].rearrange("p a b -> p (a b)"), lhsT=ones_f[:64, :64],
                                                              rhs=rk[:, 2 * hp:2 * hp + 2, :].rearrange("p a b -> p (a b)"), start=True, stop=True), w=[ps_], r=[ones_f, rk])
                        p.V(lambda e, hp=hp, ps_=ps_: e.tensor_tensor(out=bon[:, 2 * hp:2 * hp + 2, :], in0=ps_[:], in1=v_[:, 2 * hp:2 * hp + 2, :], op=ALU.mult),
                            w=[bon], r=[ps_, v_])
                    p.A(lambda e: e.activation(out=gdt[:], in_=gdt[:], func=AF.Sigmoid), w=[gdt], r=[gdt])
                    for hp in range(4):
                        ps_ = nps()
                        for hh in range(2):
                            h = hp * 2 + hh
                            p.T(lambda e, h=h, hh=hh, ps_=ps_: e.matmul(ps_[:, hh, :], lhsT=g2[:, h * 64:(h + 1) * 64], rhs=gdt[:], start=True, stop=True), w=[ps_], r=[g2, gdt])
                        p.A(lambda e, hp=hp, ps_=ps_: e.copy(out=g_[:, 2 * hp:2 * hp + 2, :], in_=ps_[:]), w=[g_], r=[ps_])
                    for d in range(2):
                        p.A(lambda e, d=d: e.activation(out=th[:], in_=wd[d][:], func=AF.Tanh), w=[th], r=[wd[d]])
                        for hp in range(4):
                            ps_ = nps()
                            for hh in range(2):
                                h = hp * 2 + hh
                                p.T(lambda e, h=h, hh=hh, ps_=ps_, d=d: e.matmul(ps_[:, hh, :], lhsT=w2[:, d, h * 64:(h + 1) * 64], rhs=th[:], start=True, stop=True), w=[ps_], r=[w2, th])
                            for hh in range(2):
                                h = hp * 2 + hh
                                p.A(lambda e, h=h, hh=hh, ps_=ps_, d=d: e.activation(out=lw[d][:, h, :], in_=ps_[:, hh, :], func=AF.Sigmoid, bias=w0[:, d, h:h + 1], scale=1.0),
                                    w=[lw[d]], r=[ps_, w0])
                        p.V(lambda e, d=d: e.tensor_scalar(out=lw[d][:], in0=lw[d][:], scalar1=-math.exp(-0.5), scalar2=None, op0=ALU.mult), w=[lw[d]], r=[lw[d]])
                    for qi, (nm, tl) in enumerate((("R", r_), ("K", k_), ("V", v_), ("AN", an), ("B", b_), ("LW0", lw[0]), ("LW1", lw[1]), ("BON", bon), ("G", g_))):
                        p.dma(RWS[nm][:, :, b0:b0 + n], tl[:], w=[RWS[nm]], r=[tl], q=("sp" if qi % 2 == 0 else "pool"))
                p.barrier()

        def stage_rw_scan(l):
            with ExitStack() as st:
                mk = lambda nm, shp=(64, 8, 64), dt=F32: [p.sb(nm, list(shp), dt, es=st) for _ in range(2)]
                ST = mk("ST"); STb = mk("STb", dt=BF16); Vb = mk("Vb", dt=BF16); Xb = mk("Xb", dt=BF16)
                for d in range(2):
                    p.G(lambda e, d=d: e.memset(STb[d][:], 0.0), w=[STb[d]])
                for d in range(2):
                    p.G(lambda e, d=d: e.memset(ST[d][:], 0.0), w=[ST[d]])
                rmask = p.sb("rmask", [64, 8, 64], es=st)
                p.V(lambda e: e.memset(rmask[:], 1.0), w=[rmask]); p.V(lambda e: e.memset(rmask[:, :, 0:1], 0.0), w=[rmask])
                ld = {nm: mk("l" + nm) for nm in ("R", "K", "V", "AN", "B", "LW")}
                Ft, Et, Gt, Ht = mk("F"), mk("E"), mk("G"), mk("H")
                ex = [mk("ex0"), mk("ex1")]
                AR = mk("AR", (64, 8, 2, 64), BF16); bt, kt_, bh, kh = mk("bt", dt=BF16), mk("kt", dt=BF16), mk("bh", dt=BF16), mk("kh", dt=BF16)
                Vtm, Btm, Ktm = mk("Vtm", dt=BF16), mk("Btm", dt=BF16), mk("Ktm", dt=BF16)
                W1 = mk("W1", (64, 8, 128), BF16); W2 = mk("W2", (64, 8, 128), BF16); Lm = mk("Lm", dt=BF16); Xm = mk("Xm")
                Pb = [mk("Pb0", dt=BF16), mk("Pb1", dt=BF16)]; PTb = [mk("PTb0", dt=BF16), mk("PTb1", dt=BF16)]
                RH, Um, yo = mk("RH", dt=BF16), mk("Um", dt=BF16), mk("yo")
                gC = mk("gC", (64, 8))
                pool = [p.ps("prs", [64, 8, 64], es=st) for _ in range(6)]
                poolb = [p.ps("prsb", [64, 8, 64], BF16, es=st) for _ in range(2)]
                pi = [0, 0]

                def nps():
                    pi[0] += 1
                    return pool[pi[0] % 6]

                def npsb():
                    pi[1] += 1
                    return poolb[pi[1] % 2]
                f2 = lambda b: b[:].rearrange("p h t -> p (h t)")
                bwd = [3, 2, 1, 0] + list(range(35, 3, -1))
                idb = ident[:64, :64].unsqueeze(1).broadcast_to([64, 8, 64])
                ei = [0]

                def exp_to(d, src, sgn):
                    e_ = ex[ei[0] % 2][d]; ei[0] += 1
                    p.A(lambda e: e.activation(out=e_[:], in_=src[:], func=AF.Exp, scale=sgn), w=[e_], r=[src])
                    return e_
                for step in range(36):
                    cd = (step, bwd[step])
                    for d in range(2):
                        t0 = cd[d] * 64
                        for qi, nm in enumerate(("R", "K", "V", "AN", "B", "LW")):
                            src = RWS["LW%d" % d] if nm == "LW" else RWS[nm]
                            p.dma(ld[nm][d][:], src[:, :, t0:t0 + 64], w=[ld[nm][d]], r=[src], q=("sp" if qi % 2 == 0 else "pool"))
                    for d in range(2):
                        lw = ld["LW"][d]; F_, E_, G_, H_ = Ft[d], Et[d], Gt[d], Ht[d]
                        p.V(lambda e: e.tensor_tensor_scan(out=f2(F_), data0=f2(rmask), data1=f2(lw), initial=0.0, op0=ALU.mult, op1=ALU.add), w=[F_], r=[rmask, lw])
                        p.G(lambda e: e.tensor_tensor(out=E_[:], in0=F_[:], in1=lw[:], op=ALU.subtract), w=[E_], r=[F_, lw])
                        fend = F_[:, :, 63:64].broadcast_to([64, 8, 64])
                        p.V(lambda e: e.tensor_tensor(out=G_[:], in0=fend, in1=F_[:], op=ALU.subtract), w=[G_], r=[F_])
                        p.A(lambda e: e.activation(out=gC[d][:], in_=F_[:, :, 63], func=AF.Exp), w=[gC[d]], r=[F_])
                        if d == 1:
                            p.V(lambda e: e.tensor_tensor(out=H_[:], in0=E_[:], in1=fend, op=ALU.subtract), w=[H_], r=[E_, F_])
                        R_, K_, AN_, B_ = ld["R"][d], ld["K"][d], ld["AN"][d], ld["B"][d]
                        specs = ((E_, 1.0), (F_, -1.0), (F_, 1.0), (G_, 1.0)) if d == 0 else ((G_, 1.0), (H_, 1.0), (H_, -1.0), (E_, 1.0))
                        e1 = exp_to(d, *specs[0])
                        p.V(lambda e: e.tensor_tensor(out=AR[d][:, :, 0, :], in0=AN_[:], in1=e1[:], op=ALU.mult), w=[AR[d]], r=[AN_, e1])
                        e2 = exp_to(d, *specs[1])
                        p.V(lambda e: e.tensor_tensor(out=bt[d][:], in0=B_[:], in1=e2[:], op=ALU.mult), w=[bt[d]], r=[B_, e2])
                        p.G(lambda e: e.tensor_tensor(out=kt_[d][:], in0=K_[:], in1=e2[:], op=ALU.mult), w=[kt_[d]], r=[K_, e2])
                        e3 = exp_to(d, *specs[2])
                        p.V(lambda e: e.tensor_tensor(out=AR[d][:, :, 1, :], in0=R_[:], in1=e3[:], op=ALU.mult), w=[AR[d]], r=[R_, e3])
                        e4 = exp_to(d, *specs[3])
                        p.V(lambda e: e.tensor_tensor(out=bh[d][:], in0=B_[:], in1=e4[:], op=ALU.mult), w=[bh[d]], r=[B_, e4])
                        p.G(lambda e: e.tensor_tensor(out=kh[d][:], in0=K_[:], in1=e4[:], op=ALU.mult), w=[kh[d]], r=[K_, e4])
                    for d in range(2):
                        p.A(lambda e: e.copy(out=Vb[d][:], in_=ld["V"][d][:]), w=[Vb[d]], r=[ld["V"][d]])
                        for (src, dst) in ((Vb[d], Vtm[d]), (bh[d], Btm[d]), (kh[d], Ktm[d])):
                            ps_ = npsb()
                            for h in range(8):
                                p.T(lambda e, h=h, ps_=ps_, src=src: e.transpose(ps_[:, h, :], src[:, h, :], ident_bf[:64, :64]), w=[ps_], r=[src, ident_bf])
                            p.A(lambda e, ps_=ps_, dst=dst: e.copy(out=dst[:], in_=ps_[:]), w=[dst], r=[ps_])
                    for d in range(2):
                        cm = masks[:, 2 * d:2 * d + 2, :].rearrange("p a b -> p (a b)").unsqueeze(1).broadcast_to([64, 4, 128])
                        for (lh, Wd) in ((bt[d], W1[d]), (kt_[d], W2[d])):
                            for half in range(2):
                                ps_ = nps()
                                pv = ps_[:].rearrange("p h t -> p (h t)").rearrange("p (a b) -> p a b", b=128)
                                for hh in range(4):
                                    h = half * 4 + hh
                                    p.T(lambda e, h=h, hh=hh, pv=pv, lh=lh: e.matmul(pv[:, hh, :], lhsT=lh[:, h, :], rhs=AR[d][:, h, :, :].rearrange("p a b -> p (a b)"),
                                                                                   start=True, stop=True), w=[ps_], r=[lh, AR[d]])
                                p.V(lambda e, pv=pv, Wd=Wd, half=half: e.tensor_tensor(out=Wd[:, half * 4:(half + 1) * 4, :], in0=pv, in1=cm, op=ALU.mult), w=[Wd], r=[ps_, masks])
                        ps_ = nps()
                        for h in range(8):
                            p.T(lambda e, h=h, ps_=ps_: e.matmul(ps_[:, h, :], lhsT=AR[d][:, h, 0, :], rhs=bt[d][:, h, :], start=True, stop=True), w=[ps_], r=[AR[d], bt[d]])
                        mnt = masks[:, 2 - 2 * d, :].unsqueeze(1).broadcast_to([64, 8, 64])
                        p.V(lambda e, ps_=ps_: e.tensor_tensor(out=Lm[d][:], in0=ps_[:], in1=mnt, op=ALU.mult), w=[Lm[d]], r=[ps_, masks])
                        p.G(lambda e: e.tensor_tensor(out=Xm[d][:], in0=W1[d][:, :, 0:64], in1=idb, op=ALU.add), w=[Xm[d]], r=[W1[d], ident])
                        p.A(lambda e: e.copy(out=Xb[d][:], in_=Xm[d][:]), w=[Xb[d]], r=[Xm[d]])
                    cur = [(lambda h, d=d: W1[d][:, h, 0:64], W1[d], lambda h, d=d: Lm[d][:, h, :], Lm[d]) for d in range(2)]
                    for i in range(1, 6):
                        for d in range(2):
                            Pf, Pbuf, PTf, PTbuf = cur[d]
                            nP, nPT = Pb[i % 2][d], PTb[i % 2][d]
                            if i < 5:
                                ps_ = nps()
                                for h in range(8):
                                    p.T(lambda e, h=h, ps_=ps_: e.matmul(ps_[:, h, :], lhsT=PTf(h), rhs=Pf(h), start=True, stop=True), w=[ps_], r=[Pbuf, PTbuf])
                                p.A(lambda e, ps_=ps_, nP=nP: e.copy(out=nP[:], in_=ps_[:]), w=[nP], r=[ps_])
                            ps2 = nps()
                            for h in range(8):
                                p.T(lambda e, h=h, ps2=ps2: e.matmul(ps2[:, h, :], lhsT=Pf(h), rhs=PTf(h), start=True, stop=True), w=[ps2], r=[Pbuf, PTbuf])
                            p.V(lambda e, ps2=ps2, nPT=nPT: e.tensor_copy(out=nPT[:], in_=ps2[:]), w=[nPT], r=[ps2])
                            ps3 = nps()
                            for h in range(8):
                                p.T(lambda e, h=h, ps3=ps3, nPT=nPT: e.matmul(ps3[:, h, :], lhsT=nPT[:, h, :], rhs=Xb[d][:, h, :], start=True, stop=True), w=[ps3], r=[nPT, Xb[d]])
                            p.V(lambda e, ps3=ps3: e.tensor_tensor(out=Xm[d][:], in0=Xm[d][:], in1=ps3[:], op=ALU.add), w=[Xm[d]], r=[Xm[d], ps3])
                            p.A(lambda e: e.copy(out=Xb[d][:], in_=Xm[d][:]), w=[Xb[d]], r=[Xm[d]])
                            cur[d] = (lambda h, nP=nP: nP[:, h, :], nP, lambda h, nPT=nPT: nPT[:, h, :], nPT)
                    for d in range(2):
                        ps_ = nps()
                        for h in range(8):
                            p.T(lambda e, h=h, ps_=ps_: e.matmul(ps_[:, h, :], lhsT=AR[d][:, h, 0, :], rhs=STb[d][:, h, :], start=True, stop=False), w=[ps_], r=[AR[d], STb[d]])
                            p.T(lambda e, h=h, ps_=ps_: e.matmul(ps_[:, h, :], lhsT=W2[d][:, h, 0:64], rhs=Vtm[d][:, h, :], start=False, stop=True), w=[ps_], r=[W2[d], Vtm[d]])
                        p.A(lambda e, ps_=ps_: e.copy(out=RH[d][:], in_=ps_[:]), w=[RH[d]], r=[ps_])
                        ps_ = nps()
                        for h in range(8):
                            p.T(lambda e, h=h, ps_=ps_: e.matmul(ps_[:, h, :], lhsT=Xb[d][:, h, :], rhs=RH[d][:, h, :], start=True, stop=True), w=[ps_], r=[Xb[d], RH[d]])
                        p.V(lambda e, ps_=ps_: e.tensor_copy(out=Um[d][:], in_=ps_[:]), w=[Um[d]], r=[ps_])
                        ps_ = nps()
                        for h in range(8):
                            p.T(lambda e, h=h, ps_=ps_: e.matmul(ps_[:, h, :], lhsT=STb[d][:, h, :], rhs=AR[d][:, h, 1, :], start=True, stop=False), w=[ps_], r=[STb[d], AR[d]])
                            p.T(lambda e, h=h, ps_=ps_: e.matmul(ps_[:, h, :], lhsT=Um[d][:, h, :], rhs=W1[d][:, h, 64:128], start=False, stop=False), w=[ps_], r=[Um[d], W1[d]])
                            p.T(lambda e, h=h, ps_=ps_: e.matmul(ps_[:, h, :], lhsT=Vtm[d][:, h, :], rhs=W2[d][:, h, 64:128], start=False, stop=True), w=[ps_], r=[Vtm[d], W2[d]])
                        p.A(lambda e, ps_=ps_: e.copy(out=yo[d][:], in_=ps_[:]), w=[yo[d]], r=[ps_])
                        t0 = cd[d] * 64
                        p.dma(YRW[d][:, :, t0:t0 + 64], yo[d][:], w=[YRW[d]], r=[yo[d]], q=("sp" if d == 0 else "pool"))
                        ps_ = nps()
                        for h in range(8):
                            p.T(lambda e, h=h, ps_=ps_: e.matmul(ps_[:, h, :], lhsT=Btm[d][:, h, :], rhs=Um[d][:, h, :], start=True, stop=False), w=[ps_], r=[Btm[d], Um[d]])
                            p.T(lambda e, h=h, ps_=ps_: e.matmul(ps_[:, h, :], lhsT=Ktm[d][:, h, :], rhs=Vtm[d][:, h, :], start=False, stop=True), w=[ps_], r=[Ktm[d], Vtm[d]])
                        p.V(lambda e: e.tensor_tensor(out=ST[d][:], in0=ST[d][:], in1=gC[d][:].unsqueeze(2).broadcast_to([64, 8, 64]), op=ALU.mult), w=[ST[d]], r=[ST[d], gC[d]])
                        p.V(lambda e, ps_=ps_: e.tensor_tensor(out=ST[d][:], in0=ST[d][:], in1=ps_[:], op=ALU.add), w=[ST[d]], r=[ST[d], ps_])
                        p.A(lambda e: e.copy(out=STb[d][:], in_=ST[d][:]), w=[STb[d]], r=[ST[d]])
                p.barrier()

        def stage_rw_fin(l):
            with ExitStack() as st:
                vec = p.sb("rwvec", [64, 8, 8], es=st); p.dma(vec[:], I["rw_vecT"][l], w=[vec], r=[I["rw_vecT"]])
                wproj = load_w_bf(st, I["rw_proj"][l].rearrange("(h k) c -> k h c", k=64), 8, D, "rwp", pk=64)
                bt = alloc_branch_tiles(st)
                n = 256
                mk = lambda nm: p.sb(nm, [64, 8, n], es=st)
                ya, yb, bo, gg, sq2 = mk("ya"), mk("yb"), mk("bo"), mk("gg"), mk("sq2")
                zT = p.sb("zTr", [64, 8, 512], BF16, es=st)
                pool = [p.ps("prf", [64, 2, n], es=st) for _ in range(2)]
                bc = lambda i: vec[:, i, :].unsqueeze(2).broadcast_to([64, 8, n])
                pi = 0
                for blk in range(9):
                    b0 = blk * n
                    p.dma(ya[:], YRW[0][:, :, b0:b0 + n], w=[ya], r=[YRW[0]]); p.dma(yb[:], YRW[1][:, :, b0:b0 + n], w=[yb], r=[YRW[1]], q="pool")
                    p.dma(bo[:], RWS["BON"][:, :, b0:b0 + n], w=[bo], r=[RWS["BON"]]); p.dma(gg[:], RWS["G"][:, :, b0:b0 + n], w=[gg], r=[RWS["G"]], q="pool")
                    p.V(lambda e: e.tensor_tensor(out=ya[:], in0=ya[:], in1=yb[:], op=ALU.add), w=[ya], r=[ya, yb])
                    for hp in range(4):
                        ps_ = pool[pi % 2]; pi += 1
                        p.T(lambda e, hp=hp, ps_=ps_: e.matmul(ps_[:].rearrange("p a b -> p (a b)"), lhsT=ones_f[:64, :64],
                                                              rhs=ya[:, 2 * hp:2 * hp + 2, :].rearrange("p a b -> p (a b)"), start=True, stop=True), w=[ps_], r=[ones_f, ya])
                        p.V(lambda e, hp=hp, ps_=ps_: e.scalar_tensor_tensor(out=yb[:, 2 * hp:2 * hp + 2, :], in0=ps_[:], scalar=-1.0 / 64, in1=ya[:, 2 * hp:2 * hp + 2, :],
                                                                            op0=ALU.mult, op1=ALU.add), w=[yb], r=[ps_, ya])
                    p.A(lambda e: e.activation(out=sq2[:], in_=yb[:], func=AF.Square), w=[sq2], r=[yb])
                    for hp in range(4):
                        ps_ = pool[pi % 2]; pi += 1
                        p.T(lambda e, hp=hp, ps_=ps_: e.matmul(ps_[:].rearrange("p a b -> p (a b)"), lhsT=ones_f[:64, :64],
                                                              rhs=sq2[:, 2 * hp:2 * hp + 2, :].rearrange("p a b -> p (a b)"), start=True, stop=True), w=[ps_], r=[ones_f, sq2])
                        p.A(lambda e, hp=hp, ps_=ps_: e.activation(out=ya[:, 2 * hp:2 * hp + 2, :], in_=ps_[:], func=AF.Sqrt, bias=64e-5, scale=1.0 / 64), w=[ya], r=[ps_])
                    p.V(lambda e: e.reciprocal(out=ya[:], in_=ya[:]), w=[ya], r=[ya])
                    p.V(lambda e: e.tensor_tensor(out=yb[:], in0=yb[:], in1=ya[:], op=ALU.mult), w=[yb], r=[yb, ya])
                    p.V(lambda e: e.tensor_tensor(out=yb[:], in0=yb[:], in1=bc(4), op=ALU.mult), w=[yb], r=[yb, vec])
                    p.V(lambda e: e.tensor_tensor(out=yb[:], in0=yb[:], in1=bc(5), op=ALU.add), w=[yb], r=[yb, vec])
                    p.V(lambda e: e.tensor_tensor(out=yb[:], in0=yb[:], in1=bo[:], op=ALU.add), w=[yb], r=[yb, bo])
                    p.V(lambda e: e.tensor_tensor(out=zT[:, :, :n], in0=yb[:], in1=gg[:], op=ALU.mult), w=[zT], r=[yb, gg])
                    branch_out(bt, zT, 8, 64, wproj, 1, b0, n)
                p.barrier()


        def stage_merge(l):
            with ExitStack() as st:
                wo = load_w_bf(st, I["w_out"][l].rearrange("(k p) c -> p k c", p=128), 8, D, "wo")
                ya = [p.sb("mya", [128, 8, 512], es=st) for _ in range(3)]
                mT = p.sb("mT", [128, 8, 512], BF16, es=st)
                xb = p.sb("mxb", [128, 8, 512], es=st)
                po = [p.ps("mpo", [128, 512], es=st) for _ in range(2)]
                blks = BLKS if l == 0 else BLKS[1:]
                for (b0, n, seg) in blks:
                    for i in range(3):
                        p.dma(ya[i][:, :, :n], YG[i][:, b0:b0 + n].rearrange("(kc p) t -> p kc t", p=128), w=[ya[i]], r=[YG[i]], q=("sp" if i != 1 else "pool"))
                    p.dma(xb[:, :, :n], X[:, b0:b0 + n].rearrange("(kc p) t -> p kc t", p=128), w=[xb], r=[X], q="pool")
                    p.V(lambda e: e.tensor_tensor(out=ya[0][:, :, :n], in0=ya[0][:, :, :n], in1=ya[1][:, :, :n], op=ALU.add), w=[ya[0]], r=[ya[0], ya[1]])
                    p.V(lambda e: e.tensor_tensor(out=mT[:, :, :n], in0=ya[0][:, :, :n], in1=ya[2][:, :, :n], op=ALU.add), w=[mT], r=[ya[0], ya[2]])
                    for oc in range(8):
                        po_ = po[oc % 2]
                        for kc in range(8):
                            p.T(lambda e, kc=kc, oc=oc, po_=po_: e.matmul(po_[:, :n], lhsT=wo[:, kc, oc * 128:(oc + 1) * 128], rhs=mT[:, kc, :n],
                                                                         start=(kc == 0), stop=(kc == 7)), w=[po_], r=[wo, mT])
                        p.V(lambda e, oc=oc, po_=po_: e.scalar_tensor_tensor(out=xb[:, oc, :n], in0=po_[:, :n], scalar=mod[l][:, 16 + oc, seg:seg + 1], in1=xb[:, oc, :n],
                                                                            op0=ALU.mult, op1=ALU.add), w=[xb], r=[po_, mod[l], xb])
                    p.dma(X[:, b0:b0 + n].rearrange("(kc p) t -> p kc t", p=128), xb[:, :, :n], w=[X], r=[xb])
                p.barrier()

        def stage_ffn(l):
            moe = (l % 2 == 1)
            blks = BLKS[1:] if moe else BLKS
            nexp = NE if moe else 1
            UTs = p.dram("UT%d" % l, [nexp, FF, T], BF16)
            with ExitStack() as st:
                hT = p.sb("hTf", [128, 8, T], BF16, es=st)
                combT = p.sb("combT", [8, TL], es=st) if moe else None
                with ExitStack() as st2:
                    nt = alloc_norm_tiles(st2)
                    m = mod[l]
                    if moe:
                        h32 = p.sb("h32", [128, 8, 512], es=st2)
                        rt = p.sb("rt", [128, 8, NE], es=st2)
                        p.dma(rt[:], I["moe_router"][0].rearrange("(kc p) e -> p kc e", p=128), w=[rt], r=[I["moe_router"]])
                        lg = p.sb("lg", [128, 16, NE], es=st2)
                        plg = p.ps("plg", [128, NE], es=st2)
                    for bi, (b0, n, seg) in enumerate(blks):
                        norm_block(nt, b0, n, lambda kc, seg=seg: gsf[l][:, kc, seg:seg + 1], lambda kc, seg=seg: m[:, 24 + kc, seg:seg + 1], hT,
                                   h32=(h32 if moe else None))
                        if moe:
                            for j in range(4):
                                for kc in range(8):
                                    p.T(lambda e, kc=kc, j=j: e.matmul(plg[:], lhsT=h32[:, kc, j * 128:(j + 1) * 128], rhs=rt[:, kc, :], start=(kc == 0), stop=(kc == 7)),
                                        w=[plg], r=[h32, rt])
                                p.V(lambda e, j=j, bi=bi: e.tensor_copy(out=lg[:, bi * 4 + j, :], in_=plg[:]), w=[lg], r=[plg])
                    if moe:
                        m1 = p.sb("m1", [128, 16], es=st2); eq = p.sb("eq", [128, 16, NE], es=st2); l2 = p.sb("l2", [128, 16, NE], es=st2)
                        m2 = p.sb("m2", [128, 16], es=st2); ex = p.sb("exr", [128, 16, NE], es=st2); sm = p.sb("sm", [128, 16], es=st2)
                        b3 = lambda t_: t_[:].unsqueeze(2).broadcast_to([128, 16, NE])
                        p.V(lambda e: e.reduce_max(out=m1[:], in_=lg[:], axis=AX.X), w=[m1], r=[lg])
                        p.V(lambda e: e.tensor_tensor(out=eq[:], in0=lg[:], in1=b3(m1), op=ALU.is_equal), w=[eq], r=[lg, m1])
                        p.V(lambda e: e.scalar_tensor_tensor(out=l2[:], in0=eq[:], scalar=-1e30, in1=lg[:], op0=ALU.mult, op1=ALU.add), w=[l2], r=[eq, lg])
                        p.V(lambda e: e.reduce_max(out=m2[:], in_=l2[:], axis=AX.X), w=[m2], r=[l2])
                        p.V(lambda e: e.tensor_tensor(out=eq[:], in0=lg[:], in1=b3(m2), op=ALU.is_ge), w=[eq], r=[lg, m2])
                        p.V(lambda e: e.tensor_tensor(out=l2[:], in0=lg[:], in1=b3(m1), op=ALU.subtract), w=[l2], r=[lg, m1])
                        p.A(lambda e: e.activation(out=ex[:], in_=l2[:], func=AF.Exp), w=[ex], r=[l2])
                        p.V(lambda e: e.tensor_tensor(out=ex[:], in0=ex[:], in1=eq[:], op=ALU.mult), w=[ex], r=[ex, eq])
                        p.V(lambda e: e.reduce_sum(out=sm[:], in_=ex[:], axis=AX.X), w=[sm], r=[ex])
                        p.V(lambda e: e.reciprocal(out=sm[:], in_=sm[:]), w=[sm], r=[sm])
                        p.V(lambda e: e.tensor_tensor(out=ex[:], in0=ex[:], in1=b3(sm), op=ALU.mult), w=[ex], r=[ex, sm])
                        pct = p.ps("pct", [8, 512], es=st2)
                        for g4 in range(4):
                            for j in range(4):
                                p.T(lambda e, g4=g4, j=j: e.transpose(pct[:, j * 128:(j + 1) * 128], ex[:, g4 * 4 + j, :], ident[:]), w=[pct], r=[ex, ident])
                            p.V(lambda e, g4=g4: e.tensor_copy(out=combT[:, g4 * 512:(g4 + 1) * 512], in_=pct[:]), w=[combT], r=[pct])
                    p.barrier()
                if "comb" in dbg and moe:
                    cdd = p.dram("dbg_comb", [8, TL], F32, kind="ExternalOutput")
                    p.dma(cdd[:], combT[:], w=[cdd], r=[combT])
                with ExitStack() as st2:
                    wf1 = [p.sb("wf1", [128, 8, 512], es=st2) for _ in range(2)]; wf3 = [p.sb("wf3", [128, 8, 512], es=st2) for _ in range(2)]
                    wb1 = [p.sb("wb1", [128, 8, 512], BF16, es=st2) for _ in range(2)]; wb3 = [p.sb("wb3", [128, 8, 512], BF16, es=st2) for _ in range(2)]
                    pa = [p.ps("pa", [128, 512], es=st2) for _ in range(2)]; pb = [p.ps("pb", [128, 512], es=st2) for _ in range(2)]
                    pcb = p.ps("pcb", [128, 512], es=st2)
                    sl = [p.sb("sl", [128, 512], es=st2) for _ in range(2)]; ut = [p.sb("ut", [128, 512], BF16, es=st2) for _ in range(2)]
                    sel = p.sb("sel8", [8, 8, 128], es=st2)
                    p.dma(sel[:], I["sel8"][:].rearrange("e k m -> k e m"), w=[sel], r=[I["sel8"]])
                    cbc = p.sb("cbc", [128, TL], es=st2) if moe else None
                    gi = 0; ui = 0
                    for ex_ in range(nexp):
                        w1s = I["moe_w1"][0, ex_] if moe else I["ffn_w1"][0]
                        w3s = I["moe_w3"][0, ex_] if moe else I["ffn_w3"][0]
                        if moe:
                            for g4 in range(4):
                                p.T(lambda e, g4=g4, ex_=ex_: e.matmul(pcb[:], lhsT=sel[:, ex_, :], rhs=combT[:, g4 * 512:(g4 + 1) * 512], start=True, stop=True), w=[pcb], r=[sel, combT])
                                p.A(lambda e, g4=g4: e.copy(out=cbc[:, g4 * 512:(g4 + 1) * 512], in_=pcb[:]), w=[cbc], r=[pcb])
                        for fg in range(6):
                            f0 = fg * 512
                            nf = min(512, FF - f0)
                            a1, a3, c1, c3 = wf1[gi % 2], wf3[gi % 2], wb1[gi % 2], wb3[gi % 2]
                            gi += 1
                            p.dma(a1[:, :, :nf], w1s[:, f0:f0 + nf].rearrange("(kc p) c -> p kc c", p=128), w=[a1], r=[], q="pool")
                            p.dma(a3[:, :, :nf], w3s[:, f0:f0 + nf].rearrange("(kc p) c -> p kc c", p=128), w=[a3], r=[], q="pool")
                            for kc in range(8):
                                p.V(lambda e, kc=kc: e.tensor_copy(out=c1[:, kc, :nf], in_=a1[:, kc, :nf]), w=[c1], r=[a1])
                                p.A(lambda e, kc=kc: e.copy(out=c3[:, kc, :nf], in_=a3[:, kc, :nf]), w=[c3], r=[a3])
                            for sub in range(nf // 128):
                                fb = fg * 4 + sub
                                for (b0, n, seg) in blks:
                                    pa_, pb_, sl_, ut_ = pa[ui % 2], pb[ui % 2], sl[ui % 2], ut[ui % 2]
                                    ui += 1
                                    for kc in range(8):
                                        p.T(lambda e, kc=kc, sub=sub, pa_=pa_: e.matmul(pa_[:, :n], lhsT=c1[:, kc, sub * 128:(sub + 1) * 128], rhs=hT[:, kc, b0:b0 + n],
                                                                                       start=(kc == 0), stop=(kc == 7)), w=[pa_], r=[c1, hT])
                                    for kc in range(8):
                                        p.T(lambda e, kc=kc, sub=sub, pb_=pb_: e.matmul(pb_[:, :n], lhsT=c3[:, kc, sub * 128:(sub + 1) * 128], rhs=hT[:, kc, b0:b0 + n],
                                                                                       start=(kc == 0), stop=(kc == 7)), w=[pb_], r=[c3, hT])
                                    p.A(lambda e, pa_=pa_, sl_=sl_: e.activation(out=sl_[:, :n], in_=pa_[:, :n], func=AF.Silu), w=[sl_], r=[pa_])
                                    if moe:
                                        p.V(lambda e, sl_=sl_, b0=b0: e.tensor_tensor(out=sl_[:, :n], in0=sl_[:, :n], in1=cbc[:, b0 - TC:b0 - TC + n], op=ALU.mult), w=[sl_], r=[sl_, cbc])
                                    p.V(lambda e, pb_=pb_, sl_=sl_, ut_=ut_: e.tensor_tensor(out=ut_[:, :n], in0=sl_[:, :n], in1=pb_[:, :n], op=ALU.mult), w=[ut_], r=[sl_, pb_])
                                    p.dma(UTs[ex_, fb * 128:(fb + 1) * 128, b0:b0 + n], ut_[:, :n], w=[UTs], r=[ut_])
                    p.barrier()
            with ExitStack() as st:
                w2f = [p.sb("w2f", [128, 2, D], es=st) for _ in range(2)]
                w2bs = [p.sb("w2b", [128, 22, D], BF16, es=st) for _ in range(2 if nexp > 1 else 1)]
                uu = [p.sb("uu", [128, 22, 512], BF16, es=st) for _ in range(2)]
                acc = p.sb("facc", [128, 8, 512], es=st); xb = p.sb("fxb", [128, 8, 512], es=st)
                po = [p.ps("fpo", [128, 512], es=st) for _ in range(4)]
                ui = 0; oi = 0

                def load_w2(ex_):
                    w2s = I["moe_w2"][0, ex_] if moe else I["ffn_w2"][0]
                    w2b_ = w2bs[ex_ % len(w2bs)]
                    for f2_ in range(11):
                        wf_ = w2f[f2_ % 2]
                        p.dma(wf_[:], w2s[f2_ * 256:(f2_ + 1) * 256, :].rearrange("(k p) c -> p k c", p=128), w=[wf_], r=[], q="pool")
                        p.V(lambda e, f2_=f2_, wf_=wf_: e.tensor_copy(out=w2b_[:, 2 * f2_, :], in_=wf_[:, 0, :]), w=[w2b_], r=[wf_])
                        p.A(lambda e, f2_=f2_, wf_=wf_: e.copy(out=w2b_[:, 2 * f2_ + 1, :], in_=wf_[:, 1, :]), w=[w2b_], r=[wf_])
                load_w2(0)
                for ex_ in range(nexp):
                    w2b = w2bs[ex_ % len(w2bs)]
                    if ex_ + 1 < nexp:
                        load_w2(ex_ + 1)
                    for (b0, n, seg) in blks:
                        u_ = uu[ui % 2]; ui += 1
                        p.dma(u_[:, :, :n], UTs[ex_, :, b0:b0 + n].rearrange("(k p) t -> p k t", p=128), w=[u_], r=[UTs], q="pool")
                        if ex_ > 0:
                            p.dma(acc[:, :, :n], FACC[:, b0:b0 + n].rearrange("(kc p) t -> p kc t", p=128), w=[acc], r=[FACC])
                        last = (ex_ == nexp - 1)
                        if last:
                            p.dma(xb[:, :, :n], X[:, b0:b0 + n].rearrange("(kc p) t -> p kc t", p=128), w=[xb], r=[X])
                        for oc in range(8):
                            po_ = po[oi % 4]; oi += 1
                            for k in range(22):
                                p.T(lambda e, k=k, oc=oc, po_=po_, u_=u_: e.matmul(po_[:, :n], lhsT=w2b[:, k, oc * 128:(oc + 1) * 128], rhs=u_[:, k, :n],
                                                                                  start=(k == 0), stop=(k == 21)), w=[po_], r=[w2b, u_])
                            if ex_ == 0:
                                p.A(lambda e, oc=oc, po_=po_: e.copy(out=acc[:, oc, :n], in_=po_[:, :n]), w=[acc], r=[po_])
                            else:
                                p.V(lambda e, oc=oc, po_=po_: e.tensor_tensor(out=acc[:, oc, :n], in0=acc[:, oc, :n], in1=po_[:, :n], op=ALU.add), w=[acc], r=[acc, po_])
                            if last:
                                p.V(lambda e, oc=oc: e.scalar_tensor_tensor(out=xb[:, oc, :n], in0=acc[:, oc, :n], scalar=mod[l][:, 40 + oc, seg:seg + 1], in1=xb[:, oc, :n],
                                                                           op0=ALU.mult, op1=ALU.add), w=[xb], r=[acc, mod[l], xb])
                        if last:
                            p.dma(X[:, b0:b0 + n].rearrange("(kc p) t -> p kc t", p=128), xb[:, :, :n], w=[X], r=[xb])
                        else:
                            p.dma(FACC[:, b0:b0 + n].rearrange("(kc p) t -> p kc t", p=128), acc[:, :, :n], w=[FACC], r=[acc])
                p.barrier()

        def stage_final():
            with ExitStack() as st:
                nt = alloc_norm_tiles(st)
                xb, sq, pss, rstd, tmp = nt
                for (b0, n, seg) in BLKS[1:]:
                    norm_block(nt, b0, n, lambda kc: fng[:, kc:kc + 1], None, None)
                    p.dma(outT[:, b0 - TC:b0 - TC + n].rearrange("(kc p) t -> p kc t", p=128), tmp[:, :, :n], w=[outT], r=[tmp])
                p.barrier()

        for l in range(n_layers):
            stage_ada(l)
        if "mod" in dbg:
            md = p.dram("dbg_mod", [2, 128, 96], F32, kind="ExternalOutput")
            for l in range(2):
                p.dma(md[l], mod[l][:].rearrange("p a b -> p (a b)"), w=[md], r=[mod[l]])
        for l in range(n_layers):
            stage_proj(l)
            if stop_after == ("proj", l):
                break
            if "hg" not in skip:
                stage_hg(l)
            if stop_after == ("hg", l):
                break
            if "da" not in skip:
                stage_da(l)
            if stop_after == ("da", l):
                break
            if "rw" not in skip:
                stage_rw_prep(l)
                stage_rw_scan(l)
                stage_rw_fin(l)
            if stop_after == ("rw", l):
                break
            if "merge" not in skip:
                stage_merge(l)
            if stop_after == ("merge", l):
                break
            if "ffn" not in skip:
                stage_ffn(l)
            if stop_after == ("ffn", l):
                break
        if stop_after is None:
            stage_final()
        if "X" in dbg:
            xd = p.dram("dbg_X", [D, T], F32, kind="ExternalOutput")
            p.dma(xd[:], X[:], w=[xd], r=[X])
        if "YG1" in dbg:
            yd = p.dram("dbg_YG1", [D, T], F32, kind="ExternalOutput")
            p.dma(yd[:], YG[1][:], w=[yd], r=[YG[1]])
            for nm in RWN:
                dd = p.dram("dbg_RWS_" + nm, [64, 8, T], F32, kind="ExternalOutput")
                p.dma(dd[:], RWS[nm][:], w=[dd], r=[RWS[nm]])
            for d in range(2):
                dd = p.dram("dbg_YRW%d" % d, [64, 8, T], F32, kind="ExternalOutput")
                p.dma(dd[:], YRW[d][:], w=[dd], r=[YRW[d]])
        if "YG2" in dbg:
            yd = p.dram("dbg_YG2", [D, T], F32, kind="ExternalOutput")
            p.dma(yd[:], YG[2][:], w=[yd], r=[YG[2]])
        if "YG0" in dbg:
            yd = p.dram("dbg_YG0", [D, T], F32, kind="ExternalOutput")
            p.dma(yd[:], YG[0][:], w=[yd], r=[YG[0]])
            od = p.dram("dbg_OHG", [2, 512, T], F32, kind="ExternalOutput")
            p.dma(od[:], OHG[:], w=[od], r=[OHG])
        if "PT" in dbg:
            pd = p.dram("dbg_PT", [INC, T], F32, kind="ExternalOutput")
            p.dma(pd[:], PT[:], w=[pd], r=[PT])
            vd = p.dram("dbg_VHG", [T, 512], F32, kind="ExternalOutput")
            p.dma(vd[:], VHG[:], w=[vd], r=[VHG])
        p.barrier()
        print("instrs", p.ninstr, "waits", p.nwait)
    return nc


def host_inputs(inputs, b):
    f = np.float32
    g = {}
    x = np.asarray(inputs["x"][b], f); ctx = np.asarray(inputs["ctx"][b], f)
    g["xT"] = np.ascontiguousarray(np.concatenate([ctx.T, x.T], axis=1))
    c2 = np.stack([np.asarray(inputs["c"][b], f), np.asarray(inputs["c_ctx"], f)], axis=-1)
    g["c2"] = np.ascontiguousarray(c2.reshape(8, 128, 2).transpose(1, 0, 2))
    return g


def shared_inputs(inputs):
    f = np.float32
    g = {}
    A = lambda k: np.asarray(inputs[k], f)
    g["ada_w"] = A("ada_w")
    g["ada_bT"] = np.ascontiguousarray(A("ada_b").reshape(2, 48, 128).transpose(0, 2, 1))
    g["nmgT"] = np.ascontiguousarray(A("norm_mix_g").reshape(2, 8, 128).transpose(0, 2, 1))
    g["nfgT"] = np.ascontiguousarray(A("norm_ffn_g").reshape(2, 8, 128).transpose(0, 2, 1))
    g["fngT"] = np.ascontiguousarray(A("final_norm_g").reshape(8, 128).T)
    g["w_in"] = A("w_in")
    mu_full = np.zeros((2, 71 * 128), f)
    mu_full[:, RW0:DA0] = A("rw_mu")
    g["muT"] = np.ascontiguousarray(mu_full.reshape(2, 71, 128).transpose(0, 2, 1))
    g["ident"] = np.eye(128, dtype=f)
    i = np.arange(64)[:, None]; j = np.arange(64)[None, :]
    g["masks"] = np.ascontiguousarray(np.stack([(i < j), (i <= j), (i > j), (i >= j)], axis=1).astype(f))
    g["hg_lbT"] = np.ascontiguousarray(A("hg_lb_logits").reshape(2, 2, 4, 128).transpose(0, 1, 3, 2))
    g["hg_ngT"] = np.ascontiguousarray(A("hg_norm_g").reshape(2, 128, 1))
    g["hg_proj"] = A("hg_proj")
    g["w_out"] = A("w_out")
    for k in ("ffn_w1", "ffn_w3", "ffn_w2", "moe_router", "moe_w1", "moe_w3", "moe_w2", "da_proj", "rw_w2", "rw_a2", "rw_g2", "rw_proj"):
        g[k] = A(k)
    sel = np.zeros((8, 8, 128), f)
    for e in range(8):
        sel[e, e, :] = 1.0
    g["sel8"] = sel
    g["da_lambda"] = A("da_lambda").reshape(2, 256)
    g["da_sg"] = A("da_subln_g")
    t = np.arange(TL)
    rowi = (t // 64).astype(f); coli = (t % 64).astype(f)
    inv_freq = (1.0 / (10000.0 ** (np.arange(0, 32, 2, dtype=f) / f(32)))).astype(f)
    ang = np.zeros((64, TL), f)
    for d in range(64):
        jj = d % 16
        ang[d] = (rowi if d < 32 else coli) * inv_freq[jj]
    g["ropeCS"] = np.ascontiguousarray(np.stack([np.cos(ang), np.sin(ang)], axis=1).astype(f))
    R = np.zeros((64, 64), f)
    for base in (0, 32):
        for q in range(16):
            R[base + q, base + 16 + q] = -1.0
            R[base + 16 + q, base + q] = 1.0
    g["ropeR"] = np.ascontiguousarray(R.T)
    vec = np.zeros((2, 8, 512), f)
    vec[:, 0] = A("rw_k_k"); vec[:, 1] = A("rw_k_a"); vec[:, 2] = A("rw_a0"); vec[:, 3] = A("rw_r_k").reshape(2, 512)
    vec[:, 4] = A("rw_ln_g"); vec[:, 5] = A("rw_ln_b")
    g["rw_vecT"] = np.ascontiguousarray(vec.reshape(2, 8, 8, 64).transpose(0, 3, 1, 2))
    g["rw_w0T"] = np.ascontiguousarray(A("rw_w0").reshape(2, 2, 8, 64).transpose(0, 1, 3, 2))
    return g


_NC_CACHE = {}


def kernel(**inputs):
    if "nc" not in _NC_CACHE:
        _NC_CACHE["nc"] = build()
    nc = _NC_CACHE["nc"]
    sh = shared_inputs(inputs)
    in_maps = []
    for b in range(8):
        m = dict(sh)
        m.update(host_inputs(inputs, b))
        in_maps.append(m)
    res = run_bass_kernel_spmd(nc, in_maps, core_ids=list(range(8)))
    out = np.stack([np.ascontiguousarray(r["outT"].T) for r in res.results], axis=0)
    return out.astype(np.float32)
```
